# Optimizing a Trainium2 kernel written in Bass

```python
import math
import jax, jax.numpy as jnp
from jax import lax
import numpy as np

D_MODEL = 2048
BATCH = 2
SEQ = 4096
DEPTH = 1
DEC_BATCH = 32
DEC_SEQ = 4
PAST_LEN = 8192
PAGE_SIZE = 128

N_HEADS = 16
N_KV = 4
HPG = N_HEADS // N_KV
HEAD_DIM = 64
NSA_WIDTH = N_HEADS * HEAD_DIM
CMP_LEN = 32
CMP_STRIDE = 16
CMP_HID = 128
SEL_BLOCK = 64
N_SEL = 16
WINDOW = 512
QBLK = 128
KV_SLOTS = 4
HG_HEADS = 8
HG_DK = 128
HG_DV = 128
HG_WIDTH = HG_HEADS * HG_DV
HG_CHUNK = 32
N_GROUPS = 4
EXP_PER_GROUP = 8
N_EXPERTS = N_GROUPS * EXP_PER_GROUP
D_EXPERT = 512
TOP_K_IN_GROUP = 2
DN_ALPHA = (2.0 * DEPTH) ** 0.25
DN_BETA = (8.0 * DEPTH) ** -0.25
LN_EPS = 1e-5
SCALE = HEAD_DIM ** -0.5
NEG = -1e30
BIG = 1e30
SPLIT_SIZES = (NSA_WIDTH, 6 * N_KV * HEAD_DIM, 3 * N_HEADS, HG_HEADS * HG_DK, HG_HEADS * HG_DK, HG_WIDTH, HG_WIDTH, 2 * D_MODEL)
PROJ_COLS = NSA_WIDTH + 6 * N_KV * HEAD_DIM + 3 * N_HEADS + 2 * HG_HEADS * HG_DK + 2 * HG_WIDTH + 2 * D_MODEL

kernel_name = 'nsa_hgrn2_hier_moe_deepnorm_step'


def layer_norm(x, g, b):
    xf = x.astype(jnp.float32)
    mu = jnp.mean(xf, -1, keepdims=True)
    var = jnp.mean(jnp.square(xf - mu), -1, keepdims=True)
    return ((xf - mu) * lax.rsqrt(var + LN_EPS) * g + b).astype(x.dtype)


def alibi_slopes():
    return jnp.asarray(2.0 ** (-8.0 * np.arange(1, N_HEADS + 1) / N_HEADS), dtype=jnp.float32).reshape(N_KV, HPG)


def masked_softmax(s, valid):
    s = jnp.where(valid, s, NEG)
    m = jnp.max(s, -1, keepdims=True)
    e = jnp.where(valid, jnp.exp(s - m), 0.0)
    return e / jnp.maximum(jnp.sum(e, -1, keepdims=True), 1e-30)


def project(x, w, b):
    B, T, _ = x.shape
    offs = [int(v) for v in np.cumsum(SPLIT_SIZES)[:-1]]
    proj = jnp.einsum('btd,dc->btc', x, w) + b
    q, kv, ng, hq, hf, hi, hg, mg = jnp.split(proj, offs, axis=-1)
    return (q.reshape(B, T, N_HEADS, HEAD_DIM), kv.reshape(B, T, 6, N_KV, HEAD_DIM),
            ng.reshape(B, T, 3, N_KV, HPG), hq, hf, hi, hg, mg.reshape(B, T, 2, D_MODEL))


def compress_kv(kvc, w1, w2, pe):
    B, T = kvc.shape[:2]
    R = CMP_LEN // CMP_STRIDE
    n_cmp = (T - CMP_LEN) // CMP_STRIDE + 1
    n_ch = -(-T // CMP_STRIDE)
    kvc = jnp.pad(kvc, ((0, 0), (0, n_ch * CMP_STRIDE - T), (0, 0), (0, 0), (0, 0)))
    ch = kvc.reshape(B, n_ch, CMP_STRIDE, 2, N_KV, HEAD_DIM)
    w1r = w1.reshape(2, R, CMP_STRIDE, HEAD_DIM, CMP_HID)
    pre = jnp.einsum('cjd,cjdh->ch', pe, w1)[:, None, None, None, :]
    for r in range(R):
        pre = pre + jnp.einsum('bnjcgd,cjdh->cbngh', ch[:, r:r + n_cmp], w1r[:, r])
    out = jnp.einsum('cbngh,chd->cbngd', jax.nn.gelu(pre), w2)
    return out[0], out[1], n_cmp


def nsa_sparse(q, kv_full, qpos, w1, w2, pe, slopes):
    B, T = kv_full.shape[:2]
    Tq = q.shape[1]
    kc, vc, n_cmp = compress_kv(kv_full[:, :, 0:2], w1, w2, pe)
    cpos = jnp.arange(n_cmp, dtype=jnp.int32) * CMP_STRIDE + (CMP_LEN - 1)
    nsb = -(-T // SEL_BLOCK)
    k_sel = min(N_SEL, nsb)
    st = np.arange(n_cmp) * CMP_STRIDE
    bs = np.arange(nsb) * SEL_BLOCK
    overlap = jnp.asarray(((st[:, None] <= bs[None, :] + SEL_BLOCK - 1) & (st[:, None] + CMP_LEN - 1 >= bs[None, :])).astype(np.float32))
    ksv = jnp.pad(kv_full[:, :, 2:4], ((0, 0), (0, nsb * SEL_BLOCK - T), (0, 0), (0, 0), (0, 0)))
    ksv = ksv.reshape(B, nsb, SEL_BLOCK, 2, N_KV, HEAD_DIM).transpose(0, 4, 1, 2, 3, 5)
    blk_ids = jnp.arange(nsb, dtype=jnp.int32)
    offs = jnp.arange(SEL_BLOCK, dtype=jnp.int32)
    qb_len = min(QBLK, Tq)
    nb = Tq // qb_len

    def one_block(args):
        qb, pb = args
        qg = qb.reshape(B, qb_len, N_KV, HPG, HEAD_DIM)
        dist_c = pb[:, None] - cpos[None, :]
        s = jnp.einsum('bqghd,bngd->bghqn', qg, kc).astype(jnp.float32) * SCALE
        s = s - slopes[None, :, :, None, None] * dist_c.astype(jnp.float32)
        p = masked_softmax(s, dist_c >= 0)
        o_cmp = jnp.einsum('bghqn,bngd->bqghd', p.astype(vc.dtype), vc)
        imp = jnp.einsum('bghqn,nj->bgqj', p, overlap)
        cur = pb // SEL_BLOCK
        imp = jnp.where(blk_ids[None, :] > cur[:, None], NEG, imp)
        imp = jnp.where((blk_ids[None, :] == cur[:, None]) | (blk_ids[None, :] == 0), BIG, imp)
        _, idx = lax.top_k(imp, k_sel)
        gath = jax.vmap(jax.vmap(lambda kb, ib: kb[ib]))(ksv, idx)
        s2 = jnp.einsum('bqghd,bgqnpd->bghqnp', qg, gath[..., 0, :]).astype(jnp.float32) * SCALE
        spos = idx[..., None] * SEL_BLOCK + offs
        dist_s = pb[None, None, :, None, None] - spos
        s2 = s2 - slopes[None, :, :, None, None, None] * dist_s[:, :, None].astype(jnp.float32)
        s2 = s2.reshape(B, N_KV, HPG, qb_len, k_sel * SEL_BLOCK)
        valid2 = (dist_s >= 0).reshape(B, N_KV, qb_len, k_sel * SEL_BLOCK)[:, :, None]
        p2 = masked_softmax(s2, valid2)
        vg = gath[..., 1, :].reshape(B, N_KV, qb_len, k_sel * SEL_BLOCK, HEAD_DIM)
        o_slc = jnp.einsum('bghqm,bgqmd->bqghd', p2.astype(vg.dtype), vg)
        return o_cmp, o_slc

    qblocks = q.reshape(B, nb, qb_len, N_HEADS, HEAD_DIM).swapaxes(0, 1)
    pblocks = qpos.reshape(nb, qb_len)
    o_cmp, o_slc = lax.map(one_block, (qblocks, pblocks))
    o_cmp = o_cmp.swapaxes(0, 1).reshape(B, Tq, N_KV, HPG, HEAD_DIM)
    o_slc = o_slc.swapaxes(0, 1).reshape(B, Tq, N_KV, HPG, HEAD_DIM)
    return o_cmp, o_slc


def window_attend(q, k, v, qpos, kpos, slopes):
    B, Tq = q.shape[:2]
    qg = q.reshape(B, Tq, N_KV, HPG, HEAD_DIM)
    dist = qpos[:, None] - kpos[None, :]
    s = jnp.einsum('bqghd,bkgd->bghqk', qg, k).astype(jnp.float32) * SCALE
    s = s - slopes[None, :, :, None, None] * dist.astype(jnp.float32)
    valid = (dist >= 0) & (dist < WINDOW) & (kpos[None, :] >= 0)
    p = masked_softmax(s, valid)
    return jnp.einsum('bghqk,bkgd->bqghd', p.astype(v.dtype), v)


def prompt_window(q, kw, vw, slopes):
    B, S = q.shape[:2]
    nb = S // QBLK
    wb = WINDOW // QBLK
    kp = jnp.pad(kw, ((0, 0), (wb * QBLK, 0), (0, 0), (0, 0))).reshape(B, nb + wb, QBLK, N_KV, HEAD_DIM)
    vp = jnp.pad(vw, ((0, 0), (wb * QBLK, 0), (0, 0), (0, 0))).reshape(B, nb + wb, QBLK, N_KV, HEAD_DIM)
    kband = jnp.concatenate([kp[:, i:i + nb] for i in range(wb + 1)], axis=2)
    vband = jnp.concatenate([vp[:, i:i + nb] for i in range(wb + 1)], axis=2)
    kpos = (jnp.arange(nb, dtype=jnp.int32)[:, None] - wb) * QBLK + jnp.arange((wb + 1) * QBLK, dtype=jnp.int32)[None, :]
    qpos = jnp.arange(S, dtype=jnp.int32).reshape(nb, QBLK)
    o = jax.vmap(window_attend, in_axes=(1, 1, 1, 0, 0, None), out_axes=1)(
        q.reshape(B, nb, QBLK, N_HEADS, HEAD_DIM), kband, vband, qpos, kpos, slopes)
    return o.reshape(B, S, N_KV, HPG, HEAD_DIM)


def nsa_combine(gates, o_cmp, o_slc, o_win):
    g = jax.nn.sigmoid(gates)[..., None]
    o = g[:, :, 0] * o_cmp + g[:, :, 1] * o_slc + g[:, :, 2] * o_win
    B, T = o.shape[:2]
    return o.reshape(B, T, NSA_WIDTH)


def chunked_gla(q, k, v, logf, s0):
    B, T, H, _ = q.shape
    C = HG_CHUNK
    nc = -(-T // C)
    pad = nc * C - T

    def prep(a):
        a = jnp.pad(a, ((0, 0), (0, pad), (0, 0), (0, 0)))
        return a.reshape(B, nc, C, H, a.shape[-1]).transpose(1, 0, 3, 2, 4)

    mask = jnp.tril(jnp.ones((C, C), dtype=bool))

    def step(S, inp):
        qc, kc, vc, lc = inp
        bc = jnp.cumsum(lc, axis=-2)
        qe = qc * jnp.exp(bc)
        ke = kc * jnp.exp(-bc)
        att = jnp.where(mask, jnp.einsum('bhtd,bhsd->bhts', qe, ke), 0.0)
        o = jnp.einsum('bhtd,bhde->bhte', qe, S) + jnp.einsum('bhts,bhse->bhte', att, vc)
        bl = bc[:, :, -1:, :]
        S = jnp.exp(bl[:, :, 0, :, None]) * S + jnp.einsum('bhsd,bhse->bhde', kc * jnp.exp(bl - bc), vc)
        return S, o

    s_fin, o = lax.scan(step, s0, (prep(q), prep(k), prep(v), prep(logf)))
    o = o.transpose(1, 0, 3, 2, 4).reshape(B, nc * C, H, v.shape[-1])[:, :T]
    return o, s_fin


def hgrn2(hq, hf, hi, hg, lb, s0, norm_g):
    B, T, _ = hq.shape
    q = jax.nn.silu(hq.astype(jnp.float32).reshape(B, T, HG_HEADS, HG_DK))
    f = lb + (1.0 - lb) * jax.nn.sigmoid(hf.astype(jnp.float32).reshape(B, T, HG_HEADS, HG_DK))
    v = hi.astype(jnp.float32).reshape(B, T, HG_HEADS, HG_DV)
    o, s_fin = chunked_gla(q, 1.0 - f, v, jnp.log(f), s0.astype(jnp.float32))
    o = o * lax.rsqrt(jnp.mean(jnp.square(o), -1, keepdims=True) + LN_EPS) * norm_g
    o = o * jax.nn.silu(hg.astype(jnp.float32).reshape(B, T, HG_HEADS, HG_DV))
    return o.reshape(B, T, HG_WIDTH).astype(hq.dtype), s_fin


def hier_moe(h, w_rg, b_rg, w_re, b_re, w_gate, w_up, w_down):
    shp = h.shape
    hf = h.reshape(-1, D_MODEL)
    n_tok = hf.shape[0]
    lg = (hf @ w_rg + b_rg).astype(jnp.float32)
    grp = jnp.argmax(lg, -1)
    g_w = jnp.take_along_axis(jax.nn.softmax(lg, -1), grp[:, None], -1)
    le = (hf @ w_re + b_re).astype(jnp.float32).reshape(n_tok, N_GROUPS, EXP_PER_GROUP)
    le_sel = jnp.take_along_axis(le, grp[:, None, None], axis=1)[:, 0]
    top_v, top_i = lax.top_k(le_sel, TOP_K_IN_GROUP)
    w_sel = jax.nn.softmax(top_v, -1) * g_w
    eid = grp[:, None] * EXP_PER_GROUP + top_i
    gate = jnp.sum(jax.nn.one_hot(eid, N_EXPERTS, dtype=jnp.float32) * w_sel[..., None], axis=1)
    hid = jax.nn.silu(jnp.einsum('nd,edf->enf', hf, w_gate)) * jnp.einsum('nd,edf->enf', hf, w_up)
    hid = hid * gate.T[:, :, None].astype(hid.dtype)
    return jnp.einsum('enf,efd->nd', hid, w_down).reshape(shp)


def trunk_tail(x, o_nsa, o_hg, mg, lp):
    a = o_nsa @ lp['w_pa']
    b = o_hg @ lp['w_pb']
    y = (jax.nn.sigmoid(mg[:, :, 0]) * a + jax.nn.sigmoid(mg[:, :, 1]) * b) @ lp['w_out']
    h = layer_norm(DN_ALPHA * x + y, lp['ln1_g'], lp['ln1_b'])
    z = hier_moe(h, lp['w_rg'], lp['b_rg'], lp['w_re'], lp['b_re'], lp['w_gate'], lp['w_up'], lp['w_down'])
    return layer_norm(DN_ALPHA * h + z, lp['ln2_g'], lp['ln2_b'])


def setup_inputs(seed: int = 0) -> dict:
    key = jax.random.key(seed)
    ks = jax.random.split(key, 32)
    n_pages = PAST_LEN // PAGE_SIZE
    n_phys = (5 * DEC_BATCH * n_pages + 3) // 4
    w_buf = min(WINDOW, PAST_LEN)

    def nrm(k, shape, scale):
        return jax.random.normal(k, shape, jnp.float32) * scale

    return {
        'x_prompt': nrm(ks[0], (BATCH, SEQ, D_MODEL), 1.0),
        'x_sample': nrm(ks[1], (DEC_BATCH, DEC_SEQ, D_MODEL), 1.0),
        'cache_kv': nrm(ks[2], (DEPTH, n_phys, PAGE_SIZE, KV_SLOTS, N_KV, HEAD_DIM), 1.0),
        'cache_win': nrm(ks[3], (DEPTH, DEC_BATCH, w_buf, 2, N_KV, HEAD_DIM), 1.0),
        'state_hgrn': nrm(ks[4], (DEPTH, DEC_BATCH, HG_HEADS, HG_DK, HG_DV), 0.5),
        'page_table': jax.random.permutation(ks[5], n_phys)[:DEC_BATCH * n_pages].reshape(DEC_BATCH, n_pages).astype(jnp.int32),
        'w_in': nrm(ks[6], (DEPTH, D_MODEL, PROJ_COLS), D_MODEL ** -0.5),
        'b_in': nrm(ks[7], (DEPTH, PROJ_COLS), 0.02),
        'w_cmp1': nrm(ks[8], (DEPTH, 2, CMP_LEN, HEAD_DIM, CMP_HID), (CMP_LEN * HEAD_DIM) ** -0.5),
        'w_cmp2': nrm(ks[9], (DEPTH, 2, CMP_HID, HEAD_DIM), CMP_HID ** -0.5),
        'cmp_pe': nrm(ks[10], (DEPTH, 2, CMP_LEN, HEAD_DIM), 0.1),
        'hgrn_gamma': nrm(ks[11], (DEPTH + 1, HG_HEADS * HG_DK), 0.5),
        'hgrn_norm': 1.0 + nrm(ks[12], (DEPTH, HG_HEADS, HG_DV), 0.02),
        'w_pa': nrm(ks[13], (DEPTH, NSA_WIDTH, D_MODEL), NSA_WIDTH ** -0.5),
        'w_pb': nrm(ks[14], (DEPTH, HG_WIDTH, D_MODEL), HG_WIDTH ** -0.5),
        'w_out': nrm(ks[15], (DEPTH, D_MODEL, D_MODEL), D_MODEL ** -0.5 * DN_BETA),
        'ln1_g': 1.0 + nrm(ks[16], (DEPTH, D_MODEL), 0.02),
        'ln1_b': nrm(ks[17], (DEPTH, D_MODEL), 0.02),
        'w_rg': nrm(ks[18], (DEPTH, D_MODEL, N_GROUPS), D_MODEL ** -0.5),
        'b_rg': nrm(ks[19], (DEPTH, N_GROUPS), 0.01),
        'w_re': nrm(ks[20], (DEPTH, D_MODEL, N_EXPERTS), D_MODEL ** -0.5),
        'b_re': nrm(ks[21], (DEPTH, N_EXPERTS), 0.01),
        'w_gate': nrm(ks[22], (DEPTH, N_EXPERTS, D_MODEL, D_EXPERT), D_MODEL ** -0.5),
        'w_up': nrm(ks[23], (DEPTH, N_EXPERTS, D_MODEL, D_EXPERT), D_MODEL ** -0.5),
        'w_down': nrm(ks[24], (DEPTH, N_EXPERTS, D_EXPERT, D_MODEL), D_EXPERT ** -0.5 * DN_BETA),
        'ln2_g': 1.0 + nrm(ks[25], (DEPTH, D_MODEL), 0.02),
        'ln2_b': nrm(ks[26], (DEPTH, D_MODEL), 0.02),
    }


def reference(x_prompt, x_sample, cache_kv, cache_win, state_hgrn, page_table, w_in, b_in, w_cmp1, w_cmp2, cmp_pe,
              hgrn_gamma, hgrn_norm, w_pa, w_pb, w_out, ln1_g, ln1_b, w_rg, b_rg, w_re, b_re, w_gate, w_up, w_down,
              ln2_g, ln2_b):
    slopes = alibi_slopes()
    lower = jnp.cumsum(jax.nn.softmax(hgrn_gamma.astype(jnp.float32), axis=0), axis=0)
    n_pages = page_table.shape[1]
    past_len = n_pages * PAGE_SIZE
    w_buf = cache_win.shape[2]
    xp, xs = x_prompt, x_sample
    kv_p_l, kv_s_l, win_p_l, win_s_l, st_p_l, st_s_l = [], [], [], [], [], []
    for l in range(DEPTH):
        lp = {'w_pa': w_pa[l], 'w_pb': w_pb[l], 'w_out': w_out[l], 'ln1_g': ln1_g[l], 'ln1_b': ln1_b[l],
              'w_rg': w_rg[l], 'b_rg': b_rg[l], 'w_re': w_re[l], 'b_re': b_re[l], 'w_gate': w_gate[l],
              'w_up': w_up[l], 'w_down': w_down[l], 'ln2_g': ln2_g[l], 'ln2_b': ln2_b[l]}
        lb = lower[l].reshape(HG_HEADS, HG_DK)
        qp, kvp, gp, hqp, hfp, hip, hgp, mgp = project(xp, w_in[l], b_in[l])
        kv_full_p = kvp[:, :, :KV_SLOTS]
        oc_p, os_p = nsa_sparse(qp, kv_full_p, jnp.arange(xp.shape[1], dtype=jnp.int32), w_cmp1[l], w_cmp2[l], cmp_pe[l], slopes)
        ow_p = prompt_window(qp, kvp[:, :, 4], kvp[:, :, 5], slopes)
        o_nsa_p = nsa_combine(gp, oc_p, os_p, ow_p)
        s0 = jnp.zeros((xp.shape[0], HG_HEADS, HG_DK, HG_DV), jnp.float32)
        ohg_p, sfin_p = hgrn2(hqp, hfp, hip, hgp, lb, s0, hgrn_norm[l])
        w_keep_p = min(WINDOW, xp.shape[1])
        kv_p_l.append(kv_full_p)
        win_p_l.append(kvp[:, xp.shape[1] - w_keep_p:, 4:6])
        st_p_l.append(sfin_p.astype(xp.dtype))
        xp = trunk_tail(xp, o_nsa_p, ohg_p, mgp, lp)
        qs, kvs, gs, hqs, hfs, his, hgs, mgs = project(xs, w_in[l], b_in[l])
        past = cache_kv[l][page_table].reshape(xs.shape[0], past_len, KV_SLOTS, N_KV, HEAD_DIM)
        new_rows = kvs[:, :, :KV_SLOTS]
        kv_full_s = jnp.concatenate([past, new_rows.astype(past.dtype)], axis=1)
        qpos_s = past_len + jnp.arange(xs.shape[1], dtype=jnp.int32)
        oc_s, os_s = nsa_sparse(qs, kv_full_s, qpos_s, w_cmp1[l], w_cmp2[l], cmp_pe[l], slopes)
        win = jnp.concatenate([cache_win[l], kvs[:, :, 4:6].astype(cache_win.dtype)], axis=1)
        kpos_w = past_len - w_buf + jnp.arange(w_buf + xs.shape[1], dtype=jnp.int32)
        ow_s = window_attend(qs, win[:, :, 0], win[:, :, 1], qpos_s, kpos_w, slopes)
        o_nsa_s = nsa_combine(gs, oc_s, os_s, ow_s)
        ohg_s, sfin_s = hgrn2(hqs, hfs, his, hgs, lb, state_hgrn[l], hgrn_norm[l])
        w_keep_s = min(WINDOW, w_buf + xs.shape[1])
        kv_s_l.append(new_rows)
        win_s_l.append(win[:, win.shape[1] - w_keep_s:])
        st_s_l.append(sfin_s.astype(state_hgrn.dtype))
        xs = trunk_tail(xs, o_nsa_s, ohg_s, mgs, lp)
    new_kv_prompt = jnp.stack(kv_p_l, 0)
    new_kv_sample = jnp.stack(kv_s_l, 0)
    new_win_prompt = jnp.stack(win_p_l, 0)
    new_win_sample = jnp.stack(win_s_l, 0)
    new_state_prompt = jnp.stack(st_p_l, 0)
    new_state_sample = jnp.stack(st_s_l, 0)
    return (xp, xs, new_kv_prompt, new_kv_sample, new_win_prompt, new_win_sample, new_state_prompt, new_state_sample)
```

```python
import contextlib
import numpy as np
import concourse.bass as bass
import concourse.mybir as mybir
from concourse.bass_utils import run_bass_kernel_spmd

F32 = mybir.dt.float32
BF16 = mybir.dt.bfloat16
AF = mybir.ActivationFunctionType
ALU = mybir.AluOpType

D_MODEL = 2048
KC = D_MODEL // 128
SEQ = 4096
NCORES = 8
TOK_P = 1024
TOK_S = 16
TOK = TOK_P + TOK_S
KV_OFF, KV_W = 1024, 1536
HF_OFF, HI_OFF = 3632, 4656


class _Stop(Exception):
    pass


class Buf:
    def __init__(self, name):
        self.name = name
        self.w = {}
        self.r = {}


class Prog:
    COMPUTE = ("pe", "act", "dve", "pool")

    def __init__(self, n_dma_sems=8):
        self.ops = {e: [] for e in ("pe", "act", "dve", "pool", "sp")}
        self.cnt = {}
        self.waited = {}
        self.n_dma = n_dma_sems
        self.dma_rr = {"sp": 0, "pool": 0, "act": 0}

    def _need(self, eng, reads, writes):
        need = {}
        for b in reads:
            for e, s in b.w.items():
                need[e] = max(need.get(e, 0), s)
        for b in writes:
            for e, s in b.w.items():
                need[e] = max(need.get(e, 0), s)
            for e, s in b.r.items():
                need[e] = max(need.get(e, 0), s)
        waits = []
        for e, s in need.items():
            if e == eng and eng == "pe":
                continue
            if self.waited.get((eng, e), 0) >= s:
                continue
            self.waited[(eng, e)] = s
            waits.append((e, s))
        return waits

    def op(self, eng, fn, reads=(), writes=()):
        waits = self._need(eng, reads, writes)
        self.cnt[eng] = self.cnt.get(eng, 0) + 1
        s = self.cnt[eng]
        for b in reads:
            b.r[eng] = s
        for b in writes:
            b.w[eng] = s
        self.ops[eng].append((waits, fn, eng))

    def dma(self, queue, fn, reads=(), writes=()):
        k = self.dma_rr[queue]
        self.dma_rr[queue] = (k + 1) % self.n_dma
        v = "dma_%s_%d" % (queue, k)
        waits = self._need(queue, reads, writes)
        prev = self.cnt.get(v, 0)
        if prev and self.waited.get((queue, v), 0) < prev:
            self.waited[(queue, v)] = prev
            waits.append((v, prev))
        self.cnt[v] = prev + 1
        s = self.cnt[v]
        for b in reads:
            b.r[v] = s
        for b in writes:
            b.w[v] = s
        self.ops[queue].append((waits, fn, v))

    def barrier(self):
        targets = dict(self.cnt)
        for eng in self.ops:
            waits = []
            for e, n in targets.items():
                if e == eng and eng == "pe":
                    continue
                if n and self.waited.get((eng, e), 0) < n:
                    self.waited[(eng, e)] = n
                    waits.append((e, n))
            self.ops[eng].append((waits, None, None))

    def finish(self, queue="sp"):
        waits = []
        for v, n in self.cnt.items():
            if v.startswith("dma_") and self.waited.get((queue, v), 0) < n:
                waits.append((v, n))
        self.ops[queue].append((waits, None, None))

    def emit(self, nc):
        names = sorted(set(list(self.COMPUTE) + [v for v in self.cnt if v.startswith("dma_")]))
        with contextlib.ExitStack() as es:
            sems = {n: es.enter_context(nc.semaphore("s_" + n)) for n in names}
            block = es.enter_context(nc.Block())

            def run(eng_name, eng):
                for waits, fn, inc in self.ops[eng_name]:
                    for (e, s) in waits:
                        eng.wait_ge(sems[e], s * 16 if e.startswith("dma_") else s)
                    if fn is None:
                        continue
                    ins = fn(eng)
                    ins.then_inc(sems[inc], 16 if inc.startswith("dma_") else 1)

            @block.tensor
            def _(e):
                run("pe", e)

            @block.scalar
            def _(e):
                run("act", e)

            @block.vector
            def _(e):
                run("dve", e)

            @block.gpsimd
            def _(e):
                run("pool", e)

            @block.sync
            def _(e):
                run("sp", e)


def stage_states(nc, P, st, sbt, next_ps, ones_c, B_ones, D):
    xTb, xf = D["xTb"], D["xf"]
    wbig, B_wbig = sbt(st, "wbig", [128, KC, 2048], BF16)
    wst_sb, B_wst = sbt(st, "wst_sb", [128, KC, 512], BF16)
    bias_big, B_bias_big = sbt(st, "bias_big", [128, 2048], F32)
    bias_st, B_bias_st = sbt(st, "bias_st", [128, 512], F32)
    oml_st, B_oml_st = sbt(st, "oml_st", [128, 256], F32)
    oml_ss, B_oml_ss = sbt(st, "oml_ss", [128, 1024], F32)
    lm, B_lm = sbt(st, "lm", [128, 128], F32)
    lm4, B_lm4 = sbt(st, "lm4", [4, 4], F32)
    xs = [sbt(st, "sxs%d" % i, [128, KC, 256], BF16) for i in range(2)]
    xsm, B_xsm = sbt(st, "xsm", [128, KC, 128], BF16)
    S_sb, B_S = sbt(st, "S_sb", [128, 2, 128], F32)
    NSCR = 2
    scr = []
    for i in range(NSCR):
        d = {}
        for n, dt in (("kk", F32), ("lgf", F32), ("edd", F32), ("kd", BF16), ("vv", BF16)):
            d[n] = sbt(st, "%s%d" % (n, i), [128, 1024], dt)
        d["edl"] = sbt(st, "edl%d" % i, [128, 8], F32)
        scr.append(d)

    P.dma("sp", lambda e: e.dma_start(out=lm[:], in_=D["c_lm"]), writes=[B_lm])
    P.dma("sp", lambda e: e.dma_start(out=lm4[:], in_=D["c_lm4"]), writes=[B_lm4])
    P.dma("sp", lambda e: e.dma_start(out=bias_st[:], in_=D["b_st"].partition_broadcast(128)), writes=[B_bias_st])
    wst_v = D["w_st"].rearrange("(kc p) c -> p kc c", p=128)
    for q in range(2):
        P.dma("pool", lambda e, q=q: e.dma_start(out=wst_sb[:, 8 * q:8 * q + 8, :], in_=wst_v[:, 8 * q:8 * q + 8, :]),
              writes=[B_wst])

    def lower_bound_prep(g_ap, n, oml, B_oml):
        g0, Bg0 = scr[0]["lgf"]
        g1, Bg1 = scr[1]["lgf"]
        P.dma("sp", lambda e: e.dma_start(out=g0[:, 0:n], in_=g_ap[0].partition_broadcast(128)), writes=[Bg0])
        P.dma("sp", lambda e: e.dma_start(out=g1[:, 0:n], in_=g_ap[1].partition_broadcast(128)), writes=[Bg1])
        P.op("dve", lambda e: e.tensor_tensor(out=oml[:, 0:n], in0=g1[:, 0:n], in1=g0[:, 0:n], op=ALU.subtract),
             reads=[Bg0, Bg1], writes=[B_oml])
        P.op("act", lambda e: e.activation(out=oml[:, 0:n], in_=oml[:, 0:n], func=AF.Sigmoid), reads=[B_oml], writes=[B_oml])

    def state_chunk(si, m, xsl, W, B_W, B_x, bias, B_bias, oml, B_oml, nh, lmask, B_lmask, S_list):
        d = scr[si % NSCR]
        kk, Bkk = d["kk"]
        lgf, Blgf = d["lgf"]
        edd, Bedd = d["edd"]
        kd, Bkd = d["kd"]
        vv, Bvv = d["vv"]
        edl, Bedl = d["edl"]
        n = nh * 128
        for g in range((2 * n) // 512):
            pt, Bp = next_ps("a")
            for kc in range(KC):
                P.op("pe", lambda e, pt=pt, kc=kc, g=g: e.matmul(
                    pt[:m, :], lhsT=xsl(kc), rhs=W[:, kc, g * 512:(g + 1) * 512],
                    start=(kc == 0), stop=(kc == KC - 1)), reads=[B_x, B_W], writes=[Bp])
            c0 = g * 512
            a0, a1 = c0, min(c0 + 512, n)
            if a1 > a0:
                P.op("dve", lambda e, pt=pt, a0=a0, a1=a1, c0=c0: e.tensor_tensor(
                    out=kk[:m, a0:a1], in0=pt[:m, a0 - c0:a1 - c0], in1=bias[:m, a0:a1], op=ALU.add),
                    reads=[Bp, B_bias], writes=[Bkk])
            v0, v1 = max(c0, n), c0 + 512
            if v1 > v0:
                P.op("dve", lambda e, pt=pt, v0=v0, v1=v1, c0=c0: e.tensor_tensor(
                    out=vv[:m, v0 - n:v1 - n], in0=pt[:m, v0 - c0:v1 - c0], in1=bias[:m, v0:v1], op=ALU.add),
                    reads=[Bp, B_bias], writes=[Bvv])
        P.op("act", lambda e: e.activation(out=kk[:m, 0:n], in_=kk[:m, 0:n], func=AF.Sigmoid, scale=-1.0),
             reads=[Bkk], writes=[Bkk])
        P.op("dve", lambda e: e.tensor_tensor(out=kk[:m, 0:n], in0=kk[:m, 0:n], in1=oml[:m, 0:n], op=ALU.mult),
             reads=[Bkk, B_oml], writes=[Bkk])
        P.op("act", lambda e: e.activation(out=lgf[:m, 0:n], in_=kk[:m, 0:n], func=AF.Ln, scale=-1.0, bias=ones_c[:m, :]),
             reads=[Bkk, B_ones], writes=[Blgf])
        for g in range((n + 511) // 512):
            w = min(512, n - g * 512)
            pt, Bp = next_ps("a")
            P.op("pe", lambda e, pt=pt, g=g, w=w: e.matmul(pt[:m, 0:w], lhsT=lmask[:m, :m], rhs=lgf[:m, g * 512:g * 512 + w],
                                                           start=True, stop=True), reads=[B_lmask, Blgf], writes=[Bp])
            P.op("act", lambda e, pt=pt, g=g, w=w: e.activation(out=edd[:m, g * 512:g * 512 + w], in_=pt[:m, 0:w], func=AF.Exp),
                 reads=[Bp], writes=[Bedd])
        P.op("dve", lambda e: e.tensor_tensor(out=kd[:m, 0:n], in0=kk[:m, 0:n], in1=edd[:m, 0:n], op=ALU.mult),
             reads=[Bkk, Bedd], writes=[Bkd])
        pt, Bp = next_ps("b")
        for h in range(nh):
            P.op("pe", lambda e, pt=pt, h=h: e.matmul(pt[:, h:h + 1], lhsT=lgf[:m, h * 128:(h + 1) * 128], rhs=ones_c[:m, :],
                                                      start=True, stop=True), reads=[Blgf, B_ones], writes=[Bp])
        P.op("act", lambda e, pt=pt: e.activation(out=edl[:, 0:nh], in_=pt[:, 0:nh], func=AF.Exp), reads=[Bp], writes=[Bedl])
        for h0 in range(0, nh, 4):
            pt, Bp = next_ps("b")
            hs_ = list(range(h0, min(nh, h0 + 4)))
            for h in hs_:
                P.op("pe", lambda e, pt=pt, h=h, h0=h0: e.matmul(
                    pt[:, (h - h0) * 128:(h - h0 + 1) * 128], lhsT=kd[:m, h * 128:(h + 1) * 128], rhs=vv[:m, h * 128:(h + 1) * 128],
                    start=True, stop=True), reads=[Bkd, Bvv], writes=[Bp])
            for h in hs_:
                s_in, s_out, b_in, b_out = S_list[h]
                P.op("dve", lambda e, pt=pt, h=h, h0=h0, s_in=s_in, s_out=s_out: e.scalar_tensor_tensor(
                    out=s_out, in0=s_in, scalar=edl[:, h:h + 1], in1=pt[:, (h - h0) * 128:(h - h0 + 1) * 128],
                    op0=ALU.mult, op1=ALU.add), reads=[Bp, Bedl, b_in], writes=[b_out])

    lower_bound_prep(D["g_st"], 256, oml_st, B_oml_st)
    P.op("dve", lambda e: e.memset(S_sb[:], 0.0), writes=[B_S])
    xTb_v = xTb.rearrange("(kc p) t -> p kc t", p=128)
    si = 0
    for tt in range(SEQ // 256):
        xb, Bx = xs[tt % 2]
        for q in range(2):
            P.dma("pool", lambda e, xb=xb, tt=tt, q=q: e.dma_start(
                out=xb[:, 8 * q:8 * q + 8, :], in_=xTb_v[:, 8 * q:8 * q + 8, tt * 256:(tt + 1) * 256]), writes=[Bx])
        for c4 in range(2):
            S_list = [(S_sb[:, h, :], S_sb[:, h, :], B_S, B_S) for h in range(2)]
            state_chunk(si, 128, lambda kc, xb=xb, c4=c4: xb[:, kc, c4 * 128:(c4 + 1) * 128],
                        wst_sb, B_wst, Bx, bias_st, B_bias_st, oml_st, B_oml_st, 2, lm, B_lm, S_list)
            si += 1
    for h in range(2):
        P.dma("sp", lambda e, h=h: e.dma_start(out=D["st_p"][h], in_=S_sb[:, h, :]), reads=[B_S])

    wss_v = D["w_ss"].rearrange("(kc p) c -> p kc c", p=128)
    for q in range(4):
        P.dma("pool", lambda e, q=q: e.dma_start(out=wbig[:, 4 * q:4 * q + 4, :], in_=wss_v[:, 4 * q:4 * q + 4, :]),
              writes=[B_wbig])
    P.dma("sp", lambda e: e.dma_start(out=bias_big[:], in_=D["b_ss"].partition_broadcast(128)), writes=[B_bias_big])
    xf_v = xf.rearrange("(kc p) t -> p kc t", p=128)
    for q in range(2):
        P.dma("pool", lambda e, q=q: e.dma_start(out=xsm[:, 8 * q:8 * q + 8, :], in_=xf_v[:, 8 * q:8 * q + 8, NB * 128:(NB + 1) * 128]),
              writes=[B_xsm])
    lower_bound_prep(D["g_ss"], 1024, oml_ss, B_oml_ss)
    s0 = [sbt(st, "s0_%d" % i, [128, 8, 128], F32) for i in range(2)]
    for j in range(4):
        sj, Bsj = s0[j % 2]
        P.dma("sp", lambda e, sj=sj, j=j: e.dma_start(out=sj[:], in_=D["st_in"][j].rearrange("h k v -> k h v")), writes=[Bsj])
        S_list = [(sj[:, h, :], sj[:, h, :], Bsj, Bsj) for h in range(8)]
        state_chunk(si, 4, lambda kc, j=j: xsm[:, kc, 32 * j:32 * j + 4],
                    wbig, B_wbig, B_xsm, bias_big, B_bias_big, oml_ss, B_oml_ss, 8, lm4, B_lm4, S_list)
        si += 1
        P.dma("sp", lambda e, sj=sj, j=j: e.dma_start(out=D["st_s"][j].rearrange("h k v -> k h v"), in_=sj[:]), reads=[Bsj])


def nsa_prompt(nc, P, p4, sbt, next_ps, L):
    debug = L["debug"]
    xf = L["xf"]
    kslcT, B_kslcT, kwinT, B_kwinT = L["kslcT"], L["B_kslcT"], L["kwinT"], L["B_kwinT"]
    vslc, B_vslc, vwin, B_vwin = L["vslc"], L["B_vslc"], L["vwin"], L["B_vwin"]
    kcT, B_kcT, vca, B_vca = L["kcT"], L["B_kcT"], L["vca"], L["B_vca"]
    ident, B_ident, identb, B_identb = L["ident"], L["B_ident"], L["identb"], L["B_identb"]
    onesb, B_onesb = L["onesb"], L["B_onesb"]
    o_nsaT, B_onsaT = L["o_nsaT"], L["B_onsaT"]

    def table(name, shape, dt, src, q="sp"):
        t, B = sbt(p4, name, shape, dt)
        P.dma(q, lambda e: e.dma_start(out=t[:], in_=src), writes=[B])
        return t, B
    cb, B_cb = table("cb", [128, 256], F32, L["t_cb"])
    cmask, B_cmask = table("cmask", [128, NOWN * 512], BF16, L["t_cmask"])
    bs, B_bs = table("bs", [128, 512], F32, L["t_bs"])
    sq, B_sq = table("sq", [1, 2048], F32, L["t_sq"])
    lnt, B_lnt = table("lnt", [2, NB * 128], BF16, L["t_ln"])
    R2, B_R2 = table("R2", [2, 512], BF16, L["t_r2"])
    Et, B_Et = table("Et", [64, NB * 128], BF16, L["t_E"])
    caus, B_caus = table("caus", [128, 512], BF16, L["t_caus"])
    low, B_low = table("low", [128, 512], BF16, L["t_low"])
    keep, B_keep = table("keep", [128, NOWN * 64], F32, L["t_keep"])
    force, B_force = table("force", [128, NOWN * 64], F32, L["t_force"])

    qT, B_qT = sbt(p4, "qT", [128, 8, NOWN * 128], BF16)
    gates, B_gates = sbt(p4, "gates", [128, NOWN, 48], F32)
    pq = contextlib.ExitStack()
    with pq:
        wq, B_wq = sbt(pq, "wq", [128, KC, 1024], BF16)
        wng, B_wng = sbt(pq, "wng", [128, KC, 48], BF16)
        bq_col, B_bq = sbt(pq, "bq_col", [128, 8], F32)
        bng, B_bng = sbt(pq, "bng", [128, 48], F32)
        xo = [sbt(pq, "xo%d" % i, [128, KC, 256], BF16) for i in range(1)]
        wq_v = L["w_q"].rearrange("(kc p) c -> p kc c", p=128)
        for q in range(4):
            P.dma("pool", lambda e, q=q: e.dma_start(out=wq[:, 4 * q:4 * q + 4, :], in_=wq_v[:, 4 * q:4 * q + 4, :]), writes=[B_wq])
        P.dma("pool", lambda e: e.dma_start(out=wng[:], in_=L["w_ng"].rearrange("(kc p) c -> p kc c", p=128)), writes=[B_wng])
        with nc.allow_non_contiguous_dma(reason="tiny bias column layout"):
            P.dma("sp", lambda e: e.dma_start(out=bq_col[:], in_=L["b_q"].rearrange("(cb p) -> p cb", p=128), allow_slow_non_contiguous=True), writes=[B_bq])
        P.dma("sp", lambda e: e.dma_start(out=bng[:], in_=L["b_ng"].partition_broadcast(128)), writes=[B_bng])
        xf_v = xf.rearrange("(kc p) t -> p kc t", p=128)
        for tt in range(4):
            xb, Bx = xo[0]
            t0 = OWN0 * 128 + tt * 256
            for q in range(2):
                P.dma("pool", lambda e, xb=xb, t0=t0, q=q: e.dma_start(
                    out=xb[:, 8 * q:8 * q + 8, :], in_=xf_v[:, 8 * q:8 * q + 8, t0:t0 + 256]), writes=[Bx])
            for cbk in range(8):
                pt, Bp = next_ps("a")
                for kc in range(KC):
                    P.op("pe", lambda e, pt=pt, kc=kc, cbk=cbk, xb=xb: e.matmul(
                        pt[:, 0:256], lhsT=wq[:, kc, cbk * 128:(cbk + 1) * 128], rhs=xb[:, kc, :],
                        start=(kc == 0), stop=(kc == KC - 1)), reads=[B_wq, Bx], writes=[Bp])
                P.op("act", lambda e, pt=pt, cbk=cbk, tt=tt: e.activation(
                    out=qT[:, cbk, tt * 256:(tt + 1) * 256], in_=pt[:, 0:256], func=AF.Identity, bias=bq_col[:, cbk:cbk + 1]),
                    reads=[Bp, B_bq], writes=[B_qT])
            for bi in range(2):
                k = tt * 2 + bi
                pt, Bp = next_ps("b")
                for kc in range(KC):
                    P.op("pe", lambda e, pt=pt, kc=kc, xb=xb, bi=bi: e.matmul(
                        pt[:, 0:48], lhsT=xb[:, kc, bi * 128:(bi + 1) * 128], rhs=wng[:, kc, :],
                        start=(kc == 0), stop=(kc == KC - 1)), reads=[B_wng, Bx], writes=[Bp])
                P.op("dve", lambda e, pt=pt, k=k: e.tensor_tensor(out=gates[:, k, :], in0=pt[:, 0:48], in1=bng[:, :], op=ALU.add),
                     reads=[Bp, B_bng], writes=[B_gates])
        P.op("act", lambda e: e.activation(out=gates[:], in_=gates[:], func=AF.Sigmoid), reads=[B_gates], writes=[B_gates])
        P.barrier()

    sqt, B_sqt = sbt(p4, "sqt", [128, 512], BF16)
    runmax, B_runmax = sbt(p4, "runmax", [1, 512], F32)
    nkm, B_nkm = sbt(p4, "nkm", [1, 1], F32)
    P.op("dve", lambda e: e.memset(runmax[:], 0.0), writes=[B_runmax])
    srcs = []
    for gp in range(2):
        for s in range(NB * 128 // 512):
            srcs.append((kslcT, B_kslcT, gp, s * 512, 512))
        for s in range(3):
            srcs.append((kwinT, B_kwinT, gp, s * 512, 512))
        srcs.append((kcT, B_kcT, gp, 0, 256))
    for (src, Bs, gp, c0, w) in srcs:
        P.op("dve", lambda e, src=src, gp=gp, c0=c0, w=w: e.tensor_tensor(
            out=sqt[:, 0:w], in0=src[:, gp, c0:c0 + w], in1=src[:, gp, c0:c0 + w], op=ALU.mult), reads=[Bs], writes=[B_sqt])
        pt, Bp = next_ps("b")
        P.op("pe", lambda e, pt=pt, w=w: e.matmul(pt[0:1, 0:w], lhsT=onesb[:, 0:1], rhs=sqt[:, 0:w], start=True, stop=True),
             reads=[B_onesb, B_sqt], writes=[Bp])
        P.op("dve", lambda e, pt=pt, w=w: e.tensor_tensor(out=runmax[:, 0:w], in0=runmax[:, 0:w], in1=pt[0:1, 0:w], op=ALU.max),
             reads=[Bp, B_runmax], writes=[B_runmax])
    P.op("dve", lambda e: e.reduce_max(out=nkm[:], in_=runmax[:], axis=mybir.AxisListType.X), reads=[B_runmax], writes=[B_nkm])
    P.op("dve", lambda e: e.tensor_scalar(out=nkm[:], in0=nkm[:], scalar1=-0.5, scalar2=None, op0=ALU.mult),
         reads=[B_nkm], writes=[B_nkm])
    P.op("dve", lambda e: e.tensor_scalar(out=sq[:, :], in0=sq[:, :], scalar1=nkm[0:1, 0:1], scalar2=None, op0=ALU.add),
         reads=[B_nkm, B_sq], writes=[B_sq])

    if _LIM < 5:
        raise _Stop()
    pT = [sbt(p4, "pT%d" % i, [128, 4, 128], BF16) for i in range(4)]
    pT_rr = [0]

    def next_pT():
        k = pT_rr[0] % len(pT)
        pT_rr[0] += 1
        return pT[k]
    qsq, B_qsq = sbt(p4, "qsq", [128, 512], BF16)
    rt, B_rt = sbt(p4, "rt", [1, 512], F32)
    o_blk, B_oblk = sbt(p4, "o_blk", [128, 256], F32)
    rs, B_rs = sbt(p4, "rs", [128, 4], F32)
    wgt, B_wgt = sbt(p4, "wgt", [128, 4], F32)
    imp, B_imp = sbt(p4, "imp", [128, 64], F32)
    imp3, B_imp3 = sbt(p4, "imp3", [128, 64], F32)
    m8, B_m8 = sbt(p4, "m8", [128, 16], F32)
    nsel, B_nsel = sbt(p4, "nsel", [128, 64], F32)
    nselT, B_nselT = sbt(p4, "nselT", [64, 512], BF16)
    if debug:
        dimp, B_dimp = sbt(p4, "dimp", [128, 64], F32)

    def softmax_chunk(pt, Bp, w, hbase, col_fn, tab, B_tab):
        (t, Bt) = next_pT()
        for hh in range(4):
            P.op("act", lambda e, pt=pt, t=t, hh=hh, w=w: e.activation(
                out=t[:w, hh, :], in_=pt[:w, hh * 128:(hh + 1) * 128], func=AF.Exp, scale=SCALE,
                bias=tab[:w, col_fn(hbase + hh):col_fn(hbase + hh) + 1]), reads=[Bp, B_tab], writes=[Bt])
        return t, Bt

    def finish_branch(psO, BpO, k, g, br, first):
        P.op("dve", lambda e: e.tensor_scalar(out=rs[:, :], in0=psO[:, 0:260].rearrange("p (h c) -> p h c", c=65)[:, :, 64],
                                              scalar1=1e-30, scalar2=None, op0=ALU.max), reads=[BpO], writes=[B_rs])
        P.op("dve", lambda e: e.reciprocal(out=rs[:, :], in_=rs[:, :]), reads=[B_rs], writes=[B_rs])
        P.op("dve", lambda e: e.tensor_tensor(out=wgt[:, :], in0=rs[:, :], in1=gates[:, k, br * 16 + g * 4:br * 16 + g * 4 + 4],
                                              op=ALU.mult), reads=[B_rs, B_gates], writes=[B_wgt])
        for hh in range(4):
            oc = slice(hh * 64, hh * 64 + 64)
            if first:
                P.op("dve", lambda e, hh=hh, oc=oc: e.tensor_scalar(out=o_blk[:, oc], in0=psO[:, hh * 65:hh * 65 + 64],
                                                                    scalar1=wgt[:, hh:hh + 1], scalar2=None, op0=ALU.mult),
                     reads=[BpO, B_wgt], writes=[B_oblk])
            else:
                P.op("dve", lambda e, hh=hh, oc=oc: e.scalar_tensor_tensor(
                    out=o_blk[:, oc], in0=psO[:, hh * 65:hh * 65 + 64], scalar=wgt[:, hh:hh + 1], in1=o_blk[:, oc],
                    op0=ALU.mult, op1=ALU.add), reads=[BpO, B_wgt, B_oblk], writes=[B_oblk])

    kslc_g, B_kslcg = sbt(p4, "kslc_g", [64, NB * 128], BF16)
    kwin_g, B_kwing = sbt(p4, "kwin_g", [64, 12 * 128], BF16)
    kc_g, B_kcg = sbt(p4, "kc_g", [64, 256], BF16)
    q_g, B_qg = sbt(p4, "q_g", [64, 4, NOWN * 128], BF16)
    for g in range(4):
        gp, g2 = g // 2, g % 2
        hs0 = slice(64 * g2, 64 * g2 + 64)
        P.dma("sp", lambda e, hs0=hs0, gp=gp: e.dma_start(out=kslc_g[:, :], in_=kslcT[hs0, gp, :]), reads=[B_kslcT], writes=[B_kslcg])
        P.dma("sp", lambda e, hs0=hs0, gp=gp: e.dma_start(out=kwin_g[:, :], in_=kwinT[hs0, gp, :]), reads=[B_kwinT], writes=[B_kwing])
        P.dma("sp", lambda e, hs0=hs0, gp=gp: e.dma_start(out=kc_g[:, :], in_=kcT[hs0, gp, :]), reads=[B_kcT], writes=[B_kcg])
        P.dma("sp", lambda e, hs0=hs0, gp=gp: e.dma_start(out=q_g[:, :, :], in_=qT[hs0, gp * 4:gp * 4 + 4, :]), reads=[B_qT], writes=[B_qg])
        hs = slice(0, 64)
        for k in range(NOWN):
            if _LIM < 6 and (k, g) not in _KSEL:
                continue
            B = OWN0 + k
            tok = slice(k * 128, (k + 1) * 128)
            qv = q_g[:, :, tok]
            P.op("dve", lambda e, qv=qv, hs=hs: e.tensor_tensor(out=qsq[hs, :].rearrange("p (h q) -> p h q", h=4), in0=qv, in1=qv,
                                                                op=ALU.mult), reads=[B_qg], writes=[B_qsq])
            pt, Bp = next_ps("b")
            P.op("pe", lambda e, pt=pt, hs=hs: e.matmul(pt[0:1, :], lhsT=onesb[hs, 0:1], rhs=qsq[hs, :], start=True, stop=True),
                 reads=[B_onesb, B_qsq], writes=[Bp])
            P.op("dve", lambda e, pt=pt, g=g: e.scalar_tensor_tensor(out=R2[0:1, :], in0=pt[0:1, :], scalar=-0.5,
                                                                     in1=sq[0:1, g * 512:(g + 1) * 512], op0=ALU.mult, op1=ALU.add),
                 reads=[Bp, B_sq], writes=[B_R2])

            pts = []
            for c in range(2):
                w = 128 if c == 0 else 127
                pt, Bp = next_ps("a")
                P.op("pe", lambda e, pt=pt, c=c, w=w, hs=hs, gp=gp, qv=qv: e.matmul(
                    pt[:w, :], lhsT=kc_g[hs, c * 128:c * 128 + w], rhs=qv, start=True, stop=False),
                    reads=[B_kcg, B_qg], writes=[Bp])
                P.op("pe", lambda e, pt=pt, w=w, c=c: e.matmul(pt[:w, :], lhsT=lnt[0:1, 0:w], rhs=R2[0:1, :], start=False, stop=(c == 0)),
                     reads=[B_lnt, B_R2], writes=[Bp])
                if c == 1:
                    P.op("pe", lambda e, pt=pt, w=w, k=k: e.matmul(pt[:w, :], lhsT=identb[:w, :w], rhs=cmask[:w, k * 512:(k + 1) * 512],
                                                                   start=False, stop=True), reads=[B_identb, B_cmask], writes=[Bp])
                t, Bt = softmax_chunk(pt, Bp, w, 4 * g, lambda h, k=k, c=c: (h * NOWN + k) * 2 + c, cb, B_cb)
                pts.append((t, Bt, w))
            if _LIM < 5.2:
                continue
            psO, BpO = next_ps("b")
            psI, BpI = next_ps("b")
            for hh in range(4):
                for c, (t, Bt, w) in enumerate(pts):
                    P.op("pe", lambda e, hh=hh, c=c, t=t, w=w, g=g, psO=psO: e.matmul(
                        psO[:, hh * 65:(hh + 1) * 65], lhsT=t[:w, hh, :], rhs=vca[:w, c, g, 0:65], start=(c == 0), stop=(c == 1)),
                        reads=[Bt, B_vca], writes=[BpO])
            for hh in range(4):
                for c, (t, Bt, w) in enumerate(pts):
                    P.op("pe", lambda e, hh=hh, c=c, t=t, w=w, g=g, psI=psI: e.matmul(
                        psI[:, hh * 64:(hh + 1) * 64], lhsT=t[:w, hh, :], rhs=vca[:w, c, g, 65:129], start=(c == 0), stop=(c == 1)),
                        reads=[Bt, B_vca], writes=[BpI])
            finish_branch(psO, BpO, k, g, 0, True)
            for hh in range(4):
                if hh == 0:
                    P.op("dve", lambda e, psI=psI: e.tensor_scalar(out=imp[:, :], in0=psI[:, 0:64], scalar1=rs[:, 0:1], scalar2=None,
                                                          op0=ALU.mult), reads=[BpI, B_rs], writes=[B_imp])
                else:
                    P.op("dve", lambda e, hh=hh, psI=psI: e.scalar_tensor_tensor(
                        out=imp[:, :], in0=psI[:, hh * 64:(hh + 1) * 64], scalar=rs[:, hh:hh + 1], in1=imp[:, :],
                        op0=ALU.mult, op1=ALU.add), reads=[BpI, B_rs, B_imp], writes=[B_imp])
            if debug:
                P.op("dve", lambda e: e.tensor_copy(out=dimp[:], in_=imp[:]), reads=[B_imp], writes=[B_dimp])
                P.dma("sp", lambda e, k=k, g=g: e.dma_start(out=L["d_imp"][(k * 4 + g) * 128:(k * 4 + g + 1) * 128, :], in_=dimp[:]),
                      reads=[B_dimp])
            if _LIM < 5.3:
                continue
            P.op("dve", lambda e: e.tensor_scalar(out=imp[:, :], in0=imp[:, :], scalar1=1e-30, scalar2=None, op0=ALU.max),
                 reads=[B_imp], writes=[B_imp])
            P.op("dve", lambda e, k=k: e.tensor_tensor(out=imp[:, :], in0=imp[:, :], in1=keep[:, k * 64:(k + 1) * 64], op=ALU.mult),
                 reads=[B_imp, B_keep], writes=[B_imp])
            P.op("dve", lambda e, k=k: e.tensor_tensor(out=imp[:, :], in0=imp[:, :], in1=force[:, k * 64:(k + 1) * 64], op=ALU.add),
                 reads=[B_imp, B_force], writes=[B_imp])
            P.op("dve", lambda e: e.max(out=m8[:, 0:8], in_=imp[:, :]), reads=[B_imp], writes=[B_m8])
            P.op("dve", lambda e: e.tensor_scalar(out=imp3[:, :], in0=imp[:, :], scalar1=m8[:, 7:8], scalar2=None, op0=ALU.is_ge),
                 reads=[B_imp, B_m8], writes=[B_imp3])
            P.op("dve", lambda e: e.scalar_tensor_tensor(out=imp3[:, :], in0=imp3[:, :], scalar=-3.0e38, in1=imp[:, :],
                                                         op0=ALU.mult, op1=ALU.add), reads=[B_imp, B_imp3], writes=[B_imp3])
            P.op("dve", lambda e: e.max(out=m8[:, 8:16], in_=imp3[:, :]), reads=[B_imp3], writes=[B_m8])
            P.op("dve", lambda e: e.tensor_scalar(out=nsel[:, :], in0=imp[:, :], scalar1=m8[:, 15:16], scalar2=None, op0=ALU.is_ge),
                 reads=[B_imp, B_m8], writes=[B_nsel])
            P.op("dve", lambda e: e.tensor_scalar(out=nsel[:, :], in0=nsel[:, :], scalar1=-NEGM, scalar2=NEGM, op0=ALU.mult, op1=ALU.add),
                 reads=[B_nsel], writes=[B_nsel])
            pt, Bp = next_ps("b")
            P.op("pe", lambda e, pt=pt: e.transpose(out=pt[0:64, 0:128], in_=nsel[:, :], identity=ident[:, :]),
                 reads=[B_nsel, B_ident], writes=[Bp])
            for hh in range(4):
                P.op("act", lambda e, pt=pt, hh=hh: e.activation(out=nselT[:, hh * 128:(hh + 1) * 128], in_=pt[0:64, 0:128],
                                                                 func=AF.Identity), reads=[Bp], writes=[B_nselT])
            if _LIM < 5.4:
                continue
            psO, BpO = next_ps("b")
            for c in range(B + 1):
                pt, Bp = next_ps("a")
                P.op("pe", lambda e, pt=pt, c=c, hs=hs, gp=gp, qv=qv: e.matmul(
                    pt[:, :], lhsT=kslc_g[hs, c * 128:(c + 1) * 128], rhs=qv, start=True, stop=False),
                    reads=[B_kslcg, B_qg], writes=[Bp])
                P.op("pe", lambda e, pt=pt, c=c: e.matmul(pt[:, :], lhsT=Et[:, c * 128:(c + 1) * 128], rhs=nselT[:, :],
                                                          start=False, stop=False), reads=[B_Et, B_nselT], writes=[Bp])
                P.op("pe", lambda e, pt=pt, c=c, B=B: e.matmul(pt[:, :], lhsT=lnt[0:2, c * 128:(c + 1) * 128], rhs=R2[0:2, :],
                                                               start=False, stop=(c != B)), reads=[B_lnt, B_R2], writes=[Bp])
                if c == B:
                    P.op("pe", lambda e, pt=pt: e.matmul(pt[:, :], lhsT=identb[:, :], rhs=caus[:, :], start=False, stop=True),
                         reads=[B_identb, B_caus], writes=[Bp])
                t, Bt = softmax_chunk(pt, Bp, 128, 4 * g, lambda h, d=B - c: h * 32 + d, bs, B_bs)
                for hh in range(4):
                    P.op("pe", lambda e, hh=hh, t=t, c=c, g=g, B=B, psO=psO: e.matmul(
                        psO[:, hh * 65:(hh + 1) * 65], lhsT=t[:, hh, :], rhs=vslc[:, c, g, :], start=(c == 0), stop=(c == B)),
                        reads=[Bt, B_vslc], writes=[BpO])
            finish_branch(psO, BpO, k, g, 1, False)
            if _LIM < 5.5:
                continue
            psO, BpO = next_ps("b")
            for c in range(B - 4, B + 1):
                cw = c - 20
                pt, Bp = next_ps("a")
                P.op("pe", lambda e, pt=pt, cw=cw, hs=hs, gp=gp, qv=qv: e.matmul(
                    pt[:, :], lhsT=kwin_g[hs, cw * 128:(cw + 1) * 128], rhs=qv, start=True, stop=False),
                    reads=[B_kwing, B_qg], writes=[Bp])
                edge = (c == B) or (c == B - 4)
                P.op("pe", lambda e, pt=pt, c=c, edge=edge: e.matmul(pt[:, :], lhsT=lnt[0:2, c * 128:(c + 1) * 128], rhs=R2[0:2, :],
                                                                      start=False, stop=(not edge)), reads=[B_lnt, B_R2], writes=[Bp])
                if edge:
                    mk, Bmk = (caus, B_caus) if c == B else (low, B_low)
                    P.op("pe", lambda e, pt=pt, mk=mk: e.matmul(pt[:, :], lhsT=identb[:, :], rhs=mk[:, :], start=False, stop=True),
                         reads=[B_identb, Bmk], writes=[Bp])
                t, Bt = softmax_chunk(pt, Bp, 128, 4 * g, lambda h, d=B - c: h * 32 + d, bs, B_bs)
                for hh in range(4):
                    P.op("pe", lambda e, hh=hh, t=t, cw=cw, c=c, g=g, B=B, psO=psO: e.matmul(
                        psO[:, hh * 65:(hh + 1) * 65], lhsT=t[:, hh, :], rhs=vwin[:, cw, g, :], start=(c == B - 4), stop=(c == B)),
                        reads=[Bt, B_vwin], writes=[BpO])
            finish_branch(psO, BpO, k, g, 2, False)

            if _LIM < 5.6:
                continue
            if debug:
                P.dma("sp", lambda e, k=k, g=g: e.dma_start(out=L["d_onsa"][k * 128:(k + 1) * 128, g * 256:(g + 1) * 256], in_=o_blk[:, 0:256]),
                      reads=[B_oblk])
            for j in range(2):
                pt, Bp = next_ps("a")
                P.op("pe", lambda e, pt=pt, j=j: e.transpose(out=pt[:, 0:128], in_=o_blk[:, j * 128:(j + 1) * 128],
                                                             identity=ident[:, :]), reads=[B_oblk, B_ident], writes=[Bp])
                P.op("dve", lambda e, pt=pt, j=j, k=k, g=g: e.tensor_copy(out=o_nsaT[:, 2 * g + j, k * 128:(k + 1) * 128], in_=pt[:, 0:128]),
                     reads=[Bp], writes=[B_onsaT])


def hgrn_outputs(nc, P, sc, sbt, next_ps, L):
    xf = L["xf"]
    ident, B_ident = L["ident"], L["B_ident"]
    ones_c, B_ones = L["ones_c"], L["B_ones"]
    o_hgT, B_ohgT = L["o_hgT"], L["B_ohgT"]
    debug = L["debug"]

    def table(name, shape, dt, src, q="sp"):
        t, B = sbt(sc, name, shape, dt)
        P.dma(q, lambda e: e.dma_start(out=t[:], in_=src), writes=[B])
        return t, B
    lm, B_lm = table("h_lm", [128, 128], F32, L["c_lm"])
    u32, B_u32 = table("h_u32", [128, 128], F32, L["t_u32"])
    l32, B_l32 = table("h_l32", [128, 128], F32, L["t_l32"])
    ind4, B_ind4 = table("h_ind4", [128, 4], F32, L["t_ind4"])
    vmask, B_vmask = table("h_vmask", [128, NB + 1], F32, L["t_vmask"])
    eps_c, B_eps = sbt(sc, "eps_c", [128, 1], F32)
    P.op("dve", lambda e: e.memset(eps_c[:], LN_EPS), writes=[B_eps])

    W3, B_W3 = sbt(sc, "W3", [128, KC, 2048], BF16)
    b3, B_b3 = sbt(sc, "b3", [128, 2048], F32)
    oml, B_oml = sbt(sc, "oml3", [128, 512], F32)
    gtmp, B_gtmp = sbt(sc, "gtmp", [128, 2, 512], F32)
    ngb, B_ngb = sbt(sc, "ngb", [128, 512], F32)
    xt = [sbt(sc, "hx%d" % i, [128, KC, 128], BF16) for i in range(2)]
    names32 = ["kk", "lgf", "ebc", "ebi", "edd", "qe", "ke", "hgb", "osb", "og"]
    T = {n: sbt(sc, "h_" + n, [128, 512], F32) for n in names32}
    vv, B_vv = sbt(sc, "h_vv", [128, 512], BF16)
    kd4, B_kd4 = sbt(sc, "h_kd4", [128, 4, 512], BF16)
    kdb, B_kdb = sbt(sc, "h_kdb", [128, 512], BF16)
    qeT4, B_qeT4 = sbt(sc, "h_qeT4", [128, 4, 4, 128], BF16)
    keT, B_keT = sbt(sc, "h_keT", [128, 4, 128], BF16)
    attm, B_attm = sbt(sc, "h_attm", [128, 4, 128], BF16)
    S, B_S = sbt(sc, "h_S", [128, 4, 128], F32)
    Sb, B_Sb = sbt(sc, "h_Sb", [128, 4, 128], BF16)
    Sld, B_Sld = sbt(sc, "h_Sld", [128, 4, 4, 128], BF16)
    edl, B_edl = sbt(sc, "h_edl", [128, 16], F32)
    ss, B_ss = sbt(sc, "h_ss", [128, 4], F32)
    P.op("dve", lambda e: e.memset(qeT4[:], 0.0), writes=[B_qeT4])
    xf_v = xf.rearrange("(kc p) t -> p kc t", p=128)

    for hp in range(2):
        w3_v = L["w_h3"][hp].rearrange("(kc p) c -> p kc c", p=128)
        for q in range(4):
            P.dma("pool", lambda e, q=q, w3_v=w3_v: e.dma_start(out=W3[:, 4 * q:4 * q + 4, :], in_=w3_v[:, 4 * q:4 * q + 4, :]),
                  writes=[B_W3])
        P.dma("sp", lambda e, hp=hp: e.dma_start(out=b3[:], in_=L["b_h3"][hp].partition_broadcast(128)), writes=[B_b3])
        P.dma("sp", lambda e, hp=hp: e.dma_start(out=ngb[:], in_=L["n_h3"][hp].partition_broadcast(128)), writes=[B_ngb])
        for r in range(2):
            P.dma("sp", lambda e, hp=hp, r=r: e.dma_start(out=gtmp[:, r, :], in_=L["g_h3"][hp, r].partition_broadcast(128)),
                  writes=[B_gtmp])
        P.op("dve", lambda e: e.tensor_tensor(out=oml[:], in0=gtmp[:, 1, :], in1=gtmp[:, 0, :], op=ALU.subtract),
             reads=[B_gtmp], writes=[B_oml])
        P.op("act", lambda e: e.activation(out=oml[:], in_=oml[:], func=AF.Sigmoid), reads=[B_oml], writes=[B_oml])
        for j in range(4):
            P.dma("pool", lambda e, hp=hp, j=j: e.dma_start(out=Sld[:, j, :, :], in_=L["st_in"][j, 4 * hp:4 * hp + 4].rearrange("h k v -> k h v")),
                  writes=[B_Sld])
        P.op("dve", lambda e: e.memset(S[:], 0.0), writes=[B_S])
        P.op("dve", lambda e: e.memset(Sb[:], 0.0), writes=[B_Sb])

        for p in range(NB + 1):
            own = p >= OWN0
            sample = p == NB
            xb, Bx = xt[p % 2]
            P.dma("pool", lambda e, xb=xb, p=p: e.dma_start(out=xb[:, :, :], in_=xf_v[:, :, p * 128:(p + 1) * 128]), writes=[Bx])

            def proj(cg, xb=xb, Bx=Bx):
                pt, Bp = next_ps("a")
                for kc in range(KC):
                    P.op("pe", lambda e, pt=pt, kc=kc, cg=cg, xb=xb: e.matmul(
                        pt[:, :], lhsT=xb[:, kc, :], rhs=W3[:, kc, cg * 512:(cg + 1) * 512],
                        start=(kc == 0), stop=(kc == KC - 1)), reads=[Bx, B_W3], writes=[Bp])
                return pt, Bp

            def ew(eng, name, fn, reads, writes_name):
                t, Bt = T[writes_name]
                P.op(eng, fn, reads=reads, writes=[Bt])

            kk, Bkk = T["kk"]
            lgf, Blgf = T["lgf"]
            pt, Bp = proj(1)
            P.op("dve", lambda e, pt=pt: e.tensor_tensor(out=kk[:], in0=pt[:, :], in1=b3[:, 512:1024], op=ALU.add),
                 reads=[Bp, B_b3], writes=[Bkk])
            P.op("act", lambda e: e.activation(out=kk[:], in_=kk[:], func=AF.Sigmoid, scale=-1.0), reads=[Bkk], writes=[Bkk])
            P.op("dve", lambda e: e.tensor_tensor(out=kk[:], in0=kk[:], in1=oml[:], op=ALU.mult), reads=[Bkk, B_oml], writes=[Bkk])
            P.op("act", lambda e: e.activation(out=lgf[:], in_=kk[:], func=AF.Ln, scale=-1.0, bias=ones_c[:, :]),
                 reads=[Bkk, B_ones], writes=[Blgf])
            pt, Bp = proj(2)
            if own:
                P.op("dve", lambda e, pt=pt: e.tensor_tensor(out=vv[:], in0=pt[:, :], in1=b3[:, 1024:1536], op=ALU.add),
                     reads=[Bp, B_b3], writes=[B_vv])
            else:
                osb_, Bosb_ = T["osb"]
                P.op("dve", lambda e, pt=pt: e.tensor_tensor(out=osb_[:], in0=pt[:, :], in1=b3[:, 1024:1536], op=ALU.add),
                     reads=[Bp, B_b3], writes=[Bosb_])
                P.op("dve", lambda e, p=p: e.tensor_scalar(out=vv[:], in0=osb_[:], scalar1=vmask[:, p:p + 1], scalar2=None, op0=ALU.mult),
                     reads=[Bosb_, B_vmask], writes=[B_vv])

            if not own:
                edd, Bedd = T["edd"]
                pt, Bp = next_ps("a")
                P.op("pe", lambda e, pt=pt: e.matmul(pt[:, :], lhsT=lm[:, :], rhs=lgf[:, :], start=True, stop=True),
                     reads=[B_lm, Blgf], writes=[Bp])
                P.op("act", lambda e, pt=pt: e.activation(out=edd[:], in_=pt[:, :], func=AF.Exp), reads=[Bp], writes=[Bedd])
                P.op("dve", lambda e: e.tensor_tensor(out=kdb[:], in0=kk[:], in1=edd[:], op=ALU.mult), reads=[Bkk, Bedd], writes=[B_kdb])
                pt, Bp = next_ps("b")
                for h in range(4):
                    P.op("pe", lambda e, pt=pt, h=h: e.matmul(pt[:, h:h + 1], lhsT=lgf[:, h * 128:(h + 1) * 128], rhs=ones_c[:, :],
                                                              start=True, stop=True), reads=[Blgf, B_ones], writes=[Bp])
                P.op("act", lambda e, pt=pt: e.activation(out=edl[:, 0:4], in_=pt[:, 0:4], func=AF.Exp), reads=[Bp], writes=[B_edl])
                pt, Bp = next_ps("b")
                for h in range(4):
                    P.op("pe", lambda e, pt=pt, h=h: e.matmul(pt[:, h * 128:(h + 1) * 128], lhsT=kdb[:, h * 128:(h + 1) * 128],
                                                              rhs=vv[:, h * 128:(h + 1) * 128], start=True, stop=True),
                         reads=[B_kdb, B_vv], writes=[Bp])
                for h in range(4):
                    P.op("dve", lambda e, pt=pt, h=h: e.scalar_tensor_tensor(
                        out=S[:, h, :], in0=S[:, h, :], scalar=edl[:, h:h + 1], in1=pt[:, h * 128:(h + 1) * 128],
                        op0=ALU.mult, op1=ALU.add), reads=[Bp, B_edl, B_S], writes=[B_S])
                if p == OWN0 - 1:
                    P.op("dve", lambda e: e.tensor_copy(out=Sb[:], in_=S[:]), reads=[B_S], writes=[B_Sb])
                continue

            ebc, Bebc = T["ebc"]
            ebi, Bebi = T["ebi"]
            edd, Bedd = T["edd"]
            qe, Bqe = T["qe"]
            ke, Bke = T["ke"]
            hgb, Bhgb = T["hgb"]
            osb, Bosb = T["osb"]
            og, Bog = T["og"]
            pt, Bp = next_ps("a")
            P.op("pe", lambda e, pt=pt: e.matmul(pt[:, :], lhsT=u32[:, :], rhs=lgf[:, :], start=True, stop=True),
                 reads=[B_u32, Blgf], writes=[Bp])
            P.op("act", lambda e, pt=pt: e.activation(out=ebc[:], in_=pt[:, :], func=AF.Exp), reads=[Bp], writes=[Bebc])
            P.op("act", lambda e, pt=pt: e.activation(out=ebi[:], in_=pt[:, :], func=AF.Exp, scale=-1.0), reads=[Bp], writes=[Bebi])
            pt, Bp = next_ps("a")
            P.op("pe", lambda e, pt=pt: e.matmul(pt[:, :], lhsT=l32[:, :], rhs=lgf[:, :], start=True, stop=True),
                 reads=[B_l32, Blgf], writes=[Bp])
            P.op("act", lambda e, pt=pt: e.activation(out=edd[:], in_=pt[:, :], func=AF.Exp), reads=[Bp], writes=[Bedd])
            pt, Bp = next_ps("b")
            for h in range(4):
                P.op("pe", lambda e, pt=pt, h=h: e.matmul(pt[:, h * 4:h * 4 + 4], lhsT=lgf[:, h * 128:(h + 1) * 128], rhs=ind4[:, :],
                                                          start=True, stop=True), reads=[Blgf, B_ind4], writes=[Bp])
            P.op("act", lambda e, pt=pt: e.activation(out=edl[:, 0:16], in_=pt[:, 0:16], func=AF.Exp), reads=[Bp], writes=[B_edl])
            pt, Bp = proj(0)
            P.op("dve", lambda e, pt=pt: e.tensor_tensor(out=qe[:], in0=pt[:, :], in1=b3[:, 0:512], op=ALU.add),
                 reads=[Bp, B_b3], writes=[Bqe])
            P.op("act", lambda e: e.activation(out=og[:], in_=qe[:], func=AF.Sigmoid), reads=[Bqe], writes=[Bog])
            P.op("dve", lambda e: e.tensor_tensor(out=qe[:], in0=qe[:], in1=og[:], op=ALU.mult), reads=[Bqe, Bog], writes=[Bqe])
            P.op("dve", lambda e: e.tensor_tensor(out=qe[:], in0=qe[:], in1=ebc[:], op=ALU.mult), reads=[Bqe, Bebc], writes=[Bqe])
            P.op("dve", lambda e: e.tensor_tensor(out=ke[:], in0=kk[:], in1=ebi[:], op=ALU.mult), reads=[Bkk, Bebi], writes=[Bke])
            P.op("dve", lambda e: e.tensor_tensor(out=edd[:], in0=kk[:], in1=edd[:], op=ALU.mult), reads=[Bkk, Bedd], writes=[Bedd])
            if not sample:
                for c in range(4):
                    P.op("dve", lambda e, c=c: e.tensor_scalar(out=kd4[:, c, :], in0=edd[:], scalar1=ind4[:, c:c + 1], scalar2=None,
                                                               op0=ALU.mult), reads=[Bedd, B_ind4], writes=[B_kd4])
            pt, Bp = proj(3)
            P.op("dve", lambda e, pt=pt: e.tensor_tensor(out=hgb[:], in0=pt[:, :], in1=b3[:, 1536:2048], op=ALU.add),
                 reads=[Bp, B_b3], writes=[Bhgb])
            P.op("act", lambda e: e.activation(out=og[:], in_=hgb[:], func=AF.Sigmoid), reads=[Bhgb], writes=[Bog])
            P.op("dve", lambda e: e.tensor_tensor(out=hgb[:], in0=hgb[:], in1=og[:], op=ALU.mult), reads=[Bhgb, Bog], writes=[Bhgb])
            P.op("dve", lambda e: e.tensor_tensor(out=hgb[:], in0=hgb[:], in1=ngb[:], op=ALU.mult), reads=[Bhgb, B_ngb], writes=[Bhgb])
            for h in range(4):
                pt, Bp = next_ps("a")
                P.op("pe", lambda e, pt=pt, h=h: e.transpose(out=pt[:, 0:128], in_=qe[:, h * 128:(h + 1) * 128], identity=ident[:, :]),
                     reads=[Bqe, B_ident], writes=[Bp])
                for c in range(4):
                    P.op("act", lambda e, pt=pt, h=h, c=c: e.activation(out=qeT4[:, h, c, 32 * c:32 * c + 32], in_=pt[:, 32 * c:32 * c + 32],
                                                                         func=AF.Identity), reads=[Bp], writes=[B_qeT4])
                pt, Bp = next_ps("a")
                P.op("pe", lambda e, pt=pt, h=h: e.transpose(out=pt[:, 0:128], in_=ke[:, h * 128:(h + 1) * 128], identity=ident[:, :]),
                     reads=[Bke, B_ident], writes=[Bp])
                P.op("dve", lambda e, pt=pt, h=h: e.tensor_copy(out=keT[:, h, :], in_=pt[:, 0:128]), reads=[Bp], writes=[B_keT])
            for h in range(4):
                pt, Bp = next_ps("a")
                for c in range(4):
                    P.op("pe", lambda e, pt=pt, h=h, c=c: e.matmul(pt[:, 0:128], lhsT=keT[:, h, :], rhs=qeT4[:, h, c, :],
                                                                   start=(c == 0), stop=(c == 3)), reads=[B_keT, B_qeT4], writes=[Bp])
                P.op("dve", lambda e, pt=pt, h=h: e.tensor_tensor(out=attm[:, h, :], in0=pt[:, 0:128], in1=u32[:, :], op=ALU.mult),
                     reads=[Bp, B_u32], writes=[B_attm])
            po, Bpo = next_ps("b")
            for h in range(4):
                for c in range(4):
                    if sample:
                        rhs_fn = lambda c=c, h=h: Sld[:, c, h, :]
                        Brhs = B_Sld
                    else:
                        rhs_fn = lambda c=c, h=h: Sb[:, h, :]
                        Brhs = B_Sb
                    P.op("pe", lambda e, po=po, h=h, c=c, rhs_fn=rhs_fn: e.matmul(
                        po[:, h * 128:(h + 1) * 128], lhsT=qeT4[:, h, c, :], rhs=rhs_fn(), start=(c == 0), stop=False),
                        reads=[B_qeT4, Brhs], writes=[Bpo])
                    if not sample:
                        ps2, Bp2 = next_ps("a")
                        P.op("pe", lambda e, ps2=ps2, h=h, c=c: e.matmul(ps2[:, 0:128], lhsT=kd4[:, c, h * 128:(h + 1) * 128],
                                                                         rhs=vv[:, h * 128:(h + 1) * 128], start=True, stop=True),
                             reads=[B_kd4, B_vv], writes=[Bp2])
                        P.op("dve", lambda e, ps2=ps2, h=h, c=c: e.scalar_tensor_tensor(
                            out=S[:, h, :], in0=S[:, h, :], scalar=edl[:, h * 4 + c:h * 4 + c + 1], in1=ps2[:, 0:128],
                            op0=ALU.mult, op1=ALU.add), reads=[Bp2, B_edl, B_S], writes=[B_S])
                        P.op("act", lambda e, h=h: e.activation(out=Sb[:, h, :], in_=S[:, h, :], func=AF.Identity),
                             reads=[B_S], writes=[B_Sb])
                P.op("pe", lambda e, po=po, h=h: e.matmul(po[:, h * 128:(h + 1) * 128], lhsT=attm[:, h, :], rhs=vv[:, h * 128:(h + 1) * 128],
                                                          start=False, stop=True), reads=[B_attm, B_vv], writes=[Bpo])
            P.op("act", lambda e, po=po: e.activation(out=osb[:], in_=po[:, :], func=AF.Identity), reads=[Bpo], writes=[Bosb])
            P.op("dve", lambda e: e.tensor_tensor(out=og[:], in0=osb[:], in1=osb[:], op=ALU.mult), reads=[Bosb], writes=[Bog])
            P.op("dve", lambda e: e.reduce_sum(out=ss[:, 0:4], in_=og[:].rearrange("p (h v) -> p h v", h=4), axis=mybir.AxisListType.X),
                 reads=[Bog], writes=[B_ss])
            P.op("act", lambda e: e.activation(out=ss[:, 0:4], in_=ss[:, 0:4], func=AF.Sqrt, scale=1.0 / 128.0, bias=eps_c[:, :]),
                 reads=[B_ss, B_eps], writes=[B_ss])
            P.op("dve", lambda e: e.reciprocal(out=ss[:, 0:4], in_=ss[:, 0:4]), reads=[B_ss], writes=[B_ss])
            for h in range(4):
                P.op("dve", lambda e, h=h: e.scalar_tensor_tensor(
                    out=og[:, h * 128:(h + 1) * 128], in0=osb[:, h * 128:(h + 1) * 128], scalar=ss[:, h:h + 1],
                    in1=hgb[:, h * 128:(h + 1) * 128], op0=ALU.mult, op1=ALU.mult), reads=[Bosb, B_ss, Bhgb], writes=[Bog])
            if debug:
                k_ = p - OWN0
                P.dma("sp", lambda e, k_=k_, hp=hp: e.dma_start(out=L["d_ohg"][k_ * 128:(k_ + 1) * 128, hp * 512:(hp + 1) * 512], in_=og[:, :]),
                      reads=[Bog])
            for h in range(4):
                pt, Bp = next_ps("a")
                P.op("pe", lambda e, pt=pt, h=h: e.transpose(out=pt[:, 0:128], in_=og[:, h * 128:(h + 1) * 128], identity=ident[:, :]),
                     reads=[Bog, B_ident], writes=[Bp])
                P.op("dve", lambda e, pt=pt, h=h, p=p, hp=hp: e.tensor_copy(
                    out=o_hgT[:, 4 * hp + h, (p - OWN0) * 128:(p - OWN0 + 1) * 128], in_=pt[:, 0:128]), reads=[Bp], writes=[B_ohgT])
        P.barrier()


def tail_moe(nc, P, sc, sbt, next_ps, L):
    xf = L["xf"]
    ident, B_ident = L["ident"], L["B_ident"]
    ones_c, B_ones = L["ones_c"], L["B_ones"]
    o_nsaT, B_onsaT, o_hgT, B_ohgT = L["o_nsaT"], L["B_onsaT"], L["o_hgT"], L["B_ohgT"]
    debug = L["debug"]
    y_out = L["y_out"]
    xf_v = xf.rearrange("(kc p) t -> p kc t", p=128)
    T0 = OWN0 * 128

    zacc, B_zacc = sbt(sc, "zacc", [128, KC, NT], F32)
    hT, B_hT = L["ohT"], L["B_oh"]
    lncol, B_lncol = sbt(sc, "lncol", [128, 4, KC], F32)
    for i, nm in enumerate(("ln1_g", "ln1_b", "ln2_g", "ln2_b")):
        P.dma("sp", lambda e, i=i, nm=nm: e.dma_start(out=lncol[:, i, :], in_=L[nm].rearrange("(kc p) -> p kc", p=128),
                                                      allow_slow_non_contiguous=True), writes=[B_lncol])
    eps_c, B_eps = sbt(sc, "eps_t", [128, 1], F32)
    P.op("dve", lambda e: e.memset(eps_c[:], LN_EPS), writes=[B_eps])
    onesr, B_onesr = sbt(sc, "onesr", [1, 128], F32)
    P.op("dve", lambda e: e.memset(onesr[:], 1.0), writes=[B_onesr])

    def layer_norm(gi, bi, emit_out):
        for (t0, tw) in TT:
            p1, Bp1 = next_ps("b")
            p2, Bp2 = next_ps("b")
            for m in range(KC):
                P.op("pe", lambda e, p1=p1, m=m, t0=t0, tw=tw: e.matmul(p1[0:1, 0:tw], lhsT=ones_c[:, 0:1], rhs=zacc[:, m, t0:t0 + tw],
                                                                        start=(m == 0), stop=(m == KC - 1)),
                     reads=[B_ones, B_zacc], writes=[Bp1])
                P.op("act", lambda e, m=m, t0=t0, tw=tw: e.activation(out=sqs[:, 0:tw], in_=zacc[:, m, t0:t0 + tw], func=AF.Square),
                     reads=[B_zacc], writes=[B_sqs])
                P.op("pe", lambda e, p2=p2, m=m, tw=tw: e.matmul(p2[0:1, 0:tw], lhsT=ones_c[:, 0:1], rhs=sqs[:, 0:tw],
                                                                 start=(m == 0), stop=(m == KC - 1)),
                     reads=[B_ones, B_sqs], writes=[Bp2])
            P.op("dve", lambda e, p1=p1, t0=t0, tw=tw: e.tensor_scalar(out=stat[:, 0, t0:t0 + tw], in0=p1[0:1, 0:tw], scalar1=1.0 / D_MODEL,
                                                                       scalar2=None, op0=ALU.mult), reads=[Bp1], writes=[B_stat])
            P.op("dve", lambda e, p2=p2, t0=t0, tw=tw: e.tensor_scalar(out=stat[:, 1, t0:t0 + tw], in0=p2[0:1, 0:tw], scalar1=1.0 / D_MODEL,
                                                                       scalar2=None, op0=ALU.mult), reads=[Bp2], writes=[B_stat])
            P.op("dve", lambda e, t0=t0, tw=tw: e.tensor_tensor(out=sqs[0:1, 0:tw], in0=stat[:, 0, t0:t0 + tw], in1=stat[:, 0, t0:t0 + tw],
                                                                op=ALU.mult), reads=[B_stat], writes=[B_sqs])
            P.op("dve", lambda e, t0=t0, tw=tw: e.tensor_tensor(out=stat[:, 1, t0:t0 + tw], in0=stat[:, 1, t0:t0 + tw], in1=sqs[0:1, 0:tw],
                                                                op=ALU.subtract), reads=[B_stat, B_sqs], writes=[B_stat])
            P.op("act", lambda e, t0=t0, tw=tw: e.activation(out=stat[:, 1, t0:t0 + tw], in_=stat[:, 1, t0:t0 + tw], func=AF.Sqrt,
                                                             bias=eps_c[0:1, :]), reads=[B_stat, B_eps], writes=[B_stat])
            P.op("dve", lambda e, t0=t0, tw=tw: e.reciprocal(out=stat[:, 1, t0:t0 + tw], in_=stat[:, 1, t0:t0 + tw]),
                 reads=[B_stat], writes=[B_stat])
            for r in range(2):
                pb, Bpb = next_ps("b")
                P.op("pe", lambda e, pb=pb, r=r, t0=t0, tw=tw: e.matmul(pb[:, 0:tw], lhsT=onesr[0:1, :], rhs=stat[:, r, t0:t0 + tw],
                                                                        start=True, stop=True), reads=[B_onesr, B_stat], writes=[Bpb])
                P.op("act", lambda e, pb=pb, r=r, t0=t0, tw=tw: e.activation(out=mbc[:, r, t0:t0 + tw], in_=pb[:, 0:tw], func=AF.Identity),
                     reads=[Bpb], writes=[B_mbc])
        for m in range(KC):
            P.op("dve", lambda e, m=m: e.tensor_tensor(out=zacc[:, m, :], in0=zacc[:, m, :], in1=mbc[:, 0, :], op=ALU.subtract),
                 reads=[B_zacc, B_mbc], writes=[B_zacc])
            P.op("dve", lambda e, m=m: e.tensor_tensor(out=zacc[:, m, :], in0=zacc[:, m, :], in1=mbc[:, 1, :], op=ALU.mult),
                 reads=[B_zacc, B_mbc], writes=[B_zacc])
            P.op("dve", lambda e, m=m: e.tensor_scalar(out=zacc[:, m, :], in0=zacc[:, m, :], scalar1=lncol[:, gi, m:m + 1],
                                                       scalar2=lncol[:, bi, m:m + 1], op0=ALU.mult, op1=ALU.add),
                 reads=[B_zacc, B_lncol], writes=[B_zacc])
            emit_out(m)

    t1 = contextlib.ExitStack()
    with t1:
        mT, B_mT = sbt(t1, "mT", [128, KC, NT], BF16)
        t1a = t1.enter_context(contextlib.ExitStack())
        xt_, B_xt = sbt(t1a, "xt_", [128, KC, 512], BF16)
        wpa = [sbt(t1a, "wpa%d" % i, [128, 8, 128], BF16) for i in range(2)]
        wpb = [sbt(t1a, "wpb%d" % i, [128, 8, 128], BF16) for i in range(2)]
        wmg = [sbt(t1a, "wmg%d" % i, [128, KC, 256], BF16) for i in range(2)]
        bmg, B_bmg = sbt(t1a, "bmg", [128, 32], F32)
        sga, B_sga = sbt(t1a, "sga", [128, 512], F32)
        sgb, B_sgb = sbt(t1a, "sgb", [128, 512], F32)
        P.dma("sp", lambda e: e.dma_start(out=bmg[:], in_=L["b_mg"].rearrange("(cb p) -> p cb", p=128), allow_slow_non_contiguous=True),
              writes=[B_bmg])
        wmg_v = L["w_mg"].rearrange("(kc p) c -> p kc c", p=128)
        wpa_v = L["w_pa"].rearrange("(kc p) c -> p kc c", p=128)
        wpb_v = L["w_pb"].rearrange("(kc p) c -> p kc c", p=128)
        it = 0
        for (t0, tw) in TT:
            for q in range(2):
                P.dma("pool", lambda e, q=q, t0=t0, tw=tw: e.dma_start(out=xt_[:, 8 * q:8 * q + 8, 0:tw],
                                                                       in_=xf_v[:, 8 * q:8 * q + 8, T0 + t0:T0 + t0 + tw]), writes=[B_xt])
            for m in range(KC):
                wm, Bwm = wmg[it % 2]
                wa, Bwa = wpa[it % 2]
                wb, Bwb = wpb[it % 2]
                it += 1
                P.dma("pool", lambda e, wm=wm, m=m: e.dma_start(out=wm[:, :, 0:128], in_=wmg_v[:, :, m * 128:(m + 1) * 128]), writes=[Bwm])
                P.dma("pool", lambda e, wm=wm, m=m: e.dma_start(out=wm[:, :, 128:256], in_=wmg_v[:, :, 2048 + m * 128:2048 + (m + 1) * 128]),
                      writes=[Bwm])
                P.dma("pool", lambda e, wa=wa, m=m: e.dma_start(out=wa[:, :, :], in_=wpa_v[:, :, m * 128:(m + 1) * 128]), writes=[Bwa])
                P.dma("pool", lambda e, wb=wb, m=m: e.dma_start(out=wb[:, :, :], in_=wpb_v[:, :, m * 128:(m + 1) * 128]), writes=[Bwb])
                pA, BpA = next_ps("a")
                pB, BpB = next_ps("a")
                pga, Bpga = next_ps("a")
                pgb, Bpgb = next_ps("a")
                for kc in range(8):
                    P.op("pe", lambda e, pA=pA, kc=kc, wa=wa, t0=t0, tw=tw: e.matmul(
                        pA[:, 0:tw], lhsT=wa[:, kc, :], rhs=o_nsaT[:, kc, t0:t0 + tw], start=(kc == 0), stop=(kc == 7)),
                        reads=[Bwa, B_onsaT], writes=[BpA])
                for kc in range(8):
                    P.op("pe", lambda e, pB=pB, kc=kc, wb=wb, t0=t0, tw=tw: e.matmul(
                        pB[:, 0:tw], lhsT=wb[:, kc, :], rhs=o_hgT[:, kc, t0:t0 + tw], start=(kc == 0), stop=(kc == 7)),
                        reads=[Bwb, B_ohgT], writes=[BpB])
                for kc in range(KC):
                    P.op("pe", lambda e, pga=pga, kc=kc, wm=wm, tw=tw: e.matmul(
                        pga[:, 0:tw], lhsT=wm[:, kc, 0:128], rhs=xt_[:, kc, 0:tw], start=(kc == 0), stop=(kc == KC - 1)),
                        reads=[Bwm, B_xt], writes=[Bpga])
                for kc in range(KC):
                    P.op("pe", lambda e, pgb=pgb, kc=kc, wm=wm, tw=tw: e.matmul(
                        pgb[:, 0:tw], lhsT=wm[:, kc, 128:256], rhs=xt_[:, kc, 0:tw], start=(kc == 0), stop=(kc == KC - 1)),
                        reads=[Bwm, B_xt], writes=[Bpgb])
                P.op("act", lambda e, pga=pga, m=m, tw=tw: e.activation(out=sga[:, 0:tw], in_=pga[:, 0:tw], func=AF.Sigmoid,
                                                                        bias=bmg[:, m:m + 1]), reads=[Bpga, B_bmg], writes=[B_sga])
                P.op("act", lambda e, pgb=pgb, m=m, tw=tw: e.activation(out=sgb[:, 0:tw], in_=pgb[:, 0:tw], func=AF.Sigmoid,
                                                                        bias=bmg[:, 16 + m:17 + m]), reads=[Bpgb, B_bmg], writes=[B_sgb])
                P.op("dve", lambda e, pA=pA, tw=tw: e.tensor_tensor(out=sga[:, 0:tw], in0=sga[:, 0:tw], in1=pA[:, 0:tw], op=ALU.mult),
                     reads=[BpA, B_sga], writes=[B_sga])
                P.op("dve", lambda e, pB=pB, tw=tw: e.tensor_tensor(out=sgb[:, 0:tw], in0=sgb[:, 0:tw], in1=pB[:, 0:tw], op=ALU.mult),
                     reads=[BpB, B_sgb], writes=[B_sgb])
                P.op("dve", lambda e, m=m, t0=t0, tw=tw: e.tensor_tensor(out=mT[:, m, t0:t0 + tw], in0=sga[:, 0:tw], in1=sgb[:, 0:tw], op=ALU.add),
                     reads=[B_sga, B_sgb], writes=[B_mT])
        P.barrier()
        t1a.close()
        wout = [sbt(t1, "wout%d" % i, [128, KC, 128], BF16) for i in range(2)]
        xres = [sbt(t1, "xres%d" % i, [128, NT], F32) for i in range(2)]
        wout_v = L["w_out"].rearrange("(kc p) c -> p kc c", p=128)
        for m in range(KC):
            xr, Bxr = xres[m % 2]
            wo, Bwo = wout[m % 2]
            P.dma("pool", lambda e, wo=wo, m=m: e.dma_start(out=wo[:, :, :], in_=wout_v[:, :, m * 128:(m + 1) * 128]), writes=[Bwo])
            P.dma("sp", lambda e, xr=xr, m=m: e.dma_start(out=xr[:, :], in_=xf[m * 128:(m + 1) * 128, T0:T0 + NT]), writes=[Bxr])
            for (t0, tw) in TT:
                pt, Bp = next_ps("a")
                for kc in range(KC):
                    P.op("pe", lambda e, pt=pt, kc=kc, wo=wo, t0=t0, tw=tw: e.matmul(
                        pt[:, 0:tw], lhsT=wo[:, kc, :], rhs=mT[:, kc, t0:t0 + tw], start=(kc == 0), stop=(kc == KC - 1)),
                        reads=[Bwo, B_mT], writes=[Bp])
                P.op("dve", lambda e, pt=pt, xr=xr, m=m, t0=t0, tw=tw: e.scalar_tensor_tensor(
                    out=zacc[:, m, t0:t0 + tw], in0=xr[:, t0:t0 + tw], scalar=ALPHA, in1=pt[:, 0:tw], op0=ALU.mult, op1=ALU.add),
                    reads=[Bp, Bxr], writes=[B_zacc])
        P.barrier()

    stat, B_stat = sbt(sc, "stat", [1, 2, NT], F32)
    mbc, B_mbc = sbt(sc, "mbc", [128, 2, NT], F32)
    sqs, B_sqs = sbt(sc, "sqs", [128, 512], F32)

    def after_ln1(m):
        P.op("act", lambda e, m=m: e.activation(out=hT[:, m, :], in_=zacc[:, m, :], func=AF.Identity), reads=[B_zacc], writes=[B_hT])
        if debug:
            P.dma("sp", lambda e, m=m: e.dma_start(out=L["d_h"][m * 128:(m + 1) * 128, :], in_=zacc[:, m, :]), reads=[B_zacc])
        P.op("dve", lambda e, m=m: e.tensor_scalar(out=zacc[:, m, :], in0=zacc[:, m, :], scalar1=ALPHA, scalar2=None, op0=ALU.mult),
             reads=[B_zacc], writes=[B_zacc])
    layer_norm(0, 1, after_ln1)
    P.barrier()

    t3 = contextlib.ExitStack()
    with t3:
        wr, B_wr = sbt(t3, "wr", [128, KC, 36], BF16)
        brt, B_brt = sbt(t3, "brt", [128, 36], F32)
        lg, B_lg = sbt(t3, "lg", [128, 36], F32)
        lem, B_lem = sbt(t3, "lem", [128, 32], F32)
        sm, B_sm = sbt(t3, "sm", [128, 16], F32)
        m8, B_m8 = sbt(t3, "m8r", [128, 8], F32)
        gate, B_gate = sbt(t3, "gate", [128, 32], F32)
        gate2, B_gate2 = sbt(t3, "gate2", [128, 32], F32)
        gT, B_gT = sbt(t3, "gT", [32, NT], F32)
        gThi, B_gThi = sbt(t3, "gThi", [32, NT], BF16)
        gTlo, B_gTlo = sbt(t3, "gTlo", [32, NT], BF16)
        selE, B_selE = sbt(t3, "selE", [32, 32 * 128], BF16)
        gbc, B_gbc = sbt(t3, "gbc", [128, NT], F32)
        hid, B_hid = sbt(t3, "hid", [128, 4, NT], BF16)
        sil, B_sil = sbt(t3, "sil", [128, 512], F32)
        ug, B_ug = sbt(t3, "ugm", [128, 512], F32)
        wgu = [sbt(t3, "wgu%d" % i, [128, KC, 2, 128], BF16) for i in range(2)]
        wd = [sbt(t3, "wd%d" % i, [128, 4, D_MODEL], BF16) for i in range(1)]
        P.dma("pool", lambda e: e.dma_start(out=wr[:], in_=L["w_r"].rearrange("(kc p) c -> p kc c", p=128)), writes=[B_wr])
        P.dma("sp", lambda e: e.dma_start(out=brt[:], in_=L["b_r"].partition_broadcast(128)), writes=[B_brt])
        P.dma("sp", lambda e: e.dma_start(out=selE[:], in_=L["t_selE"]), writes=[B_selE])
        for blk in range(NOWN + 1):
            bs_ = slice(blk * 128, (blk + 1) * 128)
            pt, Bp = next_ps("b")
            for kc in range(KC):
                P.op("pe", lambda e, pt=pt, kc=kc, bs_=bs_: e.matmul(pt[:, 0:36], lhsT=hT[:, kc, bs_], rhs=wr[:, kc, :],
                                                                     start=(kc == 0), stop=(kc == KC - 1)), reads=[B_hT, B_wr], writes=[Bp])
            P.op("dve", lambda e, pt=pt: e.tensor_tensor(out=lg[:], in0=pt[:, 0:36], in1=brt[:], op=ALU.add), reads=[Bp, B_brt], writes=[B_lg])
            P.op("dve", lambda e: e.reduce_max(out=sm[:, 0:1], in_=lg[:, 0:4], axis=mybir.AxisListType.X), reads=[B_lg], writes=[B_sm])
            P.op("dve", lambda e: e.tensor_scalar(out=sm[:, 1:2], in0=sm[:, 0:1], scalar1=-1.0, scalar2=None, op0=ALU.mult),
                 reads=[B_sm], writes=[B_sm])
            P.op("act", lambda e: e.activation(out=sm[:, 4:8], in_=lg[:, 0:4], func=AF.Exp, bias=sm[:, 1:2]), reads=[B_lg, B_sm], writes=[B_sm])
            P.op("dve", lambda e: e.reduce_sum(out=sm[:, 2:3], in_=sm[:, 4:8], axis=mybir.AxisListType.X), reads=[B_sm], writes=[B_sm])
            P.op("dve", lambda e: e.reciprocal(out=sm[:, 2:3], in_=sm[:, 2:3]), reads=[B_sm], writes=[B_sm])
            P.op("dve", lambda e: e.tensor_scalar(out=sm[:, 8:12], in0=lg[:, 0:4], scalar1=sm[:, 0:1], scalar2=None, op0=ALU.is_ge),
                 reads=[B_lg, B_sm], writes=[B_sm])
            P.op("dve", lambda e: e.tensor_scalar(out=sm[:, 8:12], in0=sm[:, 8:12], scalar1=1e30, scalar2=-1e30, op0=ALU.mult, op1=ALU.add),
                 reads=[B_sm], writes=[B_sm])
            for g in range(4):
                P.op("dve", lambda e, g=g: e.tensor_scalar(out=lem[:, g * 8:(g + 1) * 8], in0=lg[:, 4 + g * 8:4 + (g + 1) * 8],
                                                           scalar1=sm[:, 8 + g:9 + g], scalar2=None, op0=ALU.add),
                     reads=[B_lg, B_sm], writes=[B_lem])
            P.op("dve", lambda e: e.max(out=m8[:, 0:8], in_=lem[:, :]), reads=[B_lem], writes=[B_m8])
            P.op("dve", lambda e: e.tensor_tensor(out=sm[:, 12:13], in0=m8[:, 0:1], in1=m8[:, 1:2], op=ALU.subtract), reads=[B_m8], writes=[B_sm])
            P.op("act", lambda e: e.activation(out=sm[:, 13:14], in_=sm[:, 12:13], func=AF.Sigmoid), reads=[B_sm], writes=[B_sm])
            P.op("act", lambda e: e.activation(out=sm[:, 14:15], in_=sm[:, 12:13], func=AF.Sigmoid, scale=-1.0), reads=[B_sm], writes=[B_sm])
            P.op("dve", lambda e: e.tensor_scalar(out=sm[:, 13:15], in0=sm[:, 13:15], scalar1=sm[:, 2:3], scalar2=None, op0=ALU.mult),
                 reads=[B_sm], writes=[B_sm])
            P.op("dve", lambda e: e.tensor_scalar(out=gate[:], in0=lem[:], scalar1=m8[:, 0:1], scalar2=sm[:, 13:14], op0=ALU.is_equal, op1=ALU.mult),
                 reads=[B_lem, B_m8, B_sm], writes=[B_gate])
            P.op("dve", lambda e: e.tensor_scalar(out=gate2[:], in0=lem[:], scalar1=m8[:, 1:2], scalar2=sm[:, 14:15], op0=ALU.is_equal, op1=ALU.mult),
                 reads=[B_lem, B_m8, B_sm], writes=[B_gate2])
            P.op("dve", lambda e: e.tensor_tensor(out=gate[:], in0=gate[:], in1=gate2[:], op=ALU.add), reads=[B_gate, B_gate2], writes=[B_gate])
            pt, Bp = next_ps("b")
            P.op("pe", lambda e, pt=pt: e.transpose(out=pt[0:32, 0:128], in_=gate[:, :], identity=ident[:, :]), reads=[B_gate, B_ident], writes=[Bp])
            P.op("dve", lambda e, pt=pt, bs_=bs_: e.tensor_copy(out=gT[:, bs_], in_=pt[0:32, 0:128]), reads=[Bp], writes=[B_gT])
        if debug:
            P.dma("sp", lambda e: e.dma_start(out=L["d_gate"], in_=gT[:, :]), reads=[B_gT])
        P.op("dve", lambda e: e.tensor_copy(out=gThi[:], in_=gT[:]), reads=[B_gT], writes=[B_gThi])
        P.op("dve", lambda e: e.tensor_tensor(out=gT[:], in0=gT[:], in1=gThi[:], op=ALU.subtract), reads=[B_gT, B_gThi], writes=[B_gT])
        P.op("dve", lambda e: e.tensor_copy(out=gTlo[:], in_=gT[:]), reads=[B_gT], writes=[B_gTlo])

        NE = L["n_experts"]
        wg_v = L["w_gate"].rearrange("e (kc p) f -> e p kc f", p=128)
        wu_v = L["w_up"].rearrange("e (kc p) f -> e p kc f", p=128)
        wd_v = L["w_down"].rearrange("e (fc p) d -> e p fc d", p=128)
        for ex in range(NE):
            wdt, Bwd = wd[0]
            for q in range(2):
                P.dma("pool", lambda e, wdt=wdt, ex=ex, q=q: e.dma_start(out=wdt[:, 2 * q:2 * q + 2, :], in_=wd_v[ex, :, 2 * q:2 * q + 2, :]), writes=[Bwd])
            for (t0, tw) in TT:
                pb, Bpb = next_ps("b")
                P.op("pe", lambda e, pb=pb, ex=ex, t0=t0, tw=tw: e.matmul(pb[:, 0:tw], lhsT=selE[:, ex * 128:(ex + 1) * 128], rhs=gThi[:, t0:t0 + tw],
                                                                          start=True, stop=False), reads=[B_selE, B_gThi], writes=[Bpb])
                P.op("pe", lambda e, pb=pb, ex=ex, t0=t0, tw=tw: e.matmul(pb[:, 0:tw], lhsT=selE[:, ex * 128:(ex + 1) * 128], rhs=gTlo[:, t0:t0 + tw],
                                                                          start=False, stop=True), reads=[B_selE, B_gTlo], writes=[Bpb])
                P.op("act", lambda e, pb=pb, t0=t0, tw=tw: e.activation(out=gbc[:, t0:t0 + tw], in_=pb[:, 0:tw], func=AF.Identity),
                     reads=[Bpb], writes=[B_gbc])
            for fc in range(4):
                wt, Bwt = wgu[(ex * 4 + fc) % 2]
                P.dma("pool", lambda e, wt=wt, ex=ex, fc=fc: e.dma_start(out=wt[:, :, 0, :], in_=wg_v[ex, :, :, fc * 128:(fc + 1) * 128]), writes=[Bwt])
                P.dma("pool", lambda e, wt=wt, ex=ex, fc=fc: e.dma_start(out=wt[:, :, 1, :], in_=wu_v[ex, :, :, fc * 128:(fc + 1) * 128]), writes=[Bwt])
                if True:
                    for (t0, tw) in TT:
                        pg, Bpg = next_ps("a")
                        pu, Bpu = next_ps("a")
                        for kc in range(KC):
                            P.op("pe", lambda e, pg=pg, kc=kc, wt=wt, t0=t0, tw=tw: e.matmul(
                                pg[:, 0:tw], lhsT=wt[:, kc, 0, :], rhs=hT[:, kc, t0:t0 + tw],
                                start=(kc == 0), stop=(kc == KC - 1)), reads=[Bwt, B_hT], writes=[Bpg])
                        for kc in range(KC):
                            P.op("pe", lambda e, pu=pu, kc=kc, wt=wt, t0=t0, tw=tw: e.matmul(
                                pu[:, 0:tw], lhsT=wt[:, kc, 1, :], rhs=hT[:, kc, t0:t0 + tw],
                                start=(kc == 0), stop=(kc == KC - 1)), reads=[Bwt, B_hT], writes=[Bpu])
                        P.op("act", lambda e, pg=pg, tw=tw: e.activation(out=sil[:, 0:tw], in_=pg[:, 0:tw], func=AF.Sigmoid),
                             reads=[Bpg], writes=[B_sil])
                        P.op("dve", lambda e, pg=pg, tw=tw: e.tensor_tensor(out=sil[:, 0:tw], in0=sil[:, 0:tw], in1=pg[:, 0:tw], op=ALU.mult),
                             reads=[Bpg, B_sil], writes=[B_sil])
                        P.op("dve", lambda e, pu=pu, t0=t0, tw=tw: e.tensor_tensor(out=ug[:, 0:tw], in0=gbc[:, t0:t0 + tw], in1=pu[:, 0:tw], op=ALU.mult),
                             reads=[Bpu, B_gbc], writes=[B_ug])
                        P.op("pool", lambda e, fc=fc, t0=t0, tw=tw: e.tensor_tensor(out=hid[:, fc, t0:t0 + tw], in0=sil[:, 0:tw], in1=ug[:, 0:tw], op=ALU.mult),
                             reads=[B_sil, B_ug], writes=[B_hid])
            for m in range(KC):
                for (t0, tw) in TT:
                    pt, Bp = next_ps("a")
                    for fc in range(4):
                        P.op("pe", lambda e, pt=pt, fc=fc, wdt=wdt, m=m, t0=t0, tw=tw: e.matmul(
                            pt[:, 0:tw], lhsT=wdt[:, fc, m * 128:(m + 1) * 128], rhs=hid[:, fc, t0:t0 + tw], start=(fc == 0), stop=(fc == 3)),
                            reads=[Bwd, B_hid], writes=[Bp])
                    P.op("dve", lambda e, pt=pt, m=m, t0=t0, tw=tw: e.tensor_tensor(out=zacc[:, m, t0:t0 + tw], in0=zacc[:, m, t0:t0 + tw],
                                                                                   in1=pt[:, 0:tw], op=ALU.add), reads=[Bp, B_zacc], writes=[B_zacc])
        P.barrier()

    def after_ln2(m):
        P.dma("sp", lambda e, m=m: e.dma_start(out=y_out[m * 128:(m + 1) * 128, :], in_=zacc[:, m, :]), reads=[B_zacc])
    layer_norm(2, 3, after_ln2)
    P.barrier()


def nsa_sample(nc, P, sc, sbt, next_ps, L):
    debug = L["debug"]
    xf = L["xf"]
    ident, B_ident, identb, B_identb = L["ident"], L["B_ident"], L["identb"], L["B_identb"]
    onesb, B_onesb = L["onesb"], L["B_onesb"]
    o_nsaT, B_onsaT = L["o_nsaT"], L["B_onsaT"]
    cache2d = L["cache2d"]
    SB0 = NB * 128
    SC0 = NOWN * 128

    def table(name, shape, dt, src, q="sp"):
        t, B = sbt(sc, "T" + name, shape, dt)
        P.dma(q, lambda e: e.dma_start(out=t[:], in_=src), writes=[B])
        return t, B
    cb, B_cb = table("s_cb", [128, 64], F32, L["s_cb"])
    bs, B_bs = table("s_bs", [128, 16 * 65], F32, L["s_bs"])
    sq, B_sq = table("s_sq", [1, 2048], F32, L["s_sq"])
    lnt, B_lnt = table("s_lnt", [2, 128], BF16, L["s_lnt"])
    R2, B_R2 = table("s_R2", [2, 512], BF16, L["t_r2"])
    Et, B_Et = table("s_Et", [64, NB * 128], BF16, L["t_E"])
    caus, B_caus = table("s_caus", [128, 512], BF16, L["s_caus"])
    low, B_low = table("s_low", [128, 512], BF16, L["s_low"])
    keep, B_keep = table("s_keep", [128, 128], F32, L["s_keep"])
    force, B_force = table("s_force", [128, 128], F32, L["s_force"])
    iot, B_iot = table("s_iot", [128, 1], F32, L["s_iota"])
    ptb, B_ptb = sbt(sc, "s_ptb", [128, 256], mybir.dt.int32)
    P.dma("sp", lambda e: e.dma_start(out=ptb[:], in_=L["pt_core"].partition_broadcast(128)), writes=[B_ptb])
    ptf, B_ptf = sbt(sc, "s_ptf", [128, 256], F32)
    idx, B_idx = sbt(sc, "s_idx", [128, 256], mybir.dt.int32)
    P.op("dve", lambda e: e.tensor_copy(out=ptf[:], in_=ptb[:]), reads=[B_ptb], writes=[B_ptf])
    P.op("dve", lambda e: e.tensor_scalar(out=ptf[:], in0=ptf[:], scalar1=128.0, scalar2=iot[:, 0:1], op0=ALU.mult, op1=ALU.add),
         reads=[B_ptf, B_iot], writes=[B_ptf])
    P.op("dve", lambda e: e.tensor_scalar(out=ptf[:], in0=ptf[:], scalar1=2.0, scalar2=None, op0=ALU.mult), reads=[B_ptf], writes=[B_ptf])
    P.op("dve", lambda e: e.tensor_copy(out=idx[:], in_=ptf[:]), reads=[B_ptf], writes=[B_idx])
    idx1, B_idx1 = sbt(sc, "s_idx1", [128, 256], mybir.dt.int32)
    P.op("dve", lambda e: e.tensor_scalar(out=ptf[:], in0=ptf[:], scalar1=1.0, scalar2=None, op0=ALU.add), reads=[B_ptf], writes=[B_ptf])
    P.op("dve", lambda e: e.tensor_copy(out=idx1[:], in_=ptf[:]), reads=[B_ptf], writes=[B_idx1])

    qT, B_qT = sbt(sc, "s_qT", [128, 8, 128], BF16)
    gates, B_gates = sbt(sc, "s_gates", [128, 48], F32)
    knew, B_knew = sbt(sc, "s_knew", [128, 2, 2, 128], BF16)
    vnew, B_vnew = sbt(sc, "s_vnew", [128, 2, 4, 65], BF16)
    vnj, B_vnj = sbt(sc, "s_vnj", [4, 4, 2, 4, 65], BF16)
    P.op("dve", lambda e: e.memset(vnew[:, :, :, 64:65], 1.0), writes=[B_vnew])
    pq = contextlib.ExitStack()
    with pq:
        wq, B_wq = sbt(pq, "s_wq", [128, KC, 1024], BF16)
        wng, B_wng = sbt(pq, "s_wng", [128, KC, 48], BF16)
        wk, B_wk = sbt(pq, "s_wk", [128, KC, 1024], BF16)
        bq_col, B_bq = sbt(pq, "s_bq", [128, 8], F32)
        bk_col, B_bk = sbt(pq, "s_bk", [128, 12], F32)
        bk_bc, B_bkbc = sbt(pq, "s_bkbc", [128, KV_W], F32)
        bng, B_bng = sbt(pq, "s_bng", [128, 48], F32)
        xb, Bx = sbt(pq, "s_xb", [128, KC, 128], BF16)
        wq_v = L["w_q"].rearrange("(kc p) c -> p kc c", p=128)
        wkv_v = L["w_kv"].rearrange("(kc p) c -> p kc c", p=128)
        xf_v = xf.rearrange("(kc p) t -> p kc t", p=128)
        for q in range(4):
            P.dma("pool", lambda e, q=q: e.dma_start(out=wq[:, 4 * q:4 * q + 4, :], in_=wq_v[:, 4 * q:4 * q + 4, :]), writes=[B_wq])
            P.dma("pool", lambda e, q=q: e.dma_start(out=wk[:, 4 * q:4 * q + 4, :], in_=wkv_v[:, 4 * q:4 * q + 4, 512:1536]), writes=[B_wk])
        P.dma("pool", lambda e: e.dma_start(out=wng[:], in_=L["w_ng"].rearrange("(kc p) c -> p kc c", p=128)), writes=[B_wng])
        P.dma("pool", lambda e: e.dma_start(out=xb[:], in_=xf_v[:, :, SB0:SB0 + 128]), writes=[Bx])
        P.dma("sp", lambda e: e.dma_start(out=bq_col[:], in_=L["b_q"].rearrange("(cb p) -> p cb", p=128), allow_slow_non_contiguous=True),
              writes=[B_bq])
        P.dma("sp", lambda e: e.dma_start(out=bk_col[:], in_=L["b_kv"].rearrange("(cb p) -> p cb", p=128), allow_slow_non_contiguous=True),
              writes=[B_bk])
        P.dma("sp", lambda e: e.dma_start(out=bk_bc[:], in_=L["b_kv"].partition_broadcast(128)), writes=[B_bkbc])
        P.dma("sp", lambda e: e.dma_start(out=bng[:], in_=L["b_ng"].partition_broadcast(128)), writes=[B_bng])
        for cbk in range(8):
            pt, Bp = next_ps("a")
            for kc in range(KC):
                P.op("pe", lambda e, pt=pt, kc=kc, cbk=cbk: e.matmul(pt[:, 0:128], lhsT=wq[:, kc, cbk * 128:(cbk + 1) * 128], rhs=xb[:, kc, :],
                                                                     start=(kc == 0), stop=(kc == KC - 1)), reads=[B_wq, Bx], writes=[Bp])
            P.op("act", lambda e, pt=pt, cbk=cbk: e.activation(out=qT[:, cbk, :], in_=pt[:, 0:128], func=AF.Identity, bias=bq_col[:, cbk:cbk + 1]),
                 reads=[Bp, B_bq], writes=[B_qT])
        for si, (c0, cbb) in enumerate(((0, 4), (512, 8))):
            for gp in range(2):
                pt, Bp = next_ps("a")
                for kc in range(KC):
                    P.op("pe", lambda e, pt=pt, kc=kc, c0=c0, gp=gp: e.matmul(
                        pt[:, 0:128], lhsT=wk[:, kc, c0 + gp * 128:c0 + (gp + 1) * 128], rhs=xb[:, kc, :],
                        start=(kc == 0), stop=(kc == KC - 1)), reads=[B_wk, Bx], writes=[Bp])
                P.op("act", lambda e, pt=pt, si=si, gp=gp, cbb=cbb: e.activation(
                    out=knew[:, si, gp, :], in_=pt[:, 0:128], func=AF.Identity, bias=bk_col[:, cbb + gp:cbb + gp + 1]),
                    reads=[Bp, B_bk], writes=[B_knew])
        pt, Bp = next_ps("a")
        for si, c0 in enumerate((256, 768)):
            for kc in range(KC):
                P.op("pe", lambda e, pt=pt, kc=kc, si=si, c0=c0: e.matmul(pt[:, si * 256:(si + 1) * 256], lhsT=xb[:, kc, :], rhs=wk[:, kc, c0:c0 + 256],
                                                                         start=(kc == 0), stop=(kc == KC - 1)), reads=[B_wk, Bx], writes=[Bp])
        for si, c0 in enumerate((768, 1280)):
            P.op("dve", lambda e, pt=pt, si=si, c0=c0: e.tensor_tensor(
                out=vnew[:, si, :, 0:64], in0=pt[:, si * 256:(si + 1) * 256].rearrange("p (g d) -> p g d", g=4),
                in1=bk_bc[:, c0:c0 + 256].rearrange("p (g d) -> p g d", g=4), op=ALU.add), reads=[Bp, B_bkbc], writes=[B_vnew])
        for j in range(4):
            P.dma("sp", lambda e, j=j: e.dma_start(out=vnj[:, j, :, :, :], in_=vnew[32 * j:32 * j + 4, :, :, :]), reads=[B_vnew], writes=[B_vnj])
        pt, Bp = next_ps("b")
        for kc in range(KC):
            P.op("pe", lambda e, pt=pt, kc=kc: e.matmul(pt[:, 0:48], lhsT=xb[:, kc, :], rhs=wng[:, kc, :], start=(kc == 0), stop=(kc == KC - 1)),
                 reads=[B_wng, Bx], writes=[Bp])
        P.op("dve", lambda e, pt=pt: e.tensor_tensor(out=gates[:, :], in0=pt[:, 0:48], in1=bng[:, :], op=ALU.add), reads=[Bp, B_bng], writes=[B_gates])
        P.op("act", lambda e: e.activation(out=gates[:], in_=gates[:], func=AF.Sigmoid), reads=[B_gates], writes=[B_gates])
        P.barrier()

    kcs, B_kcs = sbt(sc, "s_kc", [128, 2, 512], BF16)
    vca, B_vca = sbt(sc, "s_vca", [128, 4, 4, 193], BF16)
    P.op("dve", lambda e: e.memset(vca[:, :, :, 64:65], 1.0), writes=[B_vca])
    for c in range(4):
        for g in range(4):
            P.dma("sp", lambda e, c=c, g=g: e.dma_start(out=vca[:, c, g, 65:193], in_=L["s_ovl"][:, c * 128:(c + 1) * 128]), writes=[B_vca])
    o_s, B_os = sbt(sc, "s_os", [128, 4, 256], F32)
    P.op("dve", lambda e: e.memset(o_s[:], 0.0), writes=[B_os])
    pg = [sbt(sc, "s_pg%d" % i, [128, 512], F32) for i in range(3)]
    pef, B_pef = sbt(sc, "s_pef", [128, 2, 16], F32)
    peb, B_peb = sbt(sc, "s_peb", [128, 2, 16], BF16)
    w2k, B_w2k = sbt(sc, "s_w2k", [128, 128], BF16)
    w2v, B_w2v = sbt(sc, "s_w2v", [128, 64], BF16)
    pre0, B_pre0 = sbt(sc, "s_pre0", [128, 2], F32)
    ug, B_ug = sbt(sc, "s_ug", [128, 512], F32)
    tg, B_tg = sbt(sc, "s_tg", [128, 512], F32)
    Gb, B_Gb = sbt(sc, "s_Gb", [128, 512], BF16)
    w_c1, w_c2, c_pe = L["w_c1"], L["w_c2"], L["c_pe"]
    P.dma("sp", lambda e: e.dma_start(out=pef[:], in_=c_pe.rearrange("c (jc j2) d -> (j2 d) c jc", j2=2), allow_slow_non_contiguous=True),
          writes=[B_pef])
    P.op("dve", lambda e: e.tensor_copy(out=peb[:], in_=pef[:]), reads=[B_pef], writes=[B_peb])
    P.op("dve", lambda e: e.memset(w2k[:, 0:64], 0.0), writes=[B_w2k])
    P.dma("pool", lambda e: e.dma_start(out=w2k[:, 64:128], in_=w_c2[0]), writes=[B_w2k])
    P.dma("pool", lambda e: e.dma_start(out=w2v[:], in_=w_c2[1]), writes=[B_w2v])
    pT = [sbt(sc, "s_pT%d" % i, [128, 4, 128], BF16) for i in range(4)]
    pT_rr = [0]

    def next_pT():
        k = pT_rr[0] % len(pT)
        pT_rr[0] += 1
        return pT[k]
    qsq, B_qsq = sbt(sc, "s_qsq", [128, 512], BF16)
    o_blk, B_oblk = sbt(sc, "s_oblk", [128, 256], F32)
    rs, B_rs = sbt(sc, "s_rs", [128, 4], F32)
    wgt, B_wgt = sbt(sc, "s_wgt", [128, 4], F32)
    imp, B_imp = sbt(sc, "s_imp", [128, 128], F32)
    imp3, B_imp3 = sbt(sc, "s_imp3", [128, 128], F32)
    m8, B_m8 = sbt(sc, "s_m8", [128, 16], F32)
    nsel, B_nsel = sbt(sc, "s_nsel", [128, 128], F32)
    nselT = [sbt(sc, "s_nselT%d" % i, [64, 512], BF16) for i in range(2)]
    sqt, B_sqt = sbt(sc, "s_sqt", [128, 512], BF16)
    runmax, B_runmax = sbt(sc, "s_runmax", [1, 512], F32)
    nkm, B_nkm = sbt(sc, "s_nkm", [1, 1], F32)

    def softmax_chunk(pt, Bp, w, hbase, col_fn, tab, B_tab):
        (t, Bt) = next_pT()
        for hh in range(4):
            P.op("act", lambda e, pt=pt, t=t, hh=hh, w=w: e.activation(
                out=t[:w, hh, :], in_=pt[:w, hh * 128:(hh + 1) * 128], func=AF.Exp, scale=SCALE,
                bias=tab[:w, col_fn(hbase + hh):col_fn(hbase + hh) + 1]), reads=[Bp, B_tab], writes=[Bt])
        return t, Bt

    def finish_branch(psO, BpO, g, br, first):
        P.op("dve", lambda e: e.tensor_scalar(out=rs[:, :], in0=psO[:, 0:260].rearrange("p (h c) -> p h c", c=65)[:, :, 64],
                                              scalar1=1e-30, scalar2=None, op0=ALU.max), reads=[BpO], writes=[B_rs])
        P.op("dve", lambda e: e.reciprocal(out=rs[:, :], in_=rs[:, :]), reads=[B_rs], writes=[B_rs])
        P.op("dve", lambda e: e.tensor_tensor(out=wgt[:, :], in0=rs[:, :], in1=gates[:, br * 16 + g * 4:br * 16 + g * 4 + 4], op=ALU.mult),
             reads=[B_rs, B_gates], writes=[B_wgt])
        for hh in range(4):
            oc = slice(hh * 64, hh * 64 + 64)
            if first:
                P.op("dve", lambda e, hh=hh, oc=oc: e.tensor_scalar(out=o_blk[:, oc], in0=psO[:, hh * 65:hh * 65 + 64],
                                                                    scalar1=wgt[:, hh:hh + 1], scalar2=None, op0=ALU.mult),
                     reads=[BpO, B_wgt], writes=[B_oblk])
            else:
                P.op("dve", lambda e, hh=hh, oc=oc: e.scalar_tensor_tensor(
                    out=o_blk[:, oc], in0=psO[:, hh * 65:hh * 65 + 64], scalar=wgt[:, hh:hh + 1], in1=o_blk[:, oc],
                    op0=ALU.mult, op1=ALU.add), reads=[BpO, B_wgt, B_oblk], writes=[B_oblk])

    def gather(j, c, half, dst, Bdst):
        col = j * 64 + c
        ix, Bix = (idx, B_idx) if half == 0 else (idx1, B_idx1)
        P.dma("pool", lambda e, col=col, ix=ix, dst=dst: e.indirect_dma_start(
            out=dst[:, :], out_offset=None, in_=cache2d[:, :],
            in_offset=bass.IndirectOffsetOnAxis(ap=ix[:, col:col + 1], axis=0)), reads=[Bix], writes=[Bdst])

    for j in range(4):
        if _LIM < 6.5 and j not in _JSEL:
            continue
        jb = contextlib.ExitStack()
        with jb:
            pa = jb.enter_context(contextlib.ExitStack())
            kcmp, B_kcmp = sbt(pa, "s_kcmp", [128, 2, NCH * 128], BF16)
            vcmp, B_vcmp = sbt(pa, "s_vcmp", [128, 2, NCH * 128], BF16)
            w1d, B_w1d = sbt(pa, "s_w1d", [128, 2, 32, 128], BF16)
            w1f, B_w1f = sbt(pa, "s_w1f", [128, 2, 16, 128], BF16)
            for half in range(2):
                P.dma("pool", lambda e, half=half, w1d=w1d: e.dma_start(out=w1d[64 * half:64 * half + 64, :, :, :], in_=w_c1.rearrange("c j d h -> d c j h")),
                      writes=[B_w1d])
            P.dma("pool", lambda e, w1f=w1f: e.dma_start(out=w1f[:], in_=w_c1.rearrange("c (jc j2) d h -> (j2 d) c jc h", j2=2)), writes=[B_w1f])
            pt, Bp = next_ps("b")
            for c in range(2):
                for jc in range(16):
                    P.op("pe", lambda e, pt=pt, c=c, jc=jc, w1f=w1f: e.matmul(pt[:, c:c + 1], lhsT=w1f[:, c, jc, :], rhs=peb[:, c, jc:jc + 1],
                                                                              start=(jc == 0), stop=(jc == 15)), reads=[B_w1f, B_peb], writes=[Bp])
            P.op("dve", lambda e, pt=pt: e.tensor_copy(out=pre0[:], in_=pt[:, 0:2]), reads=[Bp], writes=[B_pre0])
            for c in range(NCH):
                pgt, Bpg = pg[c % 3]
                gather(j, c, 0, pgt, Bpg)
                for q in range(4):
                    dst, Bd = (kcmp, B_kcmp) if q < 2 else (vcmp, B_vcmp)
                    pt, Bp = next_ps("a")
                    P.op("pe", lambda e, pt=pt, q=q, pgt=pgt: e.transpose(out=pt[:, 0:128], in_=pgt[:, q * 128:(q + 1) * 128],
                                                                          identity=ident[:, :]), reads=[Bpg, B_ident], writes=[Bp])
                    eng = "act" if q % 2 == 0 else "dve"
                    if eng == "act":
                        P.op("act", lambda e, pt=pt, c=c, q=q, dst=dst: e.activation(out=dst[:, q % 2, c * 128:(c + 1) * 128], in_=pt[:, 0:128],
                                                                                      func=AF.Identity), reads=[Bp], writes=[Bd])
                    else:
                        P.op("dve", lambda e, pt=pt, c=c, q=q, dst=dst: e.tensor_copy(out=dst[:, q % 2, c * 128:(c + 1) * 128], in_=pt[:, 0:128]),
                             reads=[Bp], writes=[Bd])
            for c in range(2):
                src, Bsrc = (kcmp, B_kcmp) if c == 0 else (vcmp, B_vcmp)
                for g in range(4):
                    gp, g2 = g // 2, g % 2
                    hs = slice(64 * g2, 64 * g2 + 64)
                    pt, Bp = next_ps("a")
                    for jj in range(32):
                        P.op("pe", lambda e, pt=pt, c=c, jj=jj, hs=hs, gp=gp, src=src, w1d=w1d: e.matmul(
                            pt[:, 0:511], lhsT=w1d[hs, c, jj, :], rhs=src[hs, gp, jj:jj + 16 * 510 + 1:16],
                            start=(jj == 0), stop=(jj == 31)), reads=[B_w1d, Bsrc], writes=[Bp])
                    P.op("act", lambda e, pt=pt, c=c: e.activation(out=ug[:, 0:511], in_=pt[:, 0:511], func=AF.Identity, bias=pre0[:, c:c + 1]),
                         reads=[Bp, B_pre0], writes=[B_ug])
                    P.op("dve", lambda e: e.tensor_tensor(out=tg[:, 0:511], in0=ug[:, 0:511], in1=ug[:, 0:511], op=ALU.mult), reads=[B_ug], writes=[B_tg])
                    P.op("dve", lambda e: e.tensor_scalar(out=tg[:, 0:511], in0=tg[:, 0:511], scalar1=0.044715, scalar2=1.0, op0=ALU.mult, op1=ALU.add),
                         reads=[B_tg], writes=[B_tg])
                    P.op("dve", lambda e: e.tensor_tensor(out=tg[:, 0:511], in0=tg[:, 0:511], in1=ug[:, 0:511], op=ALU.mult), reads=[B_tg, B_ug], writes=[B_tg])
                    P.op("act", lambda e: e.activation(out=tg[:, 0:511], in_=tg[:, 0:511], func=AF.Sigmoid, scale=1.5957691216057308),
                         reads=[B_tg], writes=[B_tg])
                    P.op("dve", lambda e: e.tensor_tensor(out=Gb[:, 0:511], in0=tg[:, 0:511], in1=ug[:, 0:511], op=ALU.mult), reads=[B_tg, B_ug], writes=[B_Gb])
                    if c == 0:
                        pt2, Bp2 = next_ps("b")
                        if g2 == 0:
                            P.op("pe", lambda e, pt2=pt2: e.matmul(pt2[0:64, 0:511], lhsT=w2k[:, 64:128], rhs=Gb[:, 0:511], start=True, stop=True),
                                 reads=[B_w2k, B_Gb], writes=[Bp2])
                        else:
                            P.op("pe", lambda e, pt2=pt2: e.matmul(pt2[:, 0:511], lhsT=w2k[:, :], rhs=Gb[:, 0:511], start=True, stop=True),
                                 reads=[B_w2k, B_Gb], writes=[Bp2])
                        P.op("dve", lambda e, pt2=pt2, hs=hs, gp=gp: e.tensor_copy(out=kcs[hs, gp, 0:511], in_=pt2[hs, 0:511]), reads=[Bp2], writes=[B_kcs])
                    else:
                        pt2, Bp2 = next_ps("b")
                        for ch in range(4):
                            w = 128 if ch < 3 else 127
                            P.op("pe", lambda e, pt2=pt2, ch=ch, w=w: e.matmul(pt2[:w, ch * 64:(ch + 1) * 64], lhsT=Gb[:, ch * 128:ch * 128 + w],
                                                                               rhs=w2v[:, :], start=True, stop=True), reads=[B_w2v, B_Gb], writes=[Bp2])
                        for ch in range(4):
                            w = 128 if ch < 3 else 127
                            P.op("dve", lambda e, pt2=pt2, ch=ch, w=w, g=g: e.tensor_copy(out=vca[:w, ch, g, 0:64], in_=pt2[:w, ch * 64:(ch + 1) * 64]),
                                 reads=[Bp2], writes=[B_vca])
            P.op("dve", lambda e: e.memset(kcs[:, :, 511:512], 0.0), writes=[B_kcs])
            P.barrier()
            pa.close()

            kslc, B_kslc = sbt(jb, "s_kslc", [128, 2, NCH * 128], BF16)
            vslc, B_vslc = sbt(jb, "s_vslc", [128, NCH, 4, 65], BF16)
            kwin, B_kwin = sbt(jb, "s_kwin", [128, 2, 512], BF16)
            vwin, B_vwin = sbt(jb, "s_vwin", [128, 4, 4, 65], BF16)
            sqj, B_sqj = sbt(jb, "s_sqj", [1, 2048], F32)
            kslc_g, B_kslcg = sbt(jb, "s_kslcg", [64, (NCH + 1) * 128], BF16)
            kwin_g, B_kwing = sbt(jb, "s_kwing", [64, 5 * 128], BF16)
            kc_g, B_kcg = sbt(jb, "s_kcg", [64, 512], BF16)
            q_g, B_qg = sbt(jb, "s_qg", [64, 4, 128], BF16)
            P.op("dve", lambda e: e.memset(vslc[:, :, :, 64:65], 1.0), writes=[B_vslc])
            P.op("dve", lambda e: e.memset(vwin[:, :, :, 64:65], 1.0), writes=[B_vwin])
            for c in range(NCH):
                pgt, Bpg = pg[c % 3]
                gather(j, c, 1, pgt, Bpg)
                for q in range(2):
                    pt, Bp = next_ps("a")
                    P.op("pe", lambda e, pt=pt, q=q, pgt=pgt: e.transpose(out=pt[:, 0:128], in_=pgt[:, q * 128:(q + 1) * 128],
                                                                          identity=ident[:, :]), reads=[Bpg, B_ident], writes=[Bp])
                    P.op("act", lambda e, pt=pt, c=c, q=q: e.activation(out=kslc[:, q, c * 128:(c + 1) * 128], in_=pt[:, 0:128], func=AF.Identity),
                         reads=[Bp], writes=[B_kslc])
                P.op("dve", lambda e, pgt=pgt, c=c: e.tensor_copy(out=vslc[:, c, :, 0:64], in_=pgt[:, 256:512].rearrange("p (g d) -> p g d", g=4)),
                     reads=[Bpg], writes=[B_vslc])
            for wch in range(4):
                pgt, Bpg = pg[wch % 3]
                P.dma("sp", lambda e, pgt=pgt, j=j, wch=wch: e.dma_start(out=pgt[:, :], in_=L["cw_in"][j, wch * 128:(wch + 1) * 128, :]), writes=[Bpg])
                for q in range(2):
                    pt, Bp = next_ps("a")
                    P.op("pe", lambda e, pt=pt, q=q, pgt=pgt: e.transpose(out=pt[:, 0:128], in_=pgt[:, q * 128:(q + 1) * 128],
                                                                          identity=ident[:, :]), reads=[Bpg, B_ident], writes=[Bp])
                    P.op("act", lambda e, pt=pt, wch=wch, q=q: e.activation(out=kwin[:, q, wch * 128:(wch + 1) * 128], in_=pt[:, 0:128], func=AF.Identity),
                         reads=[Bp], writes=[B_kwin])
                P.op("dve", lambda e, pgt=pgt, wch=wch: e.tensor_copy(out=vwin[:, wch, :, 0:64], in_=pgt[:, 256:512].rearrange("p (g d) -> p g d", g=4)),
                     reads=[Bpg], writes=[B_vwin])

            P.op("dve", lambda e: e.memset(runmax[:], 0.0), writes=[B_runmax])
            srcs = []
            for gp in range(2):
                for s_ in range(NCH * 128 // 512):
                    srcs.append((kslc, B_kslc, lambda gp=gp, s_=s_: kslc[:, gp, s_ * 512:(s_ + 1) * 512], 512))
                srcs.append((kwin, B_kwin, lambda gp=gp: kwin[:, gp, :], 512))
                srcs.append((kcs, B_kcs, lambda gp=gp: kcs[:, gp, :], 512))
                srcs.append((knew, B_knew, lambda gp=gp: knew[:, 0, gp, :], 128))
                srcs.append((knew, B_knew, lambda gp=gp: knew[:, 1, gp, :], 128))
            for (src, Bs, apf, w) in srcs:
                P.op("dve", lambda e, apf=apf, w=w: e.tensor_tensor(out=sqt[:, 0:w], in0=apf(), in1=apf(), op=ALU.mult), reads=[Bs], writes=[B_sqt])
                pt, Bp = next_ps("b")
                P.op("pe", lambda e, pt=pt, w=w: e.matmul(pt[0:1, 0:w], lhsT=onesb[:, 0:1], rhs=sqt[:, 0:w], start=True, stop=True),
                     reads=[B_onesb, B_sqt], writes=[Bp])
                P.op("dve", lambda e, pt=pt, w=w: e.tensor_tensor(out=runmax[:, 0:w], in0=runmax[:, 0:w], in1=pt[0:1, 0:w], op=ALU.max),
                     reads=[Bp, B_runmax], writes=[B_runmax])
            P.op("dve", lambda e: e.reduce_max(out=nkm[:], in_=runmax[:], axis=mybir.AxisListType.X), reads=[B_runmax], writes=[B_nkm])
            P.op("dve", lambda e: e.tensor_scalar(out=nkm[:], in0=nkm[:], scalar1=-0.5, scalar2=None, op0=ALU.mult), reads=[B_nkm], writes=[B_nkm])
            P.op("dve", lambda e: e.tensor_scalar(out=sqj[:, :], in0=sq[:, :], scalar1=nkm[0:1, 0:1], scalar2=None, op0=ALU.add),
                 reads=[B_nkm, B_sq], writes=[B_sqj])

            for g in range(4):
                gp, g2 = g // 2, g % 2
                hs0 = slice(64 * g2, 64 * g2 + 64)
                P.dma("sp", lambda e, hs0=hs0, gp=gp: e.dma_start(out=kslc_g[:, 0:NCH * 128], in_=kslc[hs0, gp, :]), reads=[B_kslc], writes=[B_kslcg])
                P.dma("sp", lambda e, hs0=hs0, gp=gp, j=j: e.dma_start(out=kslc_g[:, NCH * 128:NCH * 128 + 4], in_=knew[hs0, 0, gp, 32 * j:32 * j + 4]),
                      reads=[B_knew], writes=[B_kslcg])
                P.dma("sp", lambda e, hs0=hs0, gp=gp: e.dma_start(out=kwin_g[:, 0:512], in_=kwin[hs0, gp, :]), reads=[B_kwin], writes=[B_kwing])
                P.dma("sp", lambda e, hs0=hs0, gp=gp, j=j: e.dma_start(out=kwin_g[:, 512:516], in_=knew[hs0, 1, gp, 32 * j:32 * j + 4]),
                      reads=[B_knew], writes=[B_kwing])
                P.dma("sp", lambda e, hs0=hs0, gp=gp: e.dma_start(out=kc_g[:, :], in_=kcs[hs0, gp, :]), reads=[B_kcs], writes=[B_kcg])
                P.dma("sp", lambda e, hs0=hs0, gp=gp: e.dma_start(out=q_g[:, :, :], in_=qT[hs0, gp * 4:gp * 4 + 4, :]), reads=[B_qT], writes=[B_qg])
                hs = slice(0, 64)
                qv = q_g[:, :, :]
                P.op("dve", lambda e, qv=qv: e.tensor_tensor(out=qsq[hs, :].rearrange("p (h q) -> p h q", h=4), in0=qv, in1=qv, op=ALU.mult),
                     reads=[B_qg], writes=[B_qsq])
                pt, Bp = next_ps("b")
                P.op("pe", lambda e, pt=pt: e.matmul(pt[0:1, :], lhsT=onesb[hs, 0:1], rhs=qsq[hs, :], start=True, stop=True),
                     reads=[B_onesb, B_qsq], writes=[Bp])
                P.op("dve", lambda e, pt=pt, g=g: e.scalar_tensor_tensor(out=R2[0:1, :], in0=pt[0:1, :], scalar=-0.5,
                                                                         in1=sqj[0:1, g * 512:(g + 1) * 512], op0=ALU.mult, op1=ALU.add),
                     reads=[Bp, B_sqj], writes=[B_R2])
                pts = []
                for c in range(4):
                    w = 128 if c < 3 else 127
                    pt, Bp = next_ps("a")
                    P.op("pe", lambda e, pt=pt, c=c, w=w, qv=qv: e.matmul(pt[:w, :], lhsT=kc_g[hs, c * 128:c * 128 + w], rhs=qv, start=True, stop=False),
                         reads=[B_kcg, B_qg], writes=[Bp])
                    P.op("pe", lambda e, pt=pt, w=w: e.matmul(pt[:w, :], lhsT=lnt[0:1, 0:w], rhs=R2[0:1, :], start=False, stop=True),
                         reads=[B_lnt, B_R2], writes=[Bp])
                    t, Bt = softmax_chunk(pt, Bp, w, 4 * g, lambda h, c=c: h * 4 + c, cb, B_cb)
                    pts.append((t, Bt, w))
                psO, BpO = next_ps("b")
                psI, BpI = next_ps("b")
                for hh in range(4):
                    for c, (t, Bt, w) in enumerate(pts):
                        P.op("pe", lambda e, hh=hh, c=c, t=t, w=w, g=g, psO=psO: e.matmul(
                            psO[:, hh * 65:(hh + 1) * 65], lhsT=t[:w, hh, :], rhs=vca[:w, c, g, 0:65], start=(c == 0), stop=(c == 3)),
                            reads=[Bt, B_vca], writes=[BpO])
                for hh in range(4):
                    for c, (t, Bt, w) in enumerate(pts):
                        P.op("pe", lambda e, hh=hh, c=c, t=t, w=w, g=g, psI=psI: e.matmul(
                            psI[:, hh * 128:(hh + 1) * 128], lhsT=t[:w, hh, :], rhs=vca[:w, c, g, 65:193], start=(c == 0), stop=(c == 3)),
                            reads=[Bt, B_vca], writes=[BpI])
                finish_branch(psO, BpO, g, 0, True)
                for hh in range(4):
                    if hh == 0:
                        P.op("dve", lambda e, psI=psI: e.tensor_scalar(out=imp[:, :], in0=psI[:, 0:128], scalar1=rs[:, 0:1], scalar2=None, op0=ALU.mult),
                             reads=[BpI, B_rs], writes=[B_imp])
                    else:
                        P.op("dve", lambda e, hh=hh, psI=psI: e.scalar_tensor_tensor(
                            out=imp[:, :], in0=psI[:, hh * 128:(hh + 1) * 128], scalar=rs[:, hh:hh + 1], in1=imp[:, :], op0=ALU.mult, op1=ALU.add),
                            reads=[BpI, B_rs, B_imp], writes=[B_imp])
                P.op("dve", lambda e: e.tensor_scalar(out=imp[:, :], in0=imp[:, :], scalar1=1e-30, scalar2=None, op0=ALU.max), reads=[B_imp], writes=[B_imp])
                P.op("dve", lambda e: e.tensor_tensor(out=imp[:, :], in0=imp[:, :], in1=keep[:, :], op=ALU.mult), reads=[B_imp, B_keep], writes=[B_imp])
                P.op("dve", lambda e: e.tensor_tensor(out=imp[:, :], in0=imp[:, :], in1=force[:, :], op=ALU.add), reads=[B_imp, B_force], writes=[B_imp])
                P.op("dve", lambda e: e.max(out=m8[:, 0:8], in_=imp[:, :]), reads=[B_imp], writes=[B_m8])
                P.op("dve", lambda e: e.tensor_scalar(out=imp3[:, :], in0=imp[:, :], scalar1=m8[:, 7:8], scalar2=None, op0=ALU.is_ge),
                     reads=[B_imp, B_m8], writes=[B_imp3])
                P.op("dve", lambda e: e.scalar_tensor_tensor(out=imp3[:, :], in0=imp3[:, :], scalar=-3.0e38, in1=imp[:, :], op0=ALU.mult, op1=ALU.add),
                     reads=[B_imp, B_imp3], writes=[B_imp3])
                P.op("dve", lambda e: e.max(out=m8[:, 8:16], in_=imp3[:, :]), reads=[B_imp3], writes=[B_m8])
                P.op("dve", lambda e: e.tensor_scalar(out=nsel[:, :], in0=imp[:, :], scalar1=m8[:, 14:15], scalar2=None, op0=ALU.is_ge),
                     reads=[B_imp, B_m8], writes=[B_nsel])
                P.op("dve", lambda e: e.tensor_scalar(out=nsel[:, :], in0=nsel[:, :], scalar1=-NEGM, scalar2=NEGM, op0=ALU.mult, op1=ALU.add),
                     reads=[B_nsel], writes=[B_nsel])
                for hf in range(2):
                    nt_, Bnt = nselT[hf]
                    pt, Bp = next_ps("b")
                    P.op("pe", lambda e, pt=pt, hf=hf: e.transpose(out=pt[0:64, 0:128], in_=nsel[:, hf * 64:(hf + 1) * 64], identity=ident[:, :]),
                         reads=[B_nsel, B_ident], writes=[Bp])
                    for hh in range(4):
                        P.op("act", lambda e, pt=pt, hh=hh, nt_=nt_: e.activation(out=nt_[:, hh * 128:(hh + 1) * 128], in_=pt[0:64, 0:128], func=AF.Identity),
                             reads=[Bp], writes=[Bnt])
                psO, BpO = next_ps("b")
                for c in range(NCH + 1):
                    last = c == NCH
                    w = 4 if last else 128
                    pt, Bp = next_ps("a")
                    P.op("pe", lambda e, pt=pt, c=c, w=w, qv=qv: e.matmul(pt[:w, :], lhsT=kslc_g[hs, c * 128:c * 128 + w], rhs=qv, start=True, stop=False),
                         reads=[B_kslcg, B_qg], writes=[Bp])
                    if not last:
                        nt_, Bnt = nselT[c // 32]
                        cc = c % 32
                        P.op("pe", lambda e, pt=pt, cc=cc, nt_=nt_: e.matmul(pt[:, :], lhsT=Et[:, cc * 128:(cc + 1) * 128], rhs=nt_[:, :], start=False, stop=False),
                             reads=[B_Et, Bnt], writes=[Bp])
                    P.op("pe", lambda e, pt=pt, w=w, last=last: e.matmul(pt[:w, :], lhsT=lnt[0:2, 0:w], rhs=R2[0:2, :], start=False, stop=(not last)),
                         reads=[B_lnt, B_R2], writes=[Bp])
                    if last:
                        P.op("pe", lambda e, pt=pt: e.matmul(pt[:4, :], lhsT=identb[:4, :4], rhs=caus[:4, :], start=False, stop=True),
                             reads=[B_identb, B_caus], writes=[Bp])
                    t, Bt = softmax_chunk(pt, Bp, w, 4 * g, lambda h, d=NCH - c: h * 65 + d, bs, B_bs)
                    for hh in range(4):
                        if last:
                            P.op("pe", lambda e, hh=hh, t=t, g=g, j=j, psO=psO: e.matmul(
                                psO[:, hh * 65:(hh + 1) * 65], lhsT=t[:4, hh, :], rhs=vnj[0:4, j, 0, g, :], start=False, stop=True),
                                reads=[Bt, B_vnj], writes=[BpO])
                        else:
                            P.op("pe", lambda e, hh=hh, t=t, c=c, g=g, psO=psO: e.matmul(
                                psO[:, hh * 65:(hh + 1) * 65], lhsT=t[:, hh, :], rhs=vslc[:, c, g, :], start=(c == 0), stop=False),
                                reads=[Bt, B_vslc], writes=[BpO])
                finish_branch(psO, BpO, g, 1, False)
                psO, BpO = next_ps("b")
                for c in range(5):
                    last = c == 4
                    w = 4 if last else 128
                    pt, Bp = next_ps("a")
                    P.op("pe", lambda e, pt=pt, c=c, w=w, qv=qv: e.matmul(pt[:w, :], lhsT=kwin_g[hs, c * 128:c * 128 + w], rhs=qv, start=True, stop=False),
                         reads=[B_kwing, B_qg], writes=[Bp])
                    edge = last or c == 0
                    P.op("pe", lambda e, pt=pt, w=w, edge=edge: e.matmul(pt[:w, :], lhsT=lnt[0:2, 0:w], rhs=R2[0:2, :], start=False, stop=(not edge)),
                         reads=[B_lnt, B_R2], writes=[Bp])
                    if edge:
                        mk, Bmk = (caus, B_caus) if last else (low, B_low)
                        P.op("pe", lambda e, pt=pt, mk=mk, w=w: e.matmul(pt[:w, :], lhsT=identb[:w, :w], rhs=mk[:w, :], start=False, stop=True),
                             reads=[B_identb, Bmk], writes=[Bp])
                    t, Bt = softmax_chunk(pt, Bp, w, 4 * g, lambda h, d=4 - c: h * 65 + d, bs, B_bs)
                    for hh in range(4):
                        if last:
                            P.op("pe", lambda e, hh=hh, t=t, g=g, j=j, psO=psO: e.matmul(
                                psO[:, hh * 65:(hh + 1) * 65], lhsT=t[:4, hh, :], rhs=vnj[0:4, j, 1, g, :], start=False, stop=True),
                                reads=[Bt, B_vnj], writes=[BpO])
                        else:
                            P.op("pe", lambda e, hh=hh, t=t, c=c, g=g, psO=psO: e.matmul(
                                psO[:, hh * 65:(hh + 1) * 65], lhsT=t[:, hh, :], rhs=vwin[:, c, g, :], start=(c == 0), stop=False),
                                reads=[Bt, B_vwin], writes=[BpO])
                finish_branch(psO, BpO, g, 2, False)
                P.dma("sp", lambda e, j=j, g=g: e.dma_start(out=o_s[32 * j:32 * j + 4, g, :], in_=o_blk[32 * j:32 * j + 4, :]),
                      reads=[B_oblk], writes=[B_os])
            P.barrier()
    for g in range(4):
        if debug:
            P.dma("sp", lambda e, g=g: e.dma_start(out=L["d_onsa_s"][:, g * 256:(g + 1) * 256], in_=o_s[:, g, :]), reads=[B_os])
        for jj in range(2):
            pt, Bp = next_ps("a")
            P.op("pe", lambda e, pt=pt, g=g, jj=jj: e.transpose(out=pt[:, 0:128], in_=o_s[:, g, jj * 128:(jj + 1) * 128], identity=ident[:, :]),
                 reads=[B_os, B_ident], writes=[Bp])
            P.op("dve", lambda e, pt=pt, g=g, jj=jj: e.tensor_copy(out=o_nsaT[:, 2 * g + jj, SC0:SC0 + 128], in_=pt[:, 0:128]),
                 reads=[Bp], writes=[B_onsaT])
    P.barrier()


NEGM = -30000.0
_LIM = 99
_NEXP = 32
_KSEL = [(0, 0)]
_JSEL = [0]
SCALE = 0.125
NB = 32
OWN0 = 24
NOWN = 8
XF_T = (NB + 1) * 128
QOFF, NGOFF, HQOFF, HGOFF, MGOFF = 0, 2560, 2608, 5680, 6704
SLOPES = [2.0 ** (-8.0 * (h + 1) / 16) for h in range(16)]
NT = (NOWN + 1) * 128
TT = [(0, 512), (512, 512), (1024, 128)]
ALPHA = 2.0 ** 0.25
LN_EPS = 1e-5
N_EXPERTS = 32
NCH = 64
N_PHYS = 2560


def build_nc(debug=False):
    nc = bass.Bass("TRN2", target_bir_lowering=False)
    P = Prog()

    def din(name, shape, dt=F32):
        return nc.dram_tensor(name, list(shape), dt, kind="ExternalInput").ap()

    def dout(name, shape, dt=F32):
        return nc.dram_tensor(name, list(shape), dt, kind="ExternalOutput").ap()

    xf = din("xf", [D_MODEL, XF_T])
    xTb = din("xTb", [D_MODEL, SEQ])
    w_kv = din("w_kv", [D_MODEL, KV_W])
    b_kv = din("b_kv", [KV_W])
    w_q = din("w_q", [D_MODEL, 1024])
    b_q = din("b_q", [1024])
    w_ng = din("w_ng", [D_MODEL, 48])
    b_ng = din("b_ng", [48])
    w_st = din("w_st", [D_MODEL, 512])
    b_st = din("b_st", [512])
    g_st = din("g_st", [2, 256])
    w_ss = din("w_ss", [D_MODEL, 2048])
    b_ss = din("b_ss", [2048])
    g_ss = din("g_ss", [2, 1024])
    st_in = din("st_in", [4, 8, 128, 128])
    cw_in = din("cw_in", [4, 512, 512])
    c_lm = din("c_lm", [128, 128])
    c_lm4 = din("c_lm4", [4, 4])
    w_c1 = din("w_c1", [2, 32, 64, 128])
    w_c2 = din("w_c2", [2, 128, 64])
    c_pe = din("c_pe", [2, 32, 64])
    t_cb = din("t_cb", [128, 256])
    t_cmask = din("t_cmask", [128, NOWN * 512], BF16)
    t_bs = din("t_bs", [128, 512])
    t_sq = din("t_sq", [1, 2048])
    t_ln = din("t_ln", [2, NB * 128], BF16)
    t_r2 = din("t_r2", [2, 512], BF16)
    t_E = din("t_E", [64, NB * 128], BF16)
    t_caus = din("t_caus", [128, 512], BF16)
    t_low = din("t_low", [128, 512], BF16)
    t_keep = din("t_keep", [128, NOWN * 64])
    t_force = din("t_force", [128, NOWN * 64])
    t_ovl = din("t_ovl", [128, 2 * 64], BF16)
    w_h3 = din("w_h3", [2, D_MODEL, 2048])
    b_h3 = din("b_h3", [2, 2048])
    n_h3 = din("n_h3", [2, 512])
    g_h3 = din("g_h3", [2, 2, 512])
    t_u32 = din("t_u32", [128, 128])
    t_l32 = din("t_l32", [128, 128])
    t_ind4 = din("t_ind4", [128, 4])
    t_vmask = din("t_vmask", [128, NB + 1])
    w_pa = din("w_pa", [1024, D_MODEL])
    w_pb = din("w_pb", [1024, D_MODEL])
    w_mg = din("w_mg", [D_MODEL, 4096])
    b_mg = din("b_mg", [4096])
    w_out = din("w_out", [D_MODEL, D_MODEL])
    ln1_g = din("ln1_g", [D_MODEL])
    ln1_b = din("ln1_b", [D_MODEL])
    ln2_g = din("ln2_g", [D_MODEL])
    ln2_b = din("ln2_b", [D_MODEL])
    w_r = din("w_r", [D_MODEL, 36])
    b_r = din("b_r", [36])
    t_selE = din("t_selE", [32, 32 * 128], BF16)
    n_experts = _NEXP
    w_gate = din("w_gate", [n_experts, D_MODEL, 512])
    w_up = din("w_up", [n_experts, D_MODEL, 512])
    w_down = din("w_down", [n_experts, 512, D_MODEL])
    cache2d = din("cache2d", [N_PHYS * 128 * 2, 512])
    pt_core = din("pt_core", [256], mybir.dt.int32)
    s_cb = din("s_cb", [128, 64])
    s_bs = din("s_bs", [128, 16 * 65])
    s_sq = din("s_sq", [1, 2048])
    s_lnt = din("s_lnt", [2, 128], BF16)
    s_caus = din("s_caus", [128, 512], BF16)
    s_low = din("s_low", [128, 512], BF16)
    s_keep = din("s_keep", [128, 128])
    s_force = din("s_force", [128, 128])
    s_ovl = din("s_ovl", [128, 4 * 128], BF16)
    s_iota = din("s_iota", [128, 1])
    t_id = din("t_id", [128, 128])
    t_idb = din("t_idb", [128, 128], BF16)

    kv_out = dout("kv_out", [(NOWN + 1) * 128, KV_W])
    win_s = dout("win_s", [4, 512, 512])
    st_p = dout("st_p", [2, 128, 128])
    st_s = dout("st_s", [4, 8, 128, 128])
    y_out = dout("y_out", [D_MODEL, NT])
    if debug:
        d_onsa = dout("d_onsa", [NOWN * 128, 1024])
        d_kc = dout("d_kc", [128, 2 * 256])
        d_vc = dout("d_vc", [128, 2 * 4 * 129])
        d_imp = dout("d_imp", [NOWN * 4 * 128, 64])
        d_ohg = dout("d_ohg", [NT, 1024])
        d_onsa_s = dout("d_onsa_s", [128, 1024])
        d_h = dout("d_h", [D_MODEL, NT])
        d_gate = dout("d_gate", [32, NT])

    top = contextlib.ExitStack()
    stopped = False
    if True:
      try:
          _names = {}

          def sbt(stack, name, shape, dt=F32):
              n = _names.get(name, 0)
              _names[name] = n + 1
              if n:
                  name = "%s_r%d" % (name, n)
              return stack.enter_context(nc.sbuf_tensor(name, list(shape), dt)), Buf(name)

          ps = [top.enter_context(nc.psum_tensor("ps%d" % i, [128, 512], F32)) for i in range(8)]
          B_ps = [Buf("ps%d" % i) for i in range(8)]
          pools = {"a": [0, 1, 2, 3, 4], "b": [5, 6, 7]}
          rr = {"a": 0, "b": 0}

          def next_ps(pool="a"):
              lst = pools[pool]
              k = lst[rr[pool] % len(lst)]
              rr[pool] += 1
              return ps[k], B_ps[k]

          ones_c, B_ones = sbt(top, "ones_c", [128, 1], F32)
          onesb, B_onesb = sbt(top, "onesb", [128, 128], BF16)
          ident, B_ident = sbt(top, "ident", [128, 128], F32)
          identb, B_identb = sbt(top, "identb", [128, 128], BF16)
          P.op("dve", lambda e: e.memset(ones_c[:], 1.0), writes=[B_ones])
          P.op("dve", lambda e: e.memset(onesb[:], 1.0), writes=[B_onesb])
          P.dma("sp", lambda e: e.dma_start(out=ident[:], in_=t_id), writes=[B_ident])
          P.dma("sp", lambda e: e.dma_start(out=identb[:], in_=t_idb), writes=[B_identb])
          ohT, B_oh = sbt(top, "ohT", [128, 16, (NOWN + 1) * 128], BF16)
          P.op("pool", lambda e: e.memset(ohT[:], 0.0), writes=[B_oh])
          o_nsaT, B_onsaT = ohT[:, 0:8, :], B_oh
          o_hgT, B_ohgT = ohT[:, 8:16, :], B_oh

          st = contextlib.ExitStack()
          with st:
              stage_states(nc, P, st, sbt, next_ps, ones_c, B_ones,
                           dict(xTb=xTb, xf=xf, w_st=w_st, b_st=b_st, g_st=g_st, w_ss=w_ss, b_ss=b_ss, g_ss=g_ss,
                                st_in=st_in, c_lm=c_lm, c_lm4=c_lm4, st_p=st_p, st_s=st_s))
              P.barrier()

          ns = contextlib.ExitStack()
          with ns:
              kslcT, B_kslcT = sbt(ns, "kslcT", [128, 2, NB * 128], BF16)
              kwinT, B_kwinT = sbt(ns, "kwinT", [128, 2, 12 * 128], BF16)
              vslc, B_vslc = sbt(ns, "vslc", [128, NB, 4, 65], BF16)
              vwin, B_vwin = sbt(ns, "vwin", [128, 12, 4, 65], BF16)
              kcT, B_kcT = sbt(ns, "kcT", [128, 2, 256], BF16)
              vca, B_vca = sbt(ns, "vca", [128, 2, 4, 129], BF16)
              P.op("dve", lambda e: e.memset(vslc[:, :, :, 64:65], 1.0), writes=[B_vslc])
              P.op("dve", lambda e: e.memset(vwin[:, :, :, 64:65], 1.0), writes=[B_vwin])
              P.op("dve", lambda e: e.memset(vca[:, :, :, 64:65], 1.0), writes=[B_vca])
              for c in range(2):
                  for g in range(4):
                      P.dma("sp", lambda e, c=c, g=g: e.dma_start(out=vca[:, c, g, 65:129], in_=t_ovl[:, c * 64:(c + 1) * 64]),
                            writes=[B_vca])

              p1 = contextlib.ExitStack()
              with p1:
                  p1a = p1.enter_context(contextlib.ExitStack())
                  kcmpT, B_kcmpT = sbt(p1, "kcmpT", [128, 2, NB * 128], BF16)
                  vcmpT, B_vcmpT = sbt(p1, "vcmpT", [128, 2, NB * 128], BF16)
                  wkv, B_wkv = sbt(p1a, "wkv", [128, KC, KV_W], BF16)
                  bkv_bc, B_bkvbc = sbt(p1a, "bkv_bc", [128, KV_W], F32)
                  bkv_col, B_bkvcol = sbt(p1a, "bkv_col", [128, 12], F32)
                  xs = [sbt(p1a, "xs%d" % i, [128, KC, 256], BF16) for i in range(2)]
                  osb = [sbt(p1a, "osb%d" % i, [128, KV_W], F32) for i in range(2)]
                  wkv_v = w_kv.rearrange("(kc p) c -> p kc c", p=128)
                  for q in range(4):
                      P.dma("pool", lambda e, q=q: e.dma_start(out=wkv[:, 4 * q:4 * q + 4, :], in_=wkv_v[:, 4 * q:4 * q + 4, :]),
                            writes=[B_wkv])
                  P.dma("sp", lambda e: e.dma_start(out=bkv_bc[:], in_=b_kv.partition_broadcast(128)), writes=[B_bkvbc])
                  with nc.allow_non_contiguous_dma(reason="tiny bias column layout"):
                      P.dma("sp", lambda e: e.dma_start(out=bkv_col[:], in_=b_kv.rearrange("(cb p) -> p cb", p=128), allow_slow_non_contiguous=True),
                            writes=[B_bkvcol])
                  xf_v = xf.rearrange("(kc p) t -> p kc t", p=128)
                  nti = 0
                  for tt in range((NB + 1) * 128 // 256 + 1):
                      t0 = tt * 256
                      if t0 >= XF_T:
                          break
                      tw = min(256, XF_T - t0)
                      (xb, Bx) = xs[tt % 2]
                      for q in range(2):
                          P.dma("pool", lambda e, xb=xb, t0=t0, tw=tw, q=q: e.dma_start(
                              out=xb[:, 8 * q:8 * q + 8, 0:tw], in_=xf_v[:, 8 * q:8 * q + 8, t0:t0 + tw]), writes=[Bx])
                      is_prompt = t0 < NB * 128
                      if is_prompt:
                          for slot, dst, Bd, lo in ((0, kcmpT, B_kcmpT, 0), (1, vcmpT, B_vcmpT, 0), (2, kslcT, B_kslcT, 0),
                                                    (4, kwinT, B_kwinT, 20 * 128)):
                              if t0 < lo:
                                  continue
                              for gp in range(2):
                                  cb = slot * 2 + gp
                                  pt, Bp = next_ps("a")
                                  for kc in range(KC):
                                      P.op("pe", lambda e, pt=pt, kc=kc, cb=cb, xb=xb, tw=tw: e.matmul(
                                          pt[:, 0:tw], lhsT=wkv[:, kc, cb * 128:(cb + 1) * 128], rhs=xb[:, kc, 0:tw],
                                          start=(kc == 0), stop=(kc == KC - 1)), reads=[B_wkv, Bx], writes=[Bp])
                                  P.op("act", lambda e, pt=pt, dst=dst, gp=gp, cb=cb, t0=t0, tw=tw, lo=lo: e.activation(
                                      out=dst[:, gp, t0 - lo:t0 - lo + tw], in_=pt[:, 0:tw], func=AF.Identity,
                                      bias=bkv_col[:, cb:cb + 1]), reads=[Bp, B_bkvcol], writes=[Bd])
                      for bi in range(tw // 128):
                          p = (t0 + bi * 128) // 128
                          tl = slice(bi * 128, (bi + 1) * 128)
                          if p < NB:
                              pt, Bp = next_ps("a")
                              for half, c0 in ((0, 768), (1, 1280)):
                                  for kc in range(KC):
                                      P.op("pe", lambda e, pt=pt, kc=kc, xb=xb, tl=tl, half=half, c0=c0: e.matmul(
                                          pt[:, half * 256:(half + 1) * 256], lhsT=xb[:, kc, tl], rhs=wkv[:, kc, c0:c0 + 256],
                                          start=(kc == 0), stop=(kc == KC - 1)), reads=[B_wkv, Bx], writes=[Bp])
                              P.op("dve", lambda e, pt=pt, p=p: e.tensor_tensor(
                                  out=vslc[:, p, :, 0:64], in0=pt[:, 0:256].rearrange("p (g d) -> p g d", g=4),
                                  in1=bkv_bc[:, 768:1024].rearrange("p (g d) -> p g d", g=4), op=ALU.add),
                                  reads=[Bp, B_bkvbc], writes=[B_vslc])
                              if p >= 20:
                                  P.op("dve", lambda e, pt=pt, p=p: e.tensor_tensor(
                                      out=vwin[:, p - 20, :, 0:64], in0=pt[:, 256:512].rearrange("p (g d) -> p g d", g=4),
                                      in1=bkv_bc[:, 1280:1536].rearrange("p (g d) -> p g d", g=4), op=ALU.add),
                                      reads=[Bp, B_bkvbc], writes=[B_vwin])
                          if p >= OWN0:
                              (o, Bo) = osb[nti % 2]
                              nti += 1
                              for cg in range(3):
                                  pt, Bp = next_ps("a")
                                  for kc in range(KC):
                                      P.op("pe", lambda e, pt=pt, kc=kc, xb=xb, tl=tl, cg=cg: e.matmul(
                                          pt[:, :], lhsT=xb[:, kc, tl], rhs=wkv[:, kc, cg * 512:(cg + 1) * 512],
                                          start=(kc == 0), stop=(kc == KC - 1)), reads=[B_wkv, Bx], writes=[Bp])
                                  P.op("dve", lambda e, pt=pt, o=o, cg=cg: e.tensor_tensor(
                                      out=o[:, cg * 512:(cg + 1) * 512], in0=pt[:, :], in1=bkv_bc[:, cg * 512:(cg + 1) * 512],
                                      op=ALU.add), reads=[Bp, B_bkvbc], writes=[Bo])
                              r0 = (p - OWN0) * 128
                              P.dma("sp", lambda e, o=o, r0=r0: e.dma_start(out=kv_out[r0:r0 + 128, :], in_=o[:, :]), reads=[Bo])
                              if p == NB:
                                  for j in range(4):
                                      P.dma("sp", lambda e, o=o, j=j: e.dma_start(
                                          out=win_s[j, 508:512, :], in_=o[32 * j:32 * j + 4, 1024:1536]), reads=[Bo])
                  for j in range(4):
                      P.dma("sp", lambda e, j=j: e.dma_start(out=win_s[j, 0:508, :], in_=cw_in[j, 4:512, :]))

                  P.barrier()
                  p1a.close()
                  if _LIM < 3:
                      raise _Stop()
                  w1d, B_w1d = sbt(p1, "w1d", [128, 2, 32, 128], BF16)
                  w1f, B_w1f = sbt(p1, "w1f", [128, 2, 16, 128], BF16)
                  pef, B_pef = sbt(p1, "pef", [128, 2, 16], F32)
                  peb, B_peb = sbt(p1, "peb", [128, 2, 16], BF16)
                  w2k, B_w2k = sbt(p1, "w2k", [128, 128], BF16)
                  w2v, B_w2v = sbt(p1, "w2v", [128, 64], BF16)
                  pre0, B_pre0 = sbt(p1, "pre0", [128, 2], F32)
                  ug, B_ug = sbt(p1, "ug", [128, 256], F32)
                  tg, B_tg = sbt(p1, "tg", [128, 256], F32)
                  Gb, B_Gb = sbt(p1, "Gb", [128, 256], BF16)
                  for half in range(2):
                      P.dma("pool", lambda e, half=half: e.dma_start(
                          out=w1d[64 * half:64 * half + 64, :, :, :], in_=w_c1.rearrange("c j d h -> d c j h")), writes=[B_w1d])
                  P.dma("pool", lambda e: e.dma_start(out=w1f[:], in_=w_c1.rearrange("c (jc j2) d h -> (j2 d) c jc h", j2=2)),
                        writes=[B_w1f])
                  with nc.allow_non_contiguous_dma(reason="tiny pe layout"):
                      P.dma("sp", lambda e: e.dma_start(out=pef[:], in_=c_pe.rearrange("c (jc j2) d -> (j2 d) c jc", j2=2), allow_slow_non_contiguous=True),
                            writes=[B_pef])
                  P.op("dve", lambda e: e.tensor_copy(out=peb[:], in_=pef[:]), reads=[B_pef], writes=[B_peb])
                  P.op("dve", lambda e: e.memset(w2k[:, 0:64], 0.0), writes=[B_w2k])
                  P.dma("pool", lambda e: e.dma_start(out=w2k[:, 64:128], in_=w_c2[0]), writes=[B_w2k])
                  P.dma("pool", lambda e: e.dma_start(out=w2v[:], in_=w_c2[1]), writes=[B_w2v])
                  pt, Bp = next_ps("b")
                  for c in range(2):
                      for jc in range(16):
                          P.op("pe", lambda e, pt=pt, c=c, jc=jc: e.matmul(
                              pt[:, c:c + 1], lhsT=w1f[:, c, jc, :], rhs=peb[:, c, jc:jc + 1], start=(jc == 0), stop=(jc == 15)),
                              reads=[B_w1f, B_peb], writes=[Bp])
                  P.op("dve", lambda e, pt=pt: e.tensor_copy(out=pre0[:], in_=pt[:, 0:2]), reads=[Bp], writes=[B_pre0])
                  for c in range(2):
                      src, Bsrc = (kcmpT, B_kcmpT) if c == 0 else (vcmpT, B_vcmpT)
                      for g in range(4):
                          gp, g2 = g // 2, g % 2
                          hs = slice(64 * g2, 64 * g2 + 64)
                          pt, Bp = next_ps("a")
                          for j in range(32):
                              P.op("pe", lambda e, pt=pt, c=c, j=j, hs=hs, gp=gp, src=src: e.matmul(
                                  pt[:, 0:255], lhsT=w1d[hs, c, j, :], rhs=src[hs, gp, j:j + 16 * 254 + 1:16],
                                  start=(j == 0), stop=(j == 31)), reads=[B_w1d, Bsrc], writes=[Bp])
                          P.op("act", lambda e, pt=pt, c=c: e.activation(out=ug[:, 0:255], in_=pt[:, 0:255], func=AF.Identity,
                                                                         bias=pre0[:, c:c + 1]), reads=[Bp, B_pre0], writes=[B_ug])
                          P.op("dve", lambda e: e.tensor_tensor(out=tg[:, 0:255], in0=ug[:, 0:255], in1=ug[:, 0:255], op=ALU.mult),
                               reads=[B_ug], writes=[B_tg])
                          P.op("dve", lambda e: e.tensor_scalar(out=tg[:, 0:255], in0=tg[:, 0:255], scalar1=0.044715, scalar2=1.0,
                                                                op0=ALU.mult, op1=ALU.add), reads=[B_tg], writes=[B_tg])
                          P.op("dve", lambda e: e.tensor_tensor(out=tg[:, 0:255], in0=tg[:, 0:255], in1=ug[:, 0:255], op=ALU.mult),
                               reads=[B_tg, B_ug], writes=[B_tg])
                          P.op("act", lambda e: e.activation(out=tg[:, 0:255], in_=tg[:, 0:255], func=AF.Sigmoid,
                                                             scale=1.5957691216057308), reads=[B_tg], writes=[B_tg])
                          P.op("dve", lambda e: e.tensor_tensor(out=Gb[:, 0:255], in0=tg[:, 0:255], in1=ug[:, 0:255], op=ALU.mult),
                               reads=[B_tg, B_ug], writes=[B_Gb])
                          if c == 0:
                              pt2, Bp2 = next_ps("b")
                              if g2 == 0:
                                  P.op("pe", lambda e, pt2=pt2: e.matmul(pt2[0:64, 0:255], lhsT=w2k[:, 64:128], rhs=Gb[:, 0:255],
                                                                         start=True, stop=True), reads=[B_w2k, B_Gb], writes=[Bp2])
                              else:
                                  P.op("pe", lambda e, pt2=pt2: e.matmul(pt2[:, 0:255], lhsT=w2k[:, :], rhs=Gb[:, 0:255],
                                                                         start=True, stop=True), reads=[B_w2k, B_Gb], writes=[Bp2])
                              P.op("dve", lambda e, pt2=pt2, hs=hs, gp=gp: e.tensor_copy(out=kcT[hs, gp, 0:255], in_=pt2[hs, 0:255]),
                                   reads=[Bp2], writes=[B_kcT])
                          else:
                              pt2, Bp2 = next_ps("b")
                              for ch in range(2):
                                  w = 128 if ch == 0 else 127
                                  P.op("pe", lambda e, pt2=pt2, ch=ch, w=w: e.matmul(
                                      pt2[:w, ch * 64:(ch + 1) * 64], lhsT=Gb[:, ch * 128:ch * 128 + w], rhs=w2v[:, :],
                                      start=True, stop=True), reads=[B_w2v, B_Gb], writes=[Bp2])
                              for ch in range(2):
                                  w = 128 if ch == 0 else 127
                                  P.op("dve", lambda e, pt2=pt2, ch=ch, w=w, g=g: e.tensor_copy(
                                      out=vca[:w, ch, g, 0:64], in_=pt2[:w, ch * 64:(ch + 1) * 64]), reads=[Bp2], writes=[B_vca])
                  P.op("dve", lambda e: e.memset(kcT[:, :, 255:256], 0.0), writes=[B_kcT])
                  if debug:
                      dk, B_dk = sbt(p1, "dk", [128, 512], F32)
                      P.op("dve", lambda e: e.tensor_copy(out=dk[:], in_=kcT[:].rearrange("p a b -> p (a b)")), reads=[B_kcT], writes=[B_dk])
                      P.dma("sp", lambda e: e.dma_start(out=d_kc, in_=dk[:]), reads=[B_dk])
                      dv, B_dv = sbt(p1, "dv", [128, 2 * 4 * 129], F32)
                      P.op("dve", lambda e: e.tensor_copy(out=dv[:], in_=vca[:].rearrange("p a b c -> p (a b c)")), reads=[B_vca], writes=[B_dv])
                      P.dma("sp", lambda e: e.dma_start(out=d_vc, in_=dv[:]), reads=[B_dv])
                  P.barrier()

              p4 = contextlib.ExitStack()
              with p4:
                  if _LIM < 4:
                      raise _Stop()
                  nsa_prompt(nc, P, p4, sbt, next_ps, locals())
                  P.barrier()

          if _LIM < 6.2:
              raise _Stop()
          sm = contextlib.ExitStack()
          with sm:
              nsa_sample(nc, P, sm, sbt, next_ps, locals())
          if _LIM < 7:
              raise _Stop()
          hg = contextlib.ExitStack()
          with hg:
              hgrn_outputs(nc, P, hg, sbt, next_ps, locals())
          if _LIM < 8:
              raise _Stop()
          tl = contextlib.ExitStack()
          with tl:
              tail_moe(nc, P, tl, sbt, next_ps, locals())

      except _Stop:
        stopped = True
      if True:
          P.finish("sp")
          P.emit(nc)
      if not stopped:
          top.close()
    return nc


def _bf16(a):
    import ml_dtypes
    return np.ascontiguousarray(np.asarray(a, np.float32).astype(ml_dtypes.bfloat16))


def sample_tables():
    T = {}
    nl = np.arange(128)
    cb = np.zeros((128, 64), np.float32)
    for h in range(16):
        for c in range(4):
            n = 128 * c + nl
            v = SLOPES[h] * (16.0 * n + 31 - 8192)
            cb[:, h * 4 + c] = np.where(n >= 511, NEGM, v)
    T["s_cb"] = cb
    bsx = np.zeros((128, 16 * 65), np.float32)
    for h in range(16):
        for d in range(65):
            bsx[:, h * 65 + d] = SLOPES[h] * (nl - 128.0 * d)
    T["s_bs"] = bsx
    col = np.arange(128)
    sv = col % 32
    sq = np.zeros((16, 128), np.float32)
    for h in range(16):
        sq[h] = -SLOPES[h] * sv / SCALE
    T["s_sq"] = sq.reshape(1, 2048)
    ln = np.zeros((2, 128), np.float32)
    ln[0] = 1.0
    T["s_lnt"] = _bf16(ln)
    T["s_caus"] = _bf16(np.tile(np.where(nl[:, None] > sv[None, :], NEGM, 0.0), (1, 4)))
    T["s_low"] = _bf16(np.tile(np.where(nl[:, None] <= sv[None, :], NEGM, 0.0), (1, 4)))
    keep = np.ones((128, 128), np.float32)
    force = np.zeros((128, 128), np.float32)
    keep[:, 0] = 0.0
    force[:, 0] = 1e30
    T["s_keep"], T["s_force"] = keep, force
    j = np.arange(128)[None, :]
    ov = np.zeros((128, 4, 128), np.float32)
    for c in range(4):
        n = (128 * c + nl)[:, None]
        ov[:, c, :] = ((n >= 4 * j - 1) & (n <= 4 * j + 3)).astype(np.float32)
    T["s_ovl"] = _bf16(ov.reshape(128, 512))
    T["s_iota"] = nl.astype(np.float32).reshape(128, 1)
    return T


def host_tables(nnull):
    T = {}
    nl = np.arange(128)
    t_cb = np.zeros((128, 256), np.float32)
    for h in range(16):
        for k in range(NOWN):
            B = OWN0 + k
            for c in range(2):
                n = 128 * c + nl
                v = SLOPES[h] * (16.0 * n + 31 - 128 * B)
                v = np.where((n < 8 * nnull) | (n >= 255), NEGM, v)
                t_cb[:, (h * NOWN + k) * 2 + c] = v
    T["t_cb"] = t_cb
    cm = np.zeros((128, NOWN, 4, 128), np.float32)
    ql = np.arange(128)
    for k in range(NOWN):
        B = OWN0 + k
        inval = (16 * (128 + nl)[:, None] + 31) > (128 * B + ql[None, :])
        cm[:, k, :, :] = np.where(inval, NEGM, 0.0)[:, None, :]
    T["t_cmask"] = _bf16(cm.reshape(128, NOWN * 512))
    t_bs = np.zeros((128, 512), np.float32)
    for h in range(16):
        for d in range(32):
            t_bs[:, h * 32 + d] = SLOPES[h] * (nl - 128.0 * d)
    T["t_bs"] = t_bs
    sq = np.zeros((16, 128), np.float32)
    for h in range(16):
        sq[h] = -SLOPES[h] * ql / SCALE
    T["t_sq"] = sq.reshape(1, 2048)
    ln = np.zeros((2, NB, 128), np.float32)
    ln[0] = 1.0
    ln[1, :nnull] = 1.0
    T["t_ln"] = _bf16(ln.reshape(2, NB * 128))
    r2 = np.zeros((2, 512), np.float32)
    r2[1] = NEGM
    T["t_r2"] = _bf16(r2)
    E = np.zeros((64, NB, 128), np.float32)
    for c in range(NB):
        E[2 * c, c, :64] = 1.0
        E[2 * c + 1, c, 64:] = 1.0
    T["t_E"] = _bf16(E.reshape(64, NB * 128))
    tq = nl[:, None] > ql[None, :]
    T["t_caus"] = _bf16(np.tile(np.where(tq, NEGM, 0.0), (1, 4)))
    T["t_low"] = _bf16(np.tile(np.where(~tq, NEGM, 0.0), (1, 4)))
    keep = np.zeros((128, NOWN, 64), np.float32)
    force = np.zeros((128, NOWN, 64), np.float32)
    j = np.arange(64)[None, :]
    j0 = 2 * nnull
    for k in range(NOWN):
        B = OWN0 + k
        cur = (2 * B + (ql >= 64))[:, None]
        neg = (j > cur) | (j < j0)
        big = ((j == cur) | (j == j0)) & ~neg
        keep[:, k, :] = np.where(neg | big, 0.0, 1.0)
        force[:, k, :] = np.where(neg, -1e30, np.where(big, 1e30, 0.0))
    T["t_keep"] = keep.reshape(128, NOWN * 64)
    T["t_force"] = force.reshape(128, NOWN * 64)
    ov = np.zeros((128, 2, 64), np.float32)
    for c in range(2):
        n = (128 * c + nl)[:, None]
        ov[:, c, :] = ((n >= 4 * j - 1) & (n <= 4 * j + 3)).astype(np.float32)
    T["t_ovl"] = _bf16(ov.reshape(128, 128))
    a = np.arange(128)
    same = (a[:, None] // 32) == (a[None, :] // 32)
    T["t_u32"] = (same & (a[:, None] <= a[None, :])).astype(np.float32)
    T["t_l32"] = (same & (a[:, None] > a[None, :])).astype(np.float32)
    T["t_ind4"] = ((a[:, None] // 32) == np.arange(4)[None, :]).astype(np.float32)
    vm = np.ones((128, NB + 1), np.float32)
    vm[:, :nnull] = 0.0
    T["t_vmask"] = vm
    se = np.zeros((32, 32, 128), np.float32)
    for e_ in range(32):
        se[e_, e_, :] = 1.0
    T["t_selE"] = _bf16(se.reshape(32, 32 * 128))
    T["t_id"] = np.eye(128, dtype=np.float32)
    T["t_idb"] = _bf16(np.eye(128))
    return T


_NC_CACHE = {}
_TAB_CACHE = {}


def _run(inputs, debug=False):
    f = lambda a: np.ascontiguousarray(np.asarray(a, dtype=np.float32))
    x_prompt, x_sample = f(inputs["x_prompt"]), f(inputs["x_sample"])
    w_in, b_in = f(inputs["w_in"])[0], f(inputs["b_in"])[0]
    hgrn_gamma, state_hgrn, cache_win = f(inputs["hgrn_gamma"]), f(inputs["state_hgrn"])[0], f(inputs["cache_win"])[0]
    key = "nc_dbg" if debug else "nc"
    if key not in _NC_CACHE:
        _NC_CACHE[key] = build_nc(debug)
    nc = _NC_CACHE[key]

    lmask = np.tril(np.ones((128, 128), np.float32), -1)
    w_kv = np.ascontiguousarray(w_in[:, KV_OFF:KV_OFF + KV_W])
    b_kv = np.ascontiguousarray(b_in[KV_OFF:KV_OFF + KV_W])
    perm = np.zeros(1024, np.int64)
    for gp in range(2):
        for hh in range(4):
            for g2 in range(2):
                dst = ((gp * 4 + hh) * 2 + g2) * 64
                src = (4 * (2 * gp + g2) + hh) * 64
                perm[dst:dst + 64] = np.arange(src, src + 64)
    w_q = np.ascontiguousarray(w_in[:, QOFF:QOFF + 1024][:, perm])
    b_q = np.ascontiguousarray(b_in[QOFF:QOFF + 1024][perm])
    w_ng = np.ascontiguousarray(w_in[:, NGOFF:NGOFF + 48])
    b_ng = np.ascontiguousarray(b_in[NGOFF:NGOFF + 48])
    w_ss = np.ascontiguousarray(np.concatenate([w_in[:, HF_OFF:HF_OFF + 1024], w_in[:, HI_OFF:HI_OFF + 1024]], axis=1))
    b_ss = np.ascontiguousarray(np.concatenate([b_in[HF_OFF:HF_OFF + 1024], b_in[HI_OFF:HI_OFF + 1024]]))
    xTb_all = [np.ascontiguousarray(x_prompt[b].T) for b in range(2)]
    hn = f(inputs["hgrn_norm"])[0].reshape(1024)
    w_h3, b_h3, n_h3, g_h3 = [], [], [], []
    for hp in range(2):
        cs = slice(512 * hp, 512 * (hp + 1))
        w_h3.append(np.concatenate([w_in[:, o:o + 1024][:, cs] for o in (HQOFF, HF_OFF, HI_OFF, HGOFF)], axis=1))
        b_h3.append(np.concatenate([b_in[o:o + 1024][cs] for o in (HQOFF, HF_OFF, HI_OFF, HGOFF)]))
        n_h3.append(hn[cs])
        g_h3.append(hgrn_gamma[:, cs])
    shared = {
        "w_h3": np.ascontiguousarray(np.stack(w_h3)), "b_h3": np.ascontiguousarray(np.stack(b_h3)),
        "n_h3": np.ascontiguousarray(np.stack(n_h3)), "g_h3": np.ascontiguousarray(np.stack(g_h3)),
        "w_pa": f(inputs["w_pa"])[0], "w_pb": f(inputs["w_pb"])[0],
        "w_mg": np.ascontiguousarray(w_in[:, MGOFF:MGOFF + 4096]), "b_mg": np.ascontiguousarray(b_in[MGOFF:MGOFF + 4096]),
        "w_out": f(inputs["w_out"])[0],
        "ln1_g": f(inputs["ln1_g"])[0], "ln1_b": f(inputs["ln1_b"])[0], "ln2_g": f(inputs["ln2_g"])[0], "ln2_b": f(inputs["ln2_b"])[0],
        "w_r": np.ascontiguousarray(np.concatenate([f(inputs["w_rg"])[0], f(inputs["w_re"])[0]], axis=1)),
        "b_r": np.ascontiguousarray(np.concatenate([f(inputs["b_rg"])[0], f(inputs["b_re"])[0]])),
        "w_gate": np.ascontiguousarray(f(inputs["w_gate"])[0][:_NEXP]), "w_up": np.ascontiguousarray(f(inputs["w_up"])[0][:_NEXP]),
        "w_down": np.ascontiguousarray(f(inputs["w_down"])[0][:_NEXP]),
    }
    shared.update(sample_tables())
    shared["cache2d"] = np.ascontiguousarray(f(inputs["cache_kv"])[0].reshape(N_PHYS * 128 * 2, 512))
    page_table = np.ascontiguousarray(np.asarray(inputs["page_table"], dtype=np.int32))
    in_maps = []
    for c in range(NCORES):
        b, i = c // 4, c % 4
        nnull = OWN0 - 8 * i
        if nnull not in _TAB_CACHE:
            _TAB_CACHE[nnull] = host_tables(nnull)
        xfr = np.zeros((XF_T, D_MODEL), np.float32)
        nreal = (NB - nnull) * 128
        xfr[nnull * 128:NB * 128] = x_prompt[b, 0:nreal]
        for j in range(4):
            xfr[NB * 128 + 32 * j:NB * 128 + 32 * j + 4] = x_sample[4 * c + j]
        hs = slice(256 * i, 256 * (i + 1))
        w_st = np.concatenate([w_in[:, HF_OFF:HF_OFF + 1024][:, hs], w_in[:, HI_OFF:HI_OFF + 1024][:, hs]], axis=1)
        b_st = np.concatenate([b_in[HF_OFF:HF_OFF + 1024][hs], b_in[HI_OFF:HI_OFF + 1024][hs]])
        m = {
            "xf": np.ascontiguousarray(xfr.T), "xTb": xTb_all[b],
            "w_kv": w_kv, "b_kv": b_kv, "w_q": w_q, "b_q": b_q, "w_ng": w_ng, "b_ng": b_ng,
            "w_st": np.ascontiguousarray(w_st), "b_st": np.ascontiguousarray(b_st),
            "g_st": np.ascontiguousarray(hgrn_gamma[:, hs]),
            "w_ss": w_ss, "b_ss": b_ss, "g_ss": hgrn_gamma,
            "st_in": np.ascontiguousarray(state_hgrn[4 * c:4 * c + 4]),
            "cw_in": np.ascontiguousarray(cache_win[4 * c:4 * c + 4].reshape(4, 512, 512)),
            "c_lm": lmask, "c_lm4": np.ascontiguousarray(lmask[:4, :4]),
            "w_c1": f(inputs["w_cmp1"])[0], "w_c2": f(inputs["w_cmp2"])[0], "c_pe": f(inputs["cmp_pe"])[0],
        }
        m.update(_TAB_CACHE[nnull])
        m.update(shared)
        m["pt_core"] = np.ascontiguousarray(page_table[4 * c:4 * c + 4].reshape(256))
        in_maps.append(m)
    res = run_bass_kernel_spmd(nc, in_maps, core_ids=list(range(NCORES)))
    return res.results


def _assemble(R):
    y_prompt = np.zeros((2, SEQ, D_MODEL), np.float32)
    y_sample = np.zeros((32, 4, D_MODEL), np.float32)
    new_kv_prompt = np.zeros((1, 2, SEQ, 4, 4, 64), np.float32)
    new_kv_sample = np.zeros((1, 32, 4, 4, 4, 64), np.float32)
    new_win_prompt = np.zeros((1, 2, 512, 2, 4, 64), np.float32)
    new_win_sample = np.zeros((1, 32, 512, 2, 4, 64), np.float32)
    new_state_prompt = np.zeros((1, 2, 8, 128, 128), np.float32)
    new_state_sample = np.zeros((1, 32, 8, 128, 128), np.float32)
    for c in range(NCORES):
        b, i = c // 4, c % 4
        kvo = np.asarray(R[c]["kv_out"])
        new_kv_prompt[0, b, 1024 * i:1024 * (i + 1)] = kvo[:1024, :1024].reshape(1024, 4, 4, 64)
        smp = kvo[1024:].reshape(4, 32, KV_W)[:, :4]
        new_kv_sample[0, 4 * c:4 * c + 4] = smp[:, :, :1024].reshape(4, 4, 4, 4, 64)
        if i == 3:
            new_win_prompt[0, b] = kvo[512:1024, 1024:1536].reshape(512, 2, 4, 64)
        new_win_sample[0, 4 * c:4 * c + 4] = np.asarray(R[c]["win_s"]).reshape(4, 512, 2, 4, 64)
        new_state_prompt[0, b, 2 * i:2 * i + 2] = np.asarray(R[c]["st_p"])
        new_state_sample[0, 4 * c:4 * c + 4] = np.asarray(R[c]["st_s"])
        if "y_out" in R[c]:
            yo = np.asarray(R[c]["y_out"])
            y_prompt[b, 1024 * i:1024 * (i + 1)] = yo[:, :1024].T
            y_sample[4 * c:4 * c + 4] = yo[:, 1024:].T.reshape(4, 32, D_MODEL)[:, :4]
    return (y_prompt, y_sample, new_kv_prompt, new_kv_sample, new_win_prompt, new_win_sample,
            new_state_prompt, new_state_sample)


def kernel(**inputs):
    return _assemble(_run(inputs, debug=False))
```

```python
import contextlib
import numpy as np
import concourse.bass as bass
import concourse.mybir as mybir
from concourse.bass_utils import run_bass_kernel_spmd

F32 = mybir.dt.float32
BF16 = mybir.dt.bfloat16
AF = mybir.ActivationFunctionType
ALU = mybir.AluOpType

D_MODEL = 2048
KC = D_MODEL // 128
SEQ = 4096
NCORES = 8
TOK_P = 1024
TOK_S = 16
TOK = TOK_P + TOK_S
KV_OFF, KV_W = 1024, 1536
HF_OFF, HI_OFF = 3632, 4656


class _Stop(Exception):
    pass


class Buf:
    def __init__(self, name):
        self.name = name
        self.w = {}
        self.r = {}


class Prog:
    COMPUTE = ("pe", "act", "dve", "pool")

    def __init__(self, n_dma_sems=8):
        self.ops = {e: [] for e in ("pe", "act", "dve", "pool", "sp")}
        self.cnt = {}
        self.waited = {}
        self.n_dma = n_dma_sems
        self.dma_rr = {"sp": 0, "pool": 0, "act": 0}

    def _need(self, eng, reads, writes):
        need = {}
        for b in reads:
            for e, s in b.w.items():
                need[e] = max(need.get(e, 0), s)
        for b in writes:
            for e, s in b.w.items():
                need[e] = max(need.get(e, 0), s)
            for e, s in b.r.items():
                need[e] = max(need.get(e, 0), s)
        waits = []
        for e, s in need.items():
            if e == eng and eng == "pe":
                continue
            if self.waited.get((eng, e), 0) >= s:
                continue
            self.waited[(eng, e)] = s
            waits.append((e, s))
        return waits

    def op(self, eng, fn, reads=(), writes=()):
        waits = self._need(eng, reads, writes)
        self.cnt[eng] = self.cnt.get(eng, 0) + 1
        s = self.cnt[eng]
        for b in reads:
            b.r[eng] = s
        for b in writes:
            b.w[eng] = s
        self.ops[eng].append((waits, fn, eng))

    def dma(self, queue, fn, reads=(), writes=()):
        k = self.dma_rr[queue]
        self.dma_rr[queue] = (k + 1) % self.n_dma
        v = "dma_%s_%d" % (queue, k)
        waits = self._need(queue, reads, writes)
        prev = self.cnt.get(v, 0)
        if prev and self.waited.get((queue, v), 0) < prev:
            self.waited[(queue, v)] = prev
            waits.append((v, prev))
        self.cnt[v] = prev + 1
        s = self.cnt[v]
        for b in reads:
            b.r[v] = s
        for b in writes:
            b.w[v] = s
        self.ops[queue].append((waits, fn, v))

    def barrier(self):
        targets = dict(self.cnt)
        for eng in self.ops:
            waits = []
            for e, n in targets.items():
                if e == eng and eng == "pe":
                    continue
                if n and self.waited.get((eng, e), 0) < n:
                    self.waited[(eng, e)] = n
                    waits.append((e, n))
            self.ops[eng].append((waits, None, None))

    def finish(self, queue="sp"):
        waits = []
        for v, n in self.cnt.items():
            if v.startswith("dma_") and self.waited.get((queue, v), 0) < n:
                waits.append((v, n))
        self.ops[queue].append((waits, None, None))

    def emit(self, nc):
        names = sorted(set(list(self.COMPUTE) + [v for v in self.cnt if v.startswith("dma_")]))
        with contextlib.ExitStack() as es:
            sems = {n: es.enter_context(nc.semaphore("s_" + n)) for n in names}
            block = es.enter_context(nc.Block())

            def run(eng_name, eng):
                for waits, fn, inc in self.ops[eng_name]:
                    for (e, s) in waits:
                        eng.wait_ge(sems[e], s * 16 if e.startswith("dma_") else s)
                    if fn is None:
                        continue
                    ins = fn(eng)
                    ins.then_inc(sems[inc], 16 if inc.startswith("dma_") else 1)

            @block.tensor
            def _(e):
                run("pe", e)

            @block.scalar
            def _(e):
                run("act", e)

            @block.vector
            def _(e):
                run("dve", e)

            @block.gpsimd
            def _(e):
                run("pool", e)

            @block.sync
            def _(e):
                run("sp", e)


def stage_states(nc, P, st, sbt, next_ps, ones_c, B_ones, D):
    xTb, xf = D["xTb"], D["xf"]
    wbig, B_wbig = sbt(st, "wbig", [128, KC, 2048], BF16)
    wst_sb, B_wst = sbt(st, "wst_sb", [128, KC, 512], BF16)
    bias_big, B_bias_big = sbt(st, "bias_big", [128, 2048], F32)
    bias_st, B_bias_st = sbt(st, "bias_st", [128, 512], F32)
    oml_st, B_oml_st = sbt(st, "oml_st", [128, 256], F32)
    oml_ss, B_oml_ss = sbt(st, "oml_ss", [128, 1024], F32)
    lm, B_lm = sbt(st, "lm", [128, 128], F32)
    lm4, B_lm4 = sbt(st, "lm4", [4, 4], F32)
    xs = [sbt(st, "sxs%d" % i, [128, KC, 256], BF16) for i in range(2)]
    xsm, B_xsm = sbt(st, "xsm", [128, KC, 128], BF16)
    S_sb, B_S = sbt(st, "S_sb", [128, 2, 128], F32)
    NSCR = 2
    scr = []
    for i in range(NSCR):
        d = {}
        for n, dt in (("kk", F32), ("lgf", F32), ("edd", F32), ("kd", BF16), ("vv", BF16)):
            d[n] = sbt(st, "%s%d" % (n, i), [128, 1024], dt)
        d["edl"] = sbt(st, "edl%d" % i, [128, 8], F32)
        scr.append(d)

    P.dma("sp", lambda e: e.dma_start(out=lm[:], in_=D["c_lm"]), writes=[B_lm])
    P.dma("sp", lambda e: e.dma_start(out=lm4[:], in_=D["c_lm4"]), writes=[B_lm4])
    P.dma("sp", lambda e: e.dma_start(out=bias_st[:], in_=D["b_st"].partition_broadcast(128)), writes=[B_bias_st])
    wst_v = D["w_st"].rearrange("(kc p) c -> p kc c", p=128)
    for q in range(2):
        P.dma("pool", lambda e, q=q: e.dma_start(out=wst_sb[:, 8 * q:8 * q + 8, :], in_=wst_v[:, 8 * q:8 * q + 8, :]),
              writes=[B_wst])

    def lower_bound_prep(g_ap, n, oml, B_oml):
        g0, Bg0 = scr[0]["lgf"]
        g1, Bg1 = scr[1]["lgf"]
        P.dma("sp", lambda e: e.dma_start(out=g0[:, 0:n], in_=g_ap[0].partition_broadcast(128)), writes=[Bg0])
        P.dma("sp", lambda e: e.dma_start(out=g1[:, 0:n], in_=g_ap[1].partition_broadcast(128)), writes=[Bg1])
        P.op("dve", lambda e: e.tensor_tensor(out=oml[:, 0:n], in0=g1[:, 0:n], in1=g0[:, 0:n], op=ALU.subtract),
             reads=[Bg0, Bg1], writes=[B_oml])
        P.op("act", lambda e: e.activation(out=oml[:, 0:n], in_=oml[:, 0:n], func=AF.Sigmoid), reads=[B_oml], writes=[B_oml])

    def state_chunk(si, m, xsl, W, B_W, B_x, bias, B_bias, oml, B_oml, nh, lmask, B_lmask, S_list):
        d = scr[si % NSCR]
        kk, Bkk = d["kk"]
        lgf, Blgf = d["lgf"]
        edd, Bedd = d["edd"]
        kd, Bkd = d["kd"]
        vv, Bvv = d["vv"]
        edl, Bedl = d["edl"]
        n = nh * 128
        for g in range((2 * n) // 512):
            pt, Bp = next_ps("a")
            for kc in range(KC):
                P.op("pe", lambda e, pt=pt, kc=kc, g=g: e.matmul(
                    pt[:m, :], lhsT=xsl(kc), rhs=W[:, kc, g * 512:(g + 1) * 512],
                    start=(kc == 0), stop=(kc == KC - 1)), reads=[B_x, B_W], writes=[Bp])
            c0 = g * 512
            a0, a1 = c0, min(c0 + 512, n)
            if a1 > a0:
                P.op("dve", lambda e, pt=pt, a0=a0, a1=a1, c0=c0: e.tensor_tensor(
                    out=kk[:m, a0:a1], in0=pt[:m, a0 - c0:a1 - c0], in1=bias[:m, a0:a1], op=ALU.add),
                    reads=[Bp, B_bias], writes=[Bkk])
            v0, v1 = max(c0, n), c0 + 512
            if v1 > v0:
                P.op("dve", lambda e, pt=pt, v0=v0, v1=v1, c0=c0: e.tensor_tensor(
                    out=vv[:m, v0 - n:v1 - n], in0=pt[:m, v0 - c0:v1 - c0], in1=bias[:m, v0:v1], op=ALU.add),
                    reads=[Bp, B_bias], writes=[Bvv])
        P.op("act", lambda e: e.activation(out=kk[:m, 0:n], in_=kk[:m, 0:n], func=AF.Sigmoid, scale=-1.0),
             reads=[Bkk], writes=[Bkk])
        P.op("dve", lambda e: e.tensor_tensor(out=kk[:m, 0:n], in0=kk[:m, 0:n], in1=oml[:m, 0:n], op=ALU.mult),
             reads=[Bkk, B_oml], writes=[Bkk])
        P.op("act", lambda e: e.activation(out=lgf[:m, 0:n], in_=kk[:m, 0:n], func=AF.Ln, scale=-1.0, bias=ones_c[:m, :]),
             reads=[Bkk, B_ones], writes=[Blgf])
        for g in range((n + 511) // 512):
            w = min(512, n - g * 512)
            pt, Bp = next_ps("a")
            P.op("pe", lambda e, pt=pt, g=g, w=w: e.matmul(pt[:m, 0:w], lhsT=lmask[:m, :m], rhs=lgf[:m, g * 512:g * 512 + w],
                                                           start=True, stop=True), reads=[B_lmask, Blgf], writes=[Bp])
            P.op("act", lambda e, pt=pt, g=g, w=w: e.activation(out=edd[:m, g * 512:g * 512 + w], in_=pt[:m, 0:w], func=AF.Exp),
                 reads=[Bp], writes=[Bedd])
        P.op("dve", lambda e: e.tensor_tensor(out=kd[:m, 0:n], in0=kk[:m, 0:n], in1=edd[:m, 0:n], op=ALU.mult),
             reads=[Bkk, Bedd], writes=[Bkd])
        pt, Bp = next_ps("b")
        for h in range(nh):
            P.op("pe", lambda e, pt=pt, h=h: e.matmul(pt[:, h:h + 1], lhsT=lgf[:m, h * 128:(h + 1) * 128], rhs=ones_c[:m, :],
                                                      start=True, stop=True), reads=[Blgf, B_ones], writes=[Bp])
        P.op("act", lambda e, pt=pt: e.activation(out=edl[:, 0:nh], in_=pt[:, 0:nh], func=AF.Exp), reads=[Bp], writes=[Bedl])
        for h0 in range(0, nh, 4):
            pt, Bp = next_ps("b")
            hs_ = list(range(h0, min(nh, h0 + 4)))
            for h in hs_:
                P.op("pe", lambda e, pt=pt, h=h, h0=h0: e.matmul(
                    pt[:, (h - h0) * 128:(h - h0 + 1) * 128], lhsT=kd[:m, h * 128:(h + 1) * 128], rhs=vv[:m, h * 128:(h + 1) * 128],
                    start=True, stop=True), reads=[Bkd, Bvv], writes=[Bp])
            for h in hs_:
                s_in, s_out, b_in, b_out = S_list[h]
                P.op("dve", lambda e, pt=pt, h=h, h0=h0, s_in=s_in, s_out=s_out: e.scalar_tensor_tensor(
                    out=s_out, in0=s_in, scalar=edl[:, h:h + 1], in1=pt[:, (h - h0) * 128:(h - h0 + 1) * 128],
                    op0=ALU.mult, op1=ALU.add), reads=[Bp, Bedl, b_in], writes=[b_out])

    lower_bound_prep(D["g_st"], 256, oml_st, B_oml_st)
    P.op("dve", lambda e: e.memset(S_sb[:], 0.0), writes=[B_S])
    xTb_v = xTb.rearrange("(kc p) t -> p kc t", p=128)
    si = 0
    for tt in range(SEQ // 256):
        xb, Bx = xs[tt % 2]
        for q in range(2):
            P.dma("pool", lambda e, xb=xb, tt=tt, q=q: e.dma_start(
                out=xb[:, 8 * q:8 * q + 8, :], in_=xTb_v[:, 8 * q:8 * q + 8, tt * 256:(tt + 1) * 256]), writes=[Bx])
        for c4 in range(2):
            S_list = [(S_sb[:, h, :], S_sb[:, h, :], B_S, B_S) for h in range(2)]
            state_chunk(si, 128, lambda kc, xb=xb, c4=c4: xb[:, kc, c4 * 128:(c4 + 1) * 128],
                        wst_sb, B_wst, Bx, bias_st, B_bias_st, oml_st, B_oml_st, 2, lm, B_lm, S_list)
            si += 1
    for h in range(2):
        P.dma("sp", lambda e, h=h: e.dma_start(out=D["st_p"][h], in_=S_sb[:, h, :]), reads=[B_S])

    wss_v = D["w_ss"].rearrange("(kc p) c -> p kc c", p=128)
    for q in range(4):
        P.dma("pool", lambda e, q=q: e.dma_start(out=wbig[:, 4 * q:4 * q + 4, :], in_=wss_v[:, 4 * q:4 * q + 4, :]),
              writes=[B_wbig])
    P.dma("sp", lambda e: e.dma_start(out=bias_big[:], in_=D["b_ss"].partition_broadcast(128)), writes=[B_bias_big])
    xf_v = xf.rearrange("(kc p) t -> p kc t", p=128)
    for q in range(2):
        P.dma("pool", lambda e, q=q: e.dma_start(out=xsm[:, 8 * q:8 * q + 8, :], in_=xf_v[:, 8 * q:8 * q + 8, NB * 128:(NB + 1) * 128]),
              writes=[B_xsm])
    lower_bound_prep(D["g_ss"], 1024, oml_ss, B_oml_ss)
    s0 = [sbt(st, "s0_%d" % i, [128, 8, 128], F32) for i in range(2)]
    for j in range(4):
        sj, Bsj = s0[j % 2]
        P.dma("sp", lambda e, sj=sj, j=j: e.dma_start(out=sj[:], in_=D["st_in"][j].rearrange("h k v -> k h v")), writes=[Bsj])
        S_list = [(sj[:, h, :], sj[:, h, :], Bsj, Bsj) for h in range(8)]
        state_chunk(si, 4, lambda kc, j=j: xsm[:, kc, 32 * j:32 * j + 4],
                    wbig, B_wbig, B_xsm, bias_big, B_bias_big, oml_ss, B_oml_ss, 8, lm4, B_lm4, S_list)
        si += 1
        P.dma("sp", lambda e, sj=sj, j=j: e.dma_start(out=D["st_s"][j].rearrange("h k v -> k h v"), in_=sj[:]), reads=[Bsj])


def nsa_prompt(nc, P, p4, sbt, next_ps, L):
    debug = L["debug"]
    xf = L["xf"]
    kslcT, B_kslcT, kwinT, B_kwinT = L["kslcT"], L["B_kslcT"], L["kwinT"], L["B_kwinT"]
    vslc, B_vslc, vwin, B_vwin = L["vslc"], L["B_vslc"], L["vwin"], L["B_vwin"]
    kcT, B_kcT, vca, B_vca = L["kcT"], L["B_kcT"], L["vca"], L["B_vca"]
    ident, B_ident, identb, B_identb = L["ident"], L["B_ident"], L["identb"], L["B_identb"]
    onesb, B_onesb = L["onesb"], L["B_onesb"]
    o_nsaT, B_onsaT = L["o_nsaT"], L["B_onsaT"]

    def table(name, shape, dt, src, q="sp"):
        t, B = sbt(p4, name, shape, dt)
        P.dma(q, lambda e: e.dma_start(out=t[:], in_=src), writes=[B])
        return t, B
    cb, B_cb = table("cb", [128, 256], F32, L["t_cb"])
    cmask, B_cmask = table("cmask", [128, NOWN * 512], BF16, L["t_cmask"])
    bs, B_bs = table("bs", [128, 512], F32, L["t_bs"])
    sq, B_sq = table("sq", [1, 2048], F32, L["t_sq"])
    lnt, B_lnt = table("lnt", [2, NB * 128], BF16, L["t_ln"])
    R2, B_R2 = table("R2", [2, 512], BF16, L["t_r2"])
    Et, B_Et = table("Et", [64, NB * 128], BF16, L["t_E"])
    caus, B_caus = table("caus", [128, 512], BF16, L["t_caus"])
    low, B_low = table("low", [128, 512], BF16, L["t_low"])
    keep, B_keep = table("keep", [128, NOWN * 64], F32, L["t_keep"])
    force, B_force = table("force", [128, NOWN * 64], F32, L["t_force"])

    qT, B_qT = sbt(p4, "qT", [128, 8, NOWN * 128], BF16)
    gates, B_gates = sbt(p4, "gates", [128, NOWN, 48], F32)
    pq = contextlib.ExitStack()
    with pq:
        wq, B_wq = sbt(pq, "wq", [128, KC, 1024], BF16)
        wng, B_wng = sbt(pq, "wng", [128, KC, 48], BF16)
        bq_col, B_bq = sbt(pq, "bq_col", [128, 8], F32)
        bng, B_bng = sbt(pq, "bng", [128, 48], F32)
        xo = [sbt(pq, "xo%d" % i, [128, KC, 256], BF16) for i in range(1)]
        wq_v = L["w_q"].rearrange("(kc p) c -> p kc c", p=128)
        for q in range(4):
            P.dma("pool", lambda e, q=q: e.dma_start(out=wq[:, 4 * q:4 * q + 4, :], in_=wq_v[:, 4 * q:4 * q + 4, :]), writes=[B_wq])
        P.dma("pool", lambda e: e.dma_start(out=wng[:], in_=L["w_ng"].rearrange("(kc p) c -> p kc c", p=128)), writes=[B_wng])
        with nc.allow_non_contiguous_dma(reason="tiny bias column layout"):
            P.dma("sp", lambda e: e.dma_start(out=bq_col[:], in_=L["b_q"].rearrange("(cb p) -> p cb", p=128), allow_slow_non_contiguous=True), writes=[B_bq])
        P.dma("sp", lambda e: e.dma_start(out=bng[:], in_=L["b_ng"].partition_broadcast(128)), writes=[B_bng])
        xf_v = xf.rearrange("(kc p) t -> p kc t", p=128)
        for tt in range(4):
            xb, Bx = xo[0]
            t0 = OWN0 * 128 + tt * 256
            for q in range(2):
                P.dma("pool", lambda e, xb=xb, t0=t0, q=q: e.dma_start(
                    out=xb[:, 8 * q:8 * q + 8, :], in_=xf_v[:, 8 * q:8 * q + 8, t0:t0 + 256]), writes=[Bx])
            for cbk in range(8):
                pt, Bp = next_ps("a")
                for kc in range(KC):
                    P.op("pe", lambda e, pt=pt, kc=kc, cbk=cbk, xb=xb: e.matmul(
                        pt[:, 0:256], lhsT=wq[:, kc, cbk * 128:(cbk + 1) * 128], rhs=xb[:, kc, :],
                        start=(kc == 0), stop=(kc == KC - 1)), reads=[B_wq, Bx], writes=[Bp])
                P.op("act", lambda e, pt=pt, cbk=cbk, tt=tt: e.activation(
                    out=qT[:, cbk, tt * 256:(tt + 1) * 256], in_=pt[:, 0:256], func=AF.Identity, bias=bq_col[:, cbk:cbk + 1]),
                    reads=[Bp, B_bq], writes=[B_qT])
            for bi in range(2):
                k = tt * 2 + bi
                pt, Bp = next_ps("b")
                for kc in range(KC):
                    P.op("pe", lambda e, pt=pt, kc=kc, xb=xb, bi=bi: e.matmul(
                        pt[:, 0:48], lhsT=xb[:, kc, bi * 128:(bi + 1) * 128], rhs=wng[:, kc, :],
                        start=(kc == 0), stop=(kc == KC - 1)), reads=[B_wng, Bx], writes=[Bp])
                P.op("dve", lambda e, pt=pt, k=k: e.tensor_tensor(out=gates[:, k, :], in0=pt[:, 0:48], in1=bng[:, :], op=ALU.add),
                     reads=[Bp, B_bng], writes=[B_gates])
        P.op("act", lambda e: e.activation(out=gates[:], in_=gates[:], func=AF.Sigmoid), reads=[B_gates], writes=[B_gates])
        P.barrier()

    sqt, B_sqt = sbt(p4, "sqt", [128, 512], BF16)
    runmax, B_runmax = sbt(p4, "runmax", [1, 512], F32)
    nkm, B_nkm = sbt(p4, "nkm", [1, 1], F32)
    P.op("dve", lambda e: e.memset(runmax[:], 0.0), writes=[B_runmax])
    srcs = []
    for gp in range(2):
        for s in range(NB * 128 // 512):
            srcs.append((kslcT, B_kslcT, gp, s * 512, 512))
        for s in range(3):
            srcs.append((kwinT, B_kwinT, gp, s * 512, 512))
        srcs.append((kcT, B_kcT, gp, 0, 256))
    for (src, Bs, gp, c0, w) in srcs:
        P.op("dve", lambda e, src=src, gp=gp, c0=c0, w=w: e.tensor_tensor(
            out=sqt[:, 0:w], in0=src[:, gp, c0:c0 + w], in1=src[:, gp, c0:c0 + w], op=ALU.mult), reads=[Bs], writes=[B_sqt])
        pt, Bp = next_ps("b")
        P.op("pe", lambda e, pt=pt, w=w: e.matmul(pt[0:1, 0:w], lhsT=onesb[:, 0:1], rhs=sqt[:, 0:w], start=True, stop=True),
             reads=[B_onesb, B_sqt], writes=[Bp])
        P.op("dve", lambda e, pt=pt, w=w: e.tensor_tensor(out=runmax[:, 0:w], in0=runmax[:, 0:w], in1=pt[0:1, 0:w], op=ALU.max),
             reads=[Bp, B_runmax], writes=[B_runmax])
    P.op("dve", lambda e: e.reduce_max(out=nkm[:], in_=runmax[:], axis=mybir.AxisListType.X), reads=[B_runmax], writes=[B_nkm])
    P.op("dve", lambda e: e.tensor_scalar(out=nkm[:], in0=nkm[:], scalar1=-0.5, scalar2=None, op0=ALU.mult),
         reads=[B_nkm], writes=[B_nkm])
    P.op("dve", lambda e: e.tensor_scalar(out=sq[:, :], in0=sq[:, :], scalar1=nkm[0:1, 0:1], scalar2=None, op0=ALU.add),
         reads=[B_nkm, B_sq], writes=[B_sq])

    if _LIM < 5:
        raise _Stop()
    pT = [sbt(p4, "pT%d" % i, [128, 4, 128], BF16) for i in range(4)]
    pT_rr = [0]

    def next_pT():
        k = pT_rr[0] % len(pT)
        pT_rr[0] += 1
        return pT[k]
    qsq, B_qsq = sbt(p4, "qsq", [128, 512], BF16)
    rt, B_rt = sbt(p4, "rt", [1, 512], F32)
    o_blk, B_oblk = sbt(p4, "o_blk", [128, 256], F32)
    rs, B_rs = sbt(p4, "rs", [128, 4], F32)
    wgt, B_wgt = sbt(p4, "wgt", [128, 4], F32)
    imp, B_imp = sbt(p4, "imp", [128, 64], F32)
    imp3, B_imp3 = sbt(p4, "imp3", [128, 64], F32)
    m8, B_m8 = sbt(p4, "m8", [128, 16], F32)
    nsel, B_nsel = sbt(p4, "nsel", [128, 64], F32)
    nselT, B_nselT = sbt(p4, "nselT", [64, 512], BF16)
    if debug:
        dimp, B_dimp = sbt(p4, "dimp", [128, 64], F32)

    def softmax_chunk(pt, Bp, w, hbase, col_fn, tab, B_tab):
        (t, Bt) = next_pT()
        for hh in range(4):
            P.op("act", lambda e, pt=pt, t=t, hh=hh, w=w: e.activation(
                out=t[:w, hh, :], in_=pt[:w, hh * 128:(hh + 1) * 128], func=AF.Exp, scale=SCALE,
                bias=tab[:w, col_fn(hbase + hh):col_fn(hbase + hh) + 1]), reads=[Bp, B_tab], writes=[Bt])
        return t, Bt

    def finish_branch(psO, BpO, k, g, br, first):
        P.op("dve", lambda e: e.tensor_scalar(out=rs[:, :], in0=psO[:, 0:260].rearrange("p (h c) -> p h c", c=65)[:, :, 64],
                                              scalar1=1e-30, scalar2=None, op0=ALU.max), reads=[BpO], writes=[B_rs])
        P.op("dve", lambda e: e.reciprocal(out=rs[:, :], in_=rs[:, :]), reads=[B_rs], writes=[B_rs])
        P.op("dve", lambda e: e.tensor_tensor(out=wgt[:, :], in0=rs[:, :], in1=gates[:, k, br * 16 + g * 4:br * 16 + g * 4 + 4],
                                              op=ALU.mult), reads=[B_rs, B_gates], writes=[B_wgt])
        for hh in range(4):
            oc = slice(hh * 64, hh * 64 + 64)
            if first:
                P.op("dve", lambda e, hh=hh, oc=oc: e.tensor_scalar(out=o_blk[:, oc], in0=psO[:, hh * 65:hh * 65 + 64],
                                                                    scalar1=wgt[:, hh:hh + 1], scalar2=None, op0=ALU.mult),
                     reads=[BpO, B_wgt], writes=[B_oblk])
            else:
                P.op("dve", lambda e, hh=hh, oc=oc: e.scalar_tensor_tensor(
                    out=o_blk[:, oc], in0=psO[:, hh * 65:hh * 65 + 64], scalar=wgt[:, hh:hh + 1], in1=o_blk[:, oc],
                    op0=ALU.mult, op1=ALU.add), reads=[BpO, B_wgt, B_oblk], writes=[B_oblk])

    kslc_g, B_kslcg = sbt(p4, "kslc_g", [64, NB * 128], BF16)
    kwin_g, B_kwing = sbt(p4, "kwin_g", [64, 12 * 128], BF16)
    kc_g, B_kcg = sbt(p4, "kc_g", [64, 256], BF16)
    q_g, B_qg = sbt(p4, "q_g", [64, 4, NOWN * 128], BF16)
    for g in range(4):
        gp, g2 = g // 2, g % 2
        hs0 = slice(64 * g2, 64 * g2 + 64)
        P.dma("sp", lambda e, hs0=hs0, gp=gp: e.dma_start(out=kslc_g[:, :], in_=kslcT[hs0, gp, :]), reads=[B_kslcT], writes=[B_kslcg])
        P.dma("sp", lambda e, hs0=hs0, gp=gp: e.dma_start(out=kwin_g[:, :], in_=kwinT[hs0, gp, :]), reads=[B_kwinT], writes=[B_kwing])
        P.dma("sp", lambda e, hs0=hs0, gp=gp: e.dma_start(out=kc_g[:, :], in_=kcT[hs0, gp, :]), reads=[B_kcT], writes=[B_kcg])
        P.dma("sp", lambda e, hs0=hs0, gp=gp: e.dma_start(out=q_g[:, :, :], in_=qT[hs0, gp * 4:gp * 4 + 4, :]), reads=[B_qT], writes=[B_qg])
        hs = slice(0, 64)
        for k in range(NOWN):
            if _LIM < 6 and (k, g) not in _KSEL:
                continue
            B = OWN0 + k
            tok = slice(k * 128, (k + 1) * 128)
            qv = q_g[:, :, tok]
            P.op("dve", lambda e, qv=qv, hs=hs: e.tensor_tensor(out=qsq[hs, :].rearrange("p (h q) -> p h q", h=4), in0=qv, in1=qv,
                                                                op=ALU.mult), reads=[B_qg], writes=[B_qsq])
            pt, Bp = next_ps("b")
            P.op("pe", lambda e, pt=pt, hs=hs: e.matmul(pt[0:1, :], lhsT=onesb[hs, 0:1], rhs=qsq[hs, :], start=True, stop=True),
                 reads=[B_onesb, B_qsq], writes=[Bp])
            P.op("dve", lambda e, pt=pt, g=g: e.scalar_tensor_tensor(out=R2[0:1, :], in0=pt[0:1, :], scalar=-0.5,
                                                                     in1=sq[0:1, g * 512:(g + 1) * 512], op0=ALU.mult, op1=ALU.add),
                 reads=[Bp, B_sq], writes=[B_R2])

            pts = []
            for c in range(2):
                w = 128 if c == 0 else 127
                pt, Bp = next_ps("a")
                P.op("pe", lambda e, pt=pt, c=c, w=w, hs=hs, gp=gp, qv=qv: e.matmul(
                    pt[:w, :], lhsT=kc_g[hs, c * 128:c * 128 + w], rhs=qv, start=True, stop=False),
                    reads=[B_kcg, B_qg], writes=[Bp])
                P.op("pe", lambda e, pt=pt, w=w, c=c: e.matmul(pt[:w, :], lhsT=lnt[0:1, 0:w], rhs=R2[0:1, :], start=False, stop=(c == 0)),
                     reads=[B_lnt, B_R2], writes=[Bp])
                if c == 1:
                    P.op("pe", lambda e, pt=pt, w=w, k=k: e.matmul(pt[:w, :], lhsT=identb[:w, :w], rhs=cmask[:w, k * 512:(k + 1) * 512],
                                                                   start=False, stop=True), reads=[B_identb, B_cmask], writes=[Bp])
                t, Bt = softmax_chunk(pt, Bp, w, 4 * g, lambda h, k=k, c=c: (h * NOWN + k) * 2 + c, cb, B_cb)
                pts.append((t, Bt, w))
            if _LIM < 5.2:
                continue
            psO, BpO = next_ps("b")
            psI, BpI = next_ps("b")
            for hh in range(4):
                for c, (t, Bt, w) in enumerate(pts):
                    P.op("pe", lambda e, hh=hh, c=c, t=t, w=w, g=g, psO=psO: e.matmul(
                        psO[:, hh * 65:(hh + 1) * 65], lhsT=t[:w, hh, :], rhs=vca[:w, c, g, 0:65], start=(c == 0), stop=(c == 1)),
                        reads=[Bt, B_vca], writes=[BpO])
            for hh in range(4):
                for c, (t, Bt, w) in enumerate(pts):
                    P.op("pe", lambda e, hh=hh, c=c, t=t, w=w, g=g, psI=psI: e.matmul(
                        psI[:, hh * 64:(hh + 1) * 64], lhsT=t[:w, hh, :], rhs=vca[:w, c, g, 65:129], start=(c == 0), stop=(c == 1)),
                        reads=[Bt, B_vca], writes=[BpI])
            finish_branch(psO, BpO, k, g, 0, True)
            for hh in range(4):
                if hh == 0:
                    P.op("dve", lambda e, psI=psI: e.tensor_scalar(out=imp[:, :], in0=psI[:, 0:64], scalar1=rs[:, 0:1], scalar2=None,
                                                          op0=ALU.mult), reads=[BpI, B_rs], writes=[B_imp])
                else:
                    P.op("dve", lambda e, hh=hh, psI=psI: e.scalar_tensor_tensor(
                        out=imp[:, :], in0=psI[:, hh * 64:(hh + 1) * 64], scalar=rs[:, hh:hh + 1], in1=imp[:, :],
                        op0=ALU.mult, op1=ALU.add), reads=[BpI, B_rs, B_imp], writes=[B_imp])
            if debug:
                P.op("dve", lambda e: e.tensor_copy(out=dimp[:], in_=imp[:]), reads=[B_imp], writes=[B_dimp])
                P.dma("sp", lambda e, k=k, g=g: e.dma_start(out=L["d_imp"][(k * 4 + g) * 128:(k * 4 + g + 1) * 128, :], in_=dimp[:]),
                      reads=[B_dimp])
            if _LIM < 5.3:
                continue
            P.op("dve", lambda e: e.tensor_scalar(out=imp[:, :], in0=imp[:, :], scalar1=1e-30, scalar2=None, op0=ALU.max),
                 reads=[B_imp], writes=[B_imp])
            P.op("dve", lambda e, k=k: e.tensor_tensor(out=imp[:, :], in0=imp[:, :], in1=keep[:, k * 64:(k + 1) * 64], op=ALU.mult),
                 reads=[B_imp, B_keep], writes=[B_imp])
            P.op("dve", lambda e, k=k: e.tensor_tensor(out=imp[:, :], in0=imp[:, :], in1=force[:, k * 64:(k + 1) * 64], op=ALU.add),
                 reads=[B_imp, B_force], writes=[B_imp])
            P.op("dve", lambda e: e.max(out=m8[:, 0:8], in_=imp[:, :]), reads=[B_imp], writes=[B_m8])
            P.op("dve", lambda e: e.tensor_scalar(out=imp3[:, :], in0=imp[:, :], scalar1=m8[:, 7:8], scalar2=None, op0=ALU.is_ge),
                 reads=[B_imp, B_m8], writes=[B_imp3])
            P.op("dve", lambda e: e.scalar_tensor_tensor(out=imp3[:, :], in0=imp3[:, :], scalar=-3.0e38, in1=imp[:, :],
                                                         op0=ALU.mult, op1=ALU.add), reads=[B_imp, B_imp3], writes=[B_imp3])
            P.op("dve", lambda e: e.max(out=m8[:, 8:16], in_=imp3[:, :]), reads=[B_imp3], writes=[B_m8])
            P.op("dve", lambda e: e.tensor_scalar(out=nsel[:, :], in0=imp[:, :], scalar1=m8[:, 15:16], scalar2=None, op0=ALU.is_ge),
                 reads=[B_imp, B_m8], writes=[B_nsel])
            P.op("dve", lambda e: e.tensor_scalar(out=nsel[:, :], in0=nsel[:, :], scalar1=-NEGM, scalar2=NEGM, op0=ALU.mult, op1=ALU.add),
                 reads=[B_nsel], writes=[B_nsel])
            pt, Bp = next_ps("b")
            P.op("pe", lambda e, pt=pt: e.transpose(out=pt[0:64, 0:128], in_=nsel[:, :], identity=ident[:, :]),
                 reads=[B_nsel, B_ident], writes=[Bp])
            for hh in range(4):
                P.op("act", lambda e, pt=pt, hh=hh: e.activation(out=nselT[:, hh * 128:(hh + 1) * 128], in_=pt[0:64, 0:128],
                                                                 func=AF.Identity), reads=[Bp], writes=[B_nselT])
            if _LIM < 5.4:
                continue
            psO, BpO = next_ps("b")
            for c in range(B + 1):
                pt, Bp = next_ps("a")
                P.op("pe", lambda e, pt=pt, c=c, hs=hs, gp=gp, qv=qv: e.matmul(
                    pt[:, :], lhsT=kslc_g[hs, c * 128:(c + 1) * 128], rhs=qv, start=True, stop=False),
                    reads=[B_kslcg, B_qg], writes=[Bp])
                P.op("pe", lambda e, pt=pt, c=c: e.matmul(pt[:, :], lhsT=Et[:, c * 128:(c + 1) * 128], rhs=nselT[:, :],
                                                          start=False, stop=False), reads=[B_Et, B_nselT], writes=[Bp])
                P.op("pe", lambda e, pt=pt, c=c, B=B: e.matmul(pt[:, :], lhsT=lnt[0:2, c * 128:(c + 1) * 128], rhs=R2[0:2, :],
                                                               start=False, stop=(c != B)), reads=[B_lnt, B_R2], writes=[Bp])
                if c == B:
                    P.op("pe", lambda e, pt=pt: e.matmul(pt[:, :], lhsT=identb[:, :], rhs=caus[:, :], start=False, stop=True),
                         reads=[B_identb, B_caus], writes=[Bp])
                t, Bt = softmax_chunk(pt, Bp, 128, 4 * g, lambda h, d=B - c: h * 32 + d, bs, B_bs)
                for hh in range(4):
                    P.op("pe", lambda e, hh=hh, t=t, c=c, g=g, B=B, psO=psO: e.matmul(
                        psO[:, hh * 65:(hh + 1) * 65], lhsT=t[:, hh, :], rhs=vslc[:, c, g, :], start=(c == 0), stop=(c == B)),
                        reads=[Bt, B_vslc], writes=[BpO])
            finish_branch(psO, BpO, k, g, 1, False)
            if _LIM < 5.5:
                continue
            psO, BpO = next_ps("b")
            for c in range(B - 4, B + 1):
                cw = c - 20
                pt, Bp = next_ps("a")
                P.op("pe", lambda e, pt=pt, cw=cw, hs=hs, gp=gp, qv=qv: e.matmul(
                    pt[:, :], lhsT=kwin_g[hs, cw * 128:(cw + 1) * 128], rhs=qv, start=True, stop=False),
                    reads=[B_kwing, B_qg], writes=[Bp])
                edge = (c == B) or (c == B - 4)
                P.op("pe", lambda e, pt=pt, c=c, edge=edge: e.matmul(pt[:, :], lhsT=lnt[0:2, c * 128:(c + 1) * 128], rhs=R2[0:2, :],
                                                                      start=False, stop=(not edge)), reads=[B_lnt, B_R2], writes=[Bp])
                if edge:
                    mk, Bmk = (caus, B_caus) if c == B else (low, B_low)
                    P.op("pe", lambda e, pt=pt, mk=mk: e.matmul(pt[:, :], lhsT=identb[:, :], rhs=mk[:, :], start=False, stop=True),
                         reads=[B_identb, Bmk], writes=[Bp])
                t, Bt = softmax_chunk(pt, Bp, 128, 4 * g, lambda h, d=B - c: h * 32 + d, bs, B_bs)
                for hh in range(4):
                    P.op("pe", lambda e, hh=hh, t=t, cw=cw, c=c, g=g, B=B, psO=psO: e.matmul(
                        psO[:, hh * 65:(hh + 1) * 65], lhsT=t[:, hh, :], rhs=vwin[:, cw, g, :], start=(c == B - 4), stop=(c == B)),
                        reads=[Bt, B_vwin], writes=[BpO])
            finish_branch(psO, BpO, k, g, 2, False)

            if _LIM < 5.6:
                continue
            if debug:
                P.dma("sp", lambda e, k=k, g=g: e.dma_start(out=L["d_onsa"][k * 128:(k + 1) * 128, g * 256:(g + 1) * 256], in_=o_blk[:, 0:256]),
                      reads=[B_oblk])
            for j in range(2):
                pt, Bp = next_ps("a")
                P.op("pe", lambda e, pt=pt, j=j: e.transpose(out=pt[:, 0:128], in_=o_blk[:, j * 128:(j + 1) * 128],
                                                             identity=ident[:, :]), reads=[B_oblk, B_ident], writes=[Bp])
                P.op("dve", lambda e, pt=pt, j=j, k=k, g=g: e.tensor_copy(out=o_nsaT[:, 2 * g + j, k * 128:(k + 1) * 128], in_=pt[:, 0:128]),
                     reads=[Bp], writes=[B_onsaT])


def hgrn_outputs(nc, P, sc, sbt, next_ps, L):
    xf = L["xf"]
    ident, B_ident = L["ident"], L["B_ident"]
    ones_c, B_ones = L["ones_c"], L["B_ones"]
    o_hgT, B_ohgT = L["o_hgT"], L["B_ohgT"]
    debug = L["debug"]

    def table(name, shape, dt, src, q="sp"):
        t, B = sbt(sc, name, shape, dt)
        P.dma(q, lambda e: e.dma_start(out=t[:], in_=src), writes=[B])
        return t, B
    lm, B_lm = table("h_lm", [128, 128], F32, L["c_lm"])
    u32, B_u32 = table("h_u32", [128, 128], F32, L["t_u32"])
    l32, B_l32 = table("h_l32", [128, 128], F32, L["t_l32"])
    ind4, B_ind4 = table("h_ind4", [128, 4], F32, L["t_ind4"])
    vmask, B_vmask = table("h_vmask", [128, NB + 1], F32, L["t_vmask"])
    eps_c, B_eps = sbt(sc, "eps_c", [128, 1], F32)
    P.op("dve", lambda e: e.memset(eps_c[:], LN_EPS), writes=[B_eps])

    W3, B_W3 = sbt(sc, "W3", [128, KC, 2048], BF16)
    b3, B_b3 = sbt(sc, "b3", [128, 2048], F32)
    oml, B_oml = sbt(sc, "oml3", [128, 512], F32)
    gtmp, B_gtmp = sbt(sc, "gtmp", [128, 2, 512], F32)
    ngb, B_ngb = sbt(sc, "ngb", [128, 512], F32)
    xt = [sbt(sc, "hx%d" % i, [128, KC, 128], BF16) for i in range(2)]
    names32 = ["kk", "lgf", "ebc", "ebi", "edd", "qe", "ke", "hgb", "osb", "og"]
    T = {n: sbt(sc, "h_" + n, [128, 512], F32) for n in names32}
    vv, B_vv = sbt(sc, "h_vv", [128, 512], BF16)
    kd4, B_kd4 = sbt(sc, "h_kd4", [128, 4, 512], BF16)
    kdb, B_kdb = sbt(sc, "h_kdb", [128, 512], BF16)
    qeT4, B_qeT4 = sbt(sc, "h_qeT4", [128, 4, 4, 128], BF16)
    keT, B_keT = sbt(sc, "h_keT", [128, 4, 128], BF16)
    attm, B_attm = sbt(sc, "h_attm", [128, 4, 128], BF16)
    S, B_S = sbt(sc, "h_S", [128, 4, 128], F32)
    Sb, B_Sb = sbt(sc, "h_Sb", [128, 4, 128], BF16)
    Sld, B_Sld = sbt(sc, "h_Sld", [128, 4, 4, 128], BF16)
    edl, B_edl = sbt(sc, "h_edl", [128, 16], F32)
    ss, B_ss = sbt(sc, "h_ss", [128, 4], F32)
    P.op("dve", lambda e: e.memset(qeT4[:], 0.0), writes=[B_qeT4])
    xf_v = xf.rearrange("(kc p) t -> p kc t", p=128)

    for hp in range(2):
        w3_v = L["w_h3"][hp].rearrange("(kc p) c -> p kc c", p=128)
        for q in range(4):
            P.dma("pool", lambda e, q=q, w3_v=w3_v: e.dma_start(out=W3[:, 4 * q:4 * q + 4, :], in_=w3_v[:, 4 * q:4 * q + 4, :]),
                  writes=[B_W3])
        P.dma("sp", lambda e, hp=hp: e.dma_start(out=b3[:], in_=L["b_h3"][hp].partition_broadcast(128)), writes=[B_b3])
        P.dma("sp", lambda e, hp=hp: e.dma_start(out=ngb[:], in_=L["n_h3"][hp].partition_broadcast(128)), writes=[B_ngb])
        for r in range(2):
            P.dma("sp", lambda e, hp=hp, r=r: e.dma_start(out=gtmp[:, r, :], in_=L["g_h3"][hp, r].partition_broadcast(128)),
                  writes=[B_gtmp])
        P.op("dve", lambda e: e.tensor_tensor(out=oml[:], in0=gtmp[:, 1, :], in1=gtmp[:, 0, :], op=ALU.subtract),
             reads=[B_gtmp], writes=[B_oml])
        P.op("act", lambda e: e.activation(out=oml[:], in_=oml[:], func=AF.Sigmoid), reads=[B_oml], writes=[B_oml])
        for j in range(4):
            P.dma("pool", lambda e, hp=hp, j=j: e.dma_start(out=Sld[:, j, :, :], in_=L["st_in"][j, 4 * hp:4 * hp + 4].rearrange("h k v -> k h v")),
                  writes=[B_Sld])
        P.op("dve", lambda e: e.memset(S[:], 0.0), writes=[B_S])
        P.op("dve", lambda e: e.memset(Sb[:], 0.0), writes=[B_Sb])

        for p in range(NB + 1):
            own = p >= OWN0
            sample = p == NB
            xb, Bx = xt[p % 2]
            P.dma("pool", lambda e, xb=xb, p=p: e.dma_start(out=xb[:, :, :], in_=xf_v[:, :, p * 128:(p + 1) * 128]), writes=[Bx])

            def proj(cg, xb=xb, Bx=Bx):
                pt, Bp = next_ps("a")
                for kc in range(KC):
                    P.op("pe", lambda e, pt=pt, kc=kc, cg=cg, xb=xb: e.matmul(
                        pt[:, :], lhsT=xb[:, kc, :], rhs=W3[:, kc, cg * 512:(cg + 1) * 512],
                        start=(kc == 0), stop=(kc == KC - 1)), reads=[Bx, B_W3], writes=[Bp])
                return pt, Bp

            def ew(eng, name, fn, reads, writes_name):
                t, Bt = T[writes_name]
                P.op(eng, fn, reads=reads, writes=[Bt])

            kk, Bkk = T["kk"]
            lgf, Blgf = T["lgf"]
            pt, Bp = proj(1)
            P.op("dve", lambda e, pt=pt: e.tensor_tensor(out=kk[:], in0=pt[:, :], in1=b3[:, 512:1024], op=ALU.add),
                 reads=[Bp, B_b3], writes=[Bkk])
            P.op("act", lambda e: e.activation(out=kk[:], in_=kk[:], func=AF.Sigmoid, scale=-1.0), reads=[Bkk], writes=[Bkk])
            P.op("dve", lambda e: e.tensor_tensor(out=kk[:], in0=kk[:], in1=oml[:], op=ALU.mult), reads=[Bkk, B_oml], writes=[Bkk])
            P.op("act", lambda e: e.activation(out=lgf[:], in_=kk[:], func=AF.Ln, scale=-1.0, bias=ones_c[:, :]),
                 reads=[Bkk, B_ones], writes=[Blgf])
            pt, Bp = proj(2)
            if own:
                P.op("dve", lambda e, pt=pt: e.tensor_tensor(out=vv[:], in0=pt[:, :], in1=b3[:, 1024:1536], op=ALU.add),
                     reads=[Bp, B_b3], writes=[B_vv])
            else:
                osb_, Bosb_ = T["osb"]
                P.op("dve", lambda e, pt=pt: e.tensor_tensor(out=osb_[:], in0=pt[:, :], in1=b3[:, 1024:1536], op=ALU.add),
                     reads=[Bp, B_b3], writes=[Bosb_])
                P.op("dve", lambda e, p=p: e.tensor_scalar(out=vv[:], in0=osb_[:], scalar1=vmask[:, p:p + 1], scalar2=None, op0=ALU.mult),
                     reads=[Bosb_, B_vmask], writes=[B_vv])

            if not own:
                edd, Bedd = T["edd"]
                pt, Bp = next_ps("a")
                P.op("pe", lambda e, pt=pt: e.matmul(pt[:, :], lhsT=lm[:, :], rhs=lgf[:, :], start=True, stop=True),
                     reads=[B_lm, Blgf], writes=[Bp])
                P.op("act", lambda e, pt=pt: e.activation(out=edd[:], in_=pt[:, :], func=AF.Exp), reads=[Bp], writes=[Bedd])
                P.op("dve", lambda e: e.tensor_tensor(out=kdb[:], in0=kk[:], in1=edd[:], op=ALU.mult), reads=[Bkk, Bedd], writes=[B_kdb])
                pt, Bp = next_ps("b")
                for h in range(4):
                    P.op("pe", lambda e, pt=pt, h=h: e.matmul(pt[:, h:h + 1], lhsT=lgf[:, h * 128:(h + 1) * 128], rhs=ones_c[:, :],
                                                              start=True, stop=True), reads=[Blgf, B_ones], writes=[Bp])
                P.op("act", lambda e, pt=pt: e.activation(out=edl[:, 0:4], in_=pt[:, 0:4], func=AF.Exp), reads=[Bp], writes=[B_edl])
                pt, Bp = next_ps("b")
                for h in range(4):
                    P.op("pe", lambda e, pt=pt, h=h: e.matmul(pt[:, h * 128:(h + 1) * 128], lhsT=kdb[:, h * 128:(h + 1) * 128],
                                                              rhs=vv[:, h * 128:(h + 1) * 128], start=True, stop=True),
                         reads=[B_kdb, B_vv], writes=[Bp])
                for h in range(4):
                    P.op("dve", lambda e, pt=pt, h=h: e.scalar_tensor_tensor(
                        out=S[:, h, :], in0=S[:, h, :], scalar=edl[:, h:h + 1], in1=pt[:, h * 128:(h + 1) * 128],
                        op0=ALU.mult, op1=ALU.add), reads=[Bp, B_edl, B_S], writes=[B_S])
                if p == OWN0 - 1:
                    P.op("dve", lambda e: e.tensor_copy(out=Sb[:], in_=S[:]), reads=[B_S], writes=[B_Sb])
                continue

            ebc, Bebc = T["ebc"]
            ebi, Bebi = T["ebi"]
            edd, Bedd = T["edd"]
            qe, Bqe = T["qe"]
            ke, Bke = T["ke"]
            hgb, Bhgb = T["hgb"]
            osb, Bosb = T["osb"]
            og, Bog = T["og"]
            pt, Bp = next_ps("a")
            P.op("pe", lambda e, pt=pt: e.matmul(pt[:, :], lhsT=u32[:, :], rhs=lgf[:, :], start=True, stop=True),
                 reads=[B_u32, Blgf], writes=[Bp])
            P.op("act", lambda e, pt=pt: e.activation(out=ebc[:], in_=pt[:, :], func=AF.Exp), reads=[Bp], writes=[Bebc])
            P.op("act", lambda e, pt=pt: e.activation(out=ebi[:], in_=pt[:, :], func=AF.Exp, scale=-1.0), reads=[Bp], writes=[Bebi])
            pt, Bp = next_ps("a")
            P.op("pe", lambda e, pt=pt: e.matmul(pt[:, :], lhsT=l32[:, :], rhs=lgf[:, :], start=True, stop=True),
                 reads=[B_l32, Blgf], writes=[Bp])
            P.op("act", lambda e, pt=pt: e.activation(out=edd[:], in_=pt[:, :], func=AF.Exp), reads=[Bp], writes=[Bedd])
            pt, Bp = next_ps("b")
            for h in range(4):
                P.op("pe", lambda e, pt=pt, h=h: e.matmul(pt[:, h * 4:h * 4 + 4], lhsT=lgf[:, h * 128:(h + 1) * 128], rhs=ind4[:, :],
                                                          start=True, stop=True), reads=[Blgf, B_ind4], writes=[Bp])
            P.op("act", lambda e, pt=pt: e.activation(out=edl[:, 0:16], in_=pt[:, 0:16], func=AF.Exp), reads=[Bp], writes=[B_edl])
            pt, Bp = proj(0)
            P.op("dve", lambda e, pt=pt: e.tensor_tensor(out=qe[:], in0=pt[:, :], in1=b3[:, 0:512], op=ALU.add),
                 reads=[Bp, B_b3], writes=[Bqe])
            P.op("act", lambda e: e.activation(out=og[:], in_=qe[:], func=AF.Sigmoid), reads=[Bqe], writes=[Bog])
            P.op("dve", lambda e: e.tensor_tensor(out=qe[:], in0=qe[:], in1=og[:], op=ALU.mult), reads=[Bqe, Bog], writes=[Bqe])
            P.op("dve", lambda e: e.tensor_tensor(out=qe[:], in0=qe[:], in1=ebc[:], op=ALU.mult), reads=[Bqe, Bebc], writes=[Bqe])
            P.op("dve", lambda e: e.tensor_tensor(out=ke[:], in0=kk[:], in1=ebi[:], op=ALU.mult), reads=[Bkk, Bebi], writes=[Bke])
            P.op("dve", lambda e: e.tensor_tensor(out=edd[:], in0=kk[:], in1=edd[:], op=ALU.mult), reads=[Bkk, Bedd], writes=[Bedd])
            if not sample:
                for c in range(4):
                    P.op("dve", lambda e, c=c: e.tensor_scalar(out=kd4[:, c, :], in0=edd[:], scalar1=ind4[:, c:c + 1], scalar2=None,
                                                               op0=ALU.mult), reads=[Bedd, B_ind4], writes=[B_kd4])
            pt, Bp = proj(3)
            P.op("dve", lambda e, pt=pt: e.tensor_tensor(out=hgb[:], in0=pt[:, :], in1=b3[:, 1536:2048], op=ALU.add),
                 reads=[Bp, B_b3], writes=[Bhgb])
            P.op("act", lambda e: e.activation(out=og[:], in_=hgb[:], func=AF.Sigmoid), reads=[Bhgb], writes=[Bog])
            P.op("dve", lambda e: e.tensor_tensor(out=hgb[:], in0=hgb[:], in1=og[:], op=ALU.mult), reads=[Bhgb, Bog], writes=[Bhgb])
            P.op("dve", lambda e: e.tensor_tensor(out=hgb[:], in0=hgb[:], in1=ngb[:], op=ALU.mult), reads=[Bhgb, B_ngb], writes=[Bhgb])
            for h in range(4):
                pt, Bp = next_ps("a")
                P.op("pe", lambda e, pt=pt, h=h: e.transpose(out=pt[:, 0:128], in_=qe[:, h * 128:(h + 1) * 128], identity=ident[:, :]),
                     reads=[Bqe, B_ident], writes=[Bp])
                for c in range(4):
                    P.op("act", lambda e, pt=pt, h=h, c=c: e.activation(out=qeT4[:, h, c, 32 * c:32 * c + 32], in_=pt[:, 32 * c:32 * c + 32],
                                                                         func=AF.Identity), reads=[Bp], writes=[B_qeT4])
                pt, Bp = next_ps("a")
                P.op("pe", lambda e, pt=pt, h=h: e.transpose(out=pt[:, 0:128], in_=ke[:, h * 128:(h + 1) * 128], identity=ident[:, :]),
                     reads=[Bke, B_ident], writes=[Bp])
                P.op("dve", lambda e, pt=pt, h=h: e.tensor_copy(out=keT[:, h, :], in_=pt[:, 0:128]), reads=[Bp], writes=[B_keT])
            for h in range(4):
                pt, Bp = next_ps("a")
                for c in range(4):
                    P.op("pe", lambda e, pt=pt, h=h, c=c: e.matmul(pt[:, 0:128], lhsT=keT[:, h, :], rhs=qeT4[:, h, c, :],
                                                                   start=(c == 0), stop=(c == 3)), reads=[B_keT, B_qeT4], writes=[Bp])
                P.op("dve", lambda e, pt=pt, h=h: e.tensor_tensor(out=attm[:, h, :], in0=pt[:, 0:128], in1=u32[:, :], op=ALU.mult),
                     reads=[Bp, B_u32], writes=[B_attm])
            po, Bpo = next_ps("b")
            for h in range(4):
                for c in range(4):
                    if sample:
                        rhs_fn = lambda c=c, h=h: Sld[:, c, h, :]
                        Brhs = B_Sld
                    else:
                        rhs_fn = lambda c=c, h=h: Sb[:, h, :]
                        Brhs = B_Sb
                    P.op("pe", lambda e, po=po, h=h, c=c, rhs_fn=rhs_fn: e.matmul(
                        po[:, h * 128:(h + 1) * 128], lhsT=qeT4[:, h, c, :], rhs=rhs_fn(), start=(c == 0), stop=False),
                        reads=[B_qeT4, Brhs], writes=[Bpo])
                    if not sample:
                        ps2, Bp2 = next_ps("a")
                        P.op("pe", lambda e, ps2=ps2, h=h, c=c: e.matmul(ps2[:, 0:128], lhsT=kd4[:, c, h * 128:(h + 1) * 128],
                                                                         rhs=vv[:, h * 128:(h + 1) * 128], start=True, stop=True),
                             reads=[B_kd4, B_vv], writes=[Bp2])
                        P.op("dve", lambda e, ps2=ps2, h=h, c=c: e.scalar_tensor_tensor(
                            out=S[:, h, :], in0=S[:, h, :], scalar=edl[:, h * 4 + c:h * 4 + c + 1], in1=ps2[:, 0:128],
                            op0=ALU.mult, op1=ALU.add), reads=[Bp2, B_edl, B_S], writes=[B_S])
                        P.op("act", lambda e, h=h: e.activation(out=Sb[:, h, :], in_=S[:, h, :], func=AF.Identity),
                             reads=[B_S], writes=[B_Sb])
                P.op("pe", lambda e, po=po, h=h: e.matmul(po[:, h * 128:(h + 1) * 128], lhsT=attm[:, h, :], rhs=vv[:, h * 128:(h + 1) * 128],
                                                          start=False, stop=True), reads=[B_attm, B_vv], writes=[Bpo])
            P.op("act", lambda e, po=po: e.activation(out=osb[:], in_=po[:, :], func=AF.Identity), reads=[Bpo], writes=[Bosb])
            P.op("dve", lambda e: e.tensor_tensor(out=og[:], in0=osb[:], in1=osb[:], op=ALU.mult), reads=[Bosb], writes=[Bog])
            P.op("dve", lambda e: e.reduce_sum(out=ss[:, 0:4], in_=og[:].rearrange("p (h v) -> p h v", h=4), axis=mybir.AxisListType.X),
                 reads=[Bog], writes=[B_ss])
            P.op("act", lambda e: e.activation(out=ss[:, 0:4], in_=ss[:, 0:4], func=AF.Sqrt, scale=1.0 / 128.0, bias=eps_c[:, :]),
                 reads=[B_ss, B_eps], writes=[B_ss])
            P.op("dve", lambda e: e.reciprocal(out=ss[:, 0:4], in_=ss[:, 0:4]), reads=[B_ss], writes=[B_ss])
            for h in range(4):
                P.op("dve", lambda e, h=h: e.scalar_tensor_tensor(
                    out=og[:, h * 128:(h + 1) * 128], in0=osb[:, h * 128:(h + 1) * 128], scalar=ss[:, h:h + 1],
                    in1=hgb[:, h * 128:(h + 1) * 128], op0=ALU.mult, op1=ALU.mult), reads=[Bosb, B_ss, Bhgb], writes=[Bog])
            if debug:
                k_ = p - OWN0
                P.dma("sp", lambda e, k_=k_, hp=hp: e.dma_start(out=L["d_ohg"][k_ * 128:(k_ + 1) * 128, hp * 512:(hp + 1) * 512], in_=og[:, :]),
                      reads=[Bog])
            for h in range(4):
                pt, Bp = next_ps("a")
                P.op("pe", lambda e, pt=pt, h=h: e.transpose(out=pt[:, 0:128], in_=og[:, h * 128:(h + 1) * 128], identity=ident[:, :]),
                     reads=[Bog, B_ident], writes=[Bp])
                P.op("dve", lambda e, pt=pt, h=h, p=p, hp=hp: e.tensor_copy(
                    out=o_hgT[:, 4 * hp + h, (p - OWN0) * 128:(p - OWN0 + 1) * 128], in_=pt[:, 0:128]), reads=[Bp], writes=[B_ohgT])
        P.barrier()


def tail_moe(nc, P, sc, sbt, next_ps, L):
    xf = L["xf"]
    ident, B_ident = L["ident"], L["B_ident"]
    ones_c, B_ones = L["ones_c"], L["B_ones"]
    o_nsaT, B_onsaT, o_hgT, B_ohgT = L["o_nsaT"], L["B_onsaT"], L["o_hgT"], L["B_ohgT"]
    debug = L["debug"]
    y_out = L["y_out"]
    xf_v = xf.rearrange("(kc p) t -> p kc t", p=128)
    T0 = OWN0 * 128

    zacc, B_zacc = sbt(sc, "zacc", [128, KC, NT], F32)
    hT, B_hT = L["ohT"], L["B_oh"]
    lncol, B_lncol = sbt(sc, "lncol", [128, 4, KC], F32)
    for i, nm in enumerate(("ln1_g", "ln1_b", "ln2_g", "ln2_b")):
        P.dma("sp", lambda e, i=i, nm=nm: e.dma_start(out=lncol[:, i, :], in_=L[nm].rearrange("(kc p) -> p kc", p=128),
                                                      allow_slow_non_contiguous=True), writes=[B_lncol])
    eps_c, B_eps = sbt(sc, "eps_t", [128, 1], F32)
    P.op("dve", lambda e: e.memset(eps_c[:], LN_EPS), writes=[B_eps])
    onesr, B_onesr = sbt(sc, "onesr", [1, 128], F32)
    P.op("dve", lambda e: e.memset(onesr[:], 1.0), writes=[B_onesr])

    def layer_norm(gi, bi, emit_out):
        for (t0, tw) in TT:
            p1, Bp1 = next_ps("b")
            p2, Bp2 = next_ps("b")
            for m in range(KC):
                P.op("pe", lambda e, p1=p1, m=m, t0=t0, tw=tw: e.matmul(p1[0:1, 0:tw], lhsT=ones_c[:, 0:1], rhs=zacc[:, m, t0:t0 + tw],
                                                                        start=(m == 0), stop=(m == KC - 1)),
                     reads=[B_ones, B_zacc], writes=[Bp1])
                P.op("act", lambda e, m=m, t0=t0, tw=tw: e.activation(out=sqs[:, 0:tw], in_=zacc[:, m, t0:t0 + tw], func=AF.Square),
                     reads=[B_zacc], writes=[B_sqs])
                P.op("pe", lambda e, p2=p2, m=m, tw=tw: e.matmul(p2[0:1, 0:tw], lhsT=ones_c[:, 0:1], rhs=sqs[:, 0:tw],
                                                                 start=(m == 0), stop=(m == KC - 1)),
                     reads=[B_ones, B_sqs], writes=[Bp2])
            P.op("dve", lambda e, p1=p1, t0=t0, tw=tw: e.tensor_scalar(out=stat[:, 0, t0:t0 + tw], in0=p1[0:1, 0:tw], scalar1=1.0 / D_MODEL,
                                                                       scalar2=None, op0=ALU.mult), reads=[Bp1], writes=[B_stat])
            P.op("dve", lambda e, p2=p2, t0=t0, tw=tw: e.tensor_scalar(out=stat[:, 1, t0:t0 + tw], in0=p2[0:1, 0:tw], scalar1=1.0 / D_MODEL,
                                                                       scalar2=None, op0=ALU.mult), reads=[Bp2], writes=[B_stat])
            P.op("dve", lambda e, t0=t0, tw=tw: e.tensor_tensor(out=sqs[0:1, 0:tw], in0=stat[:, 0, t0:t0 + tw], in1=stat[:, 0, t0:t0 + tw],
                                                                op=ALU.mult), reads=[B_stat], writes=[B_sqs])
            P.op("dve", lambda e, t0=t0, tw=tw: e.tensor_tensor(out=stat[:, 1, t0:t0 + tw], in0=stat[:, 1, t0:t0 + tw], in1=sqs[0:1, 0:tw],
                                                                op=ALU.subtract), reads=[B_stat, B_sqs], writes=[B_stat])
            P.op("act", lambda e, t0=t0, tw=tw: e.activation(out=stat[:, 1, t0:t0 + tw], in_=stat[:, 1, t0:t0 + tw], func=AF.Sqrt,
                                                             bias=eps_c[0:1, :]), reads=[B_stat, B_eps], writes=[B_stat])
            P.op("dve", lambda e, t0=t0, tw=tw: e.reciprocal(out=stat[:, 1, t0:t0 + tw], in_=stat[:, 1, t0:t0 + tw]),
                 reads=[B_stat], writes=[B_stat])
            for r in range(2):
                pb, Bpb = next_ps("b")
                P.op("pe", lambda e, pb=pb, r=r, t0=t0, tw=tw: e.matmul(pb[:, 0:tw], lhsT=onesr[0:1, :], rhs=stat[:, r, t0:t0 + tw],
                                                                        start=True, stop=True), reads=[B_onesr, B_stat], writes=[Bpb])
                P.op("act", lambda e, pb=pb, r=r, t0=t0, tw=tw: e.activation(out=mbc[:, r, t0:t0 + tw], in_=pb[:, 0:tw], func=AF.Identity),
                     reads=[Bpb], writes=[B_mbc])
        for m in range(KC):
            P.op("dve", lambda e, m=m: e.tensor_tensor(out=zacc[:, m, :], in0=zacc[:, m, :], in1=mbc[:, 0, :], op=ALU.subtract),
                 reads=[B_zacc, B_mbc], writes=[B_zacc])
            P.op("dve", lambda e, m=m: e.tensor_tensor(out=zacc[:, m, :], in0=zacc[:, m, :], in1=mbc[:, 1, :], op=ALU.mult),
                 reads=[B_zacc, B_mbc], writes=[B_zacc])
            P.op("dve", lambda e, m=m: e.tensor_scalar(out=zacc[:, m, :], in0=zacc[:, m, :], scalar1=lncol[:, gi, m:m + 1],
                                                       scalar2=lncol[:, bi, m:m + 1], op0=ALU.mult, op1=ALU.add),
                 reads=[B_zacc, B_lncol], writes=[B_zacc])
            emit_out(m)

    t1 = contextlib.ExitStack()
    with t1:
        mT, B_mT = sbt(t1, "mT", [128, KC, NT], BF16)
        t1a = t1.enter_context(contextlib.ExitStack())
        xt_, B_xt = sbt(t1a, "xt_", [128, KC, 512], BF16)
        wpa = [sbt(t1a, "wpa%d" % i, [128, 8, 128], BF16) for i in range(2)]
        wpb = [sbt(t1a, "wpb%d" % i, [128, 8, 128], BF16) for i in range(2)]
        wmg = [sbt(t1a, "wmg%d" % i, [128, KC, 256], BF16) for i in range(2)]
        bmg, B_bmg = sbt(t1a, "bmg", [128, 32], F32)
        sga, B_sga = sbt(t1a, "sga", [128, 512], F32)
        sgb, B_sgb = sbt(t1a, "sgb", [128, 512], F32)
        P.dma("sp", lambda e: e.dma_start(out=bmg[:], in_=L["b_mg"].rearrange("(cb p) -> p cb", p=128), allow_slow_non_contiguous=True),
              writes=[B_bmg])
        wmg_v = L["w_mg"].rearrange("(kc p) c -> p kc c", p=128)
        wpa_v = L["w_pa"].rearrange("(kc p) c -> p kc c", p=128)
        wpb_v = L["w_pb"].rearrange("(kc p) c -> p kc c", p=128)
        it = 0
        for (t0, tw) in TT:
            for q in range(2):
                P.dma("pool", lambda e, q=q, t0=t0, tw=tw: e.dma_start(out=xt_[:, 8 * q:8 * q + 8, 0:tw],
                                                                       in_=xf_v[:, 8 * q:8 * q + 8, T0 + t0:T0 + t0 + tw]), writes=[B_xt])
            for m in range(KC):
                wm, Bwm = wmg[it % 2]
                wa, Bwa = wpa[it % 2]
                wb, Bwb = wpb[it % 2]
                it += 1
                P.dma("pool", lambda e, wm=wm, m=m: e.dma_start(out=wm[:, :, 0:128], in_=wmg_v[:, :, m * 128:(m + 1) * 128]), writes=[Bwm])
                P.dma("pool", lambda e, wm=wm, m=m: e.dma_start(out=wm[:, :, 128:256], in_=wmg_v[:, :, 2048 + m * 128:2048 + (m + 1) * 128]),
                      writes=[Bwm])
                P.dma("pool", lambda e, wa=wa, m=m: e.dma_start(out=wa[:, :, :], in_=wpa_v[:, :, m * 128:(m + 1) * 128]), writes=[Bwa])
                P.dma("pool", lambda e, wb=wb, m=m: e.dma_start(out=wb[:, :, :], in_=wpb_v[:, :, m * 128:(m + 1) * 128]), writes=[Bwb])
                pA, BpA = next_ps("a")
                pB, BpB = next_ps("a")
                pga, Bpga = next_ps("a")
                pgb, Bpgb = next_ps("a")
                for kc in range(8):
                    P.op("pe", lambda e, pA=pA, kc=kc, wa=wa, t0=t0, tw=tw: e.matmul(
                        pA[:, 0:tw], lhsT=wa[:, kc, :], rhs=o_nsaT[:, kc, t0:t0 + tw], start=(kc == 0), stop=(kc == 7)),
                        reads=[Bwa, B_onsaT], writes=[BpA])
                for kc in range(8):
                    P.op("pe", lambda e, pB=pB, kc=kc, wb=wb, t0=t0, tw=tw: e.matmul(
                        pB[:, 0:tw], lhsT=wb[:, kc, :], rhs=o_hgT[:, kc, t0:t0 + tw], start=(kc == 0), stop=(kc == 7)),
                        reads=[Bwb, B_ohgT], writes=[BpB])
                for kc in range(KC):
                    P.op("pe", lambda e, pga=pga, kc=kc, wm=wm, tw=tw: e.matmul(
                        pga[:, 0:tw], lhsT=wm[:, kc, 0:128], rhs=xt_[:, kc, 0:tw], start=(kc == 0), stop=(kc == KC - 1)),
                        reads=[Bwm, B_xt], writes=[Bpga])
                for kc in range(KC):
                    P.op("pe", lambda e, pgb=pgb, kc=kc, wm=wm, tw=tw: e.matmul(
                        pgb[:, 0:tw], lhsT=wm[:, kc, 128:256], rhs=xt_[:, kc, 0:tw], start=(kc == 0), stop=(kc == KC - 1)),
                        reads=[Bwm, B_xt], writes=[Bpgb])
                P.op("act", lambda e, pga=pga, m=m, tw=tw: e.activation(out=sga[:, 0:tw], in_=pga[:, 0:tw], func=AF.Sigmoid,
                                                                        bias=bmg[:, m:m + 1]), reads=[Bpga, B_bmg], writes=[B_sga])
                P.op("act", lambda e, pgb=pgb, m=m, tw=tw: e.activation(out=sgb[:, 0:tw], in_=pgb[:, 0:tw], func=AF.Sigmoid,
                                                                        bias=bmg[:, 16 + m:17 + m]), reads=[Bpgb, B_bmg], writes=[B_sgb])
                P.op("dve", lambda e, pA=pA, tw=tw: e.tensor_tensor(out=sga[:, 0:tw], in0=sga[:, 0:tw], in1=pA[:, 0:tw], op=ALU.mult),
                     reads=[BpA, B_sga], writes=[B_sga])
                P.op("dve", lambda e, pB=pB, tw=tw: e.tensor_tensor(out=sgb[:, 0:tw], in0=sgb[:, 0:tw], in1=pB[:, 0:tw], op=ALU.mult),
                     reads=[BpB, B_sgb], writes=[B_sgb])
                P.op("dve", lambda e, m=m, t0=t0, tw=tw: e.tensor_tensor(out=mT[:, m, t0:t0 + tw], in0=sga[:, 0:tw], in1=sgb[:, 0:tw], op=ALU.add),
                     reads=[B_sga, B_sgb], writes=[B_mT])
        P.barrier()
        t1a.close()
        wout = [sbt(t1, "wout%d" % i, [128, KC, 128], BF16) for i in range(2)]
        xres = [sbt(t1, "xres%d" % i, [128, NT], F32) for i in range(2)]
        wout_v = L["w_out"].rearrange("(kc p) c -> p kc c", p=128)
        for m in range(KC):
            xr, Bxr = xres[m % 2]
            wo, Bwo = wout[m % 2]
            P.dma("pool", lambda e, wo=wo, m=m: e.dma_start(out=wo[:, :, :], in_=wout_v[:, :, m * 128:(m + 1) * 128]), writes=[Bwo])
            P.dma("sp", lambda e, xr=xr, m=m: e.dma_start(out=xr[:, :], in_=xf[m * 128:(m + 1) * 128, T0:T0 + NT]), writes=[Bxr])
            for (t0, tw) in TT:
                pt, Bp = next_ps("a")
                for kc in range(KC):
                    P.op("pe", lambda e, pt=pt, kc=kc, wo=wo, t0=t0, tw=tw: e.matmul(
                        pt[:, 0:tw], lhsT=wo[:, kc, :], rhs=mT[:, kc, t0:t0 + tw], start=(kc == 0), stop=(kc == KC - 1)),
                        reads=[Bwo, B_mT], writes=[Bp])
                P.op("dve", lambda e, pt=pt, xr=xr, m=m, t0=t0, tw=tw: e.scalar_tensor_tensor(
                    out=zacc[:, m, t0:t0 + tw], in0=xr[:, t0:t0 + tw], scalar=ALPHA, in1=pt[:, 0:tw], op0=ALU.mult, op1=ALU.add),
                    reads=[Bp, Bxr], writes=[B_zacc])
        P.barrier()

    stat, B_stat = sbt(sc, "stat", [1, 2, NT], F32)
    mbc, B_mbc = sbt(sc, "mbc", [128, 2, NT], F32)
    sqs, B_sqs = sbt(sc, "sqs", [128, 512], F32)

    def after_ln1(m):
        P.op("act", lambda e, m=m: e.activation(out=hT[:, m, :], in_=zacc[:, m, :], func=AF.Identity), reads=[B_zacc], writes=[B_hT])
        if debug:
            P.dma("sp", lambda e, m=m: e.dma_start(out=L["d_h"][m * 128:(m + 1) * 128, :], in_=zacc[:, m, :]), reads=[B_zacc])
        P.op("dve", lambda e, m=m: e.tensor_scalar(out=zacc[:, m, :], in0=zacc[:, m, :], scalar1=ALPHA, scalar2=None, op0=ALU.mult),
             reads=[B_zacc], writes=[B_zacc])
    layer_norm(0, 1, after_ln1)
    P.barrier()

    t3 = contextlib.ExitStack()
    with t3:
        wr, B_wr = sbt(t3, "wr", [128, KC, 36], BF16)
        brt, B_brt = sbt(t3, "brt", [128, 36], F32)
        lg, B_lg = sbt(t3, "lg", [128, 36], F32)
        lem, B_lem = sbt(t3, "lem", [128, 32], F32)
        sm, B_sm = sbt(t3, "sm", [128, 16], F32)
        m8, B_m8 = sbt(t3, "m8r", [128, 8], F32)
        gate, B_gate = sbt(t3, "gate", [128, 32], F32)
        gate2, B_gate2 = sbt(t3, "gate2", [128, 32], F32)
        gT, B_gT = sbt(t3, "gT", [32, NT], F32)
        gThi, B_gThi = sbt(t3, "gThi", [32, NT], BF16)
        gTlo, B_gTlo = sbt(t3, "gTlo", [32, NT], BF16)
        selE, B_selE = sbt(t3, "selE", [32, 32 * 128], BF16)
        gbc, B_gbc = sbt(t3, "gbc", [128, NT], F32)
        hid, B_hid = sbt(t3, "hid", [128, 4, NT], BF16)
        sil, B_sil = sbt(t3, "sil", [128, 512], F32)
        ug, B_ug = sbt(t3, "ugm", [128, 512], F32)
        wgu = [sbt(t3, "wgu%d" % i, [128, KC, 2, 128], BF16) for i in range(2)]
        wd = [sbt(t3, "wd%d" % i, [128, 4, D_MODEL], BF16) for i in range(1)]
        P.dma("pool", lambda e: e.dma_start(out=wr[:], in_=L["w_r"].rearrange("(kc p) c -> p kc c", p=128)), writes=[B_wr])
        P.dma("sp", lambda e: e.dma_start(out=brt[:], in_=L["b_r"].partition_broadcast(128)), writes=[B_brt])
        P.dma("sp", lambda e: e.dma_start(out=selE[:], in_=L["t_selE"]), writes=[B_selE])
        for blk in range(NOWN + 1):
            bs_ = slice(blk * 128, (blk + 1) * 128)
            pt, Bp = next_ps("b")
            for kc in range(KC):
                P.op("pe", lambda e, pt=pt, kc=kc, bs_=bs_: e.matmul(pt[:, 0:36], lhsT=hT[:, kc, bs_], rhs=wr[:, kc, :],
                                                                     start=(kc == 0), stop=(kc == KC - 1)), reads=[B_hT, B_wr], writes=[Bp])
            P.op("dve", lambda e, pt=pt: e.tensor_tensor(out=lg[:], in0=pt[:, 0:36], in1=brt[:], op=ALU.add), reads=[Bp, B_brt], writes=[B_lg])
            P.op("dve", lambda e: e.reduce_max(out=sm[:, 0:1], in_=lg[:, 0:4], axis=mybir.AxisListType.X), reads=[B_lg], writes=[B_sm])
            P.op("dve", lambda e: e.tensor_scalar(out=sm[:, 1:2], in0=sm[:, 0:1], scalar1=-1.0, scalar2=None, op0=ALU.mult),
                 reads=[B_sm], writes=[B_sm])
            P.op("act", lambda e: e.activation(out=sm[:, 4:8], in_=lg[:, 0:4], func=AF.Exp, bias=sm[:, 1:2]), reads=[B_lg, B_sm], writes=[B_sm])
            P.op("dve", lambda e: e.reduce_sum(out=sm[:, 2:3], in_=sm[:, 4:8], axis=mybir.AxisListType.X), reads=[B_sm], writes=[B_sm])
            P.op("dve", lambda e: e.reciprocal(out=sm[:, 2:3], in_=sm[:, 2:3]), reads=[B_sm], writes=[B_sm])
            P.op("dve", lambda e: e.tensor_scalar(out=sm[:, 8:12], in0=lg[:, 0:4], scalar1=sm[:, 0:1], scalar2=None, op0=ALU.is_ge),
                 reads=[B_lg, B_sm], writes=[B_sm])
            P.op("dve", lambda e: e.tensor_scalar(out=sm[:, 8:12], in0=sm[:, 8:12], scalar1=1e30, scalar2=-1e30, op0=ALU.mult, op1=ALU.add),
                 reads=[B_sm], writes=[B_sm])
            for g in range(4):
                P.op("dve", lambda e, g=g: e.tensor_scalar(out=lem[:, g * 8:(g + 1) * 8], in0=lg[:, 4 + g * 8:4 + (g + 1) * 8],
                                                           scalar1=sm[:, 8 + g:9 + g], scalar2=None, op0=ALU.add),
                     reads=[B_lg, B_sm], writes=[B_lem])
            P.op("dve", lambda e: e.max(out=m8[:, 0:8], in_=lem[:, :]), reads=[B_lem], writes=[B_m8])
            P.op("dve", lambda e: e.tensor_tensor(out=sm[:, 12:13], in0=m8[:, 0:1], in1=m8[:, 1:2], op=ALU.subtract), reads=[B_m8], writes=[B_sm])
            P.op("act", lambda e: e.activation(out=sm[:, 13:14], in_=sm[:, 12:13], func=AF.Sigmoid), reads=[B_sm], writes=[B_sm])
            P.op("act", lambda e: e.activation(out=sm[:, 14:15], in_=sm[:, 12:13], func=AF.Sigmoid, scale=-1.0), reads=[B_sm], writes=[B_sm])
            P.op("dve", lambda e: e.tensor_scalar(out=sm[:, 13:15], in0=sm[:, 13:15], scalar1=sm[:, 2:3], scalar2=None, op0=ALU.mult),
                 reads=[B_sm], writes=[B_sm])
            P.op("dve", lambda e: e.tensor_scalar(out=gate[:], in0=lem[:], scalar1=m8[:, 0:1], scalar2=sm[:, 13:14], op0=ALU.is_equal, op1=ALU.mult),
                 reads=[B_lem, B_m8, B_sm], writes=[B_gate])
            P.op("dve", lambda e: e.tensor_scalar(out=gate2[:], in0=lem[:], scalar1=m8[:, 1:2], scalar2=sm[:, 14:15], op0=ALU.is_equal, op1=ALU.mult),
                 reads=[B_lem, B_m8, B_sm], writes=[B_gate2])
            P.op("dve", lambda e: e.tensor_tensor(out=gate[:], in0=gate[:], in1=gate2[:], op=ALU.add), reads=[B_gate, B_gate2], writes=[B_gate])
            pt, Bp = next_ps("b")
            P.op("pe", lambda e, pt=pt: e.transpose(out=pt[0:32, 0:128], in_=gate[:, :], identity=ident[:, :]), reads=[B_gate, B_ident], writes=[Bp])
            P.op("dve", lambda e, pt=pt, bs_=bs_: e.tensor_copy(out=gT[:, bs_], in_=pt[0:32, 0:128]), reads=[Bp], writes=[B_gT])
        if debug:
            P.dma("sp", lambda e: e.dma_start(out=L["d_gate"], in_=gT[:, :]), reads=[B_gT])
        P.op("dve", lambda e: e.tensor_copy(out=gThi[:], in_=gT[:]), reads=[B_gT], writes=[B_gThi])
        P.op("dve", lambda e: e.tensor_tensor(out=gT[:], in0=gT[:], in1=gThi[:], op=ALU.subtract), reads=[B_gT, B_gThi], writes=[B_gT])
        P.op("dve", lambda e: e.tensor_copy(out=gTlo[:], in_=gT[:]), reads=[B_gT], writes=[B_gTlo])

        NE = L["n_experts"]
        wg_v = L["w_gate"].rearrange("e (kc p) f -> e p kc f", p=128)
        wu_v = L["w_up"].rearrange("e (kc p) f -> e p kc f", p=128)
        wd_v = L["w_down"].rearrange("e (fc p) d -> e p fc d", p=128)
        for ex in range(NE):
            wdt, Bwd = wd[0]
            for q in range(2):
                P.dma("pool", lambda e, wdt=wdt, ex=ex, q=q: e.dma_start(out=wdt[:, 2 * q:2 * q + 2, :], in_=wd_v[ex, :, 2 * q:2 * q + 2, :]), writes=[Bwd])
            for (t0, tw) in TT:
                pb, Bpb = next_ps("b")
                P.op("pe", lambda e, pb=pb, ex=ex, t0=t0, tw=tw: e.matmul(pb[:, 0:tw], lhsT=selE[:, ex * 128:(ex + 1) * 128], rhs=gThi[:, t0:t0 + tw],
                                                                          start=True, stop=False), reads=[B_selE, B_gThi], writes=[Bpb])
                P.op("pe", lambda e, pb=pb, ex=ex, t0=t0, tw=tw: e.matmul(pb[:, 0:tw], lhsT=selE[:, ex * 128:(ex + 1) * 128], rhs=gTlo[:, t0:t0 + tw],
                                                                          start=False, stop=True), reads=[B_selE, B_gTlo], writes=[Bpb])
                P.op("act", lambda e, pb=pb, t0=t0, tw=tw: e.activation(out=gbc[:, t0:t0 + tw], in_=pb[:, 0:tw], func=AF.Identity),
                     reads=[Bpb], writes=[B_gbc])
            for fc in range(4):
                wt, Bwt = wgu[(ex * 4 + fc) % 2]
                P.dma("pool", lambda e, wt=wt, ex=ex, fc=fc: e.dma_start(out=wt[:, :, 0, :], in_=wg_v[ex, :, :, fc * 128:(fc + 1) * 128]), writes=[Bwt])
                P.dma("pool", lambda e, wt=wt, ex=ex, fc=fc: e.dma_start(out=wt[:, :, 1, :], in_=wu_v[ex, :, :, fc * 128:(fc + 1) * 128]), writes=[Bwt])
                if True:
                    for (t0, tw) in TT:
                        pg, Bpg = next_ps("a")
                        pu, Bpu = next_ps("a")
                        for kc in range(KC):
                            P.op("pe", lambda e, pg=pg, kc=kc, wt=wt, t0=t0, tw=tw: e.matmul(
                                pg[:, 0:tw], lhsT=wt[:, kc, 0, :], rhs=hT[:, kc, t0:t0 + tw],
                                start=(kc == 0), stop=(kc == KC - 1)), reads=[Bwt, B_hT], writes=[Bpg])
                        for kc in range(KC):
                            P.op("pe", lambda e, pu=pu, kc=kc, wt=wt, t0=t0, tw=tw: e.matmul(
                                pu[:, 0:tw], lhsT=wt[:, kc, 1, :], rhs=hT[:, kc, t0:t0 + tw],
                                start=(kc == 0), stop=(kc == KC - 1)), reads=[Bwt, B_hT], writes=[Bpu])
                        P.op("act", lambda e, pg=pg, tw=tw: e.activation(out=sil[:, 0:tw], in_=pg[:, 0:tw], func=AF.Sigmoid),
                             reads=[Bpg], writes=[B_sil])
                        P.op("dve", lambda e, pg=pg, tw=tw: e.tensor_tensor(out=sil[:, 0:tw], in0=sil[:, 0:tw], in1=pg[:, 0:tw], op=ALU.mult),
                             reads=[Bpg, B_sil], writes=[B_sil])
                        P.op("dve", lambda e, pu=pu, t0=t0, tw=tw: e.tensor_tensor(out=ug[:, 0:tw], in0=gbc[:, t0:t0 + tw], in1=pu[:, 0:tw], op=ALU.mult),
                             reads=[Bpu, B_gbc], writes=[B_ug])
                        P.op("pool", lambda e, fc=fc, t0=t0, tw=tw: e.tensor_tensor(out=hid[:, fc, t0:t0 + tw], in0=sil[:, 0:tw], in1=ug[:, 0:tw], op=ALU.mult),
                             reads=[B_sil, B_ug], writes=[B_hid])
            for m in range(KC):
                for (t0, tw) in TT:
                    pt, Bp = next_ps("a")
                    for fc in range(4):
                        P.op("pe", lambda e, pt=pt, fc=fc, wdt=wdt, m=m, t0=t0, tw=tw: e.matmul(
                            pt[:, 0:tw], lhsT=wdt[:, fc, m * 128:(m + 1) * 128], rhs=hid[:, fc, t0:t0 + tw], start=(fc == 0), stop=(fc == 3)),
                            reads=[Bwd, B_hid], writes=[Bp])
                    P.op("dve", lambda e, pt=pt, m=m, t0=t0, tw=tw: e.tensor_tensor(out=zacc[:, m, t0:t0 + tw], in0=zacc[:, m, t0:t0 + tw],
                                                                                   in1=pt[:, 0:tw], op=ALU.add), reads=[Bp, B_zacc], writes=[B_zacc])
        P.barrier()

    def after_ln2(m):
        P.dma("sp", lambda e, m=m: e.dma_start(out=y_out[m * 128:(m + 1) * 128, :], in_=zacc[:, m, :]), reads=[B_zacc])
    layer_norm(2, 3, after_ln2)
    P.barrier()


def nsa_sample(nc, P, sc, sbt, next_ps, L):
    debug = L["debug"]
    xf = L["xf"]
    ident, B_ident, identb, B_identb = L["ident"], L["B_ident"], L["identb"], L["B_identb"]
    onesb, B_onesb = L["onesb"], L["B_onesb"]
    o_nsaT, B_onsaT = L["o_nsaT"], L["B_onsaT"]
    cache2d = L["cache2d"]
    SB0 = NB * 128
    SC0 = NOWN * 128

    def table(name, shape, dt, src, q="sp"):
        t, B = sbt(sc, "T" + name, shape, dt)
        P.dma(q, lambda e: e.dma_start(out=t[:], in_=src), writes=[B])
        return t, B
    cb, B_cb = table("s_cb", [128, 64], F32, L["s_cb"])
    bs, B_bs = table("s_bs", [128, 16 * 65], F32, L["s_bs"])
    sq, B_sq = table("s_sq", [1, 512], F32, L["s_sq"])
    lnt, B_lnt = table("s_lnt", [2, 128], BF16, L["s_lnt"])
    R2, B_R2 = table("s_R2", [2, 512], BF16, L["t_r2"])
    Et, B_Et = table("s_Et", [64, NB * 128], BF16, L["t_E"])
    caus, B_caus = table("s_caus", [128, 128], BF16, L["s_caus"])
    low, B_low = table("s_low", [128, 128], BF16, L["s_low"])
    keep, B_keep = table("s_keep", [128, 128], F32, L["s_keep"])
    force, B_force = table("s_force", [128, 128], F32, L["s_force"])
    iot, B_iot = table("s_iot", [128, 1], F32, L["s_iota"])
    ptb, B_ptb = sbt(sc, "s_ptb", [128, 256], mybir.dt.int32)
    P.dma("sp", lambda e: e.dma_start(out=ptb[:], in_=L["pt_core"].partition_broadcast(128)), writes=[B_ptb])
    ptf, B_ptf = sbt(sc, "s_ptf", [128, 256], F32)
    idx, B_idx = sbt(sc, "s_idx", [128, 256], mybir.dt.int32)
    P.op("dve", lambda e: e.tensor_copy(out=ptf[:], in_=ptb[:]), reads=[B_ptb], writes=[B_ptf])
    P.op("dve", lambda e: e.tensor_scalar(out=ptf[:], in0=ptf[:], scalar1=128.0, scalar2=iot[:, 0:1], op0=ALU.mult, op1=ALU.add),
         reads=[B_ptf, B_iot], writes=[B_ptf])
    P.op("dve", lambda e: e.tensor_scalar(out=ptf[:], in0=ptf[:], scalar1=2.0, scalar2=None, op0=ALU.mult), reads=[B_ptf], writes=[B_ptf])
    P.op("dve", lambda e: e.tensor_copy(out=idx[:], in_=ptf[:]), reads=[B_ptf], writes=[B_idx])
    idx1, B_idx1 = sbt(sc, "s_idx1", [128, 256], mybir.dt.int32)
    P.op("dve", lambda e: e.tensor_scalar(out=ptf[:], in0=ptf[:], scalar1=1.0, scalar2=None, op0=ALU.add), reads=[B_ptf], writes=[B_ptf])
    P.op("dve", lambda e: e.tensor_copy(out=idx1[:], in_=ptf[:]), reads=[B_ptf], writes=[B_idx1])

    qT, B_qT = sbt(sc, "s_qT", [128, 8, 128], BF16)
    gates, B_gates = sbt(sc, "s_gates", [128, 48], F32)
    knew, B_knew = sbt(sc, "s_knew", [128, 2, 2, 128], BF16)
    vnew, B_vnew = sbt(sc, "s_vnew", [128, 2, 4, 65], BF16)
    vnj, B_vnj = sbt(sc, "s_vnj", [4, 4, 2, 4, 65], BF16)
    P.op("dve", lambda e: e.memset(vnew[:, :, :, 64:65], 1.0), writes=[B_vnew])
    pq = contextlib.ExitStack()
    with pq:
        wq, B_wq = sbt(pq, "s_wq", [128, KC, 1024], BF16)
        wng, B_wng = sbt(pq, "s_wng", [128, KC, 48], BF16)
        wk, B_wk = sbt(pq, "s_wk", [128, KC, 1024], BF16)
        bq_col, B_bq = sbt(pq, "s_bq", [128, 8], F32)
        bk_col, B_bk = sbt(pq, "s_bk", [128, 12], F32)
        bk_bc, B_bkbc = sbt(pq, "s_bkbc", [128, KV_W], F32)
        bng, B_bng = sbt(pq, "s_bng", [128, 48], F32)
        xb, Bx = sbt(pq, "s_xb", [128, KC, 128], BF16)
        wq_v = L["w_q"].rearrange("(kc p) c -> p kc c", p=128)
        wkv_v = L["w_kv"].rearrange("(kc p) c -> p kc c", p=128)
        xf_v = xf.rearrange("(kc p) t -> p kc t", p=128)
        for q in range(4):
            P.dma("pool", lambda e, q=q: e.dma_start(out=wq[:, 4 * q:4 * q + 4, :], in_=wq_v[:, 4 * q:4 * q + 4, :]), writes=[B_wq])
            P.dma("pool", lambda e, q=q: e.dma_start(out=wk[:, 4 * q:4 * q + 4, :], in_=wkv_v[:, 4 * q:4 * q + 4, 512:1536]), writes=[B_wk])
        P.dma("pool", lambda e: e.dma_start(out=wng[:], in_=L["w_ng"].rearrange("(kc p) c -> p kc c", p=128)), writes=[B_wng])
        P.dma("pool", lambda e: e.dma_start(out=xb[:], in_=xf_v[:, :, SB0:SB0 + 128]), writes=[Bx])
        P.dma("sp", lambda e: e.dma_start(out=bq_col[:], in_=L["b_q"].rearrange("(cb p) -> p cb", p=128), allow_slow_non_contiguous=True),
              writes=[B_bq])
        P.dma("sp", lambda e: e.dma_start(out=bk_col[:], in_=L["b_kv"].rearrange("(cb p) -> p cb", p=128), allow_slow_non_contiguous=True),
              writes=[B_bk])
        P.dma("sp", lambda e: e.dma_start(out=bk_bc[:], in_=L["b_kv"].partition_broadcast(128)), writes=[B_bkbc])
        P.dma("sp", lambda e: e.dma_start(out=bng[:], in_=L["b_ng"].partition_broadcast(128)), writes=[B_bng])
        for cbk in range(8):
            pt, Bp = next_ps("a")
            for kc in range(KC):
                P.op("pe", lambda e, pt=pt, kc=kc, cbk=cbk: e.matmul(pt[:, 0:128], lhsT=wq[:, kc, cbk * 128:(cbk + 1) * 128], rhs=xb[:, kc, :],
                                                                     start=(kc == 0), stop=(kc == KC - 1)), reads=[B_wq, Bx], writes=[Bp])
            P.op("act", lambda e, pt=pt, cbk=cbk: e.activation(out=qT[:, cbk, :], in_=pt[:, 0:128], func=AF.Identity, bias=bq_col[:, cbk:cbk + 1]),
                 reads=[Bp, B_bq], writes=[B_qT])
        for si, (c0, cbb) in enumerate(((0, 4), (512, 8))):
            for gp in range(2):
                pt, Bp = next_ps("a")
                for kc in range(KC):
                    P.op("pe", lambda e, pt=pt, kc=kc, c0=c0, gp=gp: e.matmul(
                        pt[:, 0:128], lhsT=wk[:, kc, c0 + gp * 128:c0 + (gp + 1) * 128], rhs=xb[:, kc, :],
                        start=(kc == 0), stop=(kc == KC - 1)), reads=[B_wk, Bx], writes=[Bp])
                P.op("act", lambda e, pt=pt, si=si, gp=gp, cbb=cbb: e.activation(
                    out=knew[:, si, gp, :], in_=pt[:, 0:128], func=AF.Identity, bias=bk_col[:, cbb + gp:cbb + gp + 1]),
                    reads=[Bp, B_bk], writes=[B_knew])
        pt, Bp = next_ps("a")
        for si, c0 in enumerate((256, 768)):
            for kc in range(KC):
                P.op("pe", lambda e, pt=pt, kc=kc, si=si, c0=c0: e.matmul(pt[:, si * 256:(si + 1) * 256], lhsT=xb[:, kc, :], rhs=wk[:, kc, c0:c0 + 256],
                                                                         start=(kc == 0), stop=(kc == KC - 1)), reads=[B_wk, Bx], writes=[Bp])
        for si, c0 in enumerate((768, 1280)):
            P.op("dve", lambda e, pt=pt, si=si, c0=c0: e.tensor_tensor(
                out=vnew[:, si, :, 0:64], in0=pt[:, si * 256:(si + 1) * 256].rearrange("p (g d) -> p g d", g=4),
                in1=bk_bc[:, c0:c0 + 256].rearrange("p (g d) -> p g d", g=4), op=ALU.add), reads=[Bp, B_bkbc], writes=[B_vnew])
        for j in range(4):
            P.dma("sp", lambda e, j=j: e.dma_start(out=vnj[:, j, :, :, :], in_=vnew[32 * j:32 * j + 4, :, :, :]), reads=[B_vnew], writes=[B_vnj])
        pt, Bp = next_ps("b")
        for kc in range(KC):
            P.op("pe", lambda e, pt=pt, kc=kc: e.matmul(pt[:, 0:48], lhsT=xb[:, kc, :], rhs=wng[:, kc, :], start=(kc == 0), stop=(kc == KC - 1)),
                 reads=[B_wng, Bx], writes=[Bp])
        P.op("dve", lambda e, pt=pt: e.tensor_tensor(out=gates[:, :], in0=pt[:, 0:48], in1=bng[:, :], op=ALU.add), reads=[Bp, B_bng], writes=[B_gates])
        P.op("act", lambda e: e.activation(out=gates[:], in_=gates[:], func=AF.Sigmoid), reads=[B_gates], writes=[B_gates])
        P.barrier()

    kcs, B_kcs = sbt(sc, "s_kc", [128, 2, 512], BF16)
    vca, B_vca = sbt(sc, "s_vca", [128, 4, 4, 193], BF16)
    P.op("dve", lambda e: e.memset(vca[:, :, :, 64:65], 1.0), writes=[B_vca])
    for c in range(4):
        for g in range(4):
            P.dma("sp", lambda e, c=c, g=g: e.dma_start(out=vca[:, c, g, 65:193], in_=L["s_ovl"][:, c * 128:(c + 1) * 128]), writes=[B_vca])
    o_s, B_os = sbt(sc, "s_os", [128, 4, 256], F32)
    P.op("dve", lambda e: e.memset(o_s[:], 0.0), writes=[B_os])
    pg = [sbt(sc, "s_pg%d" % i, [128, 512], F32) for i in range(3)]
    pef, B_pef = sbt(sc, "s_pef", [128, 2, 16], F32)
    peb, B_peb = sbt(sc, "s_peb", [128, 2, 16], BF16)
    w2k, B_w2k = sbt(sc, "s_w2k", [128, 128], BF16)
    w2v, B_w2v = sbt(sc, "s_w2v", [128, 64], BF16)
    pre0, B_pre0 = sbt(sc, "s_pre0", [128, 2], F32)
    ug, B_ug = sbt(sc, "s_ug", [128, 512], F32)
    tg, B_tg = sbt(sc, "s_tg", [128, 512], F32)
    Gb, B_Gb = sbt(sc, "s_Gb", [128, 512], BF16)
    w_c1, w_c2, c_pe = L["w_c1"], L["w_c2"], L["c_pe"]
    P.dma("sp", lambda e: e.dma_start(out=pef[:], in_=c_pe.rearrange("c (jc j2) d -> (j2 d) c jc", j2=2), allow_slow_non_contiguous=True),
          writes=[B_pef])
    P.op("dve", lambda e: e.tensor_copy(out=peb[:], in_=pef[:]), reads=[B_pef], writes=[B_peb])
    P.op("dve", lambda e: e.memset(w2k[:, 0:64], 0.0), writes=[B_w2k])
    P.dma("pool", lambda e: e.dma_start(out=w2k[:, 64:128], in_=w_c2[0]), writes=[B_w2k])
    P.dma("pool", lambda e: e.dma_start(out=w2v[:], in_=w_c2[1]), writes=[B_w2v])
    pT = [sbt(sc, "s_pT%d" % i, [128, 4, 128], BF16) for i in range(4)]
    pT_rr = [0]

    def next_pT():
        k = pT_rr[0] % len(pT)
        pT_rr[0] += 1
        return pT[k]
    qsq, B_qsq = sbt(sc, "s_qsq", [128, 512], BF16)
    o_blk, B_oblk = sbt(sc, "s_oblk", [128, 256], F32)
    rs, B_rs = sbt(sc, "s_rs", [128, 4], F32)
    wgt, B_wgt = sbt(sc, "s_wgt", [128, 4], F32)
    imp, B_imp = sbt(sc, "s_imp", [128, 128], F32)
    imp3, B_imp3 = sbt(sc, "s_imp3", [128, 128], F32)
    m8, B_m8 = sbt(sc, "s_m8", [128, 16], F32)
    nsel, B_nsel = sbt(sc, "s_nsel", [128, 128], F32)
    nselT = [sbt(sc, "s_nselT%d" % i, [64, 512], BF16) for i in range(2)]
    sqt, B_sqt = sbt(sc, "s_sqt", [128, 512], BF16)
    runmax, B_runmax = sbt(sc, "s_runmax", [1, 512], F32)
    nkm, B_nkm = sbt(sc, "s_nkm", [1, 1], F32)

    def softmax_chunk(pt, Bp, w, hbase, col_fn, tab, B_tab):
        (t, Bt) = next_pT()
        for hh in range(4):
            P.op("act", lambda e, pt=pt, t=t, hh=hh, w=w: e.activation(
                out=t[:w, hh, 0:32], in_=pt[:w, hh * 32:(hh + 1) * 32], func=AF.Exp, scale=SCALE,
                bias=tab[:w, col_fn(hbase + hh):col_fn(hbase + hh) + 1]), reads=[Bp, B_tab], writes=[Bt])
        return t, Bt

    def finish_branch(psO, BpO, g, br, first, gj, B_gj):
        P.op("dve", lambda e: e.tensor_scalar(out=rs[0:32, :], in0=psO[0:32, 0:260].rearrange("p (h c) -> p h c", c=65)[:, :, 64],
                                              scalar1=1e-30, scalar2=None, op0=ALU.max), reads=[BpO], writes=[B_rs])
        P.op("dve", lambda e: e.reciprocal(out=rs[0:32, :], in_=rs[0:32, :]), reads=[B_rs], writes=[B_rs])
        P.op("dve", lambda e: e.tensor_tensor(out=wgt[0:32, :], in0=rs[0:32, :], in1=gj[0:32, br * 16 + g * 4:br * 16 + g * 4 + 4], op=ALU.mult),
             reads=[B_rs, B_gj], writes=[B_wgt])
        for hh in range(4):
            oc = slice(hh * 64, hh * 64 + 64)
            if first:
                P.op("dve", lambda e, hh=hh, oc=oc: e.tensor_scalar(out=o_blk[0:32, oc], in0=psO[0:32, hh * 65:hh * 65 + 64],
                                                                    scalar1=wgt[0:32, hh:hh + 1], scalar2=None, op0=ALU.mult),
                     reads=[BpO, B_wgt], writes=[B_oblk])
            else:
                P.op("dve", lambda e, hh=hh, oc=oc: e.scalar_tensor_tensor(
                    out=o_blk[0:32, oc], in0=psO[0:32, hh * 65:hh * 65 + 64], scalar=wgt[0:32, hh:hh + 1], in1=o_blk[0:32, oc],
                    op0=ALU.mult, op1=ALU.add), reads=[BpO, B_wgt, B_oblk], writes=[B_oblk])

    def gather(j, c, half, dst, Bdst):
        col = j * 64 + c
        ix, Bix = (idx, B_idx) if half == 0 else (idx1, B_idx1)
        P.dma("pool", lambda e, col=col, ix=ix, dst=dst: e.indirect_dma_start(
            out=dst[:, :], out_offset=None, in_=cache2d[:, :],
            in_offset=bass.IndirectOffsetOnAxis(ap=ix[:, col:col + 1], axis=0)), reads=[Bix], writes=[Bdst])

    for j in range(4):
        if _LIM < 6.5 and j not in _JSEL:
            continue
        jb = contextlib.ExitStack()
        with jb:
            pa = jb.enter_context(contextlib.ExitStack())
            kcmp, B_kcmp = sbt(pa, "s_kcmp", [128, 2, NCH * 128], BF16)
            vcmp, B_vcmp = sbt(pa, "s_vcmp", [128, 2, NCH * 128], BF16)
            w1d, B_w1d = sbt(pa, "s_w1d", [128, 2, 32, 128], BF16)
            w1f, B_w1f = sbt(pa, "s_w1f", [128, 2, 16, 128], BF16)
            for half in range(2):
                P.dma("pool", lambda e, half=half, w1d=w1d: e.dma_start(out=w1d[64 * half:64 * half + 64, :, :, :], in_=w_c1.rearrange("c j d h -> d c j h")),
                      writes=[B_w1d])
            P.dma("pool", lambda e, w1f=w1f: e.dma_start(out=w1f[:], in_=w_c1.rearrange("c (jc j2) d h -> (j2 d) c jc h", j2=2)), writes=[B_w1f])
            pt, Bp = next_ps("b")
            for c in range(2):
                for jc in range(16):
                    P.op("pe", lambda e, pt=pt, c=c, jc=jc, w1f=w1f: e.matmul(pt[:, c:c + 1], lhsT=w1f[:, c, jc, :], rhs=peb[:, c, jc:jc + 1],
                                                                              start=(jc == 0), stop=(jc == 15)), reads=[B_w1f, B_peb], writes=[Bp])
            P.op("dve", lambda e, pt=pt: e.tensor_copy(out=pre0[:], in_=pt[:, 0:2]), reads=[Bp], writes=[B_pre0])
            for c in range(NCH):
                pgt, Bpg = pg[c % 3]
                gather(j, c, 0, pgt, Bpg)
                for q in range(4):
                    dst, Bd = (kcmp, B_kcmp) if q < 2 else (vcmp, B_vcmp)
                    pt, Bp = next_ps("a")
                    P.op("pe", lambda e, pt=pt, q=q, pgt=pgt: e.transpose(out=pt[:, 0:128], in_=pgt[:, q * 128:(q + 1) * 128],
                                                                          identity=ident[:, :]), reads=[Bpg, B_ident], writes=[Bp])
                    eng = "act" if q % 2 == 0 else "dve"
                    if eng == "act":
                        P.op("act", lambda e, pt=pt, c=c, q=q, dst=dst: e.activation(out=dst[:, q % 2, c * 128:(c + 1) * 128], in_=pt[:, 0:128],
                                                                                      func=AF.Identity), reads=[Bp], writes=[Bd])
                    else:
                        P.op("dve", lambda e, pt=pt, c=c, q=q, dst=dst: e.tensor_copy(out=dst[:, q % 2, c * 128:(c + 1) * 128], in_=pt[:, 0:128]),
                             reads=[Bp], writes=[Bd])
            for c in range(2):
                src, Bsrc = (kcmp, B_kcmp) if c == 0 else (vcmp, B_vcmp)
                for g in range(4):
                    gp, g2 = g // 2, g % 2
                    hs = slice(64 * g2, 64 * g2 + 64)
                    pt, Bp = next_ps("a")
                    for jj in range(32):
                        P.op("pe", lambda e, pt=pt, c=c, jj=jj, hs=hs, gp=gp, src=src, w1d=w1d: e.matmul(
                            pt[:, 0:511], lhsT=w1d[hs, c, jj, :], rhs=src[hs, gp, jj:jj + 16 * 510 + 1:16],
                            start=(jj == 0), stop=(jj == 31)), reads=[B_w1d, Bsrc], writes=[Bp])
                    P.op("act", lambda e, pt=pt, c=c: e.activation(out=ug[:, 0:511], in_=pt[:, 0:511], func=AF.Identity, bias=pre0[:, c:c + 1]),
                         reads=[Bp, B_pre0], writes=[B_ug])
                    P.op("dve", lambda e: e.tensor_tensor(out=tg[:, 0:511], in0=ug[:, 0:511], in1=ug[:, 0:511], op=ALU.mult), reads=[B_ug], writes=[B_tg])
                    P.op("dve", lambda e: e.tensor_scalar(out=tg[:, 0:511], in0=tg[:, 0:511], scalar1=0.044715, scalar2=1.0, op0=ALU.mult, op1=ALU.add),
                         reads=[B_tg], writes=[B_tg])
                    P.op("dve", lambda e: e.tensor_tensor(out=tg[:, 0:511], in0=tg[:, 0:511], in1=ug[:, 0:511], op=ALU.mult), reads=[B_tg, B_ug], writes=[B_tg])
                    P.op("act", lambda e: e.activation(out=tg[:, 0:511], in_=tg[:, 0:511], func=AF.Sigmoid, scale=1.5957691216057308),
                         reads=[B_tg], writes=[B_tg])
                    P.op("dve", lambda e: e.tensor_tensor(out=Gb[:, 0:511], in0=tg[:, 0:511], in1=ug[:, 0:511], op=ALU.mult), reads=[B_tg, B_ug], writes=[B_Gb])
                    if c == 0:
                        pt2, Bp2 = next_ps("b")
                        if g2 == 0:
                            P.op("pe", lambda e, pt2=pt2: e.matmul(pt2[0:64, 0:511], lhsT=w2k[:, 64:128], rhs=Gb[:, 0:511], start=True, stop=True),
                                 reads=[B_w2k, B_Gb], writes=[Bp2])
                        else:
                            P.op("pe", lambda e, pt2=pt2: e.matmul(pt2[:, 0:511], lhsT=w2k[:, :], rhs=Gb[:, 0:511], start=True, stop=True),
                                 reads=[B_w2k, B_Gb], writes=[Bp2])
                        P.op("dve", lambda e, pt2=pt2, hs=hs, gp=gp: e.tensor_copy(out=kcs[hs, gp, 0:511], in_=pt2[hs, 0:511]), reads=[Bp2], writes=[B_kcs])
                    else:
                        pt2, Bp2 = next_ps("b")
                        for ch in range(4):
                            w = 128 if ch < 3 else 127
                            P.op("pe", lambda e, pt2=pt2, ch=ch, w=w: e.matmul(pt2[:w, ch * 64:(ch + 1) * 64], lhsT=Gb[:, ch * 128:ch * 128 + w],
                                                                               rhs=w2v[:, :], start=True, stop=True), reads=[B_w2v, B_Gb], writes=[Bp2])
                        for ch in range(4):
                            w = 128 if ch < 3 else 127
                            P.op("dve", lambda e, pt2=pt2, ch=ch, w=w, g=g: e.tensor_copy(out=vca[:w, ch, g, 0:64], in_=pt2[:w, ch * 64:(ch + 1) * 64]),
                                 reads=[Bp2], writes=[B_vca])
            P.op("dve", lambda e: e.memset(kcs[:, :, 511:512], 0.0), writes=[B_kcs])
            P.barrier()
            pa.close()

            kslc, B_kslc = sbt(jb, "s_kslc", [128, 2, NCH * 128], BF16)
            vslc, B_vslc = sbt(jb, "s_vslc", [128, NCH, 4, 65], BF16)
            kwin, B_kwin = sbt(jb, "s_kwin", [128, 2, 512], BF16)
            vwin, B_vwin = sbt(jb, "s_vwin", [128, 4, 4, 65], BF16)
            sqj, B_sqj = sbt(jb, "s_sqj", [1, 512], F32)
            kslc_g, B_kslcg = sbt(jb, "s_kslcg", [64, (NCH + 1) * 128], BF16)
            kwin_g, B_kwing = sbt(jb, "s_kwing", [64, 5 * 128], BF16)
            kc_g, B_kcg = sbt(jb, "s_kcg", [64, 512], BF16)
            q_g, B_qg = sbt(jb, "s_qg", [64, 4, 128], BF16)
            gj, B_gj = sbt(jb, "s_gj", [32, 48], F32)
            P.dma("sp", lambda e, j=j, gj=gj: e.dma_start(out=gj[:, :], in_=gates[32 * j:32 * j + 32, :]), reads=[B_gates], writes=[B_gj])
            P.op("dve", lambda e: e.memset(vslc[:, :, :, 64:65], 1.0), writes=[B_vslc])
            P.op("dve", lambda e: e.memset(vwin[:, :, :, 64:65], 1.0), writes=[B_vwin])
            for c in range(NCH):
                pgt, Bpg = pg[c % 3]
                gather(j, c, 1, pgt, Bpg)
                for q in range(2):
                    pt, Bp = next_ps("a")
                    P.op("pe", lambda e, pt=pt, q=q, pgt=pgt: e.transpose(out=pt[:, 0:128], in_=pgt[:, q * 128:(q + 1) * 128],
                                                                          identity=ident[:, :]), reads=[Bpg, B_ident], writes=[Bp])
                    P.op("act", lambda e, pt=pt, c=c, q=q: e.activation(out=kslc[:, q, c * 128:(c + 1) * 128], in_=pt[:, 0:128], func=AF.Identity),
                         reads=[Bp], writes=[B_kslc])
                P.op("dve", lambda e, pgt=pgt, c=c: e.tensor_copy(out=vslc[:, c, :, 0:64], in_=pgt[:, 256:512].rearrange("p (g d) -> p g d", g=4)),
                     reads=[Bpg], writes=[B_vslc])
            for wch in range(4):
                pgt, Bpg = pg[wch % 3]
                P.dma("sp", lambda e, pgt=pgt, j=j, wch=wch: e.dma_start(out=pgt[:, :], in_=L["cw_in"][j, wch * 128:(wch + 1) * 128, :]), writes=[Bpg])
                for q in range(2):
                    pt, Bp = next_ps("a")
                    P.op("pe", lambda e, pt=pt, q=q, pgt=pgt: e.transpose(out=pt[:, 0:128], in_=pgt[:, q * 128:(q + 1) * 128],
                                                                          identity=ident[:, :]), reads=[Bpg, B_ident], writes=[Bp])
                    P.op("act", lambda e, pt=pt, wch=wch, q=q: e.activation(out=kwin[:, q, wch * 128:(wch + 1) * 128], in_=pt[:, 0:128], func=AF.Identity),
                         reads=[Bp], writes=[B_kwin])
                P.op("dve", lambda e, pgt=pgt, wch=wch: e.tensor_copy(out=vwin[:, wch, :, 0:64], in_=pgt[:, 256:512].rearrange("p (g d) -> p g d", g=4)),
                     reads=[Bpg], writes=[B_vwin])

            P.op("dve", lambda e: e.memset(runmax[:], 0.0), writes=[B_runmax])
            srcs = []
            for gp in range(2):
                for s_ in range(NCH * 128 // 512):
                    srcs.append((kslc, B_kslc, lambda gp=gp, s_=s_: kslc[:, gp, s_ * 512:(s_ + 1) * 512], 512))
                srcs.append((kwin, B_kwin, lambda gp=gp: kwin[:, gp, :], 512))
                srcs.append((kcs, B_kcs, lambda gp=gp: kcs[:, gp, :], 512))
                srcs.append((knew, B_knew, lambda gp=gp: knew[:, 0, gp, :], 128))
                srcs.append((knew, B_knew, lambda gp=gp: knew[:, 1, gp, :], 128))
            for (src, Bs, apf, w) in srcs:
                P.op("dve", lambda e, apf=apf, w=w: e.tensor_tensor(out=sqt[:, 0:w], in0=apf(), in1=apf(), op=ALU.mult), reads=[Bs], writes=[B_sqt])
                pt, Bp = next_ps("b")
                P.op("pe", lambda e, pt=pt, w=w: e.matmul(pt[0:1, 0:w], lhsT=onesb[:, 0:1], rhs=sqt[:, 0:w], start=True, stop=True),
                     reads=[B_onesb, B_sqt], writes=[Bp])
                P.op("dve", lambda e, pt=pt, w=w: e.tensor_tensor(out=runmax[:, 0:w], in0=runmax[:, 0:w], in1=pt[0:1, 0:w], op=ALU.max),
                     reads=[Bp, B_runmax], writes=[B_runmax])
            P.op("dve", lambda e: e.reduce_max(out=nkm[:], in_=runmax[:], axis=mybir.AxisListType.X), reads=[B_runmax], writes=[B_nkm])
            P.op("dve", lambda e: e.tensor_scalar(out=nkm[:], in0=nkm[:], scalar1=-0.5, scalar2=None, op0=ALU.mult), reads=[B_nkm], writes=[B_nkm])
            P.op("dve", lambda e: e.tensor_scalar(out=sqj[:, :], in0=sq[:, :], scalar1=nkm[0:1, 0:1], scalar2=None, op0=ALU.add),
                 reads=[B_nkm, B_sq], writes=[B_sqj])

            for g in range(4):
                gp, g2 = g // 2, g % 2
                hs0 = slice(64 * g2, 64 * g2 + 64)
                P.dma("sp", lambda e, hs0=hs0, gp=gp: e.dma_start(out=kslc_g[:, 0:NCH * 128], in_=kslc[hs0, gp, :]), reads=[B_kslc], writes=[B_kslcg])
                P.dma("sp", lambda e, hs0=hs0, gp=gp, j=j: e.dma_start(out=kslc_g[:, NCH * 128:NCH * 128 + 4], in_=knew[hs0, 0, gp, 32 * j:32 * j + 4]),
                      reads=[B_knew], writes=[B_kslcg])
                P.dma("sp", lambda e, hs0=hs0, gp=gp: e.dma_start(out=kwin_g[:, 0:512], in_=kwin[hs0, gp, :]), reads=[B_kwin], writes=[B_kwing])
                P.dma("sp", lambda e, hs0=hs0, gp=gp, j=j: e.dma_start(out=kwin_g[:, 512:516], in_=knew[hs0, 1, gp, 32 * j:32 * j + 4]),
                      reads=[B_knew], writes=[B_kwing])
                P.dma("sp", lambda e, hs0=hs0, gp=gp: e.dma_start(out=kc_g[:, :], in_=kcs[hs0, gp, :]), reads=[B_kcs], writes=[B_kcg])
                P.dma("sp", lambda e, hs0=hs0, gp=gp: e.dma_start(out=q_g[:, :, :], in_=qT[hs0, gp * 4:gp * 4 + 4, :]), reads=[B_qT], writes=[B_qg])
                hs = slice(0, 64)
                qv = q_g[:, :, 32 * j:32 * j + 32]
                P.op("dve", lambda e, qv=qv: e.tensor_tensor(out=qsq[hs, 0:128].rearrange("p (h q) -> p h q", h=4), in0=qv, in1=qv, op=ALU.mult),
                     reads=[B_qg], writes=[B_qsq])
                pt, Bp = next_ps("b")
                P.op("pe", lambda e, pt=pt: e.matmul(pt[0:1, 0:128], lhsT=onesb[hs, 0:1], rhs=qsq[hs, 0:128], start=True, stop=True),
                     reads=[B_onesb, B_qsq], writes=[Bp])
                P.op("dve", lambda e, pt=pt, g=g: e.scalar_tensor_tensor(out=R2[0:1, 0:128], in0=pt[0:1, 0:128], scalar=-0.5,
                                                                         in1=sqj[0:1, g * 128:(g + 1) * 128], op0=ALU.mult, op1=ALU.add),
                     reads=[Bp, B_sqj], writes=[B_R2])
                pts = []
                for c in range(4):
                    w = 128 if c < 3 else 127
                    pt, Bp = next_ps("a")
                    P.op("pe", lambda e, pt=pt, c=c, w=w, qv=qv: e.matmul(pt[:w, 0:128], lhsT=kc_g[hs, c * 128:c * 128 + w], rhs=qv, start=True, stop=False),
                         reads=[B_kcg, B_qg], writes=[Bp])
                    P.op("pe", lambda e, pt=pt, w=w: e.matmul(pt[:w, 0:128], lhsT=lnt[0:1, 0:w], rhs=R2[0:1, 0:128], start=False, stop=True),
                         reads=[B_lnt, B_R2], writes=[Bp])
                    t, Bt = softmax_chunk(pt, Bp, w, 4 * g, lambda h, c=c: h * 4 + c, cb, B_cb)
                    pts.append((t, Bt, w))
                psO, BpO = next_ps("b")
                psI, BpI = next_ps("b")
                for hh in range(4):
                    for c, (t, Bt, w) in enumerate(pts):
                        P.op("pe", lambda e, hh=hh, c=c, t=t, w=w, g=g, psO=psO: e.matmul(
                            psO[0:32, hh * 65:(hh + 1) * 65], lhsT=t[:w, hh, 0:32], rhs=vca[:w, c, g, 0:65], start=(c == 0), stop=(c == 3)),
                            reads=[Bt, B_vca], writes=[BpO])
                for hh in range(4):
                    for c, (t, Bt, w) in enumerate(pts):
                        P.op("pe", lambda e, hh=hh, c=c, t=t, w=w, g=g, psI=psI: e.matmul(
                            psI[0:32, hh * 128:(hh + 1) * 128], lhsT=t[:w, hh, 0:32], rhs=vca[:w, c, g, 65:193], start=(c == 0), stop=(c == 3)),
                            reads=[Bt, B_vca], writes=[BpI])
                finish_branch(psO, BpO, g, 0, True, gj, B_gj)
                for hh in range(4):
                    if hh == 0:
                        P.op("dve", lambda e, psI=psI: e.tensor_scalar(out=imp[0:32, :], in0=psI[0:32, 0:128], scalar1=rs[0:32, 0:1], scalar2=None, op0=ALU.mult),
                             reads=[BpI, B_rs], writes=[B_imp])
                    else:
                        P.op("dve", lambda e, hh=hh, psI=psI: e.scalar_tensor_tensor(
                            out=imp[0:32, :], in0=psI[0:32, hh * 128:(hh + 1) * 128], scalar=rs[0:32, hh:hh + 1], in1=imp[0:32, :], op0=ALU.mult, op1=ALU.add),
                            reads=[BpI, B_rs, B_imp], writes=[B_imp])
                P.op("dve", lambda e: e.tensor_scalar(out=imp[0:32, :], in0=imp[0:32, :], scalar1=1e-30, scalar2=None, op0=ALU.max), reads=[B_imp], writes=[B_imp])
                P.op("dve", lambda e: e.tensor_tensor(out=imp[0:32, :], in0=imp[0:32, :], in1=keep[0:32, :], op=ALU.mult), reads=[B_imp, B_keep], writes=[B_imp])
                P.op("dve", lambda e: e.tensor_tensor(out=imp[0:32, :], in0=imp[0:32, :], in1=force[0:32, :], op=ALU.add), reads=[B_imp, B_force], writes=[B_imp])
                P.op("dve", lambda e: e.max(out=m8[0:32, 0:8], in_=imp[0:32, :]), reads=[B_imp], writes=[B_m8])
                P.op("dve", lambda e: e.tensor_scalar(out=imp3[0:32, :], in0=imp[0:32, :], scalar1=m8[0:32, 7:8], scalar2=None, op0=ALU.is_ge),
                     reads=[B_imp, B_m8], writes=[B_imp3])
                P.op("dve", lambda e: e.scalar_tensor_tensor(out=imp3[0:32, :], in0=imp3[0:32, :], scalar=-3.0e38, in1=imp[0:32, :], op0=ALU.mult, op1=ALU.add),
                     reads=[B_imp, B_imp3], writes=[B_imp3])
                P.op("dve", lambda e: e.max(out=m8[0:32, 8:16], in_=imp3[0:32, :]), reads=[B_imp3], writes=[B_m8])
                P.op("dve", lambda e: e.tensor_scalar(out=nsel[0:32, :], in0=imp[0:32, :], scalar1=m8[0:32, 14:15], scalar2=None, op0=ALU.is_ge),
                     reads=[B_imp, B_m8], writes=[B_nsel])
                P.op("dve", lambda e: e.tensor_scalar(out=nsel[0:32, :], in0=nsel[0:32, :], scalar1=-NEGM, scalar2=NEGM, op0=ALU.mult, op1=ALU.add),
                     reads=[B_nsel], writes=[B_nsel])
                for hf in range(2):
                    nt_, Bnt = nselT[hf]
                    pt, Bp = next_ps("b")
                    P.op("pe", lambda e, pt=pt, hf=hf: e.transpose(out=pt[0:64, 0:32], in_=nsel[0:32, hf * 64:(hf + 1) * 64], identity=ident[0:32, 0:32]),
                         reads=[B_nsel, B_ident], writes=[Bp])
                    for hh in range(4):
                        P.op("act", lambda e, pt=pt, hh=hh, nt_=nt_: e.activation(out=nt_[:, hh * 32:(hh + 1) * 32], in_=pt[0:64, 0:32], func=AF.Identity),
                             reads=[Bp], writes=[Bnt])
                psO, BpO = next_ps("b")
                for c in range(NCH + 1):
                    last = c == NCH
                    w = 4 if last else 128
                    pt, Bp = next_ps("a")
                    P.op("pe", lambda e, pt=pt, c=c, w=w, qv=qv: e.matmul(pt[:w, 0:128], lhsT=kslc_g[hs, c * 128:c * 128 + w], rhs=qv, start=True, stop=False),
                         reads=[B_kslcg, B_qg], writes=[Bp])
                    if not last:
                        nt_, Bnt = nselT[c // 32]
                        cc = c % 32
                        P.op("pe", lambda e, pt=pt, cc=cc, nt_=nt_: e.matmul(pt[:, 0:128], lhsT=Et[:, cc * 128:(cc + 1) * 128], rhs=nt_[:, 0:128], start=False, stop=False),
                             reads=[B_Et, Bnt], writes=[Bp])
                    P.op("pe", lambda e, pt=pt, w=w, last=last: e.matmul(pt[:w, 0:128], lhsT=lnt[0:2, 0:w], rhs=R2[0:2, 0:128], start=False, stop=(not last)),
                         reads=[B_lnt, B_R2], writes=[Bp])
                    if last:
                        P.op("pe", lambda e, pt=pt: e.matmul(pt[:4, 0:128], lhsT=identb[:4, :4], rhs=caus[:4, 0:128], start=False, stop=True),
                             reads=[B_identb, B_caus], writes=[Bp])
                    t, Bt = softmax_chunk(pt, Bp, w, 4 * g, lambda h, d=NCH - c: h * 65 + d, bs, B_bs)
                    for hh in range(4):
                        if last:
                            P.op("pe", lambda e, hh=hh, t=t, g=g, j=j, psO=psO: e.matmul(
                                psO[0:32, hh * 65:(hh + 1) * 65], lhsT=t[:4, hh, 0:32], rhs=vnj[0:4, j, 0, g, :], start=False, stop=True),
                                reads=[Bt, B_vnj], writes=[BpO])
                        else:
                            P.op("pe", lambda e, hh=hh, t=t, c=c, g=g, psO=psO: e.matmul(
                                psO[0:32, hh * 65:(hh + 1) * 65], lhsT=t[:, hh, 0:32], rhs=vslc[:, c, g, :], start=(c == 0), stop=False),
                                reads=[Bt, B_vslc], writes=[BpO])
                finish_branch(psO, BpO, g, 1, False, gj, B_gj)
                psO, BpO = next_ps("b")
                for c in range(5):
                    last = c == 4
                    w = 4 if last else 128
                    pt, Bp = next_ps("a")
                    P.op("pe", lambda e, pt=pt, c=c, w=w, qv=qv: e.matmul(pt[:w, 0:128], lhsT=kwin_g[hs, c * 128:c * 128 + w], rhs=qv, start=True, stop=False),
                         reads=[B_kwing, B_qg], writes=[Bp])
                    edge = last or c == 0
                    P.op("pe", lambda e, pt=pt, w=w, edge=edge: e.matmul(pt[:w, 0:128], lhsT=lnt[0:2, 0:w], rhs=R2[0:2, 0:128], start=False, stop=(not edge)),
                         reads=[B_lnt, B_R2], writes=[Bp])
                    if edge:
                        mk, Bmk = (caus, B_caus) if last else (low, B_low)
                        P.op("pe", lambda e, pt=pt, mk=mk, w=w: e.matmul(pt[:w, 0:128], lhsT=identb[:w, :w], rhs=mk[:w, 0:128], start=False, stop=True),
                             reads=[B_identb, Bmk], writes=[Bp])
                    t, Bt = softmax_chunk(pt, Bp, w, 4 * g, lambda h, d=4 - c: h * 65 + d, bs, B_bs)
                    for hh in range(4):
                        if last:
                            P.op("pe", lambda e, hh=hh, t=t, g=g, j=j, psO=psO: e.matmul(
                                psO[0:32, hh * 65:(hh + 1) * 65], lhsT=t[:4, hh, 0:32], rhs=vnj[0:4, j, 1, g, :], start=False, stop=True),
                                reads=[Bt, B_vnj], writes=[BpO])
                        else:
                            P.op("pe", lambda e, hh=hh, t=t, c=c, g=g, psO=psO: e.matmul(
                                psO[0:32, hh * 65:(hh + 1) * 65], lhsT=t[:, hh, 0:32], rhs=vwin[:, c, g, :], start=(c == 0), stop=False),
                                reads=[Bt, B_vwin], writes=[BpO])
                finish_branch(psO, BpO, g, 2, False, gj, B_gj)
                P.dma("sp", lambda e, j=j, g=g: e.dma_start(out=o_s[32 * j:32 * j + 4, g, :], in_=o_blk[0:4, :]),
                      reads=[B_oblk], writes=[B_os])
            P.barrier()
    for g in range(4):
        if debug:
            P.dma("sp", lambda e, g=g: e.dma_start(out=L["d_onsa_s"][:, g * 256:(g + 1) * 256], in_=o_s[:, g, :]), reads=[B_os])
        for jj in range(2):
            pt, Bp = next_ps("a")
            P.op("pe", lambda e, pt=pt, g=g, jj=jj: e.transpose(out=pt[:, 0:128], in_=o_s[:, g, jj * 128:(jj + 1) * 128], identity=ident[:, :]),
                 reads=[B_os, B_ident], writes=[Bp])
            P.op("dve", lambda e, pt=pt, g=g, jj=jj: e.tensor_copy(out=o_nsaT[:, 2 * g + jj, SC0:SC0 + 128], in_=pt[:, 0:128]),
                 reads=[Bp], writes=[B_onsaT])
    P.barrier()


NEGM = -30000.0
_LIM = 99
_NEXP = 32
_KSEL = [(0, 0)]
_JSEL = [0]
SCALE = 0.125
NB = 32
OWN0 = 24
NOWN = 8
XF_T = (NB + 1) * 128
QOFF, NGOFF, HQOFF, HGOFF, MGOFF = 0, 2560, 2608, 5680, 6704
SLOPES = [2.0 ** (-8.0 * (h + 1) / 16) for h in range(16)]
NT = (NOWN + 1) * 128
TT = [(0, 512), (512, 512), (1024, 128)]
ALPHA = 2.0 ** 0.25
LN_EPS = 1e-5
N_EXPERTS = 32
NCH = 64
N_PHYS = 2560


def build_nc(debug=False):
    nc = bass.Bass("TRN2", target_bir_lowering=False)
    P = Prog()

    def din(name, shape, dt=F32):
        return nc.dram_tensor(name, list(shape), dt, kind="ExternalInput").ap()

    def dout(name, shape, dt=F32):
        return nc.dram_tensor(name, list(shape), dt, kind="ExternalOutput").ap()

    xf = din("xf", [D_MODEL, XF_T])
    xTb = din("xTb", [D_MODEL, SEQ])
    w_kv = din("w_kv", [D_MODEL, KV_W])
    b_kv = din("b_kv", [KV_W])
    w_q = din("w_q", [D_MODEL, 1024])
    b_q = din("b_q", [1024])
    w_ng = din("w_ng", [D_MODEL, 48])
    b_ng = din("b_ng", [48])
    w_st = din("w_st", [D_MODEL, 512])
    b_st = din("b_st", [512])
    g_st = din("g_st", [2, 256])
    w_ss = din("w_ss", [D_MODEL, 2048])
    b_ss = din("b_ss", [2048])
    g_ss = din("g_ss", [2, 1024])
    st_in = din("st_in", [4, 8, 128, 128])
    cw_in = din("cw_in", [4, 512, 512])
    c_lm = din("c_lm", [128, 128])
    c_lm4 = din("c_lm4", [4, 4])
    w_c1 = din("w_c1", [2, 32, 64, 128])
    w_c2 = din("w_c2", [2, 128, 64])
    c_pe = din("c_pe", [2, 32, 64])
    t_cb = din("t_cb", [128, 256])
    t_cmask = din("t_cmask", [128, NOWN * 512], BF16)
    t_bs = din("t_bs", [128, 512])
    t_sq = din("t_sq", [1, 2048])
    t_ln = din("t_ln", [2, NB * 128], BF16)
    t_r2 = din("t_r2", [2, 512], BF16)
    t_E = din("t_E", [64, NB * 128], BF16)
    t_caus = din("t_caus", [128, 512], BF16)
    t_low = din("t_low", [128, 512], BF16)
    t_keep = din("t_keep", [128, NOWN * 64])
    t_force = din("t_force", [128, NOWN * 64])
    t_ovl = din("t_ovl", [128, 2 * 64], BF16)
    w_h3 = din("w_h3", [2, D_MODEL, 2048])
    b_h3 = din("b_h3", [2, 2048])
    n_h3 = din("n_h3", [2, 512])
    g_h3 = din("g_h3", [2, 2, 512])
    t_u32 = din("t_u32", [128, 128])
    t_l32 = din("t_l32", [128, 128])
    t_ind4 = din("t_ind4", [128, 4])
    t_vmask = din("t_vmask", [128, NB + 1])
    w_pa = din("w_pa", [1024, D_MODEL])
    w_pb = din("w_pb", [1024, D_MODEL])
    w_mg = din("w_mg", [D_MODEL, 4096])
    b_mg = din("b_mg", [4096])
    w_out = din("w_out", [D_MODEL, D_MODEL])
    ln1_g = din("ln1_g", [D_MODEL])
    ln1_b = din("ln1_b", [D_MODEL])
    ln2_g = din("ln2_g", [D_MODEL])
    ln2_b = din("ln2_b", [D_MODEL])
    w_r = din("w_r", [D_MODEL, 36])
    b_r = din("b_r", [36])
    t_selE = din("t_selE", [32, 32 * 128], BF16)
    n_experts = _NEXP
    w_gate = din("w_gate", [n_experts, D_MODEL, 512])
    w_up = din("w_up", [n_experts, D_MODEL, 512])
    w_down = din("w_down", [n_experts, 512, D_MODEL])
    cache2d = din("cache2d", [N_PHYS * 128 * 2, 512])
    pt_core = din("pt_core", [256], mybir.dt.int32)
    s_cb = din("s_cb", [128, 64])
    s_bs = din("s_bs", [128, 16 * 65])
    s_sq = din("s_sq", [1, 512])
    s_lnt = din("s_lnt", [2, 128], BF16)
    s_caus = din("s_caus", [128, 128], BF16)
    s_low = din("s_low", [128, 128], BF16)
    s_keep = din("s_keep", [128, 128])
    s_force = din("s_force", [128, 128])
    s_ovl = din("s_ovl", [128, 4 * 128], BF16)
    s_iota = din("s_iota", [128, 1])
    t_id = din("t_id", [128, 128])
    t_idb = din("t_idb", [128, 128], BF16)

    kv_out = dout("kv_out", [(NOWN + 1) * 128, KV_W])
    win_s = dout("win_s", [4, 512, 512])
    st_p = dout("st_p", [2, 128, 128])
    st_s = dout("st_s", [4, 8, 128, 128])
    y_out = dout("y_out", [D_MODEL, NT])
    if debug:
        d_onsa = dout("d_onsa", [NOWN * 128, 1024])
        d_kc = dout("d_kc", [128, 2 * 256])
        d_vc = dout("d_vc", [128, 2 * 4 * 129])
        d_imp = dout("d_imp", [NOWN * 4 * 128, 64])
        d_ohg = dout("d_ohg", [NT, 1024])
        d_onsa_s = dout("d_onsa_s", [128, 1024])
        d_h = dout("d_h", [D_MODEL, NT])
        d_gate = dout("d_gate", [32, NT])

    top = contextlib.ExitStack()
    stopped = False
    if True:
      try:
          _names = {}

          def sbt(stack, name, shape, dt=F32):
              n = _names.get(name, 0)
              _names[name] = n + 1
              if n:
                  name = "%s_r%d" % (name, n)
              return stack.enter_context(nc.sbuf_tensor(name, list(shape), dt)), Buf(name)

          ps = [top.enter_context(nc.psum_tensor("ps%d" % i, [128, 512], F32)) for i in range(8)]
          B_ps = [Buf("ps%d" % i) for i in range(8)]
          pools = {"a": [0, 1, 2, 3, 4], "b": [5, 6, 7]}
          rr = {"a": 0, "b": 0}

          def next_ps(pool="a"):
              lst = pools[pool]
              k = lst[rr[pool] % len(lst)]
              rr[pool] += 1
              return ps[k], B_ps[k]

          ones_c, B_ones = sbt(top, "ones_c", [128, 1], F32)
          onesb, B_onesb = sbt(top, "onesb", [128, 128], BF16)
          ident, B_ident = sbt(top, "ident", [128, 128], F32)
          identb, B_identb = sbt(top, "identb", [128, 128], BF16)
          P.op("dve", lambda e: e.memset(ones_c[:], 1.0), writes=[B_ones])
          P.op("dve", lambda e: e.memset(onesb[:], 1.0), writes=[B_onesb])
          P.dma("sp", lambda e: e.dma_start(out=ident[:], in_=t_id), writes=[B_ident])
          P.dma("sp", lambda e: e.dma_start(out=identb[:], in_=t_idb), writes=[B_identb])
          ohT, B_oh = sbt(top, "ohT", [128, 16, (NOWN + 1) * 128], BF16)
          P.op("pool", lambda e: e.memset(ohT[:], 0.0), writes=[B_oh])
          o_nsaT, B_onsaT = ohT[:, 0:8, :], B_oh
          o_hgT, B_ohgT = ohT[:, 8:16, :], B_oh

          st = contextlib.ExitStack()
          with st:
              stage_states(nc, P, st, sbt, next_ps, ones_c, B_ones,
                           dict(xTb=xTb, xf=xf, w_st=w_st, b_st=b_st, g_st=g_st, w_ss=w_ss, b_ss=b_ss, g_ss=g_ss,
                                st_in=st_in, c_lm=c_lm, c_lm4=c_lm4, st_p=st_p, st_s=st_s))
              P.barrier()

          ns = contextlib.ExitStack()
          with ns:
              kslcT, B_kslcT = sbt(ns, "kslcT", [128, 2, NB * 128], BF16)
              kwinT, B_kwinT = sbt(ns, "kwinT", [128, 2, 12 * 128], BF16)
              vslc, B_vslc = sbt(ns, "vslc", [128, NB, 4, 65], BF16)
              vwin, B_vwin = sbt(ns, "vwin", [128, 12, 4, 65], BF16)
              kcT, B_kcT = sbt(ns, "kcT", [128, 2, 256], BF16)
              vca, B_vca = sbt(ns, "vca", [128, 2, 4, 129], BF16)
              P.op("dve", lambda e: e.memset(vslc[:, :, :, 64:65], 1.0), writes=[B_vslc])
              P.op("dve", lambda e: e.memset(vwin[:, :, :, 64:65], 1.0), writes=[B_vwin])
              P.op("dve", lambda e: e.memset(vca[:, :, :, 64:65], 1.0), writes=[B_vca])
              for c in range(2):
                  for g in range(4):
                      P.dma("sp", lambda e, c=c, g=g: e.dma_start(out=vca[:, c, g, 65:129], in_=t_ovl[:, c * 64:(c + 1) * 64]),
                            writes=[B_vca])

              p1 = contextlib.ExitStack()
              with p1:
                  p1a = p1.enter_context(contextlib.ExitStack())
                  kcmpT, B_kcmpT = sbt(p1, "kcmpT", [128, 2, NB * 128], BF16)
                  vcmpT, B_vcmpT = sbt(p1, "vcmpT", [128, 2, NB * 128], BF16)
                  wkv, B_wkv = sbt(p1a, "wkv", [128, KC, KV_W], BF16)
                  bkv_bc, B_bkvbc = sbt(p1a, "bkv_bc", [128, KV_W], F32)
                  bkv_col, B_bkvcol = sbt(p1a, "bkv_col", [128, 12], F32)
                  xs = [sbt(p1a, "xs%d" % i, [128, KC, 256], BF16) for i in range(2)]
                  osb = [sbt(p1a, "osb%d" % i, [128, KV_W], F32) for i in range(2)]
                  wkv_v = w_kv.rearrange("(kc p) c -> p kc c", p=128)
                  for q in range(4):
                      P.dma("pool", lambda e, q=q: e.dma_start(out=wkv[:, 4 * q:4 * q + 4, :], in_=wkv_v[:, 4 * q:4 * q + 4, :]),
                            writes=[B_wkv])
                  P.dma("sp", lambda e: e.dma_start(out=bkv_bc[:], in_=b_kv.partition_broadcast(128)), writes=[B_bkvbc])
                  with nc.allow_non_contiguous_dma(reason="tiny bias column layout"):
                      P.dma("sp", lambda e: e.dma_start(out=bkv_col[:], in_=b_kv.rearrange("(cb p) -> p cb", p=128), allow_slow_non_contiguous=True),
                            writes=[B_bkvcol])
                  xf_v = xf.rearrange("(kc p) t -> p kc t", p=128)
                  nti = 0
                  for tt in range((NB + 1) * 128 // 256 + 1):
                      t0 = tt * 256
                      if t0 >= XF_T:
                          break
                      tw = min(256, XF_T - t0)
                      (xb, Bx) = xs[tt % 2]
                      for q in range(2):
                          P.dma("pool", lambda e, xb=xb, t0=t0, tw=tw, q=q: e.dma_start(
                              out=xb[:, 8 * q:8 * q + 8, 0:tw], in_=xf_v[:, 8 * q:8 * q + 8, t0:t0 + tw]), writes=[Bx])
                      is_prompt = t0 < NB * 128
                      if is_prompt:
                          for slot, dst, Bd, lo in ((0, kcmpT, B_kcmpT, 0), (1, vcmpT, B_vcmpT, 0), (2, kslcT, B_kslcT, 0),
                                                    (4, kwinT, B_kwinT, 20 * 128)):
                              if t0 < lo:
                                  continue
                              for gp in range(2):
                                  cb = slot * 2 + gp
                                  pt, Bp = next_ps("a")
                                  for kc in range(KC):
                                      P.op("pe", lambda e, pt=pt, kc=kc, cb=cb, xb=xb, tw=tw: e.matmul(
                                          pt[:, 0:tw], lhsT=wkv[:, kc, cb * 128:(cb + 1) * 128], rhs=xb[:, kc, 0:tw],
                                          start=(kc == 0), stop=(kc == KC - 1)), reads=[B_wkv, Bx], writes=[Bp])
                                  P.op("act", lambda e, pt=pt, dst=dst, gp=gp, cb=cb, t0=t0, tw=tw, lo=lo: e.activation(
                                      out=dst[:, gp, t0 - lo:t0 - lo + tw], in_=pt[:, 0:tw], func=AF.Identity,
                                      bias=bkv_col[:, cb:cb + 1]), reads=[Bp, B_bkvcol], writes=[Bd])
                      for bi in range(tw // 128):
                          p = (t0 + bi * 128) // 128
                          tl = slice(bi * 128, (bi + 1) * 128)
                          if p < NB:
                              pt, Bp = next_ps("a")
                              for half, c0 in ((0, 768), (1, 1280)):
                                  for kc in range(KC):
                                      P.op("pe", lambda e, pt=pt, kc=kc, xb=xb, tl=tl, half=half, c0=c0: e.matmul(
                                          pt[:, half * 256:(half + 1) * 256], lhsT=xb[:, kc, tl], rhs=wkv[:, kc, c0:c0 + 256],
                                          start=(kc == 0), stop=(kc == KC - 1)), reads=[B_wkv, Bx], writes=[Bp])
                              P.op("dve", lambda e, pt=pt, p=p: e.tensor_tensor(
                                  out=vslc[:, p, :, 0:64], in0=pt[:, 0:256].rearrange("p (g d) -> p g d", g=4),
                                  in1=bkv_bc[:, 768:1024].rearrange("p (g d) -> p g d", g=4), op=ALU.add),
                                  reads=[Bp, B_bkvbc], writes=[B_vslc])
                              if p >= 20:
                                  P.op("dve", lambda e, pt=pt, p=p: e.tensor_tensor(
                                      out=vwin[:, p - 20, :, 0:64], in0=pt[:, 256:512].rearrange("p (g d) -> p g d", g=4),
                                      in1=bkv_bc[:, 1280:1536].rearrange("p (g d) -> p g d", g=4), op=ALU.add),
                                      reads=[Bp, B_bkvbc], writes=[B_vwin])
                          if p >= OWN0:
                              (o, Bo) = osb[nti % 2]
                              nti += 1
                              for cg in range(3):
                                  pt, Bp = next_ps("a")
                                  for kc in range(KC):
                                      P.op("pe", lambda e, pt=pt, kc=kc, xb=xb, tl=tl, cg=cg: e.matmul(
                                          pt[:, :], lhsT=xb[:, kc, tl], rhs=wkv[:, kc, cg * 512:(cg + 1) * 512],
                                          start=(kc == 0), stop=(kc == KC - 1)), reads=[B_wkv, Bx], writes=[Bp])
                                  P.op("dve", lambda e, pt=pt, o=o, cg=cg: e.tensor_tensor(
                                      out=o[:, cg * 512:(cg + 1) * 512], in0=pt[:, :], in1=bkv_bc[:, cg * 512:(cg + 1) * 512],
                                      op=ALU.add), reads=[Bp, B_bkvbc], writes=[Bo])
                              r0 = (p - OWN0) * 128
                              P.dma("sp", lambda e, o=o, r0=r0: e.dma_start(out=kv_out[r0:r0 + 128, :], in_=o[:, :]), reads=[Bo])
                              if p == NB:
                                  for j in range(4):
                                      P.dma("sp", lambda e, o=o, j=j: e.dma_start(
                                          out=win_s[j, 508:512, :], in_=o[32 * j:32 * j + 4, 1024:1536]), reads=[Bo])
                  for j in range(4):
                      P.dma("sp", lambda e, j=j: e.dma_start(out=win_s[j, 0:508, :], in_=cw_in[j, 4:512, :]))

                  P.barrier()
                  p1a.close()
                  if _LIM < 3:
                      raise _Stop()
                  w1d, B_w1d = sbt(p1, "w1d", [128, 2, 32, 128], BF16)
                  w1f, B_w1f = sbt(p1, "w1f", [128, 2, 16, 128], BF16)
                  pef, B_pef = sbt(p1, "pef", [128, 2, 16], F32)
                  peb, B_peb = sbt(p1, "peb", [128, 2, 16], BF16)
                  w2k, B_w2k = sbt(p1, "w2k", [128, 128], BF16)
                  w2v, B_w2v = sbt(p1, "w2v", [128, 64], BF16)
                  pre0, B_pre0 = sbt(p1, "pre0", [128, 2], F32)
                  ug, B_ug = sbt(p1, "ug", [128, 256], F32)
                  tg, B_tg = sbt(p1, "tg", [128, 256], F32)
                  Gb, B_Gb = sbt(p1, "Gb", [128, 256], BF16)
                  for half in range(2):
                      P.dma("pool", lambda e, half=half: e.dma_start(
                          out=w1d[64 * half:64 * half + 64, :, :, :], in_=w_c1.rearrange("c j d h -> d c j h")), writes=[B_w1d])
                  P.dma("pool", lambda e: e.dma_start(out=w1f[:], in_=w_c1.rearrange("c (jc j2) d h -> (j2 d) c jc h", j2=2)),
                        writes=[B_w1f])
                  with nc.allow_non_contiguous_dma(reason="tiny pe layout"):
                      P.dma("sp", lambda e: e.dma_start(out=pef[:], in_=c_pe.rearrange("c (jc j2) d -> (j2 d) c jc", j2=2), allow_slow_non_contiguous=True),
                            writes=[B_pef])
                  P.op("dve", lambda e: e.tensor_copy(out=peb[:], in_=pef[:]), reads=[B_pef], writes=[B_peb])
                  P.op("dve", lambda e: e.memset(w2k[:, 0:64], 0.0), writes=[B_w2k])
                  P.dma("pool", lambda e: e.dma_start(out=w2k[:, 64:128], in_=w_c2[0]), writes=[B_w2k])
                  P.dma("pool", lambda e: e.dma_start(out=w2v[:], in_=w_c2[1]), writes=[B_w2v])
                  pt, Bp = next_ps("b")
                  for c in range(2):
                      for jc in range(16):
                          P.op("pe", lambda e, pt=pt, c=c, jc=jc: e.matmul(
                              pt[:, c:c + 1], lhsT=w1f[:, c, jc, :], rhs=peb[:, c, jc:jc + 1], start=(jc == 0), stop=(jc == 15)),
                              reads=[B_w1f, B_peb], writes=[Bp])
                  P.op("dve", lambda e, pt=pt: e.tensor_copy(out=pre0[:], in_=pt[:, 0:2]), reads=[Bp], writes=[B_pre0])
                  for c in range(2):
                      src, Bsrc = (kcmpT, B_kcmpT) if c == 0 else (vcmpT, B_vcmpT)
                      for g in range(4):
                          gp, g2 = g // 2, g % 2
                          hs = slice(64 * g2, 64 * g2 + 64)
                          pt, Bp = next_ps("a")
                          for j in range(32):
                              P.op("pe", lambda e, pt=pt, c=c, j=j, hs=hs, gp=gp, src=src: e.matmul(
                                  pt[:, 0:255], lhsT=w1d[hs, c, j, :], rhs=src[hs, gp, j:j + 16 * 254 + 1:16],
                                  start=(j == 0), stop=(j == 31)), reads=[B_w1d, Bsrc], writes=[Bp])
                          P.op("act", lambda e, pt=pt, c=c: e.activation(out=ug[:, 0:255], in_=pt[:, 0:255], func=AF.Identity,
                                                                         bias=pre0[:, c:c + 1]), reads=[Bp, B_pre0], writes=[B_ug])
                          P.op("dve", lambda e: e.tensor_tensor(out=tg[:, 0:255], in0=ug[:, 0:255], in1=ug[:, 0:255], op=ALU.mult),
                               reads=[B_ug], writes=[B_tg])
                          P.op("dve", lambda e: e.tensor_scalar(out=tg[:, 0:255], in0=tg[:, 0:255], scalar1=0.044715, scalar2=1.0,
                                                                op0=ALU.mult, op1=ALU.add), reads=[B_tg], writes=[B_tg])
                          P.op("dve", lambda e: e.tensor_tensor(out=tg[:, 0:255], in0=tg[:, 0:255], in1=ug[:, 0:255], op=ALU.mult),
                               reads=[B_tg, B_ug], writes=[B_tg])
                          P.op("act", lambda e: e.activation(out=tg[:, 0:255], in_=tg[:, 0:255], func=AF.Sigmoid,
                                                             scale=1.5957691216057308), reads=[B_tg], writes=[B_tg])
                          P.op("dve", lambda e: e.tensor_tensor(out=Gb[:, 0:255], in0=tg[:, 0:255], in1=ug[:, 0:255], op=ALU.mult),
                               reads=[B_tg, B_ug], writes=[B_Gb])
                          if c == 0:
                              pt2, Bp2 = next_ps("b")
                              if g2 == 0:
                                  P.op("pe", lambda e, pt2=pt2: e.matmul(pt2[0:64, 0:255], lhsT=w2k[:, 64:128], rhs=Gb[:, 0:255],
                                                                         start=True, stop=True), reads=[B_w2k, B_Gb], writes=[Bp2])
                              else:
                                  P.op("pe", lambda e, pt2=pt2: e.matmul(pt2[:, 0:255], lhsT=w2k[:, :], rhs=Gb[:, 0:255],
                                                                         start=True, stop=True), reads=[B_w2k, B_Gb], writes=[Bp2])
                              P.op("dve", lambda e, pt2=pt2, hs=hs, gp=gp: e.tensor_copy(out=kcT[hs, gp, 0:255], in_=pt2[hs, 0:255]),
                                   reads=[Bp2], writes=[B_kcT])
                          else:
                              pt2, Bp2 = next_ps("b")
                              for ch in range(2):
                                  w = 128 if ch == 0 else 127
                                  P.op("pe", lambda e, pt2=pt2, ch=ch, w=w: e.matmul(
                                      pt2[:w, ch * 64:(ch + 1) * 64], lhsT=Gb[:, ch * 128:ch * 128 + w], rhs=w2v[:, :],
                                      start=True, stop=True), reads=[B_w2v, B_Gb], writes=[Bp2])
                              for ch in range(2):
                                  w = 128 if ch == 0 else 127
                                  P.op("dve", lambda e, pt2=pt2, ch=ch, w=w, g=g: e.tensor_copy(
                                      out=vca[:w, ch, g, 0:64], in_=pt2[:w, ch * 64:(ch + 1) * 64]), reads=[Bp2], writes=[B_vca])
                  P.op("dve", lambda e: e.memset(kcT[:, :, 255:256], 0.0), writes=[B_kcT])
                  if debug:
                      dk, B_dk = sbt(p1, "dk", [128, 512], F32)
                      P.op("dve", lambda e: e.tensor_copy(out=dk[:], in_=kcT[:].rearrange("p a b -> p (a b)")), reads=[B_kcT], writes=[B_dk])
                      P.dma("sp", lambda e: e.dma_start(out=d_kc, in_=dk[:]), reads=[B_dk])
                      dv, B_dv = sbt(p1, "dv", [128, 2 * 4 * 129], F32)
                      P.op("dve", lambda e: e.tensor_copy(out=dv[:], in_=vca[:].rearrange("p a b c -> p (a b c)")), reads=[B_vca], writes=[B_dv])
                      P.dma("sp", lambda e: e.dma_start(out=d_vc, in_=dv[:]), reads=[B_dv])
                  P.barrier()

              p4 = contextlib.ExitStack()
              with p4:
                  if _LIM < 4:
                      raise _Stop()
                  nsa_prompt(nc, P, p4, sbt, next_ps, locals())
                  P.barrier()

          if _LIM < 6.2:
              raise _Stop()
          sm = contextlib.ExitStack()
          with sm:
              nsa_sample(nc, P, sm, sbt, next_ps, locals())
          if _LIM < 7:
              raise _Stop()
          hg = contextlib.ExitStack()
          with hg:
              hgrn_outputs(nc, P, hg, sbt, next_ps, locals())
          if _LIM < 8:
              raise _Stop()
          tl = contextlib.ExitStack()
          with tl:
              tail_moe(nc, P, tl, sbt, next_ps, locals())

      except _Stop:
        stopped = True
      if True:
          P.finish("sp")
          P.emit(nc)
      if not stopped:
          top.close()
    return nc


def _bf16(a):
    import ml_dtypes
    return np.ascontiguousarray(np.asarray(a, np.float32).astype(ml_dtypes.bfloat16))


def sample_tables():
    T = {}
    nl = np.arange(128)
    cb = np.zeros((128, 64), np.float32)
    for h in range(16):
        for c in range(4):
            n = 128 * c + nl
            v = SLOPES[h] * (16.0 * n + 31 - 8192)
            cb[:, h * 4 + c] = np.where(n >= 511, NEGM, v)
    T["s_cb"] = cb
    bsx = np.zeros((128, 16 * 65), np.float32)
    for h in range(16):
        for d in range(65):
            bsx[:, h * 65 + d] = SLOPES[h] * (nl - 128.0 * d)
    T["s_bs"] = bsx
    sv = np.arange(32)
    sq = np.zeros((16, 32), np.float32)
    for h in range(16):
        sq[h] = -SLOPES[h] * sv / SCALE
    T["s_sq"] = sq.reshape(1, 512)
    ln = np.zeros((2, 128), np.float32)
    ln[0] = 1.0
    T["s_lnt"] = _bf16(ln)
    T["s_caus"] = _bf16(np.tile(np.where(nl[:, None] > sv[None, :], NEGM, 0.0), (1, 4)))
    T["s_low"] = _bf16(np.tile(np.where(nl[:, None] <= sv[None, :], NEGM, 0.0), (1, 4)))
    keep = np.ones((128, 128), np.float32)
    force = np.zeros((128, 128), np.float32)
    keep[:, 0] = 0.0
    force[:, 0] = 1e30
    T["s_keep"], T["s_force"] = keep, force
    j = np.arange(128)[None, :]
    ov = np.zeros((128, 4, 128), np.float32)
    for c in range(4):
        n = (128 * c + nl)[:, None]
        ov[:, c, :] = ((n >= 4 * j - 1) & (n <= 4 * j + 3)).astype(np.float32)
    T["s_ovl"] = _bf16(ov.reshape(128, 512))
    T["s_iota"] = nl.astype(np.float32).reshape(128, 1)
    return T


def host_tables(nnull):
    T = {}
    nl = np.arange(128)
    t_cb = np.zeros((128, 256), np.float32)
    for h in range(16):
        for k in range(NOWN):
            B = OWN0 + k
            for c in range(2):
                n = 128 * c + nl
                v = SLOPES[h] * (16.0 * n + 31 - 128 * B)
                v = np.where((n < 8 * nnull) | (n >= 255), NEGM, v)
                t_cb[:, (h * NOWN + k) * 2 + c] = v
    T["t_cb"] = t_cb
    cm = np.zeros((128, NOWN, 4, 128), np.float32)
    ql = np.arange(128)
    for k in range(NOWN):
        B = OWN0 + k
        inval = (16 * (128 + nl)[:, None] + 31) > (128 * B + ql[None, :])
        cm[:, k, :, :] = np.where(inval, NEGM, 0.0)[:, None, :]
    T["t_cmask"] = _bf16(cm.reshape(128, NOWN * 512))
    t_bs = np.zeros((128, 512), np.float32)
    for h in range(16):
        for d in range(32):
            t_bs[:, h * 32 + d] = SLOPES[h] * (nl - 128.0 * d)
    T["t_bs"] = t_bs
    sq = np.zeros((16, 128), np.float32)
    for h in range(16):
        sq[h] = -SLOPES[h] * ql / SCALE
    T["t_sq"] = sq.reshape(1, 2048)
    ln = np.zeros((2, NB, 128), np.float32)
    ln[0] = 1.0
    ln[1, :nnull] = 1.0
    T["t_ln"] = _bf16(ln.reshape(2, NB * 128))
    r2 = np.zeros((2, 512), np.float32)
    r2[1] = NEGM
    T["t_r2"] = _bf16(r2)
    E = np.zeros((64, NB, 128), np.float32)
    for c in range(NB):
        E[2 * c, c, :64] = 1.0
        E[2 * c + 1, c, 64:] = 1.0
    T["t_E"] = _bf16(E.reshape(64, NB * 128))
    tq = nl[:, None] > ql[None, :]
    T["t_caus"] = _bf16(np.tile(np.where(tq, NEGM, 0.0), (1, 4)))
    T["t_low"] = _bf16(np.tile(np.where(~tq, NEGM, 0.0), (1, 4)))
    keep = np.zeros((128, NOWN, 64), np.float32)
    force = np.zeros((128, NOWN, 64), np.float32)
    j = np.arange(64)[None, :]
    j0 = 2 * nnull
    for k in range(NOWN):
        B = OWN0 + k
        cur = (2 * B + (ql >= 64))[:, None]
        neg = (j > cur) | (j < j0)
        big = ((j == cur) | (j == j0)) & ~neg
        keep[:, k, :] = np.where(neg | big, 0.0, 1.0)
        force[:, k, :] = np.where(neg, -1e30, np.where(big, 1e30, 0.0))
    T["t_keep"] = keep.reshape(128, NOWN * 64)
    T["t_force"] = force.reshape(128, NOWN * 64)
    ov = np.zeros((128, 2, 64), np.float32)
    for c in range(2):
        n = (128 * c + nl)[:, None]
        ov[:, c, :] = ((n >= 4 * j - 1) & (n <= 4 * j + 3)).astype(np.float32)
    T["t_ovl"] = _bf16(ov.reshape(128, 128))
    a = np.arange(128)
    same = (a[:, None] // 32) == (a[None, :] // 32)
    T["t_u32"] = (same & (a[:, None] <= a[None, :])).astype(np.float32)
    T["t_l32"] = (same & (a[:, None] > a[None, :])).astype(np.float32)
    T["t_ind4"] = ((a[:, None] // 32) == np.arange(4)[None, :]).astype(np.float32)
    vm = np.ones((128, NB + 1), np.float32)
    vm[:, :nnull] = 0.0
    T["t_vmask"] = vm
    se = np.zeros((32, 32, 128), np.float32)
    for e_ in range(32):
        se[e_, e_, :] = 1.0
    T["t_selE"] = _bf16(se.reshape(32, 32 * 128))
    T["t_id"] = np.eye(128, dtype=np.float32)
    T["t_idb"] = _bf16(np.eye(128))
    return T


_NC_CACHE = {}
_TAB_CACHE = {}


def _run(inputs, debug=False):
    f = lambda a: np.ascontiguousarray(np.asarray(a, dtype=np.float32))
    x_prompt, x_sample = f(inputs["x_prompt"]), f(inputs["x_sample"])
    w_in, b_in = f(inputs["w_in"])[0], f(inputs["b_in"])[0]
    hgrn_gamma, state_hgrn, cache_win = f(inputs["hgrn_gamma"]), f(inputs["state_hgrn"])[0], f(inputs["cache_win"])[0]
    key = "nc_dbg" if debug else "nc"
    if key not in _NC_CACHE:
        _NC_CACHE[key] = build_nc(debug)
    nc = _NC_CACHE[key]

    lmask = np.tril(np.ones((128, 128), np.float32), -1)
    w_kv = np.ascontiguousarray(w_in[:, KV_OFF:KV_OFF + KV_W])
    b_kv = np.ascontiguousarray(b_in[KV_OFF:KV_OFF + KV_W])
    perm = np.zeros(1024, np.int64)
    for gp in range(2):
        for hh in range(4):
            for g2 in range(2):
                dst = ((gp * 4 + hh) * 2 + g2) * 64
                src = (4 * (2 * gp + g2) + hh) * 64
                perm[dst:dst + 64] = np.arange(src, src + 64)
    w_q = np.ascontiguousarray(w_in[:, QOFF:QOFF + 1024][:, perm])
    b_q = np.ascontiguousarray(b_in[QOFF:QOFF + 1024][perm])
    w_ng = np.ascontiguousarray(w_in[:, NGOFF:NGOFF + 48])
    b_ng = np.ascontiguousarray(b_in[NGOFF:NGOFF + 48])
    w_ss = np.ascontiguousarray(np.concatenate([w_in[:, HF_OFF:HF_OFF + 1024], w_in[:, HI_OFF:HI_OFF + 1024]], axis=1))
    b_ss = np.ascontiguousarray(np.concatenate([b_in[HF_OFF:HF_OFF + 1024], b_in[HI_OFF:HI_OFF + 1024]]))
    xTb_all = [np.ascontiguousarray(x_prompt[b].T) for b in range(2)]
    hn = f(inputs["hgrn_norm"])[0].reshape(1024)
    w_h3, b_h3, n_h3, g_h3 = [], [], [], []
    for hp in range(2):
        cs = slice(512 * hp, 512 * (hp + 1))
        w_h3.append(np.concatenate([w_in[:, o:o + 1024][:, cs] for o in (HQOFF, HF_OFF, HI_OFF, HGOFF)], axis=1))
        b_h3.append(np.concatenate([b_in[o:o + 1024][cs] for o in (HQOFF, HF_OFF, HI_OFF, HGOFF)]))
        n_h3.append(hn[cs])
        g_h3.append(hgrn_gamma[:, cs])
    shared = {
        "w_h3": np.ascontiguousarray(np.stack(w_h3)), "b_h3": np.ascontiguousarray(np.stack(b_h3)),
        "n_h3": np.ascontiguousarray(np.stack(n_h3)), "g_h3": np.ascontiguousarray(np.stack(g_h3)),
        "w_pa": f(inputs["w_pa"])[0], "w_pb": f(inputs["w_pb"])[0],
        "w_mg": np.ascontiguousarray(w_in[:, MGOFF:MGOFF + 4096]), "b_mg": np.ascontiguousarray(b_in[MGOFF:MGOFF + 4096]),
        "w_out": f(inputs["w_out"])[0],
        "ln1_g": f(inputs["ln1_g"])[0], "ln1_b": f(inputs["ln1_b"])[0], "ln2_g": f(inputs["ln2_g"])[0], "ln2_b": f(inputs["ln2_b"])[0],
        "w_r": np.ascontiguousarray(np.concatenate([f(inputs["w_rg"])[0], f(inputs["w_re"])[0]], axis=1)),
        "b_r": np.ascontiguousarray(np.concatenate([f(inputs["b_rg"])[0], f(inputs["b_re"])[0]])),
        "w_gate": np.ascontiguousarray(f(inputs["w_gate"])[0][:_NEXP]), "w_up": np.ascontiguousarray(f(inputs["w_up"])[0][:_NEXP]),
        "w_down": np.ascontiguousarray(f(inputs["w_down"])[0][:_NEXP]),
    }
    shared.update(sample_tables())
    shared["cache2d"] = np.ascontiguousarray(f(inputs["cache_kv"])[0].reshape(N_PHYS * 128 * 2, 512))
    page_table = np.ascontiguousarray(np.asarray(inputs["page_table"], dtype=np.int32))
    in_maps = []
    for c in range(NCORES):
        b, i = c // 4, c % 4
        nnull = OWN0 - 8 * i
        if nnull not in _TAB_CACHE:
            _TAB_CACHE[nnull] = host_tables(nnull)
        xfr = np.zeros((XF_T, D_MODEL), np.float32)
        nreal = (NB - nnull) * 128
        xfr[nnull * 128:NB * 128] = x_prompt[b, 0:nreal]
        for j in range(4):
            xfr[NB * 128 + 32 * j:NB * 128 + 32 * j + 4] = x_sample[4 * c + j]
        hs = slice(256 * i, 256 * (i + 1))
        w_st = np.concatenate([w_in[:, HF_OFF:HF_OFF + 1024][:, hs], w_in[:, HI_OFF:HI_OFF + 1024][:, hs]], axis=1)
        b_st = np.concatenate([b_in[HF_OFF:HF_OFF + 1024][hs], b_in[HI_OFF:HI_OFF + 1024][hs]])
        m = {
            "xf": np.ascontiguousarray(xfr.T), "xTb": xTb_all[b],
            "w_kv": w_kv, "b_kv": b_kv, "w_q": w_q, "b_q": b_q, "w_ng": w_ng, "b_ng": b_ng,
            "w_st": np.ascontiguousarray(w_st), "b_st": np.ascontiguousarray(b_st),
            "g_st": np.ascontiguousarray(hgrn_gamma[:, hs]),
            "w_ss": w_ss, "b_ss": b_ss, "g_ss": hgrn_gamma,
            "st_in": np.ascontiguousarray(state_hgrn[4 * c:4 * c + 4]),
            "cw_in": np.ascontiguousarray(cache_win[4 * c:4 * c + 4].reshape(4, 512, 512)),
            "c_lm": lmask, "c_lm4": np.ascontiguousarray(lmask[:4, :4]),
            "w_c1": f(inputs["w_cmp1"])[0], "w_c2": f(inputs["w_cmp2"])[0], "c_pe": f(inputs["cmp_pe"])[0],
        }
        m.update(_TAB_CACHE[nnull])
        m.update(shared)
        m["pt_core"] = np.ascontiguousarray(page_table[4 * c:4 * c + 4].reshape(256))
        in_maps.append(m)
    res = run_bass_kernel_spmd(nc, in_maps, core_ids=list(range(NCORES)))
    return res.results


def _assemble(R):
    y_prompt = np.zeros((2, SEQ, D_MODEL), np.float32)
    y_sample = np.zeros((32, 4, D_MODEL), np.float32)
    new_kv_prompt = np.zeros((1, 2, SEQ, 4, 4, 64), np.float32)
    new_kv_sample = np.zeros((1, 32, 4, 4, 4, 64), np.float32)
    new_win_prompt = np.zeros((1, 2, 512, 2, 4, 64), np.float32)
    new_win_sample = np.zeros((1, 32, 512, 2, 4, 64), np.float32)
    new_state_prompt = np.zeros((1, 2, 8, 128, 128), np.float32)
    new_state_sample = np.zeros((1, 32, 8, 128, 128), np.float32)
    for c in range(NCORES):
        b, i = c // 4, c % 4
        kvo = np.asarray(R[c]["kv_out"])
        new_kv_prompt[0, b, 1024 * i:1024 * (i + 1)] = kvo[:1024, :1024].reshape(1024, 4, 4, 64)
        smp = kvo[1024:].reshape(4, 32, KV_W)[:, :4]
        new_kv_sample[0, 4 * c:4 * c + 4] = smp[:, :, :1024].reshape(4, 4, 4, 4, 64)
        if i == 3:
            new_win_prompt[0, b] = kvo[512:1024, 1024:1536].reshape(512, 2, 4, 64)
        new_win_sample[0, 4 * c:4 * c + 4] = np.asarray(R[c]["win_s"]).reshape(4, 512, 2, 4, 64)
        new_state_prompt[0, b, 2 * i:2 * i + 2] = np.asarray(R[c]["st_p"])
        new_state_sample[0, 4 * c:4 * c + 4] = np.asarray(R[c]["st_s"])
        if "y_out" in R[c]:
            yo = np.asarray(R[c]["y_out"])
            y_prompt[b, 1024 * i:1024 * (i + 1)] = yo[:, :1024].T
            y_sample[4 * c:4 * c + 4] = yo[:, 1024:].T.reshape(4, 32, D_MODEL)[:, :4]
    return (y_prompt, y_sample, new_kv_prompt, new_kv_sample, new_win_prompt, new_win_sample,
            new_state_prompt, new_state_sample)


def kernel(**inputs):
    return _assemble(_run(inputs, debug=False))
```

```python
import contextlib
import numpy as np
import concourse.bass as bass
import concourse.mybir as mybir
from concourse.bass_utils import run_bass_kernel_spmd

F32 = mybir.dt.float32
BF16 = mybir.dt.bfloat16
AF = mybir.ActivationFunctionType
ALU = mybir.AluOpType

D_MODEL = 2048
KC = D_MODEL // 128
SEQ = 4096
NCORES = 8
TOK_P = 1024
TOK_S = 16
TOK = TOK_P + TOK_S
KV_OFF, KV_W = 1024, 1536
HF_OFF, HI_OFF = 3632, 4656


class _Stop(Exception):
    pass


class Buf:
    def __init__(self, name):
        self.name = name
        self.w = {}
        self.r = {}


class Prog:
    COMPUTE = ("pe", "act", "dve", "pool")

    def __init__(self, n_dma_sems=8):
        self.ops = {e: [] for e in ("pe", "act", "dve", "pool", "sp")}
        self.cnt = {}
        self.waited = {}
        self.n_dma = n_dma_sems
        self.dma_rr = {"sp": 0, "pool": 0, "act": 0}

    def _need(self, eng, reads, writes):
        need = {}
        for b in reads:
            for e, s in b.w.items():
                need[e] = max(need.get(e, 0), s)
        for b in writes:
            for e, s in b.w.items():
                need[e] = max(need.get(e, 0), s)
            for e, s in b.r.items():
                need[e] = max(need.get(e, 0), s)
        waits = []
        for e, s in need.items():
            if e == eng and eng == "pe":
                continue
            if self.waited.get((eng, e), 0) >= s:
                continue
            self.waited[(eng, e)] = s
            waits.append((e, s))
        return waits

    def op(self, eng, fn, reads=(), writes=()):
        waits = self._need(eng, reads, writes)
        self.cnt[eng] = self.cnt.get(eng, 0) + 1
        s = self.cnt[eng]
        for b in reads:
            b.r[eng] = s
        for b in writes:
            b.w[eng] = s
        self.ops[eng].append((waits, fn, eng))

    def dma(self, queue, fn, reads=(), writes=()):
        k = self.dma_rr[queue]
        self.dma_rr[queue] = (k + 1) % self.n_dma
        v = "dma_%s_%d" % (queue, k)
        waits = self._need(queue, reads, writes)
        prev = self.cnt.get(v, 0)
        if prev and self.waited.get((queue, v), 0) < prev:
            self.waited[(queue, v)] = prev
            waits.append((v, prev))
        self.cnt[v] = prev + 1
        s = self.cnt[v]
        for b in reads:
            b.r[v] = s
        for b in writes:
            b.w[v] = s
        self.ops[queue].append((waits, fn, v))

    def barrier(self):
        targets = dict(self.cnt)
        for eng in self.ops:
            waits = []
            for e, n in targets.items():
                if e == eng and eng == "pe":
                    continue
                if n and self.waited.get((eng, e), 0) < n:
                    self.waited[(eng, e)] = n
                    waits.append((e, n))
            self.ops[eng].append((waits, None, None))

    def finish(self, queue="sp"):
        waits = []
        for v, n in self.cnt.items():
            if v.startswith("dma_") and self.waited.get((queue, v), 0) < n:
                waits.append((v, n))
        self.ops[queue].append((waits, None, None))

    def emit(self, nc):
        names = sorted(set(list(self.COMPUTE) + [v for v in self.cnt if v.startswith("dma_")]))
        with contextlib.ExitStack() as es:
            sems = {n: es.enter_context(nc.semaphore("s_" + n)) for n in names}
            block = es.enter_context(nc.Block())

            def run(eng_name, eng):
                for waits, fn, inc in self.ops[eng_name]:
                    for (e, s) in waits:
                        eng.wait_ge(sems[e], s * 16 if e.startswith("dma_") else s)
                    if fn is None:
                        continue
                    ins = fn(eng)
                    ins.then_inc(sems[inc], 16 if inc.startswith("dma_") else 1)

            @block.tensor
            def _(e):
                run("pe", e)

            @block.scalar
            def _(e):
                run("act", e)

            @block.vector
            def _(e):
                run("dve", e)

            @block.gpsimd
            def _(e):
                run("pool", e)

            @block.sync
            def _(e):
                run("sp", e)


def stage_states(nc, P, st, sbt, next_ps, ones_c, B_ones, D):
    xTb, xf = D["xTb"], D["xf"]
    wbig, B_wbig = sbt(st, "wbig", [128, KC, 2048], BF16)
    wst_sb, B_wst = sbt(st, "wst_sb", [128, KC, 512], BF16)
    bias_big, B_bias_big = sbt(st, "bias_big", [128, 2048], F32)
    bias_st, B_bias_st = sbt(st, "bias_st", [128, 512], F32)
    oml_st, B_oml_st = sbt(st, "oml_st", [128, 256], F32)
    oml_ss, B_oml_ss = sbt(st, "oml_ss", [128, 1024], F32)
    lm, B_lm = sbt(st, "lm", [128, 128], F32)
    lm4, B_lm4 = sbt(st, "lm4", [4, 4], F32)
    xs = [sbt(st, "sxs%d" % i, [128, KC, 256], BF16) for i in range(2)]
    xsm, B_xsm = sbt(st, "xsm", [128, KC, 128], BF16)
    S_sb, B_S = sbt(st, "S_sb", [128, 2, 128], F32)
    NSCR = 2
    scr = []
    for i in range(NSCR):
        d = {}
        for n, dt in (("kk", F32), ("lgf", F32), ("edd", F32), ("kd", BF16), ("vv", BF16)):
            d[n] = sbt(st, "%s%d" % (n, i), [128, 1024], dt)
        d["edl"] = sbt(st, "edl%d" % i, [128, 8], F32)
        scr.append(d)

    P.dma("sp", lambda e: e.dma_start(out=lm[:], in_=D["c_lm"]), writes=[B_lm])
    P.dma("sp", lambda e: e.dma_start(out=lm4[:], in_=D["c_lm4"]), writes=[B_lm4])
    P.dma("sp", lambda e: e.dma_start(out=bias_st[:], in_=D["b_st"].partition_broadcast(128)), writes=[B_bias_st])
    wst_v = D["w_st"].rearrange("(kc p) c -> p kc c", p=128)
    for q in range(2):
        P.dma("pool", lambda e, q=q: e.dma_start(out=wst_sb[:, 8 * q:8 * q + 8, :], in_=wst_v[:, 8 * q:8 * q + 8, :]),
              writes=[B_wst])

    def lower_bound_prep(g_ap, n, oml, B_oml):
        g0, Bg0 = scr[0]["lgf"]
        g1, Bg1 = scr[1]["lgf"]
        P.dma("sp", lambda e: e.dma_start(out=g0[:, 0:n], in_=g_ap[0].partition_broadcast(128)), writes=[Bg0])
        P.dma("sp", lambda e: e.dma_start(out=g1[:, 0:n], in_=g_ap[1].partition_broadcast(128)), writes=[Bg1])
        P.op("dve", lambda e: e.tensor_tensor(out=oml[:, 0:n], in0=g1[:, 0:n], in1=g0[:, 0:n], op=ALU.subtract),
             reads=[Bg0, Bg1], writes=[B_oml])
        P.op("act", lambda e: e.activation(out=oml[:, 0:n], in_=oml[:, 0:n], func=AF.Sigmoid), reads=[B_oml], writes=[B_oml])

    def state_chunk(si, m, xsl, W, B_W, B_x, bias, B_bias, oml, B_oml, nh, lmask, B_lmask, S_list):
        d = scr[si % NSCR]
        kk, Bkk = d["kk"]
        lgf, Blgf = d["lgf"]
        edd, Bedd = d["edd"]
        kd, Bkd = d["kd"]
        vv, Bvv = d["vv"]
        edl, Bedl = d["edl"]
        n = nh * 128
        for g in range((2 * n) // 512):
            pt, Bp = next_ps("a")
            for kc in range(KC):
                P.op("pe", lambda e, pt=pt, kc=kc, g=g: e.matmul(
                    pt[:m, :], lhsT=xsl(kc), rhs=W[:, kc, g * 512:(g + 1) * 512],
                    start=(kc == 0), stop=(kc == KC - 1)), reads=[B_x, B_W], writes=[Bp])
            c0 = g * 512
            a0, a1 = c0, min(c0 + 512, n)
            if a1 > a0:
                P.op("dve", lambda e, pt=pt, a0=a0, a1=a1, c0=c0: e.tensor_tensor(
                    out=kk[:m, a0:a1], in0=pt[:m, a0 - c0:a1 - c0], in1=bias[:m, a0:a1], op=ALU.add),
                    reads=[Bp, B_bias], writes=[Bkk])
            v0, v1 = max(c0, n), c0 + 512
            if v1 > v0:
                P.op("dve", lambda e, pt=pt, v0=v0, v1=v1, c0=c0: e.tensor_tensor(
                    out=vv[:m, v0 - n:v1 - n], in0=pt[:m, v0 - c0:v1 - c0], in1=bias[:m, v0:v1], op=ALU.add),
                    reads=[Bp, B_bias], writes=[Bvv])
        P.op("act", lambda e: e.activation(out=kk[:m, 0:n], in_=kk[:m, 0:n], func=AF.Sigmoid, scale=-1.0),
             reads=[Bkk], writes=[Bkk])
        P.op("dve", lambda e: e.tensor_tensor(out=kk[:m, 0:n], in0=kk[:m, 0:n], in1=oml[:m, 0:n], op=ALU.mult),
             reads=[Bkk, B_oml], writes=[Bkk])
        P.op("act", lambda e: e.activation(out=lgf[:m, 0:n], in_=kk[:m, 0:n], func=AF.Ln, scale=-1.0, bias=ones_c[:m, :]),
             reads=[Bkk, B_ones], writes=[Blgf])
        for g in range((n + 511) // 512):
            w = min(512, n - g * 512)
            pt, Bp = next_ps("a")
            P.op("pe", lambda e, pt=pt, g=g, w=w: e.matmul(pt[:m, 0:w], lhsT=lmask[:m, :m], rhs=lgf[:m, g * 512:g * 512 + w],
                                                           start=True, stop=True), reads=[B_lmask, Blgf], writes=[Bp])
            P.op("act", lambda e, pt=pt, g=g, w=w: e.activation(out=edd[:m, g * 512:g * 512 + w], in_=pt[:m, 0:w], func=AF.Exp),
                 reads=[Bp], writes=[Bedd])
        P.op("dve", lambda e: e.tensor_tensor(out=kd[:m, 0:n], in0=kk[:m, 0:n], in1=edd[:m, 0:n], op=ALU.mult),
             reads=[Bkk, Bedd], writes=[Bkd])
        pt, Bp = next_ps("b")
        for h in range(nh):
            P.op("pe", lambda e, pt=pt, h=h: e.matmul(pt[:, h:h + 1], lhsT=lgf[:m, h * 128:(h + 1) * 128], rhs=ones_c[:m, :],
                                                      start=True, stop=True), reads=[Blgf, B_ones], writes=[Bp])
        P.op("act", lambda e, pt=pt: e.activation(out=edl[:, 0:nh], in_=pt[:, 0:nh], func=AF.Exp), reads=[Bp], writes=[Bedl])
        for h0 in range(0, nh, 4):
            pt, Bp = next_ps("b")
            hs_ = list(range(h0, min(nh, h0 + 4)))
            for h in hs_:
                P.op("pe", lambda e, pt=pt, h=h, h0=h0: e.matmul(
                    pt[:, (h - h0) * 128:(h - h0 + 1) * 128], lhsT=kd[:m, h * 128:(h + 1) * 128], rhs=vv[:m, h * 128:(h + 1) * 128],
                    start=True, stop=True), reads=[Bkd, Bvv], writes=[Bp])
            for h in hs_:
                s_in, s_out, b_in, b_out = S_list[h]
                P.op("dve", lambda e, pt=pt, h=h, h0=h0, s_in=s_in, s_out=s_out: e.scalar_tensor_tensor(
                    out=s_out, in0=s_in, scalar=edl[:, h:h + 1], in1=pt[:, (h - h0) * 128:(h - h0 + 1) * 128],
                    op0=ALU.mult, op1=ALU.add), reads=[Bp, Bedl, b_in], writes=[b_out])

    lower_bound_prep(D["g_st"], 256, oml_st, B_oml_st)
    P.op("dve", lambda e: e.memset(S_sb[:], 0.0), writes=[B_S])
    xTb_v = xTb.rearrange("(kc p) t -> p kc t", p=128)
    si = 0
    for tt in range(SEQ // 256):
        xb, Bx = xs[tt % 2]
        for q in range(2):
            P.dma("pool", lambda e, xb=xb, tt=tt, q=q: e.dma_start(
                out=xb[:, 8 * q:8 * q + 8, :], in_=xTb_v[:, 8 * q:8 * q + 8, tt * 256:(tt + 1) * 256]), writes=[Bx])
        for c4 in range(2):
            S_list = [(S_sb[:, h, :], S_sb[:, h, :], B_S, B_S) for h in range(2)]
            state_chunk(si, 128, lambda kc, xb=xb, c4=c4: xb[:, kc, c4 * 128:(c4 + 1) * 128],
                        wst_sb, B_wst, Bx, bias_st, B_bias_st, oml_st, B_oml_st, 2, lm, B_lm, S_list)
            si += 1
    for h in range(2):
        P.dma("sp", lambda e, h=h: e.dma_start(out=D["st_p"][h], in_=S_sb[:, h, :]), reads=[B_S])

    wss_v = D["w_ss"].rearrange("(kc p) c -> p kc c", p=128)
    for q in range(4):
        P.dma("pool", lambda e, q=q: e.dma_start(out=wbig[:, 4 * q:4 * q + 4, :], in_=wss_v[:, 4 * q:4 * q + 4, :]),
              writes=[B_wbig])
    P.dma("sp", lambda e: e.dma_start(out=bias_big[:], in_=D["b_ss"].partition_broadcast(128)), writes=[B_bias_big])
    xf_v = xf.rearrange("(kc p) t -> p kc t", p=128)
    for q in range(2):
        P.dma("pool", lambda e, q=q: e.dma_start(out=xsm[:, 8 * q:8 * q + 8, :], in_=xf_v[:, 8 * q:8 * q + 8, NB * 128:(NB + 1) * 128]),
              writes=[B_xsm])
    lower_bound_prep(D["g_ss"], 1024, oml_ss, B_oml_ss)
    s0 = [sbt(st, "s0_%d" % i, [128, 8, 128], F32) for i in range(2)]
    for j in range(4):
        sj, Bsj = s0[j % 2]
        P.dma("sp", lambda e, sj=sj, j=j: e.dma_start(out=sj[:], in_=D["st_in"][j].rearrange("h k v -> k h v")), writes=[Bsj])
        S_list = [(sj[:, h, :], sj[:, h, :], Bsj, Bsj) for h in range(8)]
        state_chunk(si, 4, lambda kc, j=j: xsm[:, kc, 32 * j:32 * j + 4],
                    wbig, B_wbig, B_xsm, bias_big, B_bias_big, oml_ss, B_oml_ss, 8, lm4, B_lm4, S_list)
        si += 1
        P.dma("sp", lambda e, sj=sj, j=j: e.dma_start(out=D["st_s"][j].rearrange("h k v -> k h v"), in_=sj[:]), reads=[Bsj])


def nsa_prompt(nc, P, p4, sbt, next_ps, L):
    debug = L["debug"]
    xf = L["xf"]
    kslcT, B_kslcT, kwinT, B_kwinT = L["kslcT"], L["B_kslcT"], L["kwinT"], L["B_kwinT"]
    vslc, B_vslc, vwin, B_vwin = L["vslc"], L["B_vslc"], L["vwin"], L["B_vwin"]
    kcT, B_kcT, vca, B_vca = L["kcT"], L["B_kcT"], L["vca"], L["B_vca"]
    ident, B_ident, identb, B_identb = L["ident"], L["B_ident"], L["identb"], L["B_identb"]
    onesb, B_onesb = L["onesb"], L["B_onesb"]
    o_nsaT, B_onsaT = L["o_nsaT"], L["B_onsaT"]

    def table(name, shape, dt, src, q="sp"):
        t, B = sbt(p4, name, shape, dt)
        P.dma(q, lambda e: e.dma_start(out=t[:], in_=src), writes=[B])
        return t, B
    cb, B_cb = table("cb", [128, 256], F32, L["t_cb"])
    cmask, B_cmask = table("cmask", [128, NOWN * 512], BF16, L["t_cmask"])
    bs, B_bs = table("bs", [128, 512], F32, L["t_bs"])
    sq, B_sq = table("sq", [1, 2048], F32, L["t_sq"])
    lnt, B_lnt = table("lnt", [2, NB * 128], BF16, L["t_ln"])
    R2, B_R2 = table("R2", [2, 512], BF16, L["t_r2"])
    Et, B_Et = table("Et", [64, NB * 128], BF16, L["t_E"])
    caus, B_caus = table("caus", [128, 512], BF16, L["t_caus"])
    low, B_low = table("low", [128, 512], BF16, L["t_low"])
    keep, B_keep = table("keep", [128, NOWN * 64], F32, L["t_keep"])
    force, B_force = table("force", [128, NOWN * 64], F32, L["t_force"])

    qT, B_qT = sbt(p4, "qT", [128, 8, NOWN * 128], BF16)
    gates, B_gates = sbt(p4, "gates", [128, NOWN, 48], F32)
    pq = contextlib.ExitStack()
    with pq:
        wq, B_wq = sbt(pq, "wq", [128, KC, 1024], BF16)
        wng, B_wng = sbt(pq, "wng", [128, KC, 48], BF16)
        bq_col, B_bq = sbt(pq, "bq_col", [128, 8], F32)
        bng, B_bng = sbt(pq, "bng", [128, 48], F32)
        xo = [sbt(pq, "xo%d" % i, [128, KC, 256], BF16) for i in range(1)]
        wq_v = L["w_q"].rearrange("(kc p) c -> p kc c", p=128)
        for q in range(4):
            P.dma("pool", lambda e, q=q: e.dma_start(out=wq[:, 4 * q:4 * q + 4, :], in_=wq_v[:, 4 * q:4 * q + 4, :]), writes=[B_wq])
        P.dma("pool", lambda e: e.dma_start(out=wng[:], in_=L["w_ng"].rearrange("(kc p) c -> p kc c", p=128)), writes=[B_wng])
        with nc.allow_non_contiguous_dma(reason="tiny bias column layout"):
            P.dma("sp", lambda e: e.dma_start(out=bq_col[:], in_=L["b_q"].rearrange("(cb p) -> p cb", p=128), allow_slow_non_contiguous=True), writes=[B_bq])
        P.dma("sp", lambda e: e.dma_start(out=bng[:], in_=L["b_ng"].partition_broadcast(128)), writes=[B_bng])
        xf_v = xf.rearrange("(kc p) t -> p kc t", p=128)
        for tt in range(4):
            xb, Bx = xo[0]
            t0 = OWN0 * 128 + tt * 256
            for q in range(2):
                P.dma("pool", lambda e, xb=xb, t0=t0, q=q: e.dma_start(
                    out=xb[:, 8 * q:8 * q + 8, :], in_=xf_v[:, 8 * q:8 * q + 8, t0:t0 + 256]), writes=[Bx])
            for cbk in range(8):
                pt, Bp = next_ps("a")
                for kc in range(KC):
                    P.op("pe", lambda e, pt=pt, kc=kc, cbk=cbk, xb=xb: e.matmul(
                        pt[:, 0:256], lhsT=wq[:, kc, cbk * 128:(cbk + 1) * 128], rhs=xb[:, kc, :],
                        start=(kc == 0), stop=(kc == KC - 1)), reads=[B_wq, Bx], writes=[Bp])
                P.op("act", lambda e, pt=pt, cbk=cbk, tt=tt: e.activation(
                    out=qT[:, cbk, tt * 256:(tt + 1) * 256], in_=pt[:, 0:256], func=AF.Identity, bias=bq_col[:, cbk:cbk + 1]),
                    reads=[Bp, B_bq], writes=[B_qT])
            for bi in range(2):
                k = tt * 2 + bi
                pt, Bp = next_ps("b")
                for kc in range(KC):
                    P.op("pe", lambda e, pt=pt, kc=kc, xb=xb, bi=bi: e.matmul(
                        pt[:, 0:48], lhsT=xb[:, kc, bi * 128:(bi + 1) * 128], rhs=wng[:, kc, :],
                        start=(kc == 0), stop=(kc == KC - 1)), reads=[B_wng, Bx], writes=[Bp])
                P.op("dve", lambda e, pt=pt, k=k: e.tensor_tensor(out=gates[:, k, :], in0=pt[:, 0:48], in1=bng[:, :], op=ALU.add),
                     reads=[Bp, B_bng], writes=[B_gates])
        P.op("act", lambda e: e.activation(out=gates[:], in_=gates[:], func=AF.Sigmoid), reads=[B_gates], writes=[B_gates])
        P.barrier()

    sqt, B_sqt = sbt(p4, "sqt", [128, 512], BF16)
    runmax, B_runmax = sbt(p4, "runmax", [1, 512], F32)
    nkm, B_nkm = sbt(p4, "nkm", [1, 1], F32)
    P.op("dve", lambda e: e.memset(runmax[:], 0.0), writes=[B_runmax])
    srcs = []
    for gp in range(2):
        for s in range(NB * 128 // 512):
            srcs.append((kslcT, B_kslcT, gp, s * 512, 512))
        for s in range(3):
            srcs.append((kwinT, B_kwinT, gp, s * 512, 512))
        srcs.append((kcT, B_kcT, gp, 0, 256))
    for (src, Bs, gp, c0, w) in srcs:
        P.op("dve", lambda e, src=src, gp=gp, c0=c0, w=w: e.tensor_tensor(
            out=sqt[:, 0:w], in0=src[:, gp, c0:c0 + w], in1=src[:, gp, c0:c0 + w], op=ALU.mult), reads=[Bs], writes=[B_sqt])
        pt, Bp = next_ps("b")
        P.op("pe", lambda e, pt=pt, w=w: e.matmul(pt[0:1, 0:w], lhsT=onesb[:, 0:1], rhs=sqt[:, 0:w], start=True, stop=True),
             reads=[B_onesb, B_sqt], writes=[Bp])
        P.op("dve", lambda e, pt=pt, w=w: e.tensor_tensor(out=runmax[:, 0:w], in0=runmax[:, 0:w], in1=pt[0:1, 0:w], op=ALU.max),
             reads=[Bp, B_runmax], writes=[B_runmax])
    P.op("dve", lambda e: e.reduce_max(out=nkm[:], in_=runmax[:], axis=mybir.AxisListType.X), reads=[B_runmax], writes=[B_nkm])
    P.op("dve", lambda e: e.tensor_scalar(out=nkm[:], in0=nkm[:], scalar1=-0.5, scalar2=None, op0=ALU.mult),
         reads=[B_nkm], writes=[B_nkm])
    P.op("dve", lambda e: e.tensor_scalar(out=sq[:, :], in0=sq[:, :], scalar1=nkm[0:1, 0:1], scalar2=None, op0=ALU.add),
         reads=[B_nkm, B_sq], writes=[B_sq])

    if _LIM < 5:
        raise _Stop()
    pT = [sbt(p4, "pT%d" % i, [128, 4, 128], BF16) for i in range(4)]
    pT_rr = [0]

    def next_pT():
        k = pT_rr[0] % len(pT)
        pT_rr[0] += 1
        return pT[k]
    qsq, B_qsq = sbt(p4, "qsq", [128, 512], BF16)
    rt, B_rt = sbt(p4, "rt", [1, 512], F32)
    o_blk, B_oblk = sbt(p4, "o_blk", [128, 256], F32)
    rs, B_rs = sbt(p4, "rs", [128, 4], F32)
    wgt, B_wgt = sbt(p4, "wgt", [128, 4], F32)
    imp, B_imp = sbt(p4, "imp", [128, 64], F32)
    imp3, B_imp3 = sbt(p4, "imp3", [128, 64], F32)
    m8, B_m8 = sbt(p4, "m8", [128, 16], F32)
    nsel, B_nsel = sbt(p4, "nsel", [128, 64], F32)
    nselT, B_nselT = sbt(p4, "nselT", [64, 512], BF16)
    if debug:
        dimp, B_dimp = sbt(p4, "dimp", [128, 64], F32)

    def softmax_chunk(pt, Bp, w, hbase, col_fn, tab, B_tab):
        (t, Bt0) = next_pT()
        Bt = _HB.setdefault(id(Bt0), [Buf("h%d" % i) for i in range(4)])
        for hh in range(4):
            P.op("act", lambda e, pt=pt, t=t, hh=hh, w=w: e.activation(
                out=t[:w, hh, :], in_=pt[:w, hh * 128:(hh + 1) * 128], func=AF.Exp, scale=SCALE,
                bias=tab[:w, col_fn(hbase + hh):col_fn(hbase + hh) + 1]), reads=[Bp, B_tab], writes=[Bt[hh]])
        return t, Bt

    def finish_branch(psO, BpO, k, g, br, first):
        P.op("dve", lambda e: e.tensor_scalar(out=rs[:, :], in0=psO[:, 0:260].rearrange("p (h c) -> p h c", c=65)[:, :, 64],
                                              scalar1=1e-30, scalar2=None, op0=ALU.max), reads=[BpO], writes=[B_rs])
        P.op("dve", lambda e: e.reciprocal(out=rs[:, :], in_=rs[:, :]), reads=[B_rs], writes=[B_rs])
        P.op("dve", lambda e: e.tensor_tensor(out=wgt[:, :], in0=rs[:, :], in1=gates[:, k, br * 16 + g * 4:br * 16 + g * 4 + 4],
                                              op=ALU.mult), reads=[B_rs, B_gates], writes=[B_wgt])
        for hh in range(4):
            oc = slice(hh * 64, hh * 64 + 64)
            if first:
                P.op("dve", lambda e, hh=hh, oc=oc: e.tensor_scalar(out=o_blk[:, oc], in0=psO[:, hh * 65:hh * 65 + 64],
                                                                    scalar1=wgt[:, hh:hh + 1], scalar2=None, op0=ALU.mult),
                     reads=[BpO, B_wgt], writes=[B_oblk])
            else:
                P.op("dve", lambda e, hh=hh, oc=oc: e.scalar_tensor_tensor(
                    out=o_blk[:, oc], in0=psO[:, hh * 65:hh * 65 + 64], scalar=wgt[:, hh:hh + 1], in1=o_blk[:, oc],
                    op0=ALU.mult, op1=ALU.add), reads=[BpO, B_wgt, B_oblk], writes=[B_oblk])

    kslc_g, B_kslcg = sbt(p4, "kslc_g", [64, NB * 128], BF16)
    kwin_g, B_kwing = sbt(p4, "kwin_g", [64, 12 * 128], BF16)
    kc_g, B_kcg = sbt(p4, "kc_g", [64, 256], BF16)
    q_g, B_qg = sbt(p4, "q_g", [64, 4, NOWN * 128], BF16)
    for g in range(4):
        gp, g2 = g // 2, g % 2
        hs0 = slice(64 * g2, 64 * g2 + 64)
        P.dma("sp", lambda e, hs0=hs0, gp=gp: e.dma_start(out=kslc_g[:, :], in_=kslcT[hs0, gp, :]), reads=[B_kslcT], writes=[B_kslcg])
        P.dma("sp", lambda e, hs0=hs0, gp=gp: e.dma_start(out=kwin_g[:, :], in_=kwinT[hs0, gp, :]), reads=[B_kwinT], writes=[B_kwing])
        P.dma("sp", lambda e, hs0=hs0, gp=gp: e.dma_start(out=kc_g[:, :], in_=kcT[hs0, gp, :]), reads=[B_kcT], writes=[B_kcg])
        P.dma("sp", lambda e, hs0=hs0, gp=gp: e.dma_start(out=q_g[:, :, :], in_=qT[hs0, gp * 4:gp * 4 + 4, :]), reads=[B_qT], writes=[B_qg])
        hs = slice(0, 64)
        for k in range(NOWN):
            if _LIM < 6 and (k, g) not in _KSEL:
                continue
            B = OWN0 + k
            tok = slice(k * 128, (k + 1) * 128)
            qv = q_g[:, :, tok]
            P.op("dve", lambda e, qv=qv, hs=hs: e.tensor_tensor(out=qsq[hs, :].rearrange("p (h q) -> p h q", h=4), in0=qv, in1=qv,
                                                                op=ALU.mult), reads=[B_qg], writes=[B_qsq])
            pt, Bp = next_ps("b")
            P.op("pe", lambda e, pt=pt, hs=hs: e.matmul(pt[0:1, :], lhsT=onesb[hs, 0:1], rhs=qsq[hs, :], start=True, stop=True),
                 reads=[B_onesb, B_qsq], writes=[Bp])
            P.op("dve", lambda e, pt=pt, g=g: e.scalar_tensor_tensor(out=R2[0:1, :], in0=pt[0:1, :], scalar=-0.5,
                                                                     in1=sq[0:1, g * 512:(g + 1) * 512], op0=ALU.mult, op1=ALU.add),
                 reads=[Bp, B_sq], writes=[B_R2])

            pts = []
            for c in range(2):
                w = 128 if c == 0 else 127
                pt, Bp = next_ps("a")
                P.op("pe", lambda e, pt=pt, c=c, w=w, hs=hs, gp=gp, qv=qv: e.matmul(
                    pt[:w, :], lhsT=kc_g[hs, c * 128:c * 128 + w], rhs=qv, start=True, stop=False),
                    reads=[B_kcg, B_qg], writes=[Bp])
                P.op("pe", lambda e, pt=pt, w=w, c=c: e.matmul(pt[:w, :], lhsT=lnt[0:1, 0:w], rhs=R2[0:1, :], start=False, stop=(c == 0)),
                     reads=[B_lnt, B_R2], writes=[Bp])
                if c == 1:
                    P.op("pe", lambda e, pt=pt, w=w, k=k: e.matmul(pt[:w, :], lhsT=identb[:w, :w], rhs=cmask[:w, k * 512:(k + 1) * 512],
                                                                   start=False, stop=True), reads=[B_identb, B_cmask], writes=[Bp])
                t, Bt = softmax_chunk(pt, Bp, w, 4 * g, lambda h, k=k, c=c: (h * NOWN + k) * 2 + c, cb, B_cb)
                pts.append((t, Bt, w))
            if _LIM < 5.2:
                continue
            psO, BpO = next_ps("b")
            psI, BpI = next_ps("b")
            for hh in range(4):
                for c, (t, Bt, w) in enumerate(pts):
                    P.op("pe", lambda e, hh=hh, c=c, t=t, w=w, g=g, psO=psO: e.matmul(
                        psO[:, hh * 65:(hh + 1) * 65], lhsT=t[:w, hh, :], rhs=vca[:w, c, g, 0:65], start=(c == 0), stop=(c == 1)),
                        reads=[Bt[hh], B_vca], writes=[BpO])
            for hh in range(4):
                for c, (t, Bt, w) in enumerate(pts):
                    P.op("pe", lambda e, hh=hh, c=c, t=t, w=w, g=g, psI=psI: e.matmul(
                        psI[:, hh * 64:(hh + 1) * 64], lhsT=t[:w, hh, :], rhs=vca[:w, c, g, 65:129], start=(c == 0), stop=(c == 1)),
                        reads=[Bt[hh], B_vca], writes=[BpI])
            finish_branch(psO, BpO, k, g, 0, True)
            for hh in range(4):
                if hh == 0:
                    P.op("dve", lambda e, psI=psI: e.tensor_scalar(out=imp[:, :], in0=psI[:, 0:64], scalar1=rs[:, 0:1], scalar2=None,
                                                          op0=ALU.mult), reads=[BpI, B_rs], writes=[B_imp])
                else:
                    P.op("dve", lambda e, hh=hh, psI=psI: e.scalar_tensor_tensor(
                        out=imp[:, :], in0=psI[:, hh * 64:(hh + 1) * 64], scalar=rs[:, hh:hh + 1], in1=imp[:, :],
                        op0=ALU.mult, op1=ALU.add), reads=[BpI, B_rs, B_imp], writes=[B_imp])
            if debug:
                P.op("dve", lambda e: e.tensor_copy(out=dimp[:], in_=imp[:]), reads=[B_imp], writes=[B_dimp])
                P.dma("sp", lambda e, k=k, g=g: e.dma_start(out=L["d_imp"][(k * 4 + g) * 128:(k * 4 + g + 1) * 128, :], in_=dimp[:]),
                      reads=[B_dimp])
            if _LIM < 5.3:
                continue
            P.op("dve", lambda e: e.tensor_scalar(out=imp[:, :], in0=imp[:, :], scalar1=1e-30, scalar2=None, op0=ALU.max),
                 reads=[B_imp], writes=[B_imp])
            P.op("dve", lambda e, k=k: e.tensor_tensor(out=imp[:, :], in0=imp[:, :], in1=keep[:, k * 64:(k + 1) * 64], op=ALU.mult),
                 reads=[B_imp, B_keep], writes=[B_imp])
            P.op("dve", lambda e, k=k: e.tensor_tensor(out=imp[:, :], in0=imp[:, :], in1=force[:, k * 64:(k + 1) * 64], op=ALU.add),
                 reads=[B_imp, B_force], writes=[B_imp])
            P.op("dve", lambda e: e.max(out=m8[:, 0:8], in_=imp[:, :]), reads=[B_imp], writes=[B_m8])
            P.op("dve", lambda e: e.tensor_scalar(out=imp3[:, :], in0=imp[:, :], scalar1=m8[:, 7:8], scalar2=None, op0=ALU.is_ge),
                 reads=[B_imp, B_m8], writes=[B_imp3])
            P.op("dve", lambda e: e.scalar_tensor_tensor(out=imp3[:, :], in0=imp3[:, :], scalar=-3.0e38, in1=imp[:, :],
                                                         op0=ALU.mult, op1=ALU.add), reads=[B_imp, B_imp3], writes=[B_imp3])
            P.op("dve", lambda e: e.max(out=m8[:, 8:16], in_=imp3[:, :]), reads=[B_imp3], writes=[B_m8])
            P.op("dve", lambda e: e.tensor_scalar(out=nsel[:, :], in0=imp[:, :], scalar1=m8[:, 15:16], scalar2=None, op0=ALU.is_ge),
                 reads=[B_imp, B_m8], writes=[B_nsel])
            P.op("dve", lambda e: e.tensor_scalar(out=nsel[:, :], in0=nsel[:, :], scalar1=-NEGM, scalar2=NEGM, op0=ALU.mult, op1=ALU.add),
                 reads=[B_nsel], writes=[B_nsel])
            pt, Bp = next_ps("b")
            P.op("pe", lambda e, pt=pt: e.transpose(out=pt[0:64, 0:128], in_=nsel[:, :], identity=ident[:, :]),
                 reads=[B_nsel, B_ident], writes=[Bp])
            for hh in range(4):
                P.op("act", lambda e, pt=pt, hh=hh: e.activation(out=nselT[:, hh * 128:(hh + 1) * 128], in_=pt[0:64, 0:128],
                                                                 func=AF.Identity), reads=[Bp], writes=[B_nselT])
            if _LIM < 5.4:
                continue
            psO, BpO = next_ps("b")
            for c in range(B + 1):
                pt, Bp = next_ps("a")
                P.op("pe", lambda e, pt=pt, c=c, hs=hs, gp=gp, qv=qv: e.matmul(
                    pt[:, :], lhsT=kslc_g[hs, c * 128:(c + 1) * 128], rhs=qv, start=True, stop=False),
                    reads=[B_kslcg, B_qg], writes=[Bp])
                P.op("pe", lambda e, pt=pt, c=c: e.matmul(pt[:, :], lhsT=Et[:, c * 128:(c + 1) * 128], rhs=nselT[:, :],
                                                          start=False, stop=False), reads=[B_Et, B_nselT], writes=[Bp])
                P.op("pe", lambda e, pt=pt, c=c, B=B: e.matmul(pt[:, :], lhsT=lnt[0:2, c * 128:(c + 1) * 128], rhs=R2[0:2, :],
                                                               start=False, stop=(c != B)), reads=[B_lnt, B_R2], writes=[Bp])
                if c == B:
                    P.op("pe", lambda e, pt=pt: e.matmul(pt[:, :], lhsT=identb[:, :], rhs=caus[:, :], start=False, stop=True),
                         reads=[B_identb, B_caus], writes=[Bp])
                t, Bt = softmax_chunk(pt, Bp, 128, 4 * g, lambda h, d=B - c: h * 32 + d, bs, B_bs)
                for hh in range(4):
                    P.op("pe", lambda e, hh=hh, t=t, c=c, g=g, B=B, psO=psO: e.matmul(
                        psO[:, hh * 65:(hh + 1) * 65], lhsT=t[:, hh, :], rhs=vslc[:, c, g, :], start=(c == 0), stop=(c == B)),
                        reads=[Bt[hh], B_vslc], writes=[BpO])
            finish_branch(psO, BpO, k, g, 1, False)
            if _LIM < 5.5:
                continue
            psO, BpO = next_ps("b")
            for c in range(B - 4, B + 1):
                cw = c - 20
                pt, Bp = next_ps("a")
                P.op("pe", lambda e, pt=pt, cw=cw, hs=hs, gp=gp, qv=qv: e.matmul(
                    pt[:, :], lhsT=kwin_g[hs, cw * 128:(cw + 1) * 128], rhs=qv, start=True, stop=False),
                    reads=[B_kwing, B_qg], writes=[Bp])
                edge = (c == B) or (c == B - 4)
                P.op("pe", lambda e, pt=pt, c=c, edge=edge: e.matmul(pt[:, :], lhsT=lnt[0:2, c * 128:(c + 1) * 128], rhs=R2[0:2, :],
                                                                      start=False, stop=(not edge)), reads=[B_lnt, B_R2], writes=[Bp])
                if edge:
                    mk, Bmk = (caus, B_caus) if c == B else (low, B_low)
                    P.op("pe", lambda e, pt=pt, mk=mk: e.matmul(pt[:, :], lhsT=identb[:, :], rhs=mk[:, :], start=False, stop=True),
                         reads=[B_identb, Bmk], writes=[Bp])
                t, Bt = softmax_chunk(pt, Bp, 128, 4 * g, lambda h, d=B - c: h * 32 + d, bs, B_bs)
                for hh in range(4):
                    P.op("pe", lambda e, hh=hh, t=t, cw=cw, c=c, g=g, B=B, psO=psO: e.matmul(
                        psO[:, hh * 65:(hh + 1) * 65], lhsT=t[:, hh, :], rhs=vwin[:, cw, g, :], start=(c == B - 4), stop=(c == B)),
                        reads=[Bt[hh], B_vwin], writes=[BpO])
            finish_branch(psO, BpO, k, g, 2, False)

            if _LIM < 5.6:
                continue
            if debug:
                P.dma("sp", lambda e, k=k, g=g: e.dma_start(out=L["d_onsa"][k * 128:(k + 1) * 128, g * 256:(g + 1) * 256], in_=o_blk[:, 0:256]),
                      reads=[B_oblk])
            for j in range(2):
                pt, Bp = next_ps("a")
                P.op("pe", lambda e, pt=pt, j=j: e.transpose(out=pt[:, 0:128], in_=o_blk[:, j * 128:(j + 1) * 128],
                                                             identity=ident[:, :]), reads=[B_oblk, B_ident], writes=[Bp])
                P.op("dve", lambda e, pt=pt, j=j, k=k, g=g: e.tensor_copy(out=o_nsaT[:, 2 * g + j, k * 128:(k + 1) * 128], in_=pt[:, 0:128]),
                     reads=[Bp], writes=[B_onsaT])


def hgrn_outputs(nc, P, sc, sbt, next_ps, L):
    xf = L["xf"]
    ident, B_ident = L["ident"], L["B_ident"]
    ones_c, B_ones = L["ones_c"], L["B_ones"]
    o_hgT, B_ohgT = L["o_hgT"], L["B_ohgT"]
    debug = L["debug"]

    def table(name, shape, dt, src, q="sp"):
        t, B = sbt(sc, name, shape, dt)
        P.dma(q, lambda e: e.dma_start(out=t[:], in_=src), writes=[B])
        return t, B
    lm, B_lm = table("h_lm", [128, 128], F32, L["c_lm"])
    u32, B_u32 = table("h_u32", [128, 128], F32, L["t_u32"])
    l32, B_l32 = table("h_l32", [128, 128], F32, L["t_l32"])
    ind4, B_ind4 = table("h_ind4", [128, 4], F32, L["t_ind4"])
    vmask, B_vmask = table("h_vmask", [128, NB + 1], F32, L["t_vmask"])
    eps_c, B_eps = sbt(sc, "eps_c", [128, 1], F32)
    P.op("dve", lambda e: e.memset(eps_c[:], LN_EPS), writes=[B_eps])

    W3, B_W3 = sbt(sc, "W3", [128, KC, 2048], BF16)
    b3, B_b3 = sbt(sc, "b3", [128, 2048], F32)
    oml, B_oml = sbt(sc, "oml3", [128, 512], F32)
    gtmp, B_gtmp = sbt(sc, "gtmp", [128, 2, 512], F32)
    ngb, B_ngb = sbt(sc, "ngb", [128, 512], F32)
    xt = [sbt(sc, "hx%d" % i, [128, KC, 128], BF16) for i in range(2)]
    names32 = ["kk", "lgf", "ebc", "ebi", "edd", "qe", "ke", "hgb", "osb", "og"]
    T = {n: sbt(sc, "h_" + n, [128, 512], F32) for n in names32}
    vv, B_vv = sbt(sc, "h_vv", [128, 512], BF16)
    kd4, B_kd4 = sbt(sc, "h_kd4", [128, 4, 512], BF16)
    kdb, B_kdb = sbt(sc, "h_kdb", [128, 512], BF16)
    qeT4, B_qeT4 = sbt(sc, "h_qeT4", [128, 4, 4, 128], BF16)
    keT, B_keT = sbt(sc, "h_keT", [128, 4, 128], BF16)
    attm, B_attm = sbt(sc, "h_attm", [128, 4, 128], BF16)
    S, B_S = sbt(sc, "h_S", [128, 4, 128], F32)
    Sb, B_Sb = sbt(sc, "h_Sb", [128, 4, 128], BF16)
    Sld, B_Sld = sbt(sc, "h_Sld", [128, 4, 4, 128], BF16)
    edl, B_edl = sbt(sc, "h_edl", [128, 16], F32)
    ss, B_ss = sbt(sc, "h_ss", [128, 4], F32)
    P.op("dve", lambda e: e.memset(qeT4[:], 0.0), writes=[B_qeT4])
    xf_v = xf.rearrange("(kc p) t -> p kc t", p=128)

    for hp in range(2):
        w3_v = L["w_h3"][hp].rearrange("(kc p) c -> p kc c", p=128)
        for q in range(4):
            P.dma("pool", lambda e, q=q, w3_v=w3_v: e.dma_start(out=W3[:, 4 * q:4 * q + 4, :], in_=w3_v[:, 4 * q:4 * q + 4, :]),
                  writes=[B_W3])
        P.dma("sp", lambda e, hp=hp: e.dma_start(out=b3[:], in_=L["b_h3"][hp].partition_broadcast(128)), writes=[B_b3])
        P.dma("sp", lambda e, hp=hp: e.dma_start(out=ngb[:], in_=L["n_h3"][hp].partition_broadcast(128)), writes=[B_ngb])
        for r in range(2):
            P.dma("sp", lambda e, hp=hp, r=r: e.dma_start(out=gtmp[:, r, :], in_=L["g_h3"][hp, r].partition_broadcast(128)),
                  writes=[B_gtmp])
        P.op("dve", lambda e: e.tensor_tensor(out=oml[:], in0=gtmp[:, 1, :], in1=gtmp[:, 0, :], op=ALU.subtract),
             reads=[B_gtmp], writes=[B_oml])
        P.op("act", lambda e: e.activation(out=oml[:], in_=oml[:], func=AF.Sigmoid), reads=[B_oml], writes=[B_oml])
        for j in range(4):
            P.dma("pool", lambda e, hp=hp, j=j: e.dma_start(out=Sld[:, j, :, :], in_=L["st_in"][j, 4 * hp:4 * hp + 4].rearrange("h k v -> k h v")),
                  writes=[B_Sld])
        P.op("dve", lambda e: e.memset(S[:], 0.0), writes=[B_S])
        P.op("dve", lambda e: e.memset(Sb[:], 0.0), writes=[B_Sb])

        for p in range(NB + 1):
            own = p >= OWN0
            sample = p == NB
            xb, Bx = xt[p % 2]
            P.dma("pool", lambda e, xb=xb, p=p: e.dma_start(out=xb[:, :, :], in_=xf_v[:, :, p * 128:(p + 1) * 128]), writes=[Bx])

            def proj(cg, xb=xb, Bx=Bx):
                pt, Bp = next_ps("a")
                for kc in range(KC):
                    P.op("pe", lambda e, pt=pt, kc=kc, cg=cg, xb=xb: e.matmul(
                        pt[:, :], lhsT=xb[:, kc, :], rhs=W3[:, kc, cg * 512:(cg + 1) * 512],
                        start=(kc == 0), stop=(kc == KC - 1)), reads=[Bx, B_W3], writes=[Bp])
                return pt, Bp

            def ew(eng, name, fn, reads, writes_name):
                t, Bt = T[writes_name]
                P.op(eng, fn, reads=reads, writes=[Bt])

            kk, Bkk = T["kk"]
            lgf, Blgf = T["lgf"]
            pt, Bp = proj(1)
            P.op("dve", lambda e, pt=pt: e.tensor_tensor(out=kk[:], in0=pt[:, :], in1=b3[:, 512:1024], op=ALU.add),
                 reads=[Bp, B_b3], writes=[Bkk])
            P.op("act", lambda e: e.activation(out=kk[:], in_=kk[:], func=AF.Sigmoid, scale=-1.0), reads=[Bkk], writes=[Bkk])
            P.op("dve", lambda e: e.tensor_tensor(out=kk[:], in0=kk[:], in1=oml[:], op=ALU.mult), reads=[Bkk, B_oml], writes=[Bkk])
            P.op("act", lambda e: e.activation(out=lgf[:], in_=kk[:], func=AF.Ln, scale=-1.0, bias=ones_c[:, :]),
                 reads=[Bkk, B_ones], writes=[Blgf])
            pt, Bp = proj(2)
            if own:
                P.op("dve", lambda e, pt=pt: e.tensor_tensor(out=vv[:], in0=pt[:, :], in1=b3[:, 1024:1536], op=ALU.add),
                     reads=[Bp, B_b3], writes=[B_vv])
            else:
                osb_, Bosb_ = T["osb"]
                P.op("dve", lambda e, pt=pt: e.tensor_tensor(out=osb_[:], in0=pt[:, :], in1=b3[:, 1024:1536], op=ALU.add),
                     reads=[Bp, B_b3], writes=[Bosb_])
                P.op("dve", lambda e, p=p: e.tensor_scalar(out=vv[:], in0=osb_[:], scalar1=vmask[:, p:p + 1], scalar2=None, op0=ALU.mult),
                     reads=[Bosb_, B_vmask], writes=[B_vv])

            if not own:
                edd, Bedd = T["edd"]
                pt, Bp = next_ps("a")
                P.op("pe", lambda e, pt=pt: e.matmul(pt[:, :], lhsT=lm[:, :], rhs=lgf[:, :], start=True, stop=True),
                     reads=[B_lm, Blgf], writes=[Bp])
                P.op("act", lambda e, pt=pt: e.activation(out=edd[:], in_=pt[:, :], func=AF.Exp), reads=[Bp], writes=[Bedd])
                P.op("dve", lambda e: e.tensor_tensor(out=kdb[:], in0=kk[:], in1=edd[:], op=ALU.mult), reads=[Bkk, Bedd], writes=[B_kdb])
                pt, Bp = next_ps("b")
                for h in range(4):
                    P.op("pe", lambda e, pt=pt, h=h: e.matmul(pt[:, h:h + 1], lhsT=lgf[:, h * 128:(h + 1) * 128], rhs=ones_c[:, :],
                                                              start=True, stop=True), reads=[Blgf, B_ones], writes=[Bp])
                P.op("act", lambda e, pt=pt: e.activation(out=edl[:, 0:4], in_=pt[:, 0:4], func=AF.Exp), reads=[Bp], writes=[B_edl])
                pt, Bp = next_ps("b")
                for h in range(4):
                    P.op("pe", lambda e, pt=pt, h=h: e.matmul(pt[:, h * 128:(h + 1) * 128], lhsT=kdb[:, h * 128:(h + 1) * 128],
                                                              rhs=vv[:, h * 128:(h + 1) * 128], start=True, stop=True),
                         reads=[B_kdb, B_vv], writes=[Bp])
                for h in range(4):
                    P.op("dve", lambda e, pt=pt, h=h: e.scalar_tensor_tensor(
                        out=S[:, h, :], in0=S[:, h, :], scalar=edl[:, h:h + 1], in1=pt[:, h * 128:(h + 1) * 128],
                        op0=ALU.mult, op1=ALU.add), reads=[Bp, B_edl, B_S], writes=[B_S])
                if p == OWN0 - 1:
                    P.op("dve", lambda e: e.tensor_copy(out=Sb[:], in_=S[:]), reads=[B_S], writes=[B_Sb])
                continue

            ebc, Bebc = T["ebc"]
            ebi, Bebi = T["ebi"]
            edd, Bedd = T["edd"]
            qe, Bqe = T["qe"]
            ke, Bke = T["ke"]
            hgb, Bhgb = T["hgb"]
            osb, Bosb = T["osb"]
            og, Bog = T["og"]
            pt, Bp = next_ps("a")
            P.op("pe", lambda e, pt=pt: e.matmul(pt[:, :], lhsT=u32[:, :], rhs=lgf[:, :], start=True, stop=True),
                 reads=[B_u32, Blgf], writes=[Bp])
            P.op("act", lambda e, pt=pt: e.activation(out=ebc[:], in_=pt[:, :], func=AF.Exp), reads=[Bp], writes=[Bebc])
            P.op("act", lambda e, pt=pt: e.activation(out=ebi[:], in_=pt[:, :], func=AF.Exp, scale=-1.0), reads=[Bp], writes=[Bebi])
            pt, Bp = next_ps("a")
            P.op("pe", lambda e, pt=pt: e.matmul(pt[:, :], lhsT=l32[:, :], rhs=lgf[:, :], start=True, stop=True),
                 reads=[B_l32, Blgf], writes=[Bp])
            P.op("act", lambda e, pt=pt: e.activation(out=edd[:], in_=pt[:, :], func=AF.Exp), reads=[Bp], writes=[Bedd])
            pt, Bp = next_ps("b")
            for h in range(4):
                P.op("pe", lambda e, pt=pt, h=h: e.matmul(pt[:, h * 4:h * 4 + 4], lhsT=lgf[:, h * 128:(h + 1) * 128], rhs=ind4[:, :],
                                                          start=True, stop=True), reads=[Blgf, B_ind4], writes=[Bp])
            P.op("act", lambda e, pt=pt: e.activation(out=edl[:, 0:16], in_=pt[:, 0:16], func=AF.Exp), reads=[Bp], writes=[B_edl])
            pt, Bp = proj(0)
            P.op("dve", lambda e, pt=pt: e.tensor_tensor(out=qe[:], in0=pt[:, :], in1=b3[:, 0:512], op=ALU.add),
                 reads=[Bp, B_b3], writes=[Bqe])
            P.op("act", lambda e: e.activation(out=og[:], in_=qe[:], func=AF.Sigmoid), reads=[Bqe], writes=[Bog])
            P.op("dve", lambda e: e.tensor_tensor(out=qe[:], in0=qe[:], in1=og[:], op=ALU.mult), reads=[Bqe, Bog], writes=[Bqe])
            P.op("dve", lambda e: e.tensor_tensor(out=qe[:], in0=qe[:], in1=ebc[:], op=ALU.mult), reads=[Bqe, Bebc], writes=[Bqe])
            P.op("dve", lambda e: e.tensor_tensor(out=ke[:], in0=kk[:], in1=ebi[:], op=ALU.mult), reads=[Bkk, Bebi], writes=[Bke])
            P.op("dve", lambda e: e.tensor_tensor(out=edd[:], in0=kk[:], in1=edd[:], op=ALU.mult), reads=[Bkk, Bedd], writes=[Bedd])
            if not sample:
                for c in range(4):
                    P.op("dve", lambda e, c=c: e.tensor_scalar(out=kd4[:, c, :], in0=edd[:], scalar1=ind4[:, c:c + 1], scalar2=None,
                                                               op0=ALU.mult), reads=[Bedd, B_ind4], writes=[B_kd4])
            pt, Bp = proj(3)
            P.op("dve", lambda e, pt=pt: e.tensor_tensor(out=hgb[:], in0=pt[:, :], in1=b3[:, 1536:2048], op=ALU.add),
                 reads=[Bp, B_b3], writes=[Bhgb])
            P.op("act", lambda e: e.activation(out=og[:], in_=hgb[:], func=AF.Sigmoid), reads=[Bhgb], writes=[Bog])
            P.op("dve", lambda e: e.tensor_tensor(out=hgb[:], in0=hgb[:], in1=og[:], op=ALU.mult), reads=[Bhgb, Bog], writes=[Bhgb])
            P.op("dve", lambda e: e.tensor_tensor(out=hgb[:], in0=hgb[:], in1=ngb[:], op=ALU.mult), reads=[Bhgb, B_ngb], writes=[Bhgb])
            for h in range(4):
                pt, Bp = next_ps("a")
                P.op("pe", lambda e, pt=pt, h=h: e.transpose(out=pt[:, 0:128], in_=qe[:, h * 128:(h + 1) * 128], identity=ident[:, :]),
                     reads=[Bqe, B_ident], writes=[Bp])
                for c in range(4):
                    P.op("act", lambda e, pt=pt, h=h, c=c: e.activation(out=qeT4[:, h, c, 32 * c:32 * c + 32], in_=pt[:, 32 * c:32 * c + 32],
                                                                         func=AF.Identity), reads=[Bp], writes=[B_qeT4])
                pt, Bp = next_ps("a")
                P.op("pe", lambda e, pt=pt, h=h: e.transpose(out=pt[:, 0:128], in_=ke[:, h * 128:(h + 1) * 128], identity=ident[:, :]),
                     reads=[Bke, B_ident], writes=[Bp])
                P.op("dve", lambda e, pt=pt, h=h: e.tensor_copy(out=keT[:, h, :], in_=pt[:, 0:128]), reads=[Bp], writes=[B_keT])
            for h in range(4):
                pt, Bp = next_ps("a")
                for c in range(4):
                    P.op("pe", lambda e, pt=pt, h=h, c=c: e.matmul(pt[:, 0:128], lhsT=keT[:, h, :], rhs=qeT4[:, h, c, :],
                                                                   start=(c == 0), stop=(c == 3)), reads=[B_keT, B_qeT4], writes=[Bp])
                P.op("dve", lambda e, pt=pt, h=h: e.tensor_tensor(out=attm[:, h, :], in0=pt[:, 0:128], in1=u32[:, :], op=ALU.mult),
                     reads=[Bp, B_u32], writes=[B_attm])
            po, Bpo = next_ps("b")
            for h in range(4):
                for c in range(4):
                    if sample:
                        rhs_fn = lambda c=c, h=h: Sld[:, c, h, :]
                        Brhs = B_Sld
                    else:
                        rhs_fn = lambda c=c, h=h: Sb[:, h, :]
                        Brhs = B_Sb
                    P.op("pe", lambda e, po=po, h=h, c=c, rhs_fn=rhs_fn: e.matmul(
                        po[:, h * 128:(h + 1) * 128], lhsT=qeT4[:, h, c, :], rhs=rhs_fn(), start=(c == 0), stop=False),
                        reads=[B_qeT4, Brhs], writes=[Bpo])
                    if not sample:
                        ps2, Bp2 = next_ps("a")
                        P.op("pe", lambda e, ps2=ps2, h=h, c=c: e.matmul(ps2[:, 0:128], lhsT=kd4[:, c, h * 128:(h + 1) * 128],
                                                                         rhs=vv[:, h * 128:(h + 1) * 128], start=True, stop=True),
                             reads=[B_kd4, B_vv], writes=[Bp2])
                        P.op("dve", lambda e, ps2=ps2, h=h, c=c: e.scalar_tensor_tensor(
                            out=S[:, h, :], in0=S[:, h, :], scalar=edl[:, h * 4 + c:h * 4 + c + 1], in1=ps2[:, 0:128],
                            op0=ALU.mult, op1=ALU.add), reads=[Bp2, B_edl, B_S], writes=[B_S])
                        P.op("act", lambda e, h=h: e.activation(out=Sb[:, h, :], in_=S[:, h, :], func=AF.Identity),
                             reads=[B_S], writes=[B_Sb])
                P.op("pe", lambda e, po=po, h=h: e.matmul(po[:, h * 128:(h + 1) * 128], lhsT=attm[:, h, :], rhs=vv[:, h * 128:(h + 1) * 128],
                                                          start=False, stop=True), reads=[B_attm, B_vv], writes=[Bpo])
            P.op("act", lambda e, po=po: e.activation(out=osb[:], in_=po[:, :], func=AF.Identity), reads=[Bpo], writes=[Bosb])
            P.op("dve", lambda e: e.tensor_tensor(out=og[:], in0=osb[:], in1=osb[:], op=ALU.mult), reads=[Bosb], writes=[Bog])
            P.op("dve", lambda e: e.reduce_sum(out=ss[:, 0:4], in_=og[:].rearrange("p (h v) -> p h v", h=4), axis=mybir.AxisListType.X),
                 reads=[Bog], writes=[B_ss])
            P.op("act", lambda e: e.activation(out=ss[:, 0:4], in_=ss[:, 0:4], func=AF.Sqrt, scale=1.0 / 128.0, bias=eps_c[:, :]),
                 reads=[B_ss, B_eps], writes=[B_ss])
            P.op("dve", lambda e: e.reciprocal(out=ss[:, 0:4], in_=ss[:, 0:4]), reads=[B_ss], writes=[B_ss])
            for h in range(4):
                P.op("dve", lambda e, h=h: e.scalar_tensor_tensor(
                    out=og[:, h * 128:(h + 1) * 128], in0=osb[:, h * 128:(h + 1) * 128], scalar=ss[:, h:h + 1],
                    in1=hgb[:, h * 128:(h + 1) * 128], op0=ALU.mult, op1=ALU.mult), reads=[Bosb, B_ss, Bhgb], writes=[Bog])
            if debug:
                k_ = p - OWN0
                P.dma("sp", lambda e, k_=k_, hp=hp: e.dma_start(out=L["d_ohg"][k_ * 128:(k_ + 1) * 128, hp * 512:(hp + 1) * 512], in_=og[:, :]),
                      reads=[Bog])
            for h in range(4):
                pt, Bp = next_ps("a")
                P.op("pe", lambda e, pt=pt, h=h: e.transpose(out=pt[:, 0:128], in_=og[:, h * 128:(h + 1) * 128], identity=ident[:, :]),
                     reads=[Bog, B_ident], writes=[Bp])
                P.op("dve", lambda e, pt=pt, h=h, p=p, hp=hp: e.tensor_copy(
                    out=o_hgT[:, 4 * hp + h, (p - OWN0) * 128:(p - OWN0 + 1) * 128], in_=pt[:, 0:128]), reads=[Bp], writes=[B_ohgT])
        P.barrier()


def tail_moe(nc, P, sc, sbt, next_ps, L):
    xf = L["xf"]
    ident, B_ident = L["ident"], L["B_ident"]
    ones_c, B_ones = L["ones_c"], L["B_ones"]
    o_nsaT, B_onsaT, o_hgT, B_ohgT = L["o_nsaT"], L["B_onsaT"], L["o_hgT"], L["B_ohgT"]
    debug = L["debug"]
    y_out = L["y_out"]
    xf_v = xf.rearrange("(kc p) t -> p kc t", p=128)
    T0 = OWN0 * 128

    zacc, B_zacc = sbt(sc, "zacc", [128, KC, NT], F32)
    hT, B_hT = L["ohT"], L["B_oh"]
    lncol, B_lncol = sbt(sc, "lncol", [128, 4, KC], F32)
    for i, nm in enumerate(("ln1_g", "ln1_b", "ln2_g", "ln2_b")):
        P.dma("sp", lambda e, i=i, nm=nm: e.dma_start(out=lncol[:, i, :], in_=L[nm].rearrange("(kc p) -> p kc", p=128),
                                                      allow_slow_non_contiguous=True), writes=[B_lncol])
    eps_c, B_eps = sbt(sc, "eps_t", [128, 1], F32)
    P.op("dve", lambda e: e.memset(eps_c[:], LN_EPS), writes=[B_eps])
    onesr, B_onesr = sbt(sc, "onesr", [1, 128], F32)
    P.op("dve", lambda e: e.memset(onesr[:], 1.0), writes=[B_onesr])

    def layer_norm(gi, bi, emit_out):
        for (t0, tw) in TT:
            p1, Bp1 = next_ps("b")
            p2, Bp2 = next_ps("b")
            for m in range(KC):
                P.op("pe", lambda e, p1=p1, m=m, t0=t0, tw=tw: e.matmul(p1[0:1, 0:tw], lhsT=ones_c[:, 0:1], rhs=zacc[:, m, t0:t0 + tw],
                                                                        start=(m == 0), stop=(m == KC - 1)),
                     reads=[B_ones, B_zacc], writes=[Bp1])
                P.op("act", lambda e, m=m, t0=t0, tw=tw: e.activation(out=sqs[:, 0:tw], in_=zacc[:, m, t0:t0 + tw], func=AF.Square),
                     reads=[B_zacc], writes=[B_sqs])
                P.op("pe", lambda e, p2=p2, m=m, tw=tw: e.matmul(p2[0:1, 0:tw], lhsT=ones_c[:, 0:1], rhs=sqs[:, 0:tw],
                                                                 start=(m == 0), stop=(m == KC - 1)),
                     reads=[B_ones, B_sqs], writes=[Bp2])
            P.op("dve", lambda e, p1=p1, t0=t0, tw=tw: e.tensor_scalar(out=stat[:, 0, t0:t0 + tw], in0=p1[0:1, 0:tw], scalar1=1.0 / D_MODEL,
                                                                       scalar2=None, op0=ALU.mult), reads=[Bp1], writes=[B_stat])
            P.op("dve", lambda e, p2=p2, t0=t0, tw=tw: e.tensor_scalar(out=stat[:, 1, t0:t0 + tw], in0=p2[0:1, 0:tw], scalar1=1.0 / D_MODEL,
                                                                       scalar2=None, op0=ALU.mult), reads=[Bp2], writes=[B_stat])
            P.op("dve", lambda e, t0=t0, tw=tw: e.tensor_tensor(out=sqs[0:1, 0:tw], in0=stat[:, 0, t0:t0 + tw], in1=stat[:, 0, t0:t0 + tw],
                                                                op=ALU.mult), reads=[B_stat], writes=[B_sqs])
            P.op("dve", lambda e, t0=t0, tw=tw: e.tensor_tensor(out=stat[:, 1, t0:t0 + tw], in0=stat[:, 1, t0:t0 + tw], in1=sqs[0:1, 0:tw],
                                                                op=ALU.subtract), reads=[B_stat, B_sqs], writes=[B_stat])
            P.op("act", lambda e, t0=t0, tw=tw: e.activation(out=stat[:, 1, t0:t0 + tw], in_=stat[:, 1, t0:t0 + tw], func=AF.Sqrt,
                                                             bias=eps_c[0:1, :]), reads=[B_stat, B_eps], writes=[B_stat])
            P.op("dve", lambda e, t0=t0, tw=tw: e.reciprocal(out=stat[:, 1, t0:t0 + tw], in_=stat[:, 1, t0:t0 + tw]),
                 reads=[B_stat], writes=[B_stat])
            for r in range(2):
                pb, Bpb = next_ps("b")
                P.op("pe", lambda e, pb=pb, r=r, t0=t0, tw=tw: e.matmul(pb[:, 0:tw], lhsT=onesr[0:1, :], rhs=stat[:, r, t0:t0 + tw],
                                                                        start=True, stop=True), reads=[B_onesr, B_stat], writes=[Bpb])
                P.op("act", lambda e, pb=pb, r=r, t0=t0, tw=tw: e.activation(out=mbc[:, r, t0:t0 + tw], in_=pb[:, 0:tw], func=AF.Identity),
                     reads=[Bpb], writes=[B_mbc])
        for m in range(KC):
            P.op("dve", lambda e, m=m: e.tensor_tensor(out=zacc[:, m, :], in0=zacc[:, m, :], in1=mbc[:, 0, :], op=ALU.subtract),
                 reads=[B_zacc, B_mbc], writes=[B_zacc])
            P.op("dve", lambda e, m=m: e.tensor_tensor(out=zacc[:, m, :], in0=zacc[:, m, :], in1=mbc[:, 1, :], op=ALU.mult),
                 reads=[B_zacc, B_mbc], writes=[B_zacc])
            P.op("dve", lambda e, m=m: e.tensor_scalar(out=zacc[:, m, :], in0=zacc[:, m, :], scalar1=lncol[:, gi, m:m + 1],
                                                       scalar2=lncol[:, bi, m:m + 1], op0=ALU.mult, op1=ALU.add),
                 reads=[B_zacc, B_lncol], writes=[B_zacc])
            emit_out(m)

    t1 = contextlib.ExitStack()
    with t1:
        mT, B_mT = sbt(t1, "mT", [128, KC, NT], BF16)
        t1a = t1.enter_context(contextlib.ExitStack())
        xt_, B_xt = sbt(t1a, "xt_", [128, KC, 512], BF16)
        wpa = [sbt(t1a, "wpa%d" % i, [128, 8, 128], BF16) for i in range(2)]
        wpb = [sbt(t1a, "wpb%d" % i, [128, 8, 128], BF16) for i in range(2)]
        wmg = [sbt(t1a, "wmg%d" % i, [128, KC, 256], BF16) for i in range(2)]
        bmg, B_bmg = sbt(t1a, "bmg", [128, 32], F32)
        sga, B_sga = sbt(t1a, "sga", [128, 512], F32)
        sgb, B_sgb = sbt(t1a, "sgb", [128, 512], F32)
        P.dma("sp", lambda e: e.dma_start(out=bmg[:], in_=L["b_mg"].rearrange("(cb p) -> p cb", p=128), allow_slow_non_contiguous=True),
              writes=[B_bmg])
        wmg_v = L["w_mg"].rearrange("(kc p) c -> p kc c", p=128)
        wpa_v = L["w_pa"].rearrange("(kc p) c -> p kc c", p=128)
        wpb_v = L["w_pb"].rearrange("(kc p) c -> p kc c", p=128)
        it = 0
        for (t0, tw) in TT:
            for q in range(2):
                P.dma("pool", lambda e, q=q, t0=t0, tw=tw: e.dma_start(out=xt_[:, 8 * q:8 * q + 8, 0:tw],
                                                                       in_=xf_v[:, 8 * q:8 * q + 8, T0 + t0:T0 + t0 + tw]), writes=[B_xt])
            for m in range(KC):
                wm, Bwm = wmg[it % 2]
                wa, Bwa = wpa[it % 2]
                wb, Bwb = wpb[it % 2]
                it += 1
                P.dma("pool", lambda e, wm=wm, m=m: e.dma_start(out=wm[:, :, 0:128], in_=wmg_v[:, :, m * 128:(m + 1) * 128]), writes=[Bwm])
                P.dma("pool", lambda e, wm=wm, m=m: e.dma_start(out=wm[:, :, 128:256], in_=wmg_v[:, :, 2048 + m * 128:2048 + (m + 1) * 128]),
                      writes=[Bwm])
                P.dma("pool", lambda e, wa=wa, m=m: e.dma_start(out=wa[:, :, :], in_=wpa_v[:, :, m * 128:(m + 1) * 128]), writes=[Bwa])
                P.dma("pool", lambda e, wb=wb, m=m: e.dma_start(out=wb[:, :, :], in_=wpb_v[:, :, m * 128:(m + 1) * 128]), writes=[Bwb])
                pA, BpA = next_ps("a")
                pB, BpB = next_ps("a")
                pga, Bpga = next_ps("a")
                pgb, Bpgb = next_ps("a")
                for kc in range(8):
                    P.op("pe", lambda e, pA=pA, kc=kc, wa=wa, t0=t0, tw=tw: e.matmul(
                        pA[:, 0:tw], lhsT=wa[:, kc, :], rhs=o_nsaT[:, kc, t0:t0 + tw], start=(kc == 0), stop=(kc == 7)),
                        reads=[Bwa, B_onsaT], writes=[BpA])
                for kc in range(8):
                    P.op("pe", lambda e, pB=pB, kc=kc, wb=wb, t0=t0, tw=tw: e.matmul(
                        pB[:, 0:tw], lhsT=wb[:, kc, :], rhs=o_hgT[:, kc, t0:t0 + tw], start=(kc == 0), stop=(kc == 7)),
                        reads=[Bwb, B_ohgT], writes=[BpB])
                for kc in range(KC):
                    P.op("pe", lambda e, pga=pga, kc=kc, wm=wm, tw=tw: e.matmul(
                        pga[:, 0:tw], lhsT=wm[:, kc, 0:128], rhs=xt_[:, kc, 0:tw], start=(kc == 0), stop=(kc == KC - 1)),
                        reads=[Bwm, B_xt], writes=[Bpga])
                for kc in range(KC):
                    P.op("pe", lambda e, pgb=pgb, kc=kc, wm=wm, tw=tw: e.matmul(
                        pgb[:, 0:tw], lhsT=wm[:, kc, 128:256], rhs=xt_[:, kc, 0:tw], start=(kc == 0), stop=(kc == KC - 1)),
                        reads=[Bwm, B_xt], writes=[Bpgb])
                P.op("act", lambda e, pga=pga, m=m, tw=tw: e.activation(out=sga[:, 0:tw], in_=pga[:, 0:tw], func=AF.Sigmoid,
                                                                        bias=bmg[:, m:m + 1]), reads=[Bpga, B_bmg], writes=[B_sga])
                P.op("act", lambda e, pgb=pgb, m=m, tw=tw: e.activation(out=sgb[:, 0:tw], in_=pgb[:, 0:tw], func=AF.Sigmoid,
                                                                        bias=bmg[:, 16 + m:17 + m]), reads=[Bpgb, B_bmg], writes=[B_sgb])
                P.op("dve", lambda e, pA=pA, tw=tw: e.tensor_tensor(out=sga[:, 0:tw], in0=sga[:, 0:tw], in1=pA[:, 0:tw], op=ALU.mult),
                     reads=[BpA, B_sga], writes=[B_sga])
                P.op("dve", lambda e, pB=pB, tw=tw: e.tensor_tensor(out=sgb[:, 0:tw], in0=sgb[:, 0:tw], in1=pB[:, 0:tw], op=ALU.mult),
                     reads=[BpB, B_sgb], writes=[B_sgb])
                P.op("dve", lambda e, m=m, t0=t0, tw=tw: e.tensor_tensor(out=mT[:, m, t0:t0 + tw], in0=sga[:, 0:tw], in1=sgb[:, 0:tw], op=ALU.add),
                     reads=[B_sga, B_sgb], writes=[B_mT])
        P.barrier()
        t1a.close()
        wout = [sbt(t1, "wout%d" % i, [128, KC, 128], BF16) for i in range(2)]
        xres = [sbt(t1, "xres%d" % i, [128, NT], F32) for i in range(2)]
        wout_v = L["w_out"].rearrange("(kc p) c -> p kc c", p=128)
        for m in range(KC):
            xr, Bxr = xres[m % 2]
            wo, Bwo = wout[m % 2]
            P.dma("pool", lambda e, wo=wo, m=m: e.dma_start(out=wo[:, :, :], in_=wout_v[:, :, m * 128:(m + 1) * 128]), writes=[Bwo])
            P.dma("sp", lambda e, xr=xr, m=m: e.dma_start(out=xr[:, :], in_=xf[m * 128:(m + 1) * 128, T0:T0 + NT]), writes=[Bxr])
            for (t0, tw) in TT:
                pt, Bp = next_ps("a")
                for kc in range(KC):
                    P.op("pe", lambda e, pt=pt, kc=kc, wo=wo, t0=t0, tw=tw: e.matmul(
                        pt[:, 0:tw], lhsT=wo[:, kc, :], rhs=mT[:, kc, t0:t0 + tw], start=(kc == 0), stop=(kc == KC - 1)),
                        reads=[Bwo, B_mT], writes=[Bp])
                P.op("dve", lambda e, pt=pt, xr=xr, m=m, t0=t0, tw=tw: e.scalar_tensor_tensor(
                    out=zacc[:, m, t0:t0 + tw], in0=xr[:, t0:t0 + tw], scalar=ALPHA, in1=pt[:, 0:tw], op0=ALU.mult, op1=ALU.add),
                    reads=[Bp, Bxr], writes=[B_zacc])
        P.barrier()

    stat, B_stat = sbt(sc, "stat", [1, 2, NT], F32)
    mbc, B_mbc = sbt(sc, "mbc", [128, 2, NT], F32)
    sqs, B_sqs = sbt(sc, "sqs", [128, 512], F32)

    def after_ln1(m):
        P.op("act", lambda e, m=m: e.activation(out=hT[:, m, :], in_=zacc[:, m, :], func=AF.Identity), reads=[B_zacc], writes=[B_hT])
        if debug:
            P.dma("sp", lambda e, m=m: e.dma_start(out=L["d_h"][m * 128:(m + 1) * 128, :], in_=zacc[:, m, :]), reads=[B_zacc])
        P.op("dve", lambda e, m=m: e.tensor_scalar(out=zacc[:, m, :], in0=zacc[:, m, :], scalar1=ALPHA, scalar2=None, op0=ALU.mult),
             reads=[B_zacc], writes=[B_zacc])
    layer_norm(0, 1, after_ln1)
    P.barrier()

    t3 = contextlib.ExitStack()
    with t3:
        wr, B_wr = sbt(t3, "wr", [128, KC, 36], BF16)
        brt, B_brt = sbt(t3, "brt", [128, 36], F32)
        lg, B_lg = sbt(t3, "lg", [128, 36], F32)
        lem, B_lem = sbt(t3, "lem", [128, 32], F32)
        sm, B_sm = sbt(t3, "sm", [128, 16], F32)
        m8, B_m8 = sbt(t3, "m8r", [128, 8], F32)
        gate, B_gate = sbt(t3, "gate", [128, 32], F32)
        gate2, B_gate2 = sbt(t3, "gate2", [128, 32], F32)
        gT, B_gT = sbt(t3, "gT", [32, NT], F32)
        gThi, B_gThi = sbt(t3, "gThi", [32, NT], BF16)
        gTlo, B_gTlo = sbt(t3, "gTlo", [32, NT], BF16)
        selE, B_selE = sbt(t3, "selE", [32, 32 * 128], BF16)
        gbc, B_gbc = sbt(t3, "gbc", [128, NT], F32)
        hid, B_hid = sbt(t3, "hid", [128, 4, NT], BF16)
        sil, B_sil = sbt(t3, "sil", [128, 512], F32)
        ug, B_ug = sbt(t3, "ugm", [128, 512], F32)
        wgu = [sbt(t3, "wgu%d" % i, [128, KC, 2, 128], BF16) for i in range(2)]
        wd = [sbt(t3, "wd%d" % i, [128, 4, D_MODEL], BF16) for i in range(1)]
        P.dma("pool", lambda e: e.dma_start(out=wr[:], in_=L["w_r"].rearrange("(kc p) c -> p kc c", p=128)), writes=[B_wr])
        P.dma("sp", lambda e: e.dma_start(out=brt[:], in_=L["b_r"].partition_broadcast(128)), writes=[B_brt])
        P.dma("sp", lambda e: e.dma_start(out=selE[:], in_=L["t_selE"]), writes=[B_selE])
        for blk in range(NOWN + 1):
            bs_ = slice(blk * 128, (blk + 1) * 128)
            pt, Bp = next_ps("b")
            for kc in range(KC):
                P.op("pe", lambda e, pt=pt, kc=kc, bs_=bs_: e.matmul(pt[:, 0:36], lhsT=hT[:, kc, bs_], rhs=wr[:, kc, :],
                                                                     start=(kc == 0), stop=(kc == KC - 1)), reads=[B_hT, B_wr], writes=[Bp])
            P.op("dve", lambda e, pt=pt: e.tensor_tensor(out=lg[:], in0=pt[:, 0:36], in1=brt[:], op=ALU.add), reads=[Bp, B_brt], writes=[B_lg])
            P.op("dve", lambda e: e.reduce_max(out=sm[:, 0:1], in_=lg[:, 0:4], axis=mybir.AxisListType.X), reads=[B_lg], writes=[B_sm])
            P.op("dve", lambda e: e.tensor_scalar(out=sm[:, 1:2], in0=sm[:, 0:1], scalar1=-1.0, scalar2=None, op0=ALU.mult),
                 reads=[B_sm], writes=[B_sm])
            P.op("act", lambda e: e.activation(out=sm[:, 4:8], in_=lg[:, 0:4], func=AF.Exp, bias=sm[:, 1:2]), reads=[B_lg, B_sm], writes=[B_sm])
            P.op("dve", lambda e: e.reduce_sum(out=sm[:, 2:3], in_=sm[:, 4:8], axis=mybir.AxisListType.X), reads=[B_sm], writes=[B_sm])
            P.op("dve", lambda e: e.reciprocal(out=sm[:, 2:3], in_=sm[:, 2:3]), reads=[B_sm], writes=[B_sm])
            P.op("dve", lambda e: e.tensor_scalar(out=sm[:, 8:12], in0=lg[:, 0:4], scalar1=sm[:, 0:1], scalar2=None, op0=ALU.is_ge),
                 reads=[B_lg, B_sm], writes=[B_sm])
            P.op("dve", lambda e: e.tensor_scalar(out=sm[:, 8:12], in0=sm[:, 8:12], scalar1=1e30, scalar2=-1e30, op0=ALU.mult, op1=ALU.add),
                 reads=[B_sm], writes=[B_sm])
            for g in range(4):
                P.op("dve", lambda e, g=g: e.tensor_scalar(out=lem[:, g * 8:(g + 1) * 8], in0=lg[:, 4 + g * 8:4 + (g + 1) * 8],
                                                           scalar1=sm[:, 8 + g:9 + g], scalar2=None, op0=ALU.add),
                     reads=[B_lg, B_sm], writes=[B_lem])
            P.op("dve", lambda e: e.max(out=m8[:, 0:8], in_=lem[:, :]), reads=[B_lem], writes=[B_m8])
            P.op("dve", lambda e: e.tensor_tensor(out=sm[:, 12:13], in0=m8[:, 0:1], in1=m8[:, 1:2], op=ALU.subtract), reads=[B_m8], writes=[B_sm])
            P.op("act", lambda e: e.activation(out=sm[:, 13:14], in_=sm[:, 12:13], func=AF.Sigmoid), reads=[B_sm], writes=[B_sm])
            P.op("act", lambda e: e.activation(out=sm[:, 14:15], in_=sm[:, 12:13], func=AF.Sigmoid, scale=-1.0), reads=[B_sm], writes=[B_sm])
            P.op("dve", lambda e: e.tensor_scalar(out=sm[:, 13:15], in0=sm[:, 13:15], scalar1=sm[:, 2:3], scalar2=None, op0=ALU.mult),
                 reads=[B_sm], writes=[B_sm])
            P.op("dve", lambda e: e.tensor_scalar(out=gate[:], in0=lem[:], scalar1=m8[:, 0:1], scalar2=sm[:, 13:14], op0=ALU.is_equal, op1=ALU.mult),
                 reads=[B_lem, B_m8, B_sm], writes=[B_gate])
            P.op("dve", lambda e: e.tensor_scalar(out=gate2[:], in0=lem[:], scalar1=m8[:, 1:2], scalar2=sm[:, 14:15], op0=ALU.is_equal, op1=ALU.mult),
                 reads=[B_lem, B_m8, B_sm], writes=[B_gate2])
            P.op("dve", lambda e: e.tensor_tensor(out=gate[:], in0=gate[:], in1=gate2[:], op=ALU.add), reads=[B_gate, B_gate2], writes=[B_gate])
            pt, Bp = next_ps("b")
            P.op("pe", lambda e, pt=pt: e.transpose(out=pt[0:32, 0:128], in_=gate[:, :], identity=ident[:, :]), reads=[B_gate, B_ident], writes=[Bp])
            P.op("dve", lambda e, pt=pt, bs_=bs_: e.tensor_copy(out=gT[:, bs_], in_=pt[0:32, 0:128]), reads=[Bp], writes=[B_gT])
        if debug:
            P.dma("sp", lambda e: e.dma_start(out=L["d_gate"], in_=gT[:, :]), reads=[B_gT])
        P.op("dve", lambda e: e.tensor_copy(out=gThi[:], in_=gT[:]), reads=[B_gT], writes=[B_gThi])
        P.op("dve", lambda e: e.tensor_tensor(out=gT[:], in0=gT[:], in1=gThi[:], op=ALU.subtract), reads=[B_gT, B_gThi], writes=[B_gT])
        P.op("dve", lambda e: e.tensor_copy(out=gTlo[:], in_=gT[:]), reads=[B_gT], writes=[B_gTlo])

        L["pools"]["a"] = [0, 1, 2, 3, 4, 5, 6]
        L["pools"]["b"] = [7]
        NE = L["n_experts"]
        wg_v = L["w_gate"].rearrange("e (kc p) f -> e p kc f", p=128)
        wu_v = L["w_up"].rearrange("e (kc p) f -> e p kc f", p=128)
        wd_v = L["w_down"].rearrange("e (fc p) d -> e p fc d", p=128)
        for ex in range(NE):
            wdt, Bwd = wd[0]
            for q in range(2):
                P.dma("pool", lambda e, wdt=wdt, ex=ex, q=q: e.dma_start(out=wdt[:, 2 * q:2 * q + 2, :], in_=wd_v[ex, :, 2 * q:2 * q + 2, :]), writes=[Bwd])
            for (t0, tw) in TT:
                pb, Bpb = next_ps("b")
                P.op("pe", lambda e, pb=pb, ex=ex, t0=t0, tw=tw: e.matmul(pb[:, 0:tw], lhsT=selE[:, ex * 128:(ex + 1) * 128], rhs=gThi[:, t0:t0 + tw],
                                                                          start=True, stop=False), reads=[B_selE, B_gThi], writes=[Bpb])
                P.op("pe", lambda e, pb=pb, ex=ex, t0=t0, tw=tw: e.matmul(pb[:, 0:tw], lhsT=selE[:, ex * 128:(ex + 1) * 128], rhs=gTlo[:, t0:t0 + tw],
                                                                          start=False, stop=True), reads=[B_selE, B_gTlo], writes=[Bpb])
                P.op("act", lambda e, pb=pb, t0=t0, tw=tw: e.activation(out=gbc[:, t0:t0 + tw], in_=pb[:, 0:tw], func=AF.Identity),
                     reads=[Bpb], writes=[B_gbc])
            for fc in range(4):
                wt, Bwt = wgu[(ex * 4 + fc) % 2]
                P.dma("pool", lambda e, wt=wt, ex=ex, fc=fc: e.dma_start(out=wt[:, :, 0, :], in_=wg_v[ex, :, :, fc * 128:(fc + 1) * 128]), writes=[Bwt])
                P.dma("pool", lambda e, wt=wt, ex=ex, fc=fc: e.dma_start(out=wt[:, :, 1, :], in_=wu_v[ex, :, :, fc * 128:(fc + 1) * 128]), writes=[Bwt])
                if True:
                    for (t0, tw) in TT:
                        pg, Bpg = next_ps("a")
                        pu, Bpu = next_ps("a")
                        for kc in range(KC):
                            P.op("pe", lambda e, pg=pg, kc=kc, wt=wt, t0=t0, tw=tw: e.matmul(
                                pg[:, 0:tw], lhsT=wt[:, kc, 0, :], rhs=hT[:, kc, t0:t0 + tw],
                                start=(kc == 0), stop=(kc == KC - 1)), reads=[Bwt, B_hT], writes=[Bpg])
                        for kc in range(KC):
                            P.op("pe", lambda e, pu=pu, kc=kc, wt=wt, t0=t0, tw=tw: e.matmul(
                                pu[:, 0:tw], lhsT=wt[:, kc, 1, :], rhs=hT[:, kc, t0:t0 + tw],
                                start=(kc == 0), stop=(kc == KC - 1)), reads=[Bwt, B_hT], writes=[Bpu])
                        P.op("act", lambda e, pg=pg, tw=tw: e.activation(out=sil[:, 0:tw], in_=pg[:, 0:tw], func=AF.Sigmoid),
                             reads=[Bpg], writes=[B_sil])
                        P.op("dve", lambda e, pg=pg, tw=tw: e.tensor_tensor(out=sil[:, 0:tw], in0=sil[:, 0:tw], in1=pg[:, 0:tw], op=ALU.mult),
                             reads=[Bpg, B_sil], writes=[B_sil])
                        P.op("dve", lambda e, pu=pu, t0=t0, tw=tw: e.tensor_tensor(out=ug[:, 0:tw], in0=gbc[:, t0:t0 + tw], in1=pu[:, 0:tw], op=ALU.mult),
                             reads=[Bpu, B_gbc], writes=[B_ug])
                        P.op("pool", lambda e, fc=fc, t0=t0, tw=tw: e.tensor_tensor(out=hid[:, fc, t0:t0 + tw], in0=sil[:, 0:tw], in1=ug[:, 0:tw], op=ALU.mult),
                             reads=[B_sil, B_ug], writes=[B_hid])
            for m in range(KC):
                for (t0, tw) in TT:
                    pt, Bp = next_ps("a")
                    for fc in range(4):
                        P.op("pe", lambda e, pt=pt, fc=fc, wdt=wdt, m=m, t0=t0, tw=tw: e.matmul(
                            pt[:, 0:tw], lhsT=wdt[:, fc, m * 128:(m + 1) * 128], rhs=hid[:, fc, t0:t0 + tw], start=(fc == 0), stop=(fc == 3)),
                            reads=[Bwd, B_hid], writes=[Bp])
                    P.op("dve", lambda e, pt=pt, m=m, t0=t0, tw=tw: e.tensor_tensor(out=zacc[:, m, t0:t0 + tw], in0=zacc[:, m, t0:t0 + tw],
                                                                                   in1=pt[:, 0:tw], op=ALU.add), reads=[Bp, B_zacc], writes=[B_zacc])
        P.barrier()

    L["pools"]["a"] = [0, 1, 2, 3, 4]
    L["pools"]["b"] = [5, 6, 7]
    def after_ln2(m):
        P.dma("sp", lambda e, m=m: e.dma_start(out=y_out[m * 128:(m + 1) * 128, :], in_=zacc[:, m, :]), reads=[B_zacc])
    layer_norm(2, 3, after_ln2)
    P.barrier()


def nsa_sample(nc, P, sc, sbt, next_ps, L):
    debug = L["debug"]
    xf = L["xf"]
    ident, B_ident, identb, B_identb = L["ident"], L["B_ident"], L["identb"], L["B_identb"]
    onesb, B_onesb = L["onesb"], L["B_onesb"]
    o_nsaT, B_onsaT = L["o_nsaT"], L["B_onsaT"]
    cache2d = L["cache2d"]
    SB0 = NB * 128
    SC0 = NOWN * 128

    def table(name, shape, dt, src, q="sp"):
        t, B = sbt(sc, "T" + name, shape, dt)
        P.dma(q, lambda e: e.dma_start(out=t[:], in_=src), writes=[B])
        return t, B
    cb, B_cb = table("s_cb", [128, 64], F32, L["s_cb"])
    bs, B_bs = table("s_bs", [128, 16 * 65], F32, L["s_bs"])
    sq, B_sq = table("s_sq", [1, 512], F32, L["s_sq"])
    lnt, B_lnt = table("s_lnt", [2, 128], BF16, L["s_lnt"])
    R2, B_R2 = table("s_R2", [2, 512], BF16, L["t_r2"])
    Et, B_Et = table("s_Et", [64, NB * 128], BF16, L["t_E"])
    caus, B_caus = table("s_caus", [128, 128], BF16, L["s_caus"])
    low, B_low = table("s_low", [128, 128], BF16, L["s_low"])
    keep, B_keep = table("s_keep", [128, 128], F32, L["s_keep"])
    force, B_force = table("s_force", [128, 128], F32, L["s_force"])
    iot, B_iot = table("s_iot", [128, 1], F32, L["s_iota"])
    ptb, B_ptb = sbt(sc, "s_ptb", [128, 256], mybir.dt.int32)
    P.dma("sp", lambda e: e.dma_start(out=ptb[:], in_=L["pt_core"].partition_broadcast(128)), writes=[B_ptb])
    ptf, B_ptf = sbt(sc, "s_ptf", [128, 256], F32)
    idx, B_idx = sbt(sc, "s_idx", [128, 256], mybir.dt.int32)
    P.op("dve", lambda e: e.tensor_copy(out=ptf[:], in_=ptb[:]), reads=[B_ptb], writes=[B_ptf])
    P.op("dve", lambda e: e.tensor_scalar(out=ptf[:], in0=ptf[:], scalar1=128.0, scalar2=iot[:, 0:1], op0=ALU.mult, op1=ALU.add),
         reads=[B_ptf, B_iot], writes=[B_ptf])
    P.op("dve", lambda e: e.tensor_scalar(out=ptf[:], in0=ptf[:], scalar1=2.0, scalar2=None, op0=ALU.mult), reads=[B_ptf], writes=[B_ptf])
    P.op("dve", lambda e: e.tensor_copy(out=idx[:], in_=ptf[:]), reads=[B_ptf], writes=[B_idx])
    idx1, B_idx1 = sbt(sc, "s_idx1", [128, 256], mybir.dt.int32)
    P.op("dve", lambda e: e.tensor_scalar(out=ptf[:], in0=ptf[:], scalar1=1.0, scalar2=None, op0=ALU.add), reads=[B_ptf], writes=[B_ptf])
    P.op("dve", lambda e: e.tensor_copy(out=idx1[:], in_=ptf[:]), reads=[B_ptf], writes=[B_idx1])

    qT, B_qT = sbt(sc, "s_qT", [128, 8, 128], BF16)
    gates, B_gates = sbt(sc, "s_gates", [128, 48], F32)
    knew, B_knew = sbt(sc, "s_knew", [128, 2, 2, 128], BF16)
    vnew, B_vnew = sbt(sc, "s_vnew", [128, 2, 4, 65], BF16)
    vnj, B_vnj = sbt(sc, "s_vnj", [4, 4, 2, 4, 65], BF16)
    P.op("dve", lambda e: e.memset(vnew[:, :, :, 64:65], 1.0), writes=[B_vnew])
    pq = contextlib.ExitStack()
    with pq:
        wq, B_wq = sbt(pq, "s_wq", [128, KC, 1024], BF16)
        wng, B_wng = sbt(pq, "s_wng", [128, KC, 48], BF16)
        wk, B_wk = sbt(pq, "s_wk", [128, KC, 1024], BF16)
        bq_col, B_bq = sbt(pq, "s_bq", [128, 8], F32)
        bk_col, B_bk = sbt(pq, "s_bk", [128, 12], F32)
        bk_bc, B_bkbc = sbt(pq, "s_bkbc", [128, KV_W], F32)
        bng, B_bng = sbt(pq, "s_bng", [128, 48], F32)
        xb, Bx = sbt(pq, "s_xb", [128, KC, 128], BF16)
        wq_v = L["w_q"].rearrange("(kc p) c -> p kc c", p=128)
        wkv_v = L["w_kv"].rearrange("(kc p) c -> p kc c", p=128)
        xf_v = xf.rearrange("(kc p) t -> p kc t", p=128)
        for q in range(4):
            P.dma("pool", lambda e, q=q: e.dma_start(out=wq[:, 4 * q:4 * q + 4, :], in_=wq_v[:, 4 * q:4 * q + 4, :]), writes=[B_wq])
            P.dma("pool", lambda e, q=q: e.dma_start(out=wk[:, 4 * q:4 * q + 4, :], in_=wkv_v[:, 4 * q:4 * q + 4, 512:1536]), writes=[B_wk])
        P.dma("pool", lambda e: e.dma_start(out=wng[:], in_=L["w_ng"].rearrange("(kc p) c -> p kc c", p=128)), writes=[B_wng])
        P.dma("pool", lambda e: e.dma_start(out=xb[:], in_=xf_v[:, :, SB0:SB0 + 128]), writes=[Bx])
        P.dma("sp", lambda e: e.dma_start(out=bq_col[:], in_=L["b_q"].rearrange("(cb p) -> p cb", p=128), allow_slow_non_contiguous=True),
              writes=[B_bq])
        P.dma("sp", lambda e: e.dma_start(out=bk_col[:], in_=L["b_kv"].rearrange("(cb p) -> p cb", p=128), allow_slow_non_contiguous=True),
              writes=[B_bk])
        P.dma("sp", lambda e: e.dma_start(out=bk_bc[:], in_=L["b_kv"].partition_broadcast(128)), writes=[B_bkbc])
        P.dma("sp", lambda e: e.dma_start(out=bng[:], in_=L["b_ng"].partition_broadcast(128)), writes=[B_bng])
        for cbk in range(8):
            pt, Bp = next_ps("a")
            for kc in range(KC):
                P.op("pe", lambda e, pt=pt, kc=kc, cbk=cbk: e.matmul(pt[:, 0:128], lhsT=wq[:, kc, cbk * 128:(cbk + 1) * 128], rhs=xb[:, kc, :],
                                                                     start=(kc == 0), stop=(kc == KC - 1)), reads=[B_wq, Bx], writes=[Bp])
            P.op("act", lambda e, pt=pt, cbk=cbk: e.activation(out=qT[:, cbk, :], in_=pt[:, 0:128], func=AF.Identity, bias=bq_col[:, cbk:cbk + 1]),
                 reads=[Bp, B_bq], writes=[B_qT])
        for si, (c0, cbb) in enumerate(((0, 4), (512, 8))):
            for gp in range(2):
                pt, Bp = next_ps("a")
                for kc in range(KC):
                    P.op("pe", lambda e, pt=pt, kc=kc, c0=c0, gp=gp: e.matmul(
                        pt[:, 0:128], lhsT=wk[:, kc, c0 + gp * 128:c0 + (gp + 1) * 128], rhs=xb[:, kc, :],
                        start=(kc == 0), stop=(kc == KC - 1)), reads=[B_wk, Bx], writes=[Bp])
                P.op("act", lambda e, pt=pt, si=si, gp=gp, cbb=cbb: e.activation(
                    out=knew[:, si, gp, :], in_=pt[:, 0:128], func=AF.Identity, bias=bk_col[:, cbb + gp:cbb + gp + 1]),
                    reads=[Bp, B_bk], writes=[B_knew])
        pt, Bp = next_ps("a")
        for si, c0 in enumerate((256, 768)):
            for kc in range(KC):
                P.op("pe", lambda e, pt=pt, kc=kc, si=si, c0=c0: e.matmul(pt[:, si * 256:(si + 1) * 256], lhsT=xb[:, kc, :], rhs=wk[:, kc, c0:c0 + 256],
                                                                         start=(kc == 0), stop=(kc == KC - 1)), reads=[B_wk, Bx], writes=[Bp])
        for si, c0 in enumerate((768, 1280)):
            P.op("dve", lambda e, pt=pt, si=si, c0=c0: e.tensor_tensor(
                out=vnew[:, si, :, 0:64], in0=pt[:, si * 256:(si + 1) * 256].rearrange("p (g d) -> p g d", g=4),
                in1=bk_bc[:, c0:c0 + 256].rearrange("p (g d) -> p g d", g=4), op=ALU.add), reads=[Bp, B_bkbc], writes=[B_vnew])
        for j in range(4):
            P.dma("sp", lambda e, j=j: e.dma_start(out=vnj[:, j, :, :, :], in_=vnew[32 * j:32 * j + 4, :, :, :]), reads=[B_vnew], writes=[B_vnj])
        pt, Bp = next_ps("b")
        for kc in range(KC):
            P.op("pe", lambda e, pt=pt, kc=kc: e.matmul(pt[:, 0:48], lhsT=xb[:, kc, :], rhs=wng[:, kc, :], start=(kc == 0), stop=(kc == KC - 1)),
                 reads=[B_wng, Bx], writes=[Bp])
        P.op("dve", lambda e, pt=pt: e.tensor_tensor(out=gates[:, :], in0=pt[:, 0:48], in1=bng[:, :], op=ALU.add), reads=[Bp, B_bng], writes=[B_gates])
        P.op("act", lambda e: e.activation(out=gates[:], in_=gates[:], func=AF.Sigmoid), reads=[B_gates], writes=[B_gates])
        P.barrier()

    kcs, B_kcs = sbt(sc, "s_kc", [128, 2, 512], BF16)
    vca, B_vca = sbt(sc, "s_vca", [128, 4, 4, 193], BF16)
    P.op("dve", lambda e: e.memset(vca[:, :, :, 64:65], 1.0), writes=[B_vca])
    for c in range(4):
        for g in range(4):
            P.dma("sp", lambda e, c=c, g=g: e.dma_start(out=vca[:, c, g, 65:193], in_=L["s_ovl"][:, c * 128:(c + 1) * 128]), writes=[B_vca])
    o_s, B_os = sbt(sc, "s_os", [128, 4, 256], F32)
    P.op("dve", lambda e: e.memset(o_s[:], 0.0), writes=[B_os])
    pg = [sbt(sc, "s_pg%d" % i, [128, 512], F32) for i in range(3)]
    pef, B_pef = sbt(sc, "s_pef", [128, 2, 16], F32)
    peb, B_peb = sbt(sc, "s_peb", [128, 2, 16], BF16)
    w2k, B_w2k = sbt(sc, "s_w2k", [128, 128], BF16)
    w2v, B_w2v = sbt(sc, "s_w2v", [128, 64], BF16)
    pre0, B_pre0 = sbt(sc, "s_pre0", [128, 2], F32)
    ug, B_ug = sbt(sc, "s_ug", [128, 512], F32)
    tg, B_tg = sbt(sc, "s_tg", [128, 512], F32)
    Gb, B_Gb = sbt(sc, "s_Gb", [128, 512], BF16)
    w_c1, w_c2, c_pe = L["w_c1"], L["w_c2"], L["c_pe"]
    P.dma("sp", lambda e: e.dma_start(out=pef[:], in_=c_pe.rearrange("c (jc j2) d -> (j2 d) c jc", j2=2), allow_slow_non_contiguous=True),
          writes=[B_pef])
    P.op("dve", lambda e: e.tensor_copy(out=peb[:], in_=pef[:]), reads=[B_pef], writes=[B_peb])
    P.op("dve", lambda e: e.memset(w2k[:, 0:64], 0.0), writes=[B_w2k])
    P.dma("pool", lambda e: e.dma_start(out=w2k[:, 64:128], in_=w_c2[0]), writes=[B_w2k])
    P.dma("pool", lambda e: e.dma_start(out=w2v[:], in_=w_c2[1]), writes=[B_w2v])
    pT = [sbt(sc, "s_pT%d" % i, [128, 4, 128], BF16) for i in range(4)]
    pT_rr = [0]

    def next_pT():
        k = pT_rr[0] % len(pT)
        pT_rr[0] += 1
        return pT[k]
    qsq, B_qsq = sbt(sc, "s_qsq", [128, 512], BF16)
    o_blk, B_oblk = sbt(sc, "s_oblk", [128, 256], F32)
    rs, B_rs = sbt(sc, "s_rs", [128, 4], F32)
    wgt, B_wgt = sbt(sc, "s_wgt", [128, 4], F32)
    imp, B_imp = sbt(sc, "s_imp", [128, 128], F32)
    imp3, B_imp3 = sbt(sc, "s_imp3", [128, 128], F32)
    m8, B_m8 = sbt(sc, "s_m8", [128, 16], F32)
    nsel, B_nsel = sbt(sc, "s_nsel", [128, 128], F32)
    nselT = [sbt(sc, "s_nselT%d" % i, [64, 512], BF16) for i in range(2)]
    sqt, B_sqt = sbt(sc, "s_sqt", [128, 512], BF16)
    runmax, B_runmax = sbt(sc, "s_runmax", [1, 512], F32)
    nkm, B_nkm = sbt(sc, "s_nkm", [1, 1], F32)

    def softmax_chunk(pt, Bp, w, hbase, col_fn, tab, B_tab):
        (t, Bt0) = next_pT()
        Bt = _HB.setdefault(id(Bt0), [Buf("h%d" % i) for i in range(4)])
        for hh in range(4):
            P.op("act", lambda e, pt=pt, t=t, hh=hh, w=w: e.activation(
                out=t[:w, hh, 0:32], in_=pt[:w, hh * 32:(hh + 1) * 32], func=AF.Exp, scale=SCALE,
                bias=tab[:w, col_fn(hbase + hh):col_fn(hbase + hh) + 1]), reads=[Bp, B_tab], writes=[Bt[hh]])
        return t, Bt

    def finish_branch(psO, BpO, g, br, first, gj, B_gj):
        P.op("dve", lambda e: e.tensor_scalar(out=rs[0:32, :], in0=psO[0:32, 0:260].rearrange("p (h c) -> p h c", c=65)[:, :, 64],
                                              scalar1=1e-30, scalar2=None, op0=ALU.max), reads=[BpO], writes=[B_rs])
        P.op("dve", lambda e: e.reciprocal(out=rs[0:32, :], in_=rs[0:32, :]), reads=[B_rs], writes=[B_rs])
        P.op("dve", lambda e: e.tensor_tensor(out=wgt[0:32, :], in0=rs[0:32, :], in1=gj[0:32, br * 16 + g * 4:br * 16 + g * 4 + 4], op=ALU.mult),
             reads=[B_rs, B_gj], writes=[B_wgt])
        for hh in range(4):
            oc = slice(hh * 64, hh * 64 + 64)
            if first:
                P.op("dve", lambda e, hh=hh, oc=oc: e.tensor_scalar(out=o_blk[0:32, oc], in0=psO[0:32, hh * 65:hh * 65 + 64],
                                                                    scalar1=wgt[0:32, hh:hh + 1], scalar2=None, op0=ALU.mult),
                     reads=[BpO, B_wgt], writes=[B_oblk])
            else:
                P.op("dve", lambda e, hh=hh, oc=oc: e.scalar_tensor_tensor(
                    out=o_blk[0:32, oc], in0=psO[0:32, hh * 65:hh * 65 + 64], scalar=wgt[0:32, hh:hh + 1], in1=o_blk[0:32, oc],
                    op0=ALU.mult, op1=ALU.add), reads=[BpO, B_wgt, B_oblk], writes=[B_oblk])

    def gather(j, c, half, dst, Bdst):
        col = j * 64 + c
        ix, Bix = (idx, B_idx) if half == 0 else (idx1, B_idx1)
        P.dma("pool", lambda e, col=col, ix=ix, dst=dst: e.indirect_dma_start(
            out=dst[:, :], out_offset=None, in_=cache2d[:, :],
            in_offset=bass.IndirectOffsetOnAxis(ap=ix[:, col:col + 1], axis=0)), reads=[Bix], writes=[Bdst])

    for j in range(4):
        if _LIM < 6.5 and j not in _JSEL:
            continue
        jb = contextlib.ExitStack()
        with jb:
            pa = jb.enter_context(contextlib.ExitStack())
            kcmp, B_kcmp = sbt(pa, "s_kcmp", [128, 2, NCH * 128], BF16)
            vcmp, B_vcmp = sbt(pa, "s_vcmp", [128, 2, NCH * 128], BF16)
            w1d, B_w1d = sbt(pa, "s_w1d", [128, 2, 32, 128], BF16)
            w1f, B_w1f = sbt(pa, "s_w1f", [128, 2, 16, 128], BF16)
            for half in range(2):
                P.dma("pool", lambda e, half=half, w1d=w1d: e.dma_start(out=w1d[64 * half:64 * half + 64, :, :, :], in_=w_c1.rearrange("c j d h -> d c j h")),
                      writes=[B_w1d])
            P.dma("pool", lambda e, w1f=w1f: e.dma_start(out=w1f[:], in_=w_c1.rearrange("c (jc j2) d h -> (j2 d) c jc h", j2=2)), writes=[B_w1f])
            pt, Bp = next_ps("b")
            for c in range(2):
                for jc in range(16):
                    P.op("pe", lambda e, pt=pt, c=c, jc=jc, w1f=w1f: e.matmul(pt[:, c:c + 1], lhsT=w1f[:, c, jc, :], rhs=peb[:, c, jc:jc + 1],
                                                                              start=(jc == 0), stop=(jc == 15)), reads=[B_w1f, B_peb], writes=[Bp])
            P.op("dve", lambda e, pt=pt: e.tensor_copy(out=pre0[:], in_=pt[:, 0:2]), reads=[Bp], writes=[B_pre0])
            for c in range(NCH):
                pgt, Bpg = pg[c % 3]
                gather(j, c, 0, pgt, Bpg)
                for q in range(4):
                    dst, Bd = (kcmp, B_kcmp) if q < 2 else (vcmp, B_vcmp)
                    pt, Bp = next_ps("a")
                    P.op("pe", lambda e, pt=pt, q=q, pgt=pgt: e.transpose(out=pt[:, 0:128], in_=pgt[:, q * 128:(q + 1) * 128],
                                                                          identity=ident[:, :]), reads=[Bpg, B_ident], writes=[Bp])
                    eng = "act" if q % 2 == 0 else "dve"
                    if eng == "act":
                        P.op("act", lambda e, pt=pt, c=c, q=q, dst=dst: e.activation(out=dst[:, q % 2, c * 128:(c + 1) * 128], in_=pt[:, 0:128],
                                                                                      func=AF.Identity), reads=[Bp], writes=[Bd])
                    else:
                        P.op("dve", lambda e, pt=pt, c=c, q=q, dst=dst: e.tensor_copy(out=dst[:, q % 2, c * 128:(c + 1) * 128], in_=pt[:, 0:128]),
                             reads=[Bp], writes=[Bd])
            for c in range(2):
                src, Bsrc = (kcmp, B_kcmp) if c == 0 else (vcmp, B_vcmp)
                for g in range(4):
                    gp, g2 = g // 2, g % 2
                    hs = slice(64 * g2, 64 * g2 + 64)
                    pt, Bp = next_ps("a")
                    for jj in range(32):
                        P.op("pe", lambda e, pt=pt, c=c, jj=jj, hs=hs, gp=gp, src=src, w1d=w1d: e.matmul(
                            pt[:, 0:511], lhsT=w1d[hs, c, jj, :], rhs=src[hs, gp, jj:jj + 16 * 510 + 1:16],
                            start=(jj == 0), stop=(jj == 31)), reads=[B_w1d, Bsrc], writes=[Bp])
                    P.op("act", lambda e, pt=pt, c=c: e.activation(out=ug[:, 0:511], in_=pt[:, 0:511], func=AF.Identity, bias=pre0[:, c:c + 1]),
                         reads=[Bp, B_pre0], writes=[B_ug])
                    P.op("dve", lambda e: e.tensor_tensor(out=tg[:, 0:511], in0=ug[:, 0:511], in1=ug[:, 0:511], op=ALU.mult), reads=[B_ug], writes=[B_tg])
                    P.op("dve", lambda e: e.tensor_scalar(out=tg[:, 0:511], in0=tg[:, 0:511], scalar1=0.044715, scalar2=1.0, op0=ALU.mult, op1=ALU.add),
                         reads=[B_tg], writes=[B_tg])
                    P.op("dve", lambda e: e.tensor_tensor(out=tg[:, 0:511], in0=tg[:, 0:511], in1=ug[:, 0:511], op=ALU.mult), reads=[B_tg, B_ug], writes=[B_tg])
                    P.op("act", lambda e: e.activation(out=tg[:, 0:511], in_=tg[:, 0:511], func=AF.Sigmoid, scale=1.5957691216057308),
                         reads=[B_tg], writes=[B_tg])
                    P.op("dve", lambda e: e.tensor_tensor(out=Gb[:, 0:511], in0=tg[:, 0:511], in1=ug[:, 0:511], op=ALU.mult), reads=[B_tg, B_ug], writes=[B_Gb])
                    if c == 0:
                        pt2, Bp2 = next_ps("b")
                        if g2 == 0:
                            P.op("pe", lambda e, pt2=pt2: e.matmul(pt2[0:64, 0:511], lhsT=w2k[:, 64:128], rhs=Gb[:, 0:511], start=True, stop=True),
                                 reads=[B_w2k, B_Gb], writes=[Bp2])
                        else:
                            P.op("pe", lambda e, pt2=pt2: e.matmul(pt2[:, 0:511], lhsT=w2k[:, :], rhs=Gb[:, 0:511], start=True, stop=True),
                                 reads=[B_w2k, B_Gb], writes=[Bp2])
                        P.op("dve", lambda e, pt2=pt2, hs=hs, gp=gp: e.tensor_copy(out=kcs[hs, gp, 0:511], in_=pt2[hs, 0:511]), reads=[Bp2], writes=[B_kcs])
                    else:
                        pt2, Bp2 = next_ps("b")
                        for ch in range(4):
                            w = 128 if ch < 3 else 127
                            P.op("pe", lambda e, pt2=pt2, ch=ch, w=w: e.matmul(pt2[:w, ch * 64:(ch + 1) * 64], lhsT=Gb[:, ch * 128:ch * 128 + w],
                                                                               rhs=w2v[:, :], start=True, stop=True), reads=[B_w2v, B_Gb], writes=[Bp2])
                        for ch in range(4):
                            w = 128 if ch < 3 else 127
                            P.op("dve", lambda e, pt2=pt2, ch=ch, w=w, g=g: e.tensor_copy(out=vca[:w, ch, g, 0:64], in_=pt2[:w, ch * 64:(ch + 1) * 64]),
                                 reads=[Bp2], writes=[B_vca])
            P.op("dve", lambda e: e.memset(kcs[:, :, 511:512], 0.0), writes=[B_kcs])
            P.barrier()
            pa.close()

            kslc, B_kslc = sbt(jb, "s_kslc", [128, 2, NCH * 128], BF16)
            vslc, B_vslc = sbt(jb, "s_vslc", [128, NCH, 4, 65], BF16)
            kwin, B_kwin = sbt(jb, "s_kwin", [128, 2, 512], BF16)
            vwin, B_vwin = sbt(jb, "s_vwin", [128, 4, 4, 65], BF16)
            sqj, B_sqj = sbt(jb, "s_sqj", [1, 512], F32)
            kslc_g, B_kslcg = sbt(jb, "s_kslcg", [64, (NCH + 1) * 128], BF16)
            kwin_g, B_kwing = sbt(jb, "s_kwing", [64, 5 * 128], BF16)
            kc_g, B_kcg = sbt(jb, "s_kcg", [64, 512], BF16)
            q_g, B_qg = sbt(jb, "s_qg", [64, 4, 128], BF16)
            gj, B_gj = sbt(jb, "s_gj", [32, 48], F32)
            P.dma("sp", lambda e, j=j, gj=gj: e.dma_start(out=gj[:, :], in_=gates[32 * j:32 * j + 32, :]), reads=[B_gates], writes=[B_gj])
            P.op("dve", lambda e: e.memset(vslc[:, :, :, 64:65], 1.0), writes=[B_vslc])
            P.op("dve", lambda e: e.memset(vwin[:, :, :, 64:65], 1.0), writes=[B_vwin])
            for c in range(NCH):
                pgt, Bpg = pg[c % 3]
                gather(j, c, 1, pgt, Bpg)
                for q in range(2):
                    pt, Bp = next_ps("a")
                    P.op("pe", lambda e, pt=pt, q=q, pgt=pgt: e.transpose(out=pt[:, 0:128], in_=pgt[:, q * 128:(q + 1) * 128],
                                                                          identity=ident[:, :]), reads=[Bpg, B_ident], writes=[Bp])
                    P.op("act", lambda e, pt=pt, c=c, q=q: e.activation(out=kslc[:, q, c * 128:(c + 1) * 128], in_=pt[:, 0:128], func=AF.Identity),
                         reads=[Bp], writes=[B_kslc])
                P.op("dve", lambda e, pgt=pgt, c=c: e.tensor_copy(out=vslc[:, c, :, 0:64], in_=pgt[:, 256:512].rearrange("p (g d) -> p g d", g=4)),
                     reads=[Bpg], writes=[B_vslc])
            for wch in range(4):
                pgt, Bpg = pg[wch % 3]
                P.dma("sp", lambda e, pgt=pgt, j=j, wch=wch: e.dma_start(out=pgt[:, :], in_=L["cw_in"][j, wch * 128:(wch + 1) * 128, :]), writes=[Bpg])
                for q in range(2):
                    pt, Bp = next_ps("a")
                    P.op("pe", lambda e, pt=pt, q=q, pgt=pgt: e.transpose(out=pt[:, 0:128], in_=pgt[:, q * 128:(q + 1) * 128],
                                                                          identity=ident[:, :]), reads=[Bpg, B_ident], writes=[Bp])
                    P.op("act", lambda e, pt=pt, wch=wch, q=q: e.activation(out=kwin[:, q, wch * 128:(wch + 1) * 128], in_=pt[:, 0:128], func=AF.Identity),
                         reads=[Bp], writes=[B_kwin])
                P.op("dve", lambda e, pgt=pgt, wch=wch: e.tensor_copy(out=vwin[:, wch, :, 0:64], in_=pgt[:, 256:512].rearrange("p (g d) -> p g d", g=4)),
                     reads=[Bpg], writes=[B_vwin])

            P.op("dve", lambda e: e.memset(runmax[:], 0.0), writes=[B_runmax])
            srcs = []
            for gp in range(2):
                for s_ in range(NCH * 128 // 512):
                    srcs.append((kslc, B_kslc, lambda gp=gp, s_=s_: kslc[:, gp, s_ * 512:(s_ + 1) * 512], 512))
                srcs.append((kwin, B_kwin, lambda gp=gp: kwin[:, gp, :], 512))
                srcs.append((kcs, B_kcs, lambda gp=gp: kcs[:, gp, :], 512))
                srcs.append((knew, B_knew, lambda gp=gp: knew[:, 0, gp, :], 128))
                srcs.append((knew, B_knew, lambda gp=gp: knew[:, 1, gp, :], 128))
            for (src, Bs, apf, w) in srcs:
                P.op("dve", lambda e, apf=apf, w=w: e.tensor_tensor(out=sqt[:, 0:w], in0=apf(), in1=apf(), op=ALU.mult), reads=[Bs], writes=[B_sqt])
                pt, Bp = next_ps("b")
                P.op("pe", lambda e, pt=pt, w=w: e.matmul(pt[0:1, 0:w], lhsT=onesb[:, 0:1], rhs=sqt[:, 0:w], start=True, stop=True),
                     reads=[B_onesb, B_sqt], writes=[Bp])
                P.op("dve", lambda e, pt=pt, w=w: e.tensor_tensor(out=runmax[:, 0:w], in0=runmax[:, 0:w], in1=pt[0:1, 0:w], op=ALU.max),
                     reads=[Bp, B_runmax], writes=[B_runmax])
            P.op("dve", lambda e: e.reduce_max(out=nkm[:], in_=runmax[:], axis=mybir.AxisListType.X), reads=[B_runmax], writes=[B_nkm])
            P.op("dve", lambda e: e.tensor_scalar(out=nkm[:], in0=nkm[:], scalar1=-0.5, scalar2=None, op0=ALU.mult), reads=[B_nkm], writes=[B_nkm])
            P.op("dve", lambda e: e.tensor_scalar(out=sqj[:, :], in0=sq[:, :], scalar1=nkm[0:1, 0:1], scalar2=None, op0=ALU.add),
                 reads=[B_nkm, B_sq], writes=[B_sqj])

            for g in range(4):
                gp, g2 = g // 2, g % 2
                hs0 = slice(64 * g2, 64 * g2 + 64)
                P.dma("sp", lambda e, hs0=hs0, gp=gp: e.dma_start(out=kslc_g[:, 0:NCH * 128], in_=kslc[hs0, gp, :]), reads=[B_kslc], writes=[B_kslcg])
                P.dma("sp", lambda e, hs0=hs0, gp=gp, j=j: e.dma_start(out=kslc_g[:, NCH * 128:NCH * 128 + 4], in_=knew[hs0, 0, gp, 32 * j:32 * j + 4]),
                      reads=[B_knew], writes=[B_kslcg])
                P.dma("sp", lambda e, hs0=hs0, gp=gp: e.dma_start(out=kwin_g[:, 0:512], in_=kwin[hs0, gp, :]), reads=[B_kwin], writes=[B_kwing])
                P.dma("sp", lambda e, hs0=hs0, gp=gp, j=j: e.dma_start(out=kwin_g[:, 512:516], in_=knew[hs0, 1, gp, 32 * j:32 * j + 4]),
                      reads=[B_knew], writes=[B_kwing])
                P.dma("sp", lambda e, hs0=hs0, gp=gp: e.dma_start(out=kc_g[:, :], in_=kcs[hs0, gp, :]), reads=[B_kcs], writes=[B_kcg])
                P.dma("sp", lambda e, hs0=hs0, gp=gp: e.dma_start(out=q_g[:, :, :], in_=qT[hs0, gp * 4:gp * 4 + 4, :]), reads=[B_qT], writes=[B_qg])
                hs = slice(0, 64)
                qv = q_g[:, :, 32 * j:32 * j + 32]
                P.op("dve", lambda e, qv=qv: e.tensor_tensor(out=qsq[hs, 0:128].rearrange("p (h q) -> p h q", h=4), in0=qv, in1=qv, op=ALU.mult),
                     reads=[B_qg], writes=[B_qsq])
                pt, Bp = next_ps("b")
                P.op("pe", lambda e, pt=pt: e.matmul(pt[0:1, 0:128], lhsT=onesb[hs, 0:1], rhs=qsq[hs, 0:128], start=True, stop=True),
                     reads=[B_onesb, B_qsq], writes=[Bp])
                P.op("dve", lambda e, pt=pt, g=g: e.scalar_tensor_tensor(out=R2[0:1, 0:128], in0=pt[0:1, 0:128], scalar=-0.5,
                                                                         in1=sqj[0:1, g * 128:(g + 1) * 128], op0=ALU.mult, op1=ALU.add),
                     reads=[Bp, B_sqj], writes=[B_R2])
                pts = []
                for c in range(4):
                    w = 128 if c < 3 else 127
                    pt, Bp = next_ps("a")
                    P.op("pe", lambda e, pt=pt, c=c, w=w, qv=qv: e.matmul(pt[:w, 0:128], lhsT=kc_g[hs, c * 128:c * 128 + w], rhs=qv, start=True, stop=False),
                         reads=[B_kcg, B_qg], writes=[Bp])
                    P.op("pe", lambda e, pt=pt, w=w: e.matmul(pt[:w, 0:128], lhsT=lnt[0:1, 0:w], rhs=R2[0:1, 0:128], start=False, stop=True),
                         reads=[B_lnt, B_R2], writes=[Bp])
                    t, Bt = softmax_chunk(pt, Bp, w, 4 * g, lambda h, c=c: h * 4 + c, cb, B_cb)
                    pts.append((t, Bt, w))
                psO, BpO = next_ps("b")
                psI, BpI = next_ps("b")
                for hh in range(4):
                    for c, (t, Bt, w) in enumerate(pts):
                        P.op("pe", lambda e, hh=hh, c=c, t=t, w=w, g=g, psO=psO: e.matmul(
                            psO[0:32, hh * 65:(hh + 1) * 65], lhsT=t[:w, hh, 0:32], rhs=vca[:w, c, g, 0:65], start=(c == 0), stop=(c == 3)),
                            reads=[Bt[hh], B_vca], writes=[BpO])
                for hh in range(4):
                    for c, (t, Bt, w) in enumerate(pts):
                        P.op("pe", lambda e, hh=hh, c=c, t=t, w=w, g=g, psI=psI: e.matmul(
                            psI[0:32, hh * 128:(hh + 1) * 128], lhsT=t[:w, hh, 0:32], rhs=vca[:w, c, g, 65:193], start=(c == 0), stop=(c == 3)),
                            reads=[Bt[hh], B_vca], writes=[BpI])
                finish_branch(psO, BpO, g, 0, True, gj, B_gj)
                for hh in range(4):
                    if hh == 0:
                        P.op("dve", lambda e, psI=psI: e.tensor_scalar(out=imp[0:32, :], in0=psI[0:32, 0:128], scalar1=rs[0:32, 0:1], scalar2=None, op0=ALU.mult),
                             reads=[BpI, B_rs], writes=[B_imp])
                    else:
                        P.op("dve", lambda e, hh=hh, psI=psI: e.scalar_tensor_tensor(
                            out=imp[0:32, :], in0=psI[0:32, hh * 128:(hh + 1) * 128], scalar=rs[0:32, hh:hh + 1], in1=imp[0:32, :], op0=ALU.mult, op1=ALU.add),
                            reads=[BpI, B_rs, B_imp], writes=[B_imp])
                P.op("dve", lambda e: e.tensor_scalar(out=imp[0:32, :], in0=imp[0:32, :], scalar1=1e-30, scalar2=None, op0=ALU.max), reads=[B_imp], writes=[B_imp])
                P.op("dve", lambda e: e.tensor_tensor(out=imp[0:32, :], in0=imp[0:32, :], in1=keep[0:32, :], op=ALU.mult), reads=[B_imp, B_keep], writes=[B_imp])
                P.op("dve", lambda e: e.tensor_tensor(out=imp[0:32, :], in0=imp[0:32, :], in1=force[0:32, :], op=ALU.add), reads=[B_imp, B_force], writes=[B_imp])
                P.op("dve", lambda e: e.max(out=m8[0:32, 0:8], in_=imp[0:32, :]), reads=[B_imp], writes=[B_m8])
                P.op("dve", lambda e: e.tensor_scalar(out=imp3[0:32, :], in0=imp[0:32, :], scalar1=m8[0:32, 7:8], scalar2=None, op0=ALU.is_ge),
                     reads=[B_imp, B_m8], writes=[B_imp3])
                P.op("dve", lambda e: e.scalar_tensor_tensor(out=imp3[0:32, :], in0=imp3[0:32, :], scalar=-3.0e38, in1=imp[0:32, :], op0=ALU.mult, op1=ALU.add),
                     reads=[B_imp, B_imp3], writes=[B_imp3])
                P.op("dve", lambda e: e.max(out=m8[0:32, 8:16], in_=imp3[0:32, :]), reads=[B_imp3], writes=[B_m8])
                P.op("dve", lambda e: e.tensor_scalar(out=nsel[0:32, :], in0=imp[0:32, :], scalar1=m8[0:32, 14:15], scalar2=None, op0=ALU.is_ge),
                     reads=[B_imp, B_m8], writes=[B_nsel])
                P.op("dve", lambda e: e.tensor_scalar(out=nsel[0:32, :], in0=nsel[0:32, :], scalar1=-NEGM, scalar2=NEGM, op0=ALU.mult, op1=ALU.add),
                     reads=[B_nsel], writes=[B_nsel])
                for hf in range(2):
                    nt_, Bnt = nselT[hf]
                    pt, Bp = next_ps("b")
                    P.op("pe", lambda e, pt=pt, hf=hf: e.transpose(out=pt[0:64, 0:32], in_=nsel[0:32, hf * 64:(hf + 1) * 64], identity=ident[0:32, 0:32]),
                         reads=[B_nsel, B_ident], writes=[Bp])
                    for hh in range(4):
                        P.op("act", lambda e, pt=pt, hh=hh, nt_=nt_: e.activation(out=nt_[:, hh * 32:(hh + 1) * 32], in_=pt[0:64, 0:32], func=AF.Identity),
                             reads=[Bp], writes=[Bnt])
                psO, BpO = next_ps("b")
                for c in range(NCH + 1):
                    last = c == NCH
                    w = 4 if last else 128
                    pt, Bp = next_ps("a")
                    P.op("pe", lambda e, pt=pt, c=c, w=w, qv=qv: e.matmul(pt[:w, 0:128], lhsT=kslc_g[hs, c * 128:c * 128 + w], rhs=qv, start=True, stop=False),
                         reads=[B_kslcg, B_qg], writes=[Bp])
                    if not last:
                        nt_, Bnt = nselT[c // 32]
                        cc = c % 32
                        P.op("pe", lambda e, pt=pt, cc=cc, nt_=nt_: e.matmul(pt[:, 0:128], lhsT=Et[:, cc * 128:(cc + 1) * 128], rhs=nt_[:, 0:128], start=False, stop=False),
                             reads=[B_Et, Bnt], writes=[Bp])
                    P.op("pe", lambda e, pt=pt, w=w, last=last: e.matmul(pt[:w, 0:128], lhsT=lnt[0:2, 0:w], rhs=R2[0:2, 0:128], start=False, stop=(not last)),
                         reads=[B_lnt, B_R2], writes=[Bp])
                    if last:
                        P.op("pe", lambda e, pt=pt: e.matmul(pt[:4, 0:128], lhsT=identb[:4, :4], rhs=caus[:4, 0:128], start=False, stop=True),
                             reads=[B_identb, B_caus], writes=[Bp])
                    t, Bt = softmax_chunk(pt, Bp, w, 4 * g, lambda h, d=NCH - c: h * 65 + d, bs, B_bs)
                    for hh in range(4):
                        if last:
                            P.op("pe", lambda e, hh=hh, t=t, g=g, j=j, psO=psO: e.matmul(
                                psO[0:32, hh * 65:(hh + 1) * 65], lhsT=t[:4, hh, 0:32], rhs=vnj[0:4, j, 0, g, :], start=False, stop=True),
                                reads=[Bt[hh], B_vnj], writes=[BpO])
                        else:
                            P.op("pe", lambda e, hh=hh, t=t, c=c, g=g, psO=psO: e.matmul(
                                psO[0:32, hh * 65:(hh + 1) * 65], lhsT=t[:, hh, 0:32], rhs=vslc[:, c, g, :], start=(c == 0), stop=False),
                                reads=[Bt[hh], B_vslc], writes=[BpO])
                finish_branch(psO, BpO, g, 1, False, gj, B_gj)
                psO, BpO = next_ps("b")
                for c in range(5):
                    last = c == 4
                    w = 4 if last else 128
                    pt, Bp = next_ps("a")
                    P.op("pe", lambda e, pt=pt, c=c, w=w, qv=qv: e.matmul(pt[:w, 0:128], lhsT=kwin_g[hs, c * 128:c * 128 + w], rhs=qv, start=True, stop=False),
                         reads=[B_kwing, B_qg], writes=[Bp])
                    edge = last or c == 0
                    P.op("pe", lambda e, pt=pt, w=w, edge=edge: e.matmul(pt[:w, 0:128], lhsT=lnt[0:2, 0:w], rhs=R2[0:2, 0:128], start=False, stop=(not edge)),
                         reads=[B_lnt, B_R2], writes=[Bp])
                    if edge:
                        mk, Bmk = (caus, B_caus) if last else (low, B_low)
                        P.op("pe", lambda e, pt=pt, mk=mk, w=w: e.matmul(pt[:w, 0:128], lhsT=identb[:w, :w], rhs=mk[:w, 0:128], start=False, stop=True),
                             reads=[B_identb, Bmk], writes=[Bp])
                    t, Bt = softmax_chunk(pt, Bp, w, 4 * g, lambda h, d=4 - c: h * 65 + d, bs, B_bs)
                    for hh in range(4):
                        if last:
                            P.op("pe", lambda e, hh=hh, t=t, g=g, j=j, psO=psO: e.matmul(
                                psO[0:32, hh * 65:(hh + 1) * 65], lhsT=t[:4, hh, 0:32], rhs=vnj[0:4, j, 1, g, :], start=False, stop=True),
                                reads=[Bt[hh], B_vnj], writes=[BpO])
                        else:
                            P.op("pe", lambda e, hh=hh, t=t, c=c, g=g, psO=psO: e.matmul(
                                psO[0:32, hh * 65:(hh + 1) * 65], lhsT=t[:, hh, 0:32], rhs=vwin[:, c, g, :], start=(c == 0), stop=False),
                                reads=[Bt[hh], B_vwin], writes=[BpO])
                finish_branch(psO, BpO, g, 2, False, gj, B_gj)
                P.dma("sp", lambda e, j=j, g=g: e.dma_start(out=o_s[32 * j:32 * j + 4, g, :], in_=o_blk[0:4, :]),
                      reads=[B_oblk], writes=[B_os])
            P.barrier()
    for g in range(4):
        if debug:
            P.dma("sp", lambda e, g=g: e.dma_start(out=L["d_onsa_s"][:, g * 256:(g + 1) * 256], in_=o_s[:, g, :]), reads=[B_os])
        for jj in range(2):
            pt, Bp = next_ps("a")
            P.op("pe", lambda e, pt=pt, g=g, jj=jj: e.transpose(out=pt[:, 0:128], in_=o_s[:, g, jj * 128:(jj + 1) * 128], identity=ident[:, :]),
                 reads=[B_os, B_ident], writes=[Bp])
            P.op("dve", lambda e, pt=pt, g=g, jj=jj: e.tensor_copy(out=o_nsaT[:, 2 * g + jj, SC0:SC0 + 128], in_=pt[:, 0:128]),
                 reads=[Bp], writes=[B_onsaT])
    P.barrier()


NEGM = -30000.0
_LIM = 99
_NEXP = 32
_KSEL = [(0, 0)]
_JSEL = [0]
_HB = {}
SCALE = 0.125
NB = 32
OWN0 = 24
NOWN = 8
XF_T = (NB + 1) * 128
QOFF, NGOFF, HQOFF, HGOFF, MGOFF = 0, 2560, 2608, 5680, 6704
SLOPES = [2.0 ** (-8.0 * (h + 1) / 16) for h in range(16)]
NT = (NOWN + 1) * 128
TT = [(0, 512), (512, 512), (1024, 128)]
ALPHA = 2.0 ** 0.25
LN_EPS = 1e-5
N_EXPERTS = 32
NCH = 64
N_PHYS = 2560


def build_nc(debug=False):
    nc = bass.Bass("TRN2", target_bir_lowering=False)
    P = Prog()

    def din(name, shape, dt=F32):
        return nc.dram_tensor(name, list(shape), dt, kind="ExternalInput").ap()

    def dout(name, shape, dt=F32):
        return nc.dram_tensor(name, list(shape), dt, kind="ExternalOutput").ap()

    xf = din("xf", [D_MODEL, XF_T])
    xTb = din("xTb", [D_MODEL, SEQ])
    w_kv = din("w_kv", [D_MODEL, KV_W])
    b_kv = din("b_kv", [KV_W])
    w_q = din("w_q", [D_MODEL, 1024])
    b_q = din("b_q", [1024])
    w_ng = din("w_ng", [D_MODEL, 48])
    b_ng = din("b_ng", [48])
    w_st = din("w_st", [D_MODEL, 512])
    b_st = din("b_st", [512])
    g_st = din("g_st", [2, 256])
    w_ss = din("w_ss", [D_MODEL, 2048])
    b_ss = din("b_ss", [2048])
    g_ss = din("g_ss", [2, 1024])
    st_in = din("st_in", [4, 8, 128, 128])
    cw_in = din("cw_in", [4, 512, 512])
    c_lm = din("c_lm", [128, 128])
    c_lm4 = din("c_lm4", [4, 4])
    w_c1 = din("w_c1", [2, 32, 64, 128])
    w_c2 = din("w_c2", [2, 128, 64])
    c_pe = din("c_pe", [2, 32, 64])
    t_cb = din("t_cb", [128, 256])
    t_cmask = din("t_cmask", [128, NOWN * 512], BF16)
    t_bs = din("t_bs", [128, 512])
    t_sq = din("t_sq", [1, 2048])
    t_ln = din("t_ln", [2, NB * 128], BF16)
    t_r2 = din("t_r2", [2, 512], BF16)
    t_E = din("t_E", [64, NB * 128], BF16)
    t_caus = din("t_caus", [128, 512], BF16)
    t_low = din("t_low", [128, 512], BF16)
    t_keep = din("t_keep", [128, NOWN * 64])
    t_force = din("t_force", [128, NOWN * 64])
    t_ovl = din("t_ovl", [128, 2 * 64], BF16)
    w_h3 = din("w_h3", [2, D_MODEL, 2048])
    b_h3 = din("b_h3", [2, 2048])
    n_h3 = din("n_h3", [2, 512])
    g_h3 = din("g_h3", [2, 2, 512])
    t_u32 = din("t_u32", [128, 128])
    t_l32 = din("t_l32", [128, 128])
    t_ind4 = din("t_ind4", [128, 4])
    t_vmask = din("t_vmask", [128, NB + 1])
    w_pa = din("w_pa", [1024, D_MODEL])
    w_pb = din("w_pb", [1024, D_MODEL])
    w_mg = din("w_mg", [D_MODEL, 4096])
    b_mg = din("b_mg", [4096])
    w_out = din("w_out", [D_MODEL, D_MODEL])
    ln1_g = din("ln1_g", [D_MODEL])
    ln1_b = din("ln1_b", [D_MODEL])
    ln2_g = din("ln2_g", [D_MODEL])
    ln2_b = din("ln2_b", [D_MODEL])
    w_r = din("w_r", [D_MODEL, 36])
    b_r = din("b_r", [36])
    t_selE = din("t_selE", [32, 32 * 128], BF16)
    n_experts = _NEXP
    w_gate = din("w_gate", [n_experts, D_MODEL, 512])
    w_up = din("w_up", [n_experts, D_MODEL, 512])
    w_down = din("w_down", [n_experts, 512, D_MODEL])
    cache2d = din("cache2d", [N_PHYS * 128 * 2, 512])
    pt_core = din("pt_core", [256], mybir.dt.int32)
    s_cb = din("s_cb", [128, 64])
    s_bs = din("s_bs", [128, 16 * 65])
    s_sq = din("s_sq", [1, 512])
    s_lnt = din("s_lnt", [2, 128], BF16)
    s_caus = din("s_caus", [128, 128], BF16)
    s_low = din("s_low", [128, 128], BF16)
    s_keep = din("s_keep", [128, 128])
    s_force = din("s_force", [128, 128])
    s_ovl = din("s_ovl", [128, 4 * 128], BF16)
    s_iota = din("s_iota", [128, 1])
    t_id = din("t_id", [128, 128])
    t_idb = din("t_idb", [128, 128], BF16)

    kv_out = dout("kv_out", [(NOWN + 1) * 128, KV_W])
    win_s = dout("win_s", [4, 512, 512])
    st_p = dout("st_p", [2, 128, 128])
    st_s = dout("st_s", [4, 8, 128, 128])
    y_out = dout("y_out", [D_MODEL, NT])
    if debug:
        d_onsa = dout("d_onsa", [NOWN * 128, 1024])
        d_kc = dout("d_kc", [128, 2 * 256])
        d_vc = dout("d_vc", [128, 2 * 4 * 129])
        d_imp = dout("d_imp", [NOWN * 4 * 128, 64])
        d_ohg = dout("d_ohg", [NT, 1024])
        d_onsa_s = dout("d_onsa_s", [128, 1024])
        d_h = dout("d_h", [D_MODEL, NT])
        d_gate = dout("d_gate", [32, NT])

    top = contextlib.ExitStack()
    stopped = False
    if True:
      try:
          _names = {}

          def sbt(stack, name, shape, dt=F32):
              n = _names.get(name, 0)
              _names[name] = n + 1
              if n:
                  name = "%s_r%d" % (name, n)
              return stack.enter_context(nc.sbuf_tensor(name, list(shape), dt)), Buf(name)

          ps = [top.enter_context(nc.psum_tensor("ps%d" % i, [128, 512], F32)) for i in range(8)]
          B_ps = [Buf("ps%d" % i) for i in range(8)]
          pools = {"a": [0, 1, 2, 3, 4], "b": [5, 6, 7]}
          rr = {"a": 0, "b": 0}

          def next_ps(pool="a"):
              lst = pools[pool]
              k = lst[rr[pool] % len(lst)]
              rr[pool] += 1
              return ps[k], B_ps[k]

          ones_c, B_ones = sbt(top, "ones_c", [128, 1], F32)
          onesb, B_onesb = sbt(top, "onesb", [128, 128], BF16)
          ident, B_ident = sbt(top, "ident", [128, 128], F32)
          identb, B_identb = sbt(top, "identb", [128, 128], BF16)
          P.op("dve", lambda e: e.memset(ones_c[:], 1.0), writes=[B_ones])
          P.op("dve", lambda e: e.memset(onesb[:], 1.0), writes=[B_onesb])
          P.dma("sp", lambda e: e.dma_start(out=ident[:], in_=t_id), writes=[B_ident])
          P.dma("sp", lambda e: e.dma_start(out=identb[:], in_=t_idb), writes=[B_identb])
          ohT, B_oh = sbt(top, "ohT", [128, 16, (NOWN + 1) * 128], BF16)
          P.op("pool", lambda e: e.memset(ohT[:], 0.0), writes=[B_oh])
          o_nsaT, B_onsaT = ohT[:, 0:8, :], B_oh
          o_hgT, B_ohgT = ohT[:, 8:16, :], B_oh

          st = contextlib.ExitStack()
          with st:
              stage_states(nc, P, st, sbt, next_ps, ones_c, B_ones,
                           dict(xTb=xTb, xf=xf, w_st=w_st, b_st=b_st, g_st=g_st, w_ss=w_ss, b_ss=b_ss, g_ss=g_ss,
                                st_in=st_in, c_lm=c_lm, c_lm4=c_lm4, st_p=st_p, st_s=st_s))
              P.barrier()

          ns = contextlib.ExitStack()
          with ns:
              kslcT, B_kslcT = sbt(ns, "kslcT", [128, 2, NB * 128], BF16)
              kwinT, B_kwinT = sbt(ns, "kwinT", [128, 2, 12 * 128], BF16)
              vslc, B_vslc = sbt(ns, "vslc", [128, NB, 4, 65], BF16)
              vwin, B_vwin = sbt(ns, "vwin", [128, 12, 4, 65], BF16)
              kcT, B_kcT = sbt(ns, "kcT", [128, 2, 256], BF16)
              vca, B_vca = sbt(ns, "vca", [128, 2, 4, 129], BF16)
              P.op("dve", lambda e: e.memset(vslc[:, :, :, 64:65], 1.0), writes=[B_vslc])
              P.op("dve", lambda e: e.memset(vwin[:, :, :, 64:65], 1.0), writes=[B_vwin])
              P.op("dve", lambda e: e.memset(vca[:, :, :, 64:65], 1.0), writes=[B_vca])
              for c in range(2):
                  for g in range(4):
                      P.dma("sp", lambda e, c=c, g=g: e.dma_start(out=vca[:, c, g, 65:129], in_=t_ovl[:, c * 64:(c + 1) * 64]),
                            writes=[B_vca])

              p1 = contextlib.ExitStack()
              with p1:
                  p1a = p1.enter_context(contextlib.ExitStack())
                  kcmpT, B_kcmpT = sbt(p1, "kcmpT", [128, 2, NB * 128], BF16)
                  vcmpT, B_vcmpT = sbt(p1, "vcmpT", [128, 2, NB * 128], BF16)
                  wkv, B_wkv = sbt(p1a, "wkv", [128, KC, KV_W], BF16)
                  bkv_bc, B_bkvbc = sbt(p1a, "bkv_bc", [128, KV_W], F32)
                  bkv_col, B_bkvcol = sbt(p1a, "bkv_col", [128, 12], F32)
                  xs = [sbt(p1a, "xs%d" % i, [128, KC, 256], BF16) for i in range(2)]
                  osb = [sbt(p1a, "osb%d" % i, [128, KV_W], F32) for i in range(2)]
                  wkv_v = w_kv.rearrange("(kc p) c -> p kc c", p=128)
                  for q in range(4):
                      P.dma("pool", lambda e, q=q: e.dma_start(out=wkv[:, 4 * q:4 * q + 4, :], in_=wkv_v[:, 4 * q:4 * q + 4, :]),
                            writes=[B_wkv])
                  P.dma("sp", lambda e: e.dma_start(out=bkv_bc[:], in_=b_kv.partition_broadcast(128)), writes=[B_bkvbc])
                  with nc.allow_non_contiguous_dma(reason="tiny bias column layout"):
                      P.dma("sp", lambda e: e.dma_start(out=bkv_col[:], in_=b_kv.rearrange("(cb p) -> p cb", p=128), allow_slow_non_contiguous=True),
                            writes=[B_bkvcol])
                  xf_v = xf.rearrange("(kc p) t -> p kc t", p=128)
                  nti = 0
                  for tt in range((NB + 1) * 128 // 256 + 1):
                      t0 = tt * 256
                      if t0 >= XF_T:
                          break
                      tw = min(256, XF_T - t0)
                      (xb, Bx) = xs[tt % 2]
                      for q in range(2):
                          P.dma("pool", lambda e, xb=xb, t0=t0, tw=tw, q=q: e.dma_start(
                              out=xb[:, 8 * q:8 * q + 8, 0:tw], in_=xf_v[:, 8 * q:8 * q + 8, t0:t0 + tw]), writes=[Bx])
                      is_prompt = t0 < NB * 128
                      if is_prompt:
                          for slot, dst, Bd, lo in ((0, kcmpT, B_kcmpT, 0), (1, vcmpT, B_vcmpT, 0), (2, kslcT, B_kslcT, 0),
                                                    (4, kwinT, B_kwinT, 20 * 128)):
                              if t0 < lo:
                                  continue
                              for gp in range(2):
                                  cb = slot * 2 + gp
                                  pt, Bp = next_ps("a")
                                  for kc in range(KC):
                                      P.op("pe", lambda e, pt=pt, kc=kc, cb=cb, xb=xb, tw=tw: e.matmul(
                                          pt[:, 0:tw], lhsT=wkv[:, kc, cb * 128:(cb + 1) * 128], rhs=xb[:, kc, 0:tw],
                                          start=(kc == 0), stop=(kc == KC - 1)), reads=[B_wkv, Bx], writes=[Bp])
                                  P.op("act", lambda e, pt=pt, dst=dst, gp=gp, cb=cb, t0=t0, tw=tw, lo=lo: e.activation(
                                      out=dst[:, gp, t0 - lo:t0 - lo + tw], in_=pt[:, 0:tw], func=AF.Identity,
                                      bias=bkv_col[:, cb:cb + 1]), reads=[Bp, B_bkvcol], writes=[Bd])
                      for bi in range(tw // 128):
                          p = (t0 + bi * 128) // 128
                          tl = slice(bi * 128, (bi + 1) * 128)
                          if p < NB:
                              pt, Bp = next_ps("a")
                              for half, c0 in ((0, 768), (1, 1280)):
                                  for kc in range(KC):
                                      P.op("pe", lambda e, pt=pt, kc=kc, xb=xb, tl=tl, half=half, c0=c0: e.matmul(
                                          pt[:, half * 256:(half + 1) * 256], lhsT=xb[:, kc, tl], rhs=wkv[:, kc, c0:c0 + 256],
                                          start=(kc == 0), stop=(kc == KC - 1)), reads=[B_wkv, Bx], writes=[Bp])
                              P.op("dve", lambda e, pt=pt, p=p: e.tensor_tensor(
                                  out=vslc[:, p, :, 0:64], in0=pt[:, 0:256].rearrange("p (g d) -> p g d", g=4),
                                  in1=bkv_bc[:, 768:1024].rearrange("p (g d) -> p g d", g=4), op=ALU.add),
                                  reads=[Bp, B_bkvbc], writes=[B_vslc])
                              if p >= 20:
                                  P.op("dve", lambda e, pt=pt, p=p: e.tensor_tensor(
                                      out=vwin[:, p - 20, :, 0:64], in0=pt[:, 256:512].rearrange("p (g d) -> p g d", g=4),
                                      in1=bkv_bc[:, 1280:1536].rearrange("p (g d) -> p g d", g=4), op=ALU.add),
                                      reads=[Bp, B_bkvbc], writes=[B_vwin])
                          if p >= OWN0:
                              (o, Bo) = osb[nti % 2]
                              nti += 1
                              for cg in range(3):
                                  pt, Bp = next_ps("a")
                                  for kc in range(KC):
                                      P.op("pe", lambda e, pt=pt, kc=kc, xb=xb, tl=tl, cg=cg: e.matmul(
                                          pt[:, :], lhsT=xb[:, kc, tl], rhs=wkv[:, kc, cg * 512:(cg + 1) * 512],
                                          start=(kc == 0), stop=(kc == KC - 1)), reads=[B_wkv, Bx], writes=[Bp])
                                  P.op("dve", lambda e, pt=pt, o=o, cg=cg: e.tensor_tensor(
                                      out=o[:, cg * 512:(cg + 1) * 512], in0=pt[:, :], in1=bkv_bc[:, cg * 512:(cg + 1) * 512],
                                      op=ALU.add), reads=[Bp, B_bkvbc], writes=[Bo])
                              r0 = (p - OWN0) * 128
                              P.dma("sp", lambda e, o=o, r0=r0: e.dma_start(out=kv_out[r0:r0 + 128, :], in_=o[:, :]), reads=[Bo])
                              if p == NB:
                                  for j in range(4):
                                      P.dma("sp", lambda e, o=o, j=j: e.dma_start(
                                          out=win_s[j, 508:512, :], in_=o[32 * j:32 * j + 4, 1024:1536]), reads=[Bo])
                  for j in range(4):
                      P.dma("sp", lambda e, j=j: e.dma_start(out=win_s[j, 0:508, :], in_=cw_in[j, 4:512, :]))

                  P.barrier()
                  p1a.close()
                  if _LIM < 3:
                      raise _Stop()
                  w1d, B_w1d = sbt(p1, "w1d", [128, 2, 32, 128], BF16)
                  w1f, B_w1f = sbt(p1, "w1f", [128, 2, 16, 128], BF16)
                  pef, B_pef = sbt(p1, "pef", [128, 2, 16], F32)
                  peb, B_peb = sbt(p1, "peb", [128, 2, 16], BF16)
                  w2k, B_w2k = sbt(p1, "w2k", [128, 128], BF16)
                  w2v, B_w2v = sbt(p1, "w2v", [128, 64], BF16)
                  pre0, B_pre0 = sbt(p1, "pre0", [128, 2], F32)
                  ug, B_ug = sbt(p1, "ug", [128, 256], F32)
                  tg, B_tg = sbt(p1, "tg", [128, 256], F32)
                  Gb, B_Gb = sbt(p1, "Gb", [128, 256], BF16)
                  for half in range(2):
                      P.dma("pool", lambda e, half=half: e.dma_start(
                          out=w1d[64 * half:64 * half + 64, :, :, :], in_=w_c1.rearrange("c j d h -> d c j h")), writes=[B_w1d])
                  P.dma("pool", lambda e: e.dma_start(out=w1f[:], in_=w_c1.rearrange("c (jc j2) d h -> (j2 d) c jc h", j2=2)),
                        writes=[B_w1f])
                  with nc.allow_non_contiguous_dma(reason="tiny pe layout"):
                      P.dma("sp", lambda e: e.dma_start(out=pef[:], in_=c_pe.rearrange("c (jc j2) d -> (j2 d) c jc", j2=2), allow_slow_non_contiguous=True),
                            writes=[B_pef])
                  P.op("dve", lambda e: e.tensor_copy(out=peb[:], in_=pef[:]), reads=[B_pef], writes=[B_peb])
                  P.op("dve", lambda e: e.memset(w2k[:, 0:64], 0.0), writes=[B_w2k])
                  P.dma("pool", lambda e: e.dma_start(out=w2k[:, 64:128], in_=w_c2[0]), writes=[B_w2k])
                  P.dma("pool", lambda e: e.dma_start(out=w2v[:], in_=w_c2[1]), writes=[B_w2v])
                  pt, Bp = next_ps("b")
                  for c in range(2):
                      for jc in range(16):
                          P.op("pe", lambda e, pt=pt, c=c, jc=jc: e.matmul(
                              pt[:, c:c + 1], lhsT=w1f[:, c, jc, :], rhs=peb[:, c, jc:jc + 1], start=(jc == 0), stop=(jc == 15)),
                              reads=[B_w1f, B_peb], writes=[Bp])
                  P.op("dve", lambda e, pt=pt: e.tensor_copy(out=pre0[:], in_=pt[:, 0:2]), reads=[Bp], writes=[B_pre0])
                  for c in range(2):
                      src, Bsrc = (kcmpT, B_kcmpT) if c == 0 else (vcmpT, B_vcmpT)
                      for g in range(4):
                          gp, g2 = g // 2, g % 2
                          hs = slice(64 * g2, 64 * g2 + 64)
                          pt, Bp = next_ps("a")
                          for j in range(32):
                              P.op("pe", lambda e, pt=pt, c=c, j=j, hs=hs, gp=gp, src=src: e.matmul(
                                  pt[:, 0:255], lhsT=w1d[hs, c, j, :], rhs=src[hs, gp, j:j + 16 * 254 + 1:16],
                                  start=(j == 0), stop=(j == 31)), reads=[B_w1d, Bsrc], writes=[Bp])
                          P.op("act", lambda e, pt=pt, c=c: e.activation(out=ug[:, 0:255], in_=pt[:, 0:255], func=AF.Identity,
                                                                         bias=pre0[:, c:c + 1]), reads=[Bp, B_pre0], writes=[B_ug])
                          P.op("dve", lambda e: e.tensor_tensor(out=tg[:, 0:255], in0=ug[:, 0:255], in1=ug[:, 0:255], op=ALU.mult),
                               reads=[B_ug], writes=[B_tg])
                          P.op("dve", lambda e: e.tensor_scalar(out=tg[:, 0:255], in0=tg[:, 0:255], scalar1=0.044715, scalar2=1.0,
                                                                op0=ALU.mult, op1=ALU.add), reads=[B_tg], writes=[B_tg])
                          P.op("dve", lambda e: e.tensor_tensor(out=tg[:, 0:255], in0=tg[:, 0:255], in1=ug[:, 0:255], op=ALU.mult),
                               reads=[B_tg, B_ug], writes=[B_tg])
                          P.op("act", lambda e: e.activation(out=tg[:, 0:255], in_=tg[:, 0:255], func=AF.Sigmoid,
                                                             scale=1.5957691216057308), reads=[B_tg], writes=[B_tg])
                          P.op("dve", lambda e: e.tensor_tensor(out=Gb[:, 0:255], in0=tg[:, 0:255], in1=ug[:, 0:255], op=ALU.mult),
                               reads=[B_tg, B_ug], writes=[B_Gb])
                          if c == 0:
                              pt2, Bp2 = next_ps("b")
                              if g2 == 0:
                                  P.op("pe", lambda e, pt2=pt2: e.matmul(pt2[0:64, 0:255], lhsT=w2k[:, 64:128], rhs=Gb[:, 0:255],
                                                                         start=True, stop=True), reads=[B_w2k, B_Gb], writes=[Bp2])
                              else:
                                  P.op("pe", lambda e, pt2=pt2: e.matmul(pt2[:, 0:255], lhsT=w2k[:, :], rhs=Gb[:, 0:255],
                                                                         start=True, stop=True), reads=[B_w2k, B_Gb], writes=[Bp2])
                              P.op("dve", lambda e, pt2=pt2, hs=hs, gp=gp: e.tensor_copy(out=kcT[hs, gp, 0:255], in_=pt2[hs, 0:255]),
                                   reads=[Bp2], writes=[B_kcT])
                          else:
                              pt2, Bp2 = next_ps("b")
                              for ch in range(2):
                                  w = 128 if ch == 0 else 127
                                  P.op("pe", lambda e, pt2=pt2, ch=ch, w=w: e.matmul(
                                      pt2[:w, ch * 64:(ch + 1) * 64], lhsT=Gb[:, ch * 128:ch * 128 + w], rhs=w2v[:, :],
                                      start=True, stop=True), reads=[B_w2v, B_Gb], writes=[Bp2])
                              for ch in range(2):
                                  w = 128 if ch == 0 else 127
                                  P.op("dve", lambda e, pt2=pt2, ch=ch, w=w, g=g: e.tensor_copy(
                                      out=vca[:w, ch, g, 0:64], in_=pt2[:w, ch * 64:(ch + 1) * 64]), reads=[Bp2], writes=[B_vca])
                  P.op("dve", lambda e: e.memset(kcT[:, :, 255:256], 0.0), writes=[B_kcT])
                  if debug:
                      dk, B_dk = sbt(p1, "dk", [128, 512], F32)
                      P.op("dve", lambda e: e.tensor_copy(out=dk[:], in_=kcT[:].rearrange("p a b -> p (a b)")), reads=[B_kcT], writes=[B_dk])
                      P.dma("sp", lambda e: e.dma_start(out=d_kc, in_=dk[:]), reads=[B_dk])
                      dv, B_dv = sbt(p1, "dv", [128, 2 * 4 * 129], F32)
                      P.op("dve", lambda e: e.tensor_copy(out=dv[:], in_=vca[:].rearrange("p a b c -> p (a b c)")), reads=[B_vca], writes=[B_dv])
                      P.dma("sp", lambda e: e.dma_start(out=d_vc, in_=dv[:]), reads=[B_dv])
                  P.barrier()

              p4 = contextlib.ExitStack()
              with p4:
                  if _LIM < 4:
                      raise _Stop()
                  nsa_prompt(nc, P, p4, sbt, next_ps, locals())
                  P.barrier()

          if _LIM < 6.2:
              raise _Stop()
          sm = contextlib.ExitStack()
          with sm:
              nsa_sample(nc, P, sm, sbt, next_ps, locals())
          if _LIM < 7:
              raise _Stop()
          hg = contextlib.ExitStack()
          with hg:
              hgrn_outputs(nc, P, hg, sbt, next_ps, locals())
          if _LIM < 8:
              raise _Stop()
          tl = contextlib.ExitStack()
          with tl:
              tail_moe(nc, P, tl, sbt, next_ps, locals())

      except _Stop:
        stopped = True
      if True:
          P.finish("sp")
          P.emit(nc)
      if not stopped:
          top.close()
    return nc


def _bf16(a):
    import ml_dtypes
    return np.ascontiguousarray(np.asarray(a, np.float32).astype(ml_dtypes.bfloat16))


def sample_tables():
    T = {}
    nl = np.arange(128)
    cb = np.zeros((128, 64), np.float32)
    for h in range(16):
        for c in range(4):
            n = 128 * c + nl
            v = SLOPES[h] * (16.0 * n + 31 - 8192)
            cb[:, h * 4 + c] = np.where(n >= 511, NEGM, v)
    T["s_cb"] = cb
    bsx = np.zeros((128, 16 * 65), np.float32)
    for h in range(16):
        for d in range(65):
            bsx[:, h * 65 + d] = SLOPES[h] * (nl - 128.0 * d)
    T["s_bs"] = bsx
    sv = np.arange(32)
    sq = np.zeros((16, 32), np.float32)
    for h in range(16):
        sq[h] = -SLOPES[h] * sv / SCALE
    T["s_sq"] = sq.reshape(1, 512)
    ln = np.zeros((2, 128), np.float32)
    ln[0] = 1.0
    T["s_lnt"] = _bf16(ln)
    T["s_caus"] = _bf16(np.tile(np.where(nl[:, None] > sv[None, :], NEGM, 0.0), (1, 4)))
    T["s_low"] = _bf16(np.tile(np.where(nl[:, None] <= sv[None, :], NEGM, 0.0), (1, 4)))
    keep = np.ones((128, 128), np.float32)
    force = np.zeros((128, 128), np.float32)
    keep[:, 0] = 0.0
    force[:, 0] = 1e30
    T["s_keep"], T["s_force"] = keep, force
    j = np.arange(128)[None, :]
    ov = np.zeros((128, 4, 128), np.float32)
    for c in range(4):
        n = (128 * c + nl)[:, None]
        ov[:, c, :] = ((n >= 4 * j - 1) & (n <= 4 * j + 3)).astype(np.float32)
    T["s_ovl"] = _bf16(ov.reshape(128, 512))
    T["s_iota"] = nl.astype(np.float32).reshape(128, 1)
    return T


def host_tables(nnull):
    T = {}
    nl = np.arange(128)
    t_cb = np.zeros((128, 256), np.float32)
    for h in range(16):
        for k in range(NOWN):
            B = OWN0 + k
            for c in range(2):
                n = 128 * c + nl
                v = SLOPES[h] * (16.0 * n + 31 - 128 * B)
                v = np.where((n < 8 * nnull) | (n >= 255), NEGM, v)
                t_cb[:, (h * NOWN + k) * 2 + c] = v
    T["t_cb"] = t_cb
    cm = np.zeros((128, NOWN, 4, 128), np.float32)
    ql = np.arange(128)
    for k in range(NOWN):
        B = OWN0 + k
        inval = (16 * (128 + nl)[:, None] + 31) > (128 * B + ql[None, :])
        cm[:, k, :, :] = np.where(inval, NEGM, 0.0)[:, None, :]
    T["t_cmask"] = _bf16(cm.reshape(128, NOWN * 512))
    t_bs = np.zeros((128, 512), np.float32)
    for h in range(16):
        for d in range(32):
            t_bs[:, h * 32 + d] = SLOPES[h] * (nl - 128.0 * d)
    T["t_bs"] = t_bs
    sq = np.zeros((16, 128), np.float32)
    for h in range(16):
        sq[h] = -SLOPES[h] * ql / SCALE
    T["t_sq"] = sq.reshape(1, 2048)
    ln = np.zeros((2, NB, 128), np.float32)
    ln[0] = 1.0
    ln[1, :nnull] = 1.0
    T["t_ln"] = _bf16(ln.reshape(2, NB * 128))
    r2 = np.zeros((2, 512), np.float32)
    r2[1] = NEGM
    T["t_r2"] = _bf16(r2)
    E = np.zeros((64, NB, 128), np.float32)
    for c in range(NB):
        E[2 * c, c, :64] = 1.0
        E[2 * c + 1, c, 64:] = 1.0
    T["t_E"] = _bf16(E.reshape(64, NB * 128))
    tq = nl[:, None] > ql[None, :]
    T["t_caus"] = _bf16(np.tile(np.where(tq, NEGM, 0.0), (1, 4)))
    T["t_low"] = _bf16(np.tile(np.where(~tq, NEGM, 0.0), (1, 4)))
    keep = np.zeros((128, NOWN, 64), np.float32)
    force = np.zeros((128, NOWN, 64), np.float32)
    j = np.arange(64)[None, :]
    j0 = 2 * nnull
    for k in range(NOWN):
        B = OWN0 + k
        cur = (2 * B + (ql >= 64))[:, None]
        neg = (j > cur) | (j < j0)
        big = ((j == cur) | (j == j0)) & ~neg
        keep[:, k, :] = np.where(neg | big, 0.0, 1.0)
        force[:, k, :] = np.where(neg, -1e30, np.where(big, 1e30, 0.0))
    T["t_keep"] = keep.reshape(128, NOWN * 64)
    T["t_force"] = force.reshape(128, NOWN * 64)
    ov = np.zeros((128, 2, 64), np.float32)
    for c in range(2):
        n = (128 * c + nl)[:, None]
        ov[:, c, :] = ((n >= 4 * j - 1) & (n <= 4 * j + 3)).astype(np.float32)
    T["t_ovl"] = _bf16(ov.reshape(128, 128))
    a = np.arange(128)
    same = (a[:, None] // 32) == (a[None, :] // 32)
    T["t_u32"] = (same & (a[:, None] <= a[None, :])).astype(np.float32)
    T["t_l32"] = (same & (a[:, None] > a[None, :])).astype(np.float32)
    T["t_ind4"] = ((a[:, None] // 32) == np.arange(4)[None, :]).astype(np.float32)
    vm = np.ones((128, NB + 1), np.float32)
    vm[:, :nnull] = 0.0
    T["t_vmask"] = vm
    se = np.zeros((32, 32, 128), np.float32)
    for e_ in range(32):
        se[e_, e_, :] = 1.0
    T["t_selE"] = _bf16(se.reshape(32, 32 * 128))
    T["t_id"] = np.eye(128, dtype=np.float32)
    T["t_idb"] = _bf16(np.eye(128))
    return T


_NC_CACHE = {}
_TAB_CACHE = {}


def _run(inputs, debug=False):
    f = lambda a: np.ascontiguousarray(np.asarray(a, dtype=np.float32))
    x_prompt, x_sample = f(inputs["x_prompt"]), f(inputs["x_sample"])
    w_in, b_in = f(inputs["w_in"])[0], f(inputs["b_in"])[0]
    hgrn_gamma, state_hgrn, cache_win = f(inputs["hgrn_gamma"]), f(inputs["state_hgrn"])[0], f(inputs["cache_win"])[0]
    key = "nc_dbg" if debug else "nc"
    if key not in _NC_CACHE:
        _NC_CACHE[key] = build_nc(debug)
    nc = _NC_CACHE[key]

    lmask = np.tril(np.ones((128, 128), np.float32), -1)
    w_kv = np.ascontiguousarray(w_in[:, KV_OFF:KV_OFF + KV_W])
    b_kv = np.ascontiguousarray(b_in[KV_OFF:KV_OFF + KV_W])
    perm = np.zeros(1024, np.int64)
    for gp in range(2):
        for hh in range(4):
            for g2 in range(2):
                dst = ((gp * 4 + hh) * 2 + g2) * 64
                src = (4 * (2 * gp + g2) + hh) * 64
                perm[dst:dst + 64] = np.arange(src, src + 64)
    w_q = np.ascontiguousarray(w_in[:, QOFF:QOFF + 1024][:, perm])
    b_q = np.ascontiguousarray(b_in[QOFF:QOFF + 1024][perm])
    w_ng = np.ascontiguousarray(w_in[:, NGOFF:NGOFF + 48])
    b_ng = np.ascontiguousarray(b_in[NGOFF:NGOFF + 48])
    w_ss = np.ascontiguousarray(np.concatenate([w_in[:, HF_OFF:HF_OFF + 1024], w_in[:, HI_OFF:HI_OFF + 1024]], axis=1))
    b_ss = np.ascontiguousarray(np.concatenate([b_in[HF_OFF:HF_OFF + 1024], b_in[HI_OFF:HI_OFF + 1024]]))
    xTb_all = [np.ascontiguousarray(x_prompt[b].T) for b in range(2)]
    hn = f(inputs["hgrn_norm"])[0].reshape(1024)
    w_h3, b_h3, n_h3, g_h3 = [], [], [], []
    for hp in range(2):
        cs = slice(512 * hp, 512 * (hp + 1))
        w_h3.append(np.concatenate([w_in[:, o:o + 1024][:, cs] for o in (HQOFF, HF_OFF, HI_OFF, HGOFF)], axis=1))
        b_h3.append(np.concatenate([b_in[o:o + 1024][cs] for o in (HQOFF, HF_OFF, HI_OFF, HGOFF)]))
        n_h3.append(hn[cs])
        g_h3.append(hgrn_gamma[:, cs])
    shared = {
        "w_h3": np.ascontiguousarray(np.stack(w_h3)), "b_h3": np.ascontiguousarray(np.stack(b_h3)),
        "n_h3": np.ascontiguousarray(np.stack(n_h3)), "g_h3": np.ascontiguousarray(np.stack(g_h3)),
        "w_pa": f(inputs["w_pa"])[0], "w_pb": f(inputs["w_pb"])[0],
        "w_mg": np.ascontiguousarray(w_in[:, MGOFF:MGOFF + 4096]), "b_mg": np.ascontiguousarray(b_in[MGOFF:MGOFF + 4096]),
        "w_out": f(inputs["w_out"])[0],
        "ln1_g": f(inputs["ln1_g"])[0], "ln1_b": f(inputs["ln1_b"])[0], "ln2_g": f(inputs["ln2_g"])[0], "ln2_b": f(inputs["ln2_b"])[0],
        "w_r": np.ascontiguousarray(np.concatenate([f(inputs["w_rg"])[0], f(inputs["w_re"])[0]], axis=1)),
        "b_r": np.ascontiguousarray(np.concatenate([f(inputs["b_rg"])[0], f(inputs["b_re"])[0]])),
        "w_gate": np.ascontiguousarray(f(inputs["w_gate"])[0][:_NEXP]), "w_up": np.ascontiguousarray(f(inputs["w_up"])[0][:_NEXP]),
        "w_down": np.ascontiguousarray(f(inputs["w_down"])[0][:_NEXP]),
    }
    shared.update(sample_tables())
    shared["cache2d"] = np.ascontiguousarray(f(inputs["cache_kv"])[0].reshape(N_PHYS * 128 * 2, 512))
    page_table = np.ascontiguousarray(np.asarray(inputs["page_table"], dtype=np.int32))
    in_maps = []
    for c in range(NCORES):
        b, i = c // 4, c % 4
        nnull = OWN0 - 8 * i
        if nnull not in _TAB_CACHE:
            _TAB_CACHE[nnull] = host_tables(nnull)
        xfr = np.zeros((XF_T, D_MODEL), np.float32)
        nreal = (NB - nnull) * 128
        xfr[nnull * 128:NB * 128] = x_prompt[b, 0:nreal]
        for j in range(4):
            xfr[NB * 128 + 32 * j:NB * 128 + 32 * j + 4] = x_sample[4 * c + j]
        hs = slice(256 * i, 256 * (i + 1))
        w_st = np.concatenate([w_in[:, HF_OFF:HF_OFF + 1024][:, hs], w_in[:, HI_OFF:HI_OFF + 1024][:, hs]], axis=1)
        b_st = np.concatenate([b_in[HF_OFF:HF_OFF + 1024][hs], b_in[HI_OFF:HI_OFF + 1024][hs]])
        m = {
            "xf": np.ascontiguousarray(xfr.T), "xTb": xTb_all[b],
            "w_kv": w_kv, "b_kv": b_kv, "w_q": w_q, "b_q": b_q, "w_ng": w_ng, "b_ng": b_ng,
            "w_st": np.ascontiguousarray(w_st), "b_st": np.ascontiguousarray(b_st),
            "g_st": np.ascontiguousarray(hgrn_gamma[:, hs]),
            "w_ss": w_ss, "b_ss": b_ss, "g_ss": hgrn_gamma,
            "st_in": np.ascontiguousarray(state_hgrn[4 * c:4 * c + 4]),
            "cw_in": np.ascontiguousarray(cache_win[4 * c:4 * c + 4].reshape(4, 512, 512)),
            "c_lm": lmask, "c_lm4": np.ascontiguousarray(lmask[:4, :4]),
            "w_c1": f(inputs["w_cmp1"])[0], "w_c2": f(inputs["w_cmp2"])[0], "c_pe": f(inputs["cmp_pe"])[0],
        }
        m.update(_TAB_CACHE[nnull])
        m.update(shared)
        m["pt_core"] = np.ascontiguousarray(page_table[4 * c:4 * c + 4].reshape(256))
        in_maps.append(m)
    res = run_bass_kernel_spmd(nc, in_maps, core_ids=list(range(NCORES)))
    return res.results


def _assemble(R):
    y_prompt = np.zeros((2, SEQ, D_MODEL), np.float32)
    y_sample = np.zeros((32, 4, D_MODEL), np.float32)
    new_kv_prompt = np.zeros((1, 2, SEQ, 4, 4, 64), np.float32)
    new_kv_sample = np.zeros((1, 32, 4, 4, 4, 64), np.float32)
    new_win_prompt = np.zeros((1, 2, 512, 2, 4, 64), np.float32)
    new_win_sample = np.zeros((1, 32, 512, 2, 4, 64), np.float32)
    new_state_prompt = np.zeros((1, 2, 8, 128, 128), np.float32)
    new_state_sample = np.zeros((1, 32, 8, 128, 128), np.float32)
    for c in range(NCORES):
        b, i = c // 4, c % 4
        kvo = np.asarray(R[c]["kv_out"])
        new_kv_prompt[0, b, 1024 * i:1024 * (i + 1)] = kvo[:1024, :1024].reshape(1024, 4, 4, 64)
        smp = kvo[1024:].reshape(4, 32, KV_W)[:, :4]
        new_kv_sample[0, 4 * c:4 * c + 4] = smp[:, :, :1024].reshape(4, 4, 4, 4, 64)
        if i == 3:
            new_win_prompt[0, b] = kvo[512:1024, 1024:1536].reshape(512, 2, 4, 64)
        new_win_sample[0, 4 * c:4 * c + 4] = np.asarray(R[c]["win_s"]).reshape(4, 512, 2, 4, 64)
        new_state_prompt[0, b, 2 * i:2 * i + 2] = np.asarray(R[c]["st_p"])
        new_state_sample[0, 4 * c:4 * c + 4] = np.asarray(R[c]["st_s"])
        if "y_out" in R[c]:
            yo = np.asarray(R[c]["y_out"])
            y_prompt[b, 1024 * i:1024 * (i + 1)] = yo[:, :1024].T
            y_sample[4 * c:4 * c + 4] = yo[:, 1024:].T.reshape(4, 32, D_MODEL)[:, :4]
    return (y_prompt, y_sample, new_kv_prompt, new_kv_sample, new_win_prompt, new_win_sample,
            new_state_prompt, new_state_sample)


def kernel(**inputs):
    return _assemble(_run(inputs, debug=False))
```

```python
import contextlib
import numpy as np
import concourse.bass as bass
import concourse.mybir as mybir
from concourse.bass_utils import run_bass_kernel_spmd

F32 = mybir.dt.float32
BF16 = mybir.dt.bfloat16
AF = mybir.ActivationFunctionType
ALU = mybir.AluOpType

D_MODEL = 2048
KC = D_MODEL // 128
SEQ = 4096
NCORES = 8
TOK_P = 1024
TOK_S = 16
TOK = TOK_P + TOK_S
KV_OFF, KV_W = 1024, 1536
HF_OFF, HI_OFF = 3632, 4656


class _Stop(Exception):
    pass


class Buf:
    def __init__(self, name):
        self.name = name
        self.w = {}
        self.r = {}


class Prog:
    COMPUTE = ("pe", "act", "dve", "pool")

    def __init__(self, n_dma_sems=8):
        self.ops = {e: [] for e in ("pe", "act", "dve", "pool", "sp")}
        self.cnt = {}
        self.waited = {}
        self.n_dma = n_dma_sems
        self.dma_rr = {"sp": 0, "pool": 0, "act": 0}

    def _need(self, eng, reads, writes):
        need = {}
        for b in reads:
            for e, s in b.w.items():
                need[e] = max(need.get(e, 0), s)
        for b in writes:
            for e, s in b.w.items():
                need[e] = max(need.get(e, 0), s)
            for e, s in b.r.items():
                need[e] = max(need.get(e, 0), s)
        waits = []
        for e, s in need.items():
            if e == eng and eng == "pe":
                continue
            if self.waited.get((eng, e), 0) >= s:
                continue
            self.waited[(eng, e)] = s
            waits.append((e, s))
        return waits

    def op(self, eng, fn, reads=(), writes=()):
        waits = self._need(eng, reads, writes)
        self.cnt[eng] = self.cnt.get(eng, 0) + 1
        s = self.cnt[eng]
        for b in reads:
            b.r[eng] = s
        for b in writes:
            b.w[eng] = s
        self.ops[eng].append((waits, fn, eng))

    def dma(self, queue, fn, reads=(), writes=()):
        k = self.dma_rr[queue]
        self.dma_rr[queue] = (k + 1) % self.n_dma
        v = "dma_%s_%d" % (queue, k)
        waits = self._need(queue, reads, writes)
        prev = self.cnt.get(v, 0)
        if prev and self.waited.get((queue, v), 0) < prev:
            self.waited[(queue, v)] = prev
            waits.append((v, prev))
        self.cnt[v] = prev + 1
        s = self.cnt[v]
        for b in reads:
            b.r[v] = s
        for b in writes:
            b.w[v] = s
        self.ops[queue].append((waits, fn, v))

    def barrier(self):
        targets = dict(self.cnt)
        for eng in self.ops:
            waits = []
            for e, n in targets.items():
                if e == eng and eng == "pe":
                    continue
                if n and self.waited.get((eng, e), 0) < n:
                    self.waited[(eng, e)] = n
                    waits.append((e, n))
            self.ops[eng].append((waits, None, None))

    def finish(self, queue="sp"):
        waits = []
        for v, n in self.cnt.items():
            if v.startswith("dma_") and self.waited.get((queue, v), 0) < n:
                waits.append((v, n))
        self.ops[queue].append((waits, None, None))

    def emit(self, nc):
        names = sorted(set(list(self.COMPUTE) + [v for v in self.cnt if v.startswith("dma_")]))
        with contextlib.ExitStack() as es:
            sems = {n: es.enter_context(nc.semaphore("s_" + n)) for n in names}
            block = es.enter_context(nc.Block())

            def run(eng_name, eng):
                for waits, fn, inc in self.ops[eng_name]:
                    for (e, s) in waits:
                        eng.wait_ge(sems[e], s * 16 if e.startswith("dma_") else s)
                    if fn is None:
                        continue
                    ins = fn(eng)
                    ins.then_inc(sems[inc], 16 if inc.startswith("dma_") else 1)

            @block.tensor
            def _(e):
                run("pe", e)

            @block.scalar
            def _(e):
                run("act", e)

            @block.vector
            def _(e):
                run("dve", e)

            @block.gpsimd
            def _(e):
                run("pool", e)

            @block.sync
            def _(e):
                run("sp", e)


def stage_states(nc, P, st, sbt, next_ps, ones_c, B_ones, D):
    xTb, xf = D["xTb"], D["xf"]
    wbig, B_wbig = sbt(st, "wbig", [128, KC, 2048], BF16)
    wst_sb, B_wst = sbt(st, "wst_sb", [128, KC, 512], BF16)
    bias_big, B_bias_big = sbt(st, "bias_big", [128, 2048], F32)
    bias_st, B_bias_st = sbt(st, "bias_st", [128, 512], F32)
    oml_st, B_oml_st = sbt(st, "oml_st", [128, 256], F32)
    oml_ss, B_oml_ss = sbt(st, "oml_ss", [128, 1024], F32)
    lm, B_lm = sbt(st, "lm", [128, 128], F32)
    lm4, B_lm4 = sbt(st, "lm4", [4, 4], F32)
    xs = [sbt(st, "sxs%d" % i, [128, KC, 256], BF16) for i in range(2)]
    xsm, B_xsm = sbt(st, "xsm", [128, KC, 128], BF16)
    S_sb, B_S = sbt(st, "S_sb", [128, 2, 128], F32)
    NSCR = 2
    scr = []
    for i in range(NSCR):
        d = {}
        for n, dt in (("kk", F32), ("lgf", F32), ("edd", F32), ("kd", BF16), ("vv", BF16)):
            d[n] = sbt(st, "%s%d" % (n, i), [128, 1024], dt)
        d["edl"] = sbt(st, "edl%d" % i, [128, 8], F32)
        scr.append(d)

    P.dma("sp", lambda e: e.dma_start(out=lm[:], in_=D["c_lm"]), writes=[B_lm])
    P.dma("sp", lambda e: e.dma_start(out=lm4[:], in_=D["c_lm4"]), writes=[B_lm4])
    P.dma("sp", lambda e: e.dma_start(out=bias_st[:], in_=D["b_st"].partition_broadcast(128)), writes=[B_bias_st])
    wst_v = D["w_st"].rearrange("(kc p) c -> p kc c", p=128)
    for q in range(2):
        P.dma("pool", lambda e, q=q: e.dma_start(out=wst_sb[:, 8 * q:8 * q + 8, :], in_=wst_v[:, 8 * q:8 * q + 8, :]),
              writes=[B_wst])

    def lower_bound_prep(g_ap, n, oml, B_oml):
        g0, Bg0 = scr[0]["lgf"]
        g1, Bg1 = scr[1]["lgf"]
        P.dma("sp", lambda e: e.dma_start(out=g0[:, 0:n], in_=g_ap[0].partition_broadcast(128)), writes=[Bg0])
        P.dma("sp", lambda e: e.dma_start(out=g1[:, 0:n], in_=g_ap[1].partition_broadcast(128)), writes=[Bg1])
        P.op("dve", lambda e: e.tensor_tensor(out=oml[:, 0:n], in0=g1[:, 0:n], in1=g0[:, 0:n], op=ALU.subtract),
             reads=[Bg0, Bg1], writes=[B_oml])
        P.op("act", lambda e: e.activation(out=oml[:, 0:n], in_=oml[:, 0:n], func=AF.Sigmoid), reads=[B_oml], writes=[B_oml])

    def state_chunk(si, m, xsl, W, B_W, B_x, bias, B_bias, oml, B_oml, nh, lmask, B_lmask, S_list):
        d = scr[si % NSCR]
        kk, Bkk = d["kk"]
        lgf, Blgf = d["lgf"]
        edd, Bedd = d["edd"]
        kd, Bkd = d["kd"]
        vv, Bvv = d["vv"]
        edl, Bedl = d["edl"]
        n = nh * 128
        for g in range((2 * n) // 512):
            pt, Bp = next_ps("a")
            for kc in range(KC):
                P.op("pe", lambda e, pt=pt, kc=kc, g=g: e.matmul(
                    pt[:m, :], lhsT=xsl(kc), rhs=W[:, kc, g * 512:(g + 1) * 512],
                    start=(kc == 0), stop=(kc == KC - 1)), reads=[B_x, B_W], writes=[Bp])
            c0 = g * 512
            a0, a1 = c0, min(c0 + 512, n)
            if a1 > a0:
                P.op("dve", lambda e, pt=pt, a0=a0, a1=a1, c0=c0: e.tensor_tensor(
                    out=kk[:m, a0:a1], in0=pt[:m, a0 - c0:a1 - c0], in1=bias[:m, a0:a1], op=ALU.add),
                    reads=[Bp, B_bias], writes=[Bkk])
            v0, v1 = max(c0, n), c0 + 512
            if v1 > v0:
                P.op("dve", lambda e, pt=pt, v0=v0, v1=v1, c0=c0: e.tensor_tensor(
                    out=vv[:m, v0 - n:v1 - n], in0=pt[:m, v0 - c0:v1 - c0], in1=bias[:m, v0:v1], op=ALU.add),
                    reads=[Bp, B_bias], writes=[Bvv])
        P.op("act", lambda e: e.activation(out=kk[:m, 0:n], in_=kk[:m, 0:n], func=AF.Sigmoid, scale=-1.0),
             reads=[Bkk], writes=[Bkk])
        P.op("dve", lambda e: e.tensor_tensor(out=kk[:m, 0:n], in0=kk[:m, 0:n], in1=oml[:m, 0:n], op=ALU.mult),
             reads=[Bkk, B_oml], writes=[Bkk])
        P.op("act", lambda e: e.activation(out=lgf[:m, 0:n], in_=kk[:m, 0:n], func=AF.Ln, scale=-1.0, bias=ones_c[:m, :]),
             reads=[Bkk, B_ones], writes=[Blgf])
        for g in range((n + 511) // 512):
            w = min(512, n - g * 512)
            pt, Bp = next_ps("a")
            P.op("pe", lambda e, pt=pt, g=g, w=w: e.matmul(pt[:m, 0:w], lhsT=lmask[:m, :m], rhs=lgf[:m, g * 512:g * 512 + w],
                                                           start=True, stop=True), reads=[B_lmask, Blgf], writes=[Bp])
            P.op("act", lambda e, pt=pt, g=g, w=w: e.activation(out=edd[:m, g * 512:g * 512 + w], in_=pt[:m, 0:w], func=AF.Exp),
                 reads=[Bp], writes=[Bedd])
        P.op("dve", lambda e: e.tensor_tensor(out=kd[:m, 0:n], in0=kk[:m, 0:n], in1=edd[:m, 0:n], op=ALU.mult),
             reads=[Bkk, Bedd], writes=[Bkd])
        pt, Bp = next_ps("b")
        for h in range(nh):
            P.op("pe", lambda e, pt=pt, h=h: e.matmul(pt[:, h:h + 1], lhsT=lgf[:m, h * 128:(h + 1) * 128], rhs=ones_c[:m, :],
                                                      start=True, stop=True), reads=[Blgf, B_ones], writes=[Bp])
        P.op("act", lambda e, pt=pt: e.activation(out=edl[:, 0:nh], in_=pt[:, 0:nh], func=AF.Exp), reads=[Bp], writes=[Bedl])
        for h0 in range(0, nh, 4):
            pt, Bp = next_ps("b")
            hs_ = list(range(h0, min(nh, h0 + 4)))
            for h in hs_:
                P.op("pe", lambda e, pt=pt, h=h, h0=h0: e.matmul(
                    pt[:, (h - h0) * 128:(h - h0 + 1) * 128], lhsT=kd[:m, h * 128:(h + 1) * 128], rhs=vv[:m, h * 128:(h + 1) * 128],
                    start=True, stop=True), reads=[Bkd, Bvv], writes=[Bp])
            for h in hs_:
                s_in, s_out, b_in, b_out = S_list[h]
                P.op("dve", lambda e, pt=pt, h=h, h0=h0, s_in=s_in, s_out=s_out: e.scalar_tensor_tensor(
                    out=s_out, in0=s_in, scalar=edl[:, h:h + 1], in1=pt[:, (h - h0) * 128:(h - h0 + 1) * 128],
                    op0=ALU.mult, op1=ALU.add), reads=[Bp, Bedl, b_in], writes=[b_out])

    lower_bound_prep(D["g_st"], 256, oml_st, B_oml_st)
    P.op("dve", lambda e: e.memset(S_sb[:], 0.0), writes=[B_S])
    xTb_v = xTb.rearrange("(kc p) t -> p kc t", p=128)
    si = 0
    for tt in range(SEQ // 256):
        xb, Bx = xs[tt % 2]
        for q in range(2):
            P.dma("pool", lambda e, xb=xb, tt=tt, q=q: e.dma_start(
                out=xb[:, 8 * q:8 * q + 8, :], in_=xTb_v[:, 8 * q:8 * q + 8, tt * 256:(tt + 1) * 256]), writes=[Bx])
        for c4 in range(2):
            S_list = [(S_sb[:, h, :], S_sb[:, h, :], B_S, B_S) for h in range(2)]
            state_chunk(si, 128, lambda kc, xb=xb, c4=c4: xb[:, kc, c4 * 128:(c4 + 1) * 128],
                        wst_sb, B_wst, Bx, bias_st, B_bias_st, oml_st, B_oml_st, 2, lm, B_lm, S_list)
            si += 1
    for h in range(2):
        P.dma("sp", lambda e, h=h: e.dma_start(out=D["st_p"][h], in_=S_sb[:, h, :]), reads=[B_S])

    wss_v = D["w_ss"].rearrange("(kc p) c -> p kc c", p=128)
    for q in range(4):
        P.dma("pool", lambda e, q=q: e.dma_start(out=wbig[:, 4 * q:4 * q + 4, :], in_=wss_v[:, 4 * q:4 * q + 4, :]),
              writes=[B_wbig])
    P.dma("sp", lambda e: e.dma_start(out=bias_big[:], in_=D["b_ss"].partition_broadcast(128)), writes=[B_bias_big])
    xf_v = xf.rearrange("(kc p) t -> p kc t", p=128)
    for q in range(2):
        P.dma("pool", lambda e, q=q: e.dma_start(out=xsm[:, 8 * q:8 * q + 8, :], in_=xf_v[:, 8 * q:8 * q + 8, NB * 128:(NB + 1) * 128]),
              writes=[B_xsm])
    lower_bound_prep(D["g_ss"], 1024, oml_ss, B_oml_ss)
    s0 = [sbt(st, "s0_%d" % i, [128, 8, 128], F32) for i in range(2)]
    for j in range(4):
        sj, Bsj = s0[j % 2]
        P.dma("sp", lambda e, sj=sj, j=j: e.dma_start(out=sj[:], in_=D["st_in"][j].rearrange("h k v -> k h v")), writes=[Bsj])
        S_list = [(sj[:, h, :], sj[:, h, :], Bsj, Bsj) for h in range(8)]
        state_chunk(si, 4, lambda kc, j=j: xsm[:, kc, 32 * j:32 * j + 4],
                    wbig, B_wbig, B_xsm, bias_big, B_bias_big, oml_ss, B_oml_ss, 8, lm4, B_lm4, S_list)
        si += 1
        P.dma("sp", lambda e, sj=sj, j=j: e.dma_start(out=D["st_s"][j].rearrange("h k v -> k h v"), in_=sj[:]), reads=[Bsj])


def nsa_prompt(nc, P, p4, sbt, next_ps, L):
    debug = L["debug"]
    xf = L["xf"]
    kslcT, B_kslcT, kwinT, B_kwinT = L["kslcT"], L["B_kslcT"], L["kwinT"], L["B_kwinT"]
    vslc, B_vslc, vwin, B_vwin = L["vslc"], L["B_vslc"], L["vwin"], L["B_vwin"]
    kcT, B_kcT, vca, B_vca = L["kcT"], L["B_kcT"], L["vca"], L["B_vca"]
    ident, B_ident, identb, B_identb = L["ident"], L["B_ident"], L["identb"], L["B_identb"]
    onesb, B_onesb = L["onesb"], L["B_onesb"]
    o_nsaT, B_onsaT = L["o_nsaT"], L["B_onsaT"]

    def table(name, shape, dt, src, q="sp"):
        t, B = sbt(p4, name, shape, dt)
        P.dma(q, lambda e: e.dma_start(out=t[:], in_=src), writes=[B])
        return t, B
    cb, B_cb = table("cb", [128, 256], F32, L["t_cb"])
    cmask, B_cmask = table("cmask", [128, NOWN * 512], BF16, L["t_cmask"])
    bs, B_bs = table("bs", [128, 512], F32, L["t_bs"])
    sq, B_sq = table("sq", [1, 2048], F32, L["t_sq"])
    lnt, B_lnt = table("lnt", [2, NB * 128], BF16, L["t_ln"])
    R2, B_R2 = table("R2", [2, 512], BF16, L["t_r2"])
    Et, B_Et = table("Et", [64, NB * 128], BF16, L["t_E"])
    caus, B_caus = table("caus", [128, 512], BF16, L["t_caus"])
    low, B_low = table("low", [128, 512], BF16, L["t_low"])
    keep, B_keep = table("keep", [128, NOWN * 64], F32, L["t_keep"])
    force, B_force = table("force", [128, NOWN * 64], F32, L["t_force"])

    qT, B_qT = sbt(p4, "qT", [128, 8, NOWN * 128], BF16)
    gates, B_gates = sbt(p4, "gates", [128, NOWN, 48], F32)
    pq = contextlib.ExitStack()
    with pq:
        wq, B_wq = sbt(pq, "wq", [128, KC, 1024], BF16)
        wng, B_wng = sbt(pq, "wng", [128, KC, 48], BF16)
        bq_col, B_bq = sbt(pq, "bq_col", [128, 8], F32)
        bng, B_bng = sbt(pq, "bng", [128, 48], F32)
        xo = [sbt(pq, "xo%d" % i, [128, KC, 256], BF16) for i in range(1)]
        wq_v = L["w_q"].rearrange("(kc p) c -> p kc c", p=128)
        for q in range(4):
            P.dma("pool", lambda e, q=q: e.dma_start(out=wq[:, 4 * q:4 * q + 4, :], in_=wq_v[:, 4 * q:4 * q + 4, :]), writes=[B_wq])
        P.dma("pool", lambda e: e.dma_start(out=wng[:], in_=L["w_ng"].rearrange("(kc p) c -> p kc c", p=128)), writes=[B_wng])
        with nc.allow_non_contiguous_dma(reason="tiny bias column layout"):
            P.dma("sp", lambda e: e.dma_start(out=bq_col[:], in_=L["b_q"].rearrange("(cb p) -> p cb", p=128), allow_slow_non_contiguous=True), writes=[B_bq])
        P.dma("sp", lambda e: e.dma_start(out=bng[:], in_=L["b_ng"].partition_broadcast(128)), writes=[B_bng])
        xf_v = xf.rearrange("(kc p) t -> p kc t", p=128)
        for tt in range(4):
            xb, Bx = xo[0]
            t0 = OWN0 * 128 + tt * 256
            for q in range(2):
                P.dma("pool", lambda e, xb=xb, t0=t0, q=q: e.dma_start(
                    out=xb[:, 8 * q:8 * q + 8, :], in_=xf_v[:, 8 * q:8 * q + 8, t0:t0 + 256]), writes=[Bx])
            for cbk in range(8):
                pt, Bp = next_ps("a")
                for kc in range(KC):
                    P.op("pe", lambda e, pt=pt, kc=kc, cbk=cbk, xb=xb: e.matmul(
                        pt[:, 0:256], lhsT=wq[:, kc, cbk * 128:(cbk + 1) * 128], rhs=xb[:, kc, :],
                        start=(kc == 0), stop=(kc == KC - 1)), reads=[B_wq, Bx], writes=[Bp])
                P.op("act", lambda e, pt=pt, cbk=cbk, tt=tt: e.activation(
                    out=qT[:, cbk, tt * 256:(tt + 1) * 256], in_=pt[:, 0:256], func=AF.Identity, bias=bq_col[:, cbk:cbk + 1]),
                    reads=[Bp, B_bq], writes=[B_qT])
            for bi in range(2):
                k = tt * 2 + bi
                pt, Bp = next_ps("b")
                for kc in range(KC):
                    P.op("pe", lambda e, pt=pt, kc=kc, xb=xb, bi=bi: e.matmul(
                        pt[:, 0:48], lhsT=xb[:, kc, bi * 128:(bi + 1) * 128], rhs=wng[:, kc, :],
                        start=(kc == 0), stop=(kc == KC - 1)), reads=[B_wng, Bx], writes=[Bp])
                P.op("dve", lambda e, pt=pt, k=k: e.tensor_tensor(out=gates[:, k, :], in0=pt[:, 0:48], in1=bng[:, :], op=ALU.add),
                     reads=[Bp, B_bng], writes=[B_gates])
        P.op("act", lambda e: e.activation(out=gates[:], in_=gates[:], func=AF.Sigmoid), reads=[B_gates], writes=[B_gates])
        P.barrier()

    sqt, B_sqt = sbt(p4, "sqt", [128, 512], BF16)
    runmax, B_runmax = sbt(p4, "runmax", [1, 512], F32)
    nkm, B_nkm = sbt(p4, "nkm", [1, 1], F32)
    P.op("dve", lambda e: e.memset(runmax[:], 0.0), writes=[B_runmax])
    srcs = []
    for gp in range(2):
        for s in range(NB * 128 // 512):
            srcs.append((kslcT, B_kslcT, gp, s * 512, 512))
        for s in range(3):
            srcs.append((kwinT, B_kwinT, gp, s * 512, 512))
        srcs.append((kcT, B_kcT, gp, 0, 256))
    for (src, Bs, gp, c0, w) in srcs:
        P.op("dve", lambda e, src=src, gp=gp, c0=c0, w=w: e.tensor_tensor(
            out=sqt[:, 0:w], in0=src[:, gp, c0:c0 + w], in1=src[:, gp, c0:c0 + w], op=ALU.mult), reads=[Bs], writes=[B_sqt])
        pt, Bp = next_ps("b")
        P.op("pe", lambda e, pt=pt, w=w: e.matmul(pt[0:1, 0:w], lhsT=onesb[:, 0:1], rhs=sqt[:, 0:w], start=True, stop=True),
             reads=[B_onesb, B_sqt], writes=[Bp])
        P.op("dve", lambda e, pt=pt, w=w: e.tensor_tensor(out=runmax[:, 0:w], in0=runmax[:, 0:w], in1=pt[0:1, 0:w], op=ALU.max),
             reads=[Bp, B_runmax], writes=[B_runmax])
    P.op("dve", lambda e: e.reduce_max(out=nkm[:], in_=runmax[:], axis=mybir.AxisListType.X), reads=[B_runmax], writes=[B_nkm])
    P.op("dve", lambda e: e.tensor_scalar(out=nkm[:], in0=nkm[:], scalar1=-0.5, scalar2=None, op0=ALU.mult),
         reads=[B_nkm], writes=[B_nkm])
    P.op("dve", lambda e: e.tensor_scalar(out=sq[:, :], in0=sq[:, :], scalar1=nkm[0:1, 0:1], scalar2=None, op0=ALU.add),
         reads=[B_nkm, B_sq], writes=[B_sq])

    if _LIM < 5:
        raise _Stop()
    pT = [sbt(p4, "pT%d" % i, [128, 4, 128], BF16) for i in range(4)]
    pT_rr = [0]

    def next_pT():
        k = pT_rr[0] % len(pT)
        pT_rr[0] += 1
        return pT[k]
    qsq, B_qsq = sbt(p4, "qsq", [128, 512], BF16)
    rt, B_rt = sbt(p4, "rt", [1, 512], F32)
    o_blk, B_oblk = sbt(p4, "o_blk", [128, 256], F32)
    rs, B_rs = sbt(p4, "rs", [128, 4], F32)
    wgt, B_wgt = sbt(p4, "wgt", [128, 4], F32)
    imp, B_imp = sbt(p4, "imp", [128, 64], F32)
    imp3, B_imp3 = sbt(p4, "imp3", [128, 64], F32)
    m8, B_m8 = sbt(p4, "m8", [128, 16], F32)
    nsel, B_nsel = sbt(p4, "nsel", [128, 64], F32)
    nselT, B_nselT = sbt(p4, "nselT", [64, 512], BF16)
    if debug:
        dimp, B_dimp = sbt(p4, "dimp", [128, 64], F32)

    def softmax_chunk(pt, Bp, w, hbase, col_fn, tab, B_tab):
        (t, Bt0) = next_pT()
        Bt = _HB.setdefault(id(Bt0), [Buf("h%d" % i) for i in range(4)])
        for hh in range(4):
            P.op("act", lambda e, pt=pt, t=t, hh=hh, w=w: e.activation(
                out=t[:w, hh, :], in_=pt[:w, hh * 128:(hh + 1) * 128], func=AF.Exp, scale=SCALE,
                bias=tab[:w, col_fn(hbase + hh):col_fn(hbase + hh) + 1]), reads=[Bp, B_tab], writes=[Bt[hh]])
        return t, Bt

    def finish_branch(psO, BpO, k, g, br, first):
        P.op("dve", lambda e: e.tensor_scalar(out=rs[:, :], in0=psO[:, 0:260].rearrange("p (h c) -> p h c", c=65)[:, :, 64],
                                              scalar1=1e-30, scalar2=None, op0=ALU.max), reads=[BpO], writes=[B_rs])
        P.op("dve", lambda e: e.reciprocal(out=rs[:, :], in_=rs[:, :]), reads=[B_rs], writes=[B_rs])
        P.op("dve", lambda e: e.tensor_tensor(out=wgt[:, :], in0=rs[:, :], in1=gates[:, k, br * 16 + g * 4:br * 16 + g * 4 + 4],
                                              op=ALU.mult), reads=[B_rs, B_gates], writes=[B_wgt])
        for hh in range(4):
            oc = slice(hh * 64, hh * 64 + 64)
            if first:
                P.op("dve", lambda e, hh=hh, oc=oc: e.tensor_scalar(out=o_blk[:, oc], in0=psO[:, hh * 65:hh * 65 + 64],
                                                                    scalar1=wgt[:, hh:hh + 1], scalar2=None, op0=ALU.mult),
                     reads=[BpO, B_wgt], writes=[B_oblk])
            else:
                P.op("dve", lambda e, hh=hh, oc=oc: e.scalar_tensor_tensor(
                    out=o_blk[:, oc], in0=psO[:, hh * 65:hh * 65 + 64], scalar=wgt[:, hh:hh + 1], in1=o_blk[:, oc],
                    op0=ALU.mult, op1=ALU.add), reads=[BpO, B_wgt, B_oblk], writes=[B_oblk])

    kslc_g, B_kslcg = sbt(p4, "kslc_g", [64, NB * 128], BF16)
    kwin_g, B_kwing = sbt(p4, "kwin_g", [64, 12 * 128], BF16)
    kc_g, B_kcg = sbt(p4, "kc_g", [64, 256], BF16)
    q_g, B_qg = sbt(p4, "q_g", [64, 4, NOWN * 128], BF16)
    for g in range(4):
        gp, g2 = g // 2, g % 2
        hs0 = slice(64 * g2, 64 * g2 + 64)
        P.dma("sp", lambda e, hs0=hs0, gp=gp: e.dma_start(out=kslc_g[:, :], in_=kslcT[hs0, gp, :]), reads=[B_kslcT], writes=[B_kslcg])
        P.dma("sp", lambda e, hs0=hs0, gp=gp: e.dma_start(out=kwin_g[:, :], in_=kwinT[hs0, gp, :]), reads=[B_kwinT], writes=[B_kwing])
        P.dma("sp", lambda e, hs0=hs0, gp=gp: e.dma_start(out=kc_g[:, :], in_=kcT[hs0, gp, :]), reads=[B_kcT], writes=[B_kcg])
        P.dma("sp", lambda e, hs0=hs0, gp=gp: e.dma_start(out=q_g[:, :, :], in_=qT[hs0, gp * 4:gp * 4 + 4, :]), reads=[B_qT], writes=[B_qg])
        hs = slice(0, 64)
        for k in range(NOWN):
            if _LIM < 6 and (k, g) not in _KSEL:
                continue
            B = OWN0 + k
            tok = slice(k * 128, (k + 1) * 128)
            qv = q_g[:, :, tok]
            P.op("dve", lambda e, qv=qv, hs=hs: e.tensor_tensor(out=qsq[hs, :].rearrange("p (h q) -> p h q", h=4), in0=qv, in1=qv,
                                                                op=ALU.mult), reads=[B_qg], writes=[B_qsq])
            pt, Bp = next_ps("b")
            P.op("pe", lambda e, pt=pt, hs=hs: e.matmul(pt[0:1, :], lhsT=onesb[hs, 0:1], rhs=qsq[hs, :], start=True, stop=True),
                 reads=[B_onesb, B_qsq], writes=[Bp])
            P.op("dve", lambda e, pt=pt, g=g: e.scalar_tensor_tensor(out=R2[0:1, :], in0=pt[0:1, :], scalar=-0.5,
                                                                     in1=sq[0:1, g * 512:(g + 1) * 512], op0=ALU.mult, op1=ALU.add),
                 reads=[Bp, B_sq], writes=[B_R2])

            pts = []
            for c in range(2):
                w = 128 if c == 0 else 127
                pt, Bp = next_ps("a")
                P.op("pe", lambda e, pt=pt, c=c, w=w, hs=hs, gp=gp, qv=qv: e.matmul(
                    pt[:w, :], lhsT=kc_g[hs, c * 128:c * 128 + w], rhs=qv, start=True, stop=False),
                    reads=[B_kcg, B_qg], writes=[Bp])
                P.op("pe", lambda e, pt=pt, w=w, c=c: e.matmul(pt[:w, :], lhsT=lnt[0:1, 0:w], rhs=R2[0:1, :], start=False, stop=(c == 0)),
                     reads=[B_lnt, B_R2], writes=[Bp])
                if c == 1:
                    P.op("pe", lambda e, pt=pt, w=w, k=k: e.matmul(pt[:w, :], lhsT=identb[:w, :w], rhs=cmask[:w, k * 512:(k + 1) * 512],
                                                                   start=False, stop=True), reads=[B_identb, B_cmask], writes=[Bp])
                t, Bt = softmax_chunk(pt, Bp, w, 4 * g, lambda h, k=k, c=c: (h * NOWN + k) * 2 + c, cb, B_cb)
                pts.append((t, Bt, w))
            if _LIM < 5.2:
                continue
            psO, BpO = next_ps("b")
            psI, BpI = next_ps("b")
            for hh in range(4):
                for c, (t, Bt, w) in enumerate(pts):
                    P.op("pe", lambda e, hh=hh, c=c, t=t, w=w, g=g, psO=psO: e.matmul(
                        psO[:, hh * 65:(hh + 1) * 65], lhsT=t[:w, hh, :], rhs=vca[:w, c, g, 0:65], start=(c == 0), stop=(c == 1)),
                        reads=[Bt[hh], B_vca], writes=[BpO])
            for hh in range(4):
                for c, (t, Bt, w) in enumerate(pts):
                    P.op("pe", lambda e, hh=hh, c=c, t=t, w=w, g=g, psI=psI: e.matmul(
                        psI[:, hh * 64:(hh + 1) * 64], lhsT=t[:w, hh, :], rhs=vca[:w, c, g, 65:129], start=(c == 0), stop=(c == 1)),
                        reads=[Bt[hh], B_vca], writes=[BpI])
            finish_branch(psO, BpO, k, g, 0, True)
            for hh in range(4):
                if hh == 0:
                    P.op("dve", lambda e, psI=psI: e.tensor_scalar(out=imp[:, :], in0=psI[:, 0:64], scalar1=rs[:, 0:1], scalar2=None,
                                                          op0=ALU.mult), reads=[BpI, B_rs], writes=[B_imp])
                else:
                    P.op("dve", lambda e, hh=hh, psI=psI: e.scalar_tensor_tensor(
                        out=imp[:, :], in0=psI[:, hh * 64:(hh + 1) * 64], scalar=rs[:, hh:hh + 1], in1=imp[:, :],
                        op0=ALU.mult, op1=ALU.add), reads=[BpI, B_rs, B_imp], writes=[B_imp])
            if debug:
                P.op("dve", lambda e: e.tensor_copy(out=dimp[:], in_=imp[:]), reads=[B_imp], writes=[B_dimp])
                P.dma("sp", lambda e, k=k, g=g: e.dma_start(out=L["d_imp"][(k * 4 + g) * 128:(k * 4 + g + 1) * 128, :], in_=dimp[:]),
                      reads=[B_dimp])
            if _LIM < 5.3:
                continue
            P.op("dve", lambda e: e.tensor_scalar(out=imp[:, :], in0=imp[:, :], scalar1=1e-30, scalar2=None, op0=ALU.max),
                 reads=[B_imp], writes=[B_imp])
            P.op("dve", lambda e, k=k: e.tensor_tensor(out=imp[:, :], in0=imp[:, :], in1=keep[:, k * 64:(k + 1) * 64], op=ALU.mult),
                 reads=[B_imp, B_keep], writes=[B_imp])
            P.op("dve", lambda e, k=k: e.tensor_tensor(out=imp[:, :], in0=imp[:, :], in1=force[:, k * 64:(k + 1) * 64], op=ALU.add),
                 reads=[B_imp, B_force], writes=[B_imp])
            P.op("dve", lambda e: e.max(out=m8[:, 0:8], in_=imp[:, :]), reads=[B_imp], writes=[B_m8])
            P.op("dve", lambda e: e.tensor_scalar(out=imp3[:, :], in0=imp[:, :], scalar1=m8[:, 7:8], scalar2=None, op0=ALU.is_ge),
                 reads=[B_imp, B_m8], writes=[B_imp3])
            P.op("dve", lambda e: e.scalar_tensor_tensor(out=imp3[:, :], in0=imp3[:, :], scalar=-3.0e38, in1=imp[:, :],
                                                         op0=ALU.mult, op1=ALU.add), reads=[B_imp, B_imp3], writes=[B_imp3])
            P.op("dve", lambda e: e.max(out=m8[:, 8:16], in_=imp3[:, :]), reads=[B_imp3], writes=[B_m8])
            P.op("dve", lambda e: e.tensor_scalar(out=nsel[:, :], in0=imp[:, :], scalar1=m8[:, 15:16], scalar2=None, op0=ALU.is_ge),
                 reads=[B_imp, B_m8], writes=[B_nsel])
            P.op("dve", lambda e: e.tensor_scalar(out=nsel[:, :], in0=nsel[:, :], scalar1=-NEGM, scalar2=NEGM, op0=ALU.mult, op1=ALU.add),
                 reads=[B_nsel], writes=[B_nsel])
            pt, Bp = next_ps("b")
            P.op("pe", lambda e, pt=pt: e.transpose(out=pt[0:64, 0:128], in_=nsel[:, :], identity=ident[:, :]),
                 reads=[B_nsel, B_ident], writes=[Bp])
            for hh in range(4):
                P.op("act", lambda e, pt=pt, hh=hh: e.activation(out=nselT[:, hh * 128:(hh + 1) * 128], in_=pt[0:64, 0:128],
                                                                 func=AF.Identity), reads=[Bp], writes=[B_nselT])
            if _LIM < 5.4:
                continue
            psO, BpO = next_ps("b")
            for c in range(B + 1):
                pt, Bp = next_ps("a")
                P.op("pe", lambda e, pt=pt, c=c, hs=hs, gp=gp, qv=qv: e.matmul(
                    pt[:, :], lhsT=kslc_g[hs, c * 128:(c + 1) * 128], rhs=qv, start=True, stop=False),
                    reads=[B_kslcg, B_qg], writes=[Bp])
                P.op("pe", lambda e, pt=pt, c=c: e.matmul(pt[:, :], lhsT=Et[:, c * 128:(c + 1) * 128], rhs=nselT[:, :],
                                                          start=False, stop=False), reads=[B_Et, B_nselT], writes=[Bp])
                P.op("pe", lambda e, pt=pt, c=c, B=B: e.matmul(pt[:, :], lhsT=lnt[0:2, c * 128:(c + 1) * 128], rhs=R2[0:2, :],
                                                               start=False, stop=(c != B)), reads=[B_lnt, B_R2], writes=[Bp])
                if c == B:
                    P.op("pe", lambda e, pt=pt: e.matmul(pt[:, :], lhsT=identb[:, :], rhs=caus[:, :], start=False, stop=True),
                         reads=[B_identb, B_caus], writes=[Bp])
                t, Bt = softmax_chunk(pt, Bp, 128, 4 * g, lambda h, d=B - c: h * 32 + d, bs, B_bs)
                for hh in range(4):
                    P.op("pe", lambda e, hh=hh, t=t, c=c, g=g, B=B, psO=psO: e.matmul(
                        psO[:, hh * 65:(hh + 1) * 65], lhsT=t[:, hh, :], rhs=vslc[:, c, g, :], start=(c == 0), stop=(c == B)),
                        reads=[Bt[hh], B_vslc], writes=[BpO])
            finish_branch(psO, BpO, k, g, 1, False)
            if _LIM < 5.5:
                continue
            psO, BpO = next_ps("b")
            for c in range(B - 4, B + 1):
                cw = c - 20
                pt, Bp = next_ps("a")
                P.op("pe", lambda e, pt=pt, cw=cw, hs=hs, gp=gp, qv=qv: e.matmul(
                    pt[:, :], lhsT=kwin_g[hs, cw * 128:(cw + 1) * 128], rhs=qv, start=True, stop=False),
                    reads=[B_kwing, B_qg], writes=[Bp])
                edge = (c == B) or (c == B - 4)
                P.op("pe", lambda e, pt=pt, c=c, edge=edge: e.matmul(pt[:, :], lhsT=lnt[0:2, c * 128:(c + 1) * 128], rhs=R2[0:2, :],
                                                                      start=False, stop=(not edge)), reads=[B_lnt, B_R2], writes=[Bp])
                if edge:
                    mk, Bmk = (caus, B_caus) if c == B else (low, B_low)
                    P.op("pe", lambda e, pt=pt, mk=mk: e.matmul(pt[:, :], lhsT=identb[:, :], rhs=mk[:, :], start=False, stop=True),
                         reads=[B_identb, Bmk], writes=[Bp])
                t, Bt = softmax_chunk(pt, Bp, 128, 4 * g, lambda h, d=B - c: h * 32 + d, bs, B_bs)
                for hh in range(4):
                    P.op("pe", lambda e, hh=hh, t=t, cw=cw, c=c, g=g, B=B, psO=psO: e.matmul(
                        psO[:, hh * 65:(hh + 1) * 65], lhsT=t[:, hh, :], rhs=vwin[:, cw, g, :], start=(c == B - 4), stop=(c == B)),
                        reads=[Bt[hh], B_vwin], writes=[BpO])
            finish_branch(psO, BpO, k, g, 2, False)

            if _LIM < 5.6:
                continue
            if debug:
                P.dma("sp", lambda e, k=k, g=g: e.dma_start(out=L["d_onsa"][k * 128:(k + 1) * 128, g * 256:(g + 1) * 256], in_=o_blk[:, 0:256]),
                      reads=[B_oblk])
            for j in range(2):
                pt, Bp = next_ps("a")
                P.op("pe", lambda e, pt=pt, j=j: e.transpose(out=pt[:, 0:128], in_=o_blk[:, j * 128:(j + 1) * 128],
                                                             identity=ident[:, :]), reads=[B_oblk, B_ident], writes=[Bp])
                P.op("dve", lambda e, pt=pt, j=j, k=k, g=g: e.tensor_copy(out=o_nsaT[:, 2 * g + j, k * 128:(k + 1) * 128], in_=pt[:, 0:128]),
                     reads=[Bp], writes=[B_onsaT])


def hgrn_outputs(nc, P, sc, sbt, next_ps, L):
    xf = L["xf"]
    ident, B_ident = L["ident"], L["B_ident"]
    ones_c, B_ones = L["ones_c"], L["B_ones"]
    o_hgT, B_ohgT = L["o_hgT"], L["B_ohgT"]
    debug = L["debug"]

    def table(name, shape, dt, src, q="sp"):
        t, B = sbt(sc, name, shape, dt)
        P.dma(q, lambda e: e.dma_start(out=t[:], in_=src), writes=[B])
        return t, B
    lm, B_lm = table("h_lm", [128, 128], F32, L["c_lm"])
    u32, B_u32 = table("h_u32", [128, 128], F32, L["t_u32"])
    l32, B_l32 = table("h_l32", [128, 128], F32, L["t_l32"])
    ind4, B_ind4 = table("h_ind4", [128, 4], F32, L["t_ind4"])
    vmask, B_vmask = table("h_vmask", [128, NB + 1], F32, L["t_vmask"])
    eps_c, B_eps = sbt(sc, "eps_c", [128, 1], F32)
    P.op("dve", lambda e: e.memset(eps_c[:], LN_EPS), writes=[B_eps])

    W3, B_W3 = sbt(sc, "W3", [128, KC, 2048], BF16)
    b3, B_b3 = sbt(sc, "b3", [128, 2048], F32)
    oml, B_oml = sbt(sc, "oml3", [128, 512], F32)
    gtmp, B_gtmp = sbt(sc, "gtmp", [128, 2, 512], F32)
    ngb, B_ngb = sbt(sc, "ngb", [128, 512], F32)
    xt = [sbt(sc, "hx%d" % i, [128, KC, 128], BF16) for i in range(2)]
    names32 = ["kk", "lgf", "ebc", "ebi", "edd", "qe", "ke", "hgb", "osb", "og"]
    T = {n: sbt(sc, "h_" + n, [128, 512], F32) for n in names32}
    vv, B_vv = sbt(sc, "h_vv", [128, 512], BF16)
    kd4, B_kd4 = sbt(sc, "h_kd4", [128, 4, 512], BF16)
    kdb, B_kdb = sbt(sc, "h_kdb", [128, 512], BF16)
    qeT4, B_qeT4 = sbt(sc, "h_qeT4", [128, 4, 4, 128], BF16)
    keT, B_keT = sbt(sc, "h_keT", [128, 4, 128], BF16)
    attm, B_attm = sbt(sc, "h_attm", [128, 4, 128], BF16)
    S, B_S = sbt(sc, "h_S", [128, 4, 128], F32)
    Sb, B_Sb = sbt(sc, "h_Sb", [128, 4, 128], BF16)
    Sld, B_Sld = sbt(sc, "h_Sld", [128, 4, 4, 128], BF16)
    edl, B_edl = sbt(sc, "h_edl", [128, 16], F32)
    ss, B_ss = sbt(sc, "h_ss", [128, 4], F32)
    P.op("dve", lambda e: e.memset(qeT4[:], 0.0), writes=[B_qeT4])
    xf_v = xf.rearrange("(kc p) t -> p kc t", p=128)

    for hp in range(2):
        w3_v = L["w_h3"][hp].rearrange("(kc p) c -> p kc c", p=128)
        for q in range(4):
            P.dma("pool", lambda e, q=q, w3_v=w3_v: e.dma_start(out=W3[:, 4 * q:4 * q + 4, :], in_=w3_v[:, 4 * q:4 * q + 4, :]),
                  writes=[B_W3])
        P.dma("sp", lambda e, hp=hp: e.dma_start(out=b3[:], in_=L["b_h3"][hp].partition_broadcast(128)), writes=[B_b3])
        P.dma("sp", lambda e, hp=hp: e.dma_start(out=ngb[:], in_=L["n_h3"][hp].partition_broadcast(128)), writes=[B_ngb])
        for r in range(2):
            P.dma("sp", lambda e, hp=hp, r=r: e.dma_start(out=gtmp[:, r, :], in_=L["g_h3"][hp, r].partition_broadcast(128)),
                  writes=[B_gtmp])
        P.op("dve", lambda e: e.tensor_tensor(out=oml[:], in0=gtmp[:, 1, :], in1=gtmp[:, 0, :], op=ALU.subtract),
             reads=[B_gtmp], writes=[B_oml])
        P.op("act", lambda e: e.activation(out=oml[:], in_=oml[:], func=AF.Sigmoid), reads=[B_oml], writes=[B_oml])
        for j in range(4):
            P.dma("pool", lambda e, hp=hp, j=j: e.dma_start(out=Sld[:, j, :, :], in_=L["st_in"][j, 4 * hp:4 * hp + 4].rearrange("h k v -> k h v")),
                  writes=[B_Sld])
        P.op("dve", lambda e: e.memset(S[:], 0.0), writes=[B_S])
        P.op("dve", lambda e: e.memset(Sb[:], 0.0), writes=[B_Sb])

        for p in range(NB + 1):
            own = p >= OWN0
            sample = p == NB
            xb, Bx = xt[p % 2]
            P.dma("pool", lambda e, xb=xb, p=p: e.dma_start(out=xb[:, :, :], in_=xf_v[:, :, p * 128:(p + 1) * 128]), writes=[Bx])

            def proj(cg, xb=xb, Bx=Bx):
                pt, Bp = next_ps("a")
                for kc in range(KC):
                    P.op("pe", lambda e, pt=pt, kc=kc, cg=cg, xb=xb: e.matmul(
                        pt[:, :], lhsT=xb[:, kc, :], rhs=W3[:, kc, cg * 512:(cg + 1) * 512],
                        start=(kc == 0), stop=(kc == KC - 1)), reads=[Bx, B_W3], writes=[Bp])
                return pt, Bp

            def ew(eng, name, fn, reads, writes_name):
                t, Bt = T[writes_name]
                P.op(eng, fn, reads=reads, writes=[Bt])

            kk, Bkk = T["kk"]
            lgf, Blgf = T["lgf"]
            pt, Bp = proj(1)
            P.op("dve", lambda e, pt=pt: e.tensor_tensor(out=kk[:], in0=pt[:, :], in1=b3[:, 512:1024], op=ALU.add),
                 reads=[Bp, B_b3], writes=[Bkk])
            P.op("act", lambda e: e.activation(out=kk[:], in_=kk[:], func=AF.Sigmoid, scale=-1.0), reads=[Bkk], writes=[Bkk])
            P.op("dve", lambda e: e.tensor_tensor(out=kk[:], in0=kk[:], in1=oml[:], op=ALU.mult), reads=[Bkk, B_oml], writes=[Bkk])
            P.op("act", lambda e: e.activation(out=lgf[:], in_=kk[:], func=AF.Ln, scale=-1.0, bias=ones_c[:, :]),
                 reads=[Bkk, B_ones], writes=[Blgf])
            pt, Bp = proj(2)
            if own:
                P.op("dve", lambda e, pt=pt: e.tensor_tensor(out=vv[:], in0=pt[:, :], in1=b3[:, 1024:1536], op=ALU.add),
                     reads=[Bp, B_b3], writes=[B_vv])
            else:
                osb_, Bosb_ = T["osb"]
                P.op("dve", lambda e, pt=pt: e.tensor_tensor(out=osb_[:], in0=pt[:, :], in1=b3[:, 1024:1536], op=ALU.add),
                     reads=[Bp, B_b3], writes=[Bosb_])
                P.op("dve", lambda e, p=p: e.tensor_scalar(out=vv[:], in0=osb_[:], scalar1=vmask[:, p:p + 1], scalar2=None, op0=ALU.mult),
                     reads=[Bosb_, B_vmask], writes=[B_vv])

            if not own:
                edd, Bedd = T["edd"]
                pt, Bp = next_ps("a")
                P.op("pe", lambda e, pt=pt: e.matmul(pt[:, :], lhsT=lm[:, :], rhs=lgf[:, :], start=True, stop=True),
                     reads=[B_lm, Blgf], writes=[Bp])
                P.op("act", lambda e, pt=pt: e.activation(out=edd[:], in_=pt[:, :], func=AF.Exp), reads=[Bp], writes=[Bedd])
                P.op("dve", lambda e: e.tensor_tensor(out=kdb[:], in0=kk[:], in1=edd[:], op=ALU.mult), reads=[Bkk, Bedd], writes=[B_kdb])
                pt, Bp = next_ps("b")
                for h in range(4):
                    P.op("pe", lambda e, pt=pt, h=h: e.matmul(pt[:, h:h + 1], lhsT=lgf[:, h * 128:(h + 1) * 128], rhs=ones_c[:, :],
                                                              start=True, stop=True), reads=[Blgf, B_ones], writes=[Bp])
                P.op("act", lambda e, pt=pt: e.activation(out=edl[:, 0:4], in_=pt[:, 0:4], func=AF.Exp), reads=[Bp], writes=[B_edl])
                pt, Bp = next_ps("b")
                for h in range(4):
                    P.op("pe", lambda e, pt=pt, h=h: e.matmul(pt[:, h * 128:(h + 1) * 128], lhsT=kdb[:, h * 128:(h + 1) * 128],
                                                              rhs=vv[:, h * 128:(h + 1) * 128], start=True, stop=True),
                         reads=[B_kdb, B_vv], writes=[Bp])
                for h in range(4):
                    P.op("dve", lambda e, pt=pt, h=h: e.scalar_tensor_tensor(
                        out=S[:, h, :], in0=S[:, h, :], scalar=edl[:, h:h + 1], in1=pt[:, h * 128:(h + 1) * 128],
                        op0=ALU.mult, op1=ALU.add), reads=[Bp, B_edl, B_S], writes=[B_S])
                if p == OWN0 - 1:
                    P.op("dve", lambda e: e.tensor_copy(out=Sb[:], in_=S[:]), reads=[B_S], writes=[B_Sb])
                continue

            ebc, Bebc = T["ebc"]
            ebi, Bebi = T["ebi"]
            edd, Bedd = T["edd"]
            qe, Bqe = T["qe"]
            ke, Bke = T["ke"]
            hgb, Bhgb = T["hgb"]
            osb, Bosb = T["osb"]
            og, Bog = T["og"]
            pt, Bp = next_ps("a")
            P.op("pe", lambda e, pt=pt: e.matmul(pt[:, :], lhsT=u32[:, :], rhs=lgf[:, :], start=True, stop=True),
                 reads=[B_u32, Blgf], writes=[Bp])
            P.op("act", lambda e, pt=pt: e.activation(out=ebc[:], in_=pt[:, :], func=AF.Exp), reads=[Bp], writes=[Bebc])
            P.op("act", lambda e, pt=pt: e.activation(out=ebi[:], in_=pt[:, :], func=AF.Exp, scale=-1.0), reads=[Bp], writes=[Bebi])
            pt, Bp = next_ps("a")
            P.op("pe", lambda e, pt=pt: e.matmul(pt[:, :], lhsT=l32[:, :], rhs=lgf[:, :], start=True, stop=True),
                 reads=[B_l32, Blgf], writes=[Bp])
            P.op("act", lambda e, pt=pt: e.activation(out=edd[:], in_=pt[:, :], func=AF.Exp), reads=[Bp], writes=[Bedd])
            pt, Bp = next_ps("b")
            for h in range(4):
                P.op("pe", lambda e, pt=pt, h=h: e.matmul(pt[:, h * 4:h * 4 + 4], lhsT=lgf[:, h * 128:(h + 1) * 128], rhs=ind4[:, :],
                                                          start=True, stop=True), reads=[Blgf, B_ind4], writes=[Bp])
            P.op("act", lambda e, pt=pt: e.activation(out=edl[:, 0:16], in_=pt[:, 0:16], func=AF.Exp), reads=[Bp], writes=[B_edl])
            pt, Bp = proj(0)
            P.op("dve", lambda e, pt=pt: e.tensor_tensor(out=qe[:], in0=pt[:, :], in1=b3[:, 0:512], op=ALU.add),
                 reads=[Bp, B_b3], writes=[Bqe])
            P.op("act", lambda e: e.activation(out=og[:], in_=qe[:], func=AF.Sigmoid), reads=[Bqe], writes=[Bog])
            P.op("dve", lambda e: e.tensor_tensor(out=qe[:], in0=qe[:], in1=og[:], op=ALU.mult), reads=[Bqe, Bog], writes=[Bqe])
            P.op("dve", lambda e: e.tensor_tensor(out=qe[:], in0=qe[:], in1=ebc[:], op=ALU.mult), reads=[Bqe, Bebc], writes=[Bqe])
            P.op("dve", lambda e: e.tensor_tensor(out=ke[:], in0=kk[:], in1=ebi[:], op=ALU.mult), reads=[Bkk, Bebi], writes=[Bke])
            P.op("dve", lambda e: e.tensor_tensor(out=edd[:], in0=kk[:], in1=edd[:], op=ALU.mult), reads=[Bkk, Bedd], writes=[Bedd])
            if not sample:
                for c in range(4):
                    P.op("dve", lambda e, c=c: e.tensor_scalar(out=kd4[:, c, :], in0=edd[:], scalar1=ind4[:, c:c + 1], scalar2=None,
                                                               op0=ALU.mult), reads=[Bedd, B_ind4], writes=[B_kd4])
            pt, Bp = proj(3)
            P.op("dve", lambda e, pt=pt: e.tensor_tensor(out=hgb[:], in0=pt[:, :], in1=b3[:, 1536:2048], op=ALU.add),
                 reads=[Bp, B_b3], writes=[Bhgb])
            P.op("act", lambda e: e.activation(out=og[:], in_=hgb[:], func=AF.Sigmoid), reads=[Bhgb], writes=[Bog])
            P.op("dve", lambda e: e.tensor_tensor(out=hgb[:], in0=hgb[:], in1=og[:], op=ALU.mult), reads=[Bhgb, Bog], writes=[Bhgb])
            P.op("dve", lambda e: e.tensor_tensor(out=hgb[:], in0=hgb[:], in1=ngb[:], op=ALU.mult), reads=[Bhgb, B_ngb], writes=[Bhgb])
            for h in range(4):
                pt, Bp = next_ps("a")
                P.op("pe", lambda e, pt=pt, h=h: e.transpose(out=pt[:, 0:128], in_=qe[:, h * 128:(h + 1) * 128], identity=ident[:, :]),
                     reads=[Bqe, B_ident], writes=[Bp])
                for c in range(4):
                    P.op("act", lambda e, pt=pt, h=h, c=c: e.activation(out=qeT4[:, h, c, 32 * c:32 * c + 32], in_=pt[:, 32 * c:32 * c + 32],
                                                                         func=AF.Identity), reads=[Bp], writes=[B_qeT4])
                pt, Bp = next_ps("a")
                P.op("pe", lambda e, pt=pt, h=h: e.transpose(out=pt[:, 0:128], in_=ke[:, h * 128:(h + 1) * 128], identity=ident[:, :]),
                     reads=[Bke, B_ident], writes=[Bp])
                P.op("dve", lambda e, pt=pt, h=h: e.tensor_copy(out=keT[:, h, :], in_=pt[:, 0:128]), reads=[Bp], writes=[B_keT])
            for h in range(4):
                pt, Bp = next_ps("a")
                for c in range(4):
                    P.op("pe", lambda e, pt=pt, h=h, c=c: e.matmul(pt[:, 0:128], lhsT=keT[:, h, :], rhs=qeT4[:, h, c, :],
                                                                   start=(c == 0), stop=(c == 3)), reads=[B_keT, B_qeT4], writes=[Bp])
                P.op("dve", lambda e, pt=pt, h=h: e.tensor_tensor(out=attm[:, h, :], in0=pt[:, 0:128], in1=u32[:, :], op=ALU.mult),
                     reads=[Bp, B_u32], writes=[B_attm])
            po, Bpo = next_ps("b")
            for h in range(4):
                for c in range(4):
                    if sample:
                        rhs_fn = lambda c=c, h=h: Sld[:, c, h, :]
                        Brhs = B_Sld
                    else:
                        rhs_fn = lambda c=c, h=h: Sb[:, h, :]
                        Brhs = B_Sb
                    P.op("pe", lambda e, po=po, h=h, c=c, rhs_fn=rhs_fn: e.matmul(
                        po[:, h * 128:(h + 1) * 128], lhsT=qeT4[:, h, c, :], rhs=rhs_fn(), start=(c == 0), stop=False),
                        reads=[B_qeT4, Brhs], writes=[Bpo])
                    if not sample:
                        ps2, Bp2 = next_ps("a")
                        P.op("pe", lambda e, ps2=ps2, h=h, c=c: e.matmul(ps2[:, 0:128], lhsT=kd4[:, c, h * 128:(h + 1) * 128],
                                                                         rhs=vv[:, h * 128:(h + 1) * 128], start=True, stop=True),
                             reads=[B_kd4, B_vv], writes=[Bp2])
                        P.op("dve", lambda e, ps2=ps2, h=h, c=c: e.scalar_tensor_tensor(
                            out=S[:, h, :], in0=S[:, h, :], scalar=edl[:, h * 4 + c:h * 4 + c + 1], in1=ps2[:, 0:128],
                            op0=ALU.mult, op1=ALU.add), reads=[Bp2, B_edl, B_S], writes=[B_S])
                        P.op("act", lambda e, h=h: e.activation(out=Sb[:, h, :], in_=S[:, h, :], func=AF.Identity),
                             reads=[B_S], writes=[B_Sb])
                P.op("pe", lambda e, po=po, h=h: e.matmul(po[:, h * 128:(h + 1) * 128], lhsT=attm[:, h, :], rhs=vv[:, h * 128:(h + 1) * 128],
                                                          start=False, stop=True), reads=[B_attm, B_vv], writes=[Bpo])
            P.op("act", lambda e, po=po: e.activation(out=osb[:], in_=po[:, :], func=AF.Identity), reads=[Bpo], writes=[Bosb])
            P.op("dve", lambda e: e.tensor_tensor(out=og[:], in0=osb[:], in1=osb[:], op=ALU.mult), reads=[Bosb], writes=[Bog])
            P.op("dve", lambda e: e.reduce_sum(out=ss[:, 0:4], in_=og[:].rearrange("p (h v) -> p h v", h=4), axis=mybir.AxisListType.X),
                 reads=[Bog], writes=[B_ss])
            P.op("act", lambda e: e.activation(out=ss[:, 0:4], in_=ss[:, 0:4], func=AF.Sqrt, scale=1.0 / 128.0, bias=eps_c[:, :]),
                 reads=[B_ss, B_eps], writes=[B_ss])
            P.op("dve", lambda e: e.reciprocal(out=ss[:, 0:4], in_=ss[:, 0:4]), reads=[B_ss], writes=[B_ss])
            for h in range(4):
                P.op("dve", lambda e, h=h: e.scalar_tensor_tensor(
                    out=og[:, h * 128:(h + 1) * 128], in0=osb[:, h * 128:(h + 1) * 128], scalar=ss[:, h:h + 1],
                    in1=hgb[:, h * 128:(h + 1) * 128], op0=ALU.mult, op1=ALU.mult), reads=[Bosb, B_ss, Bhgb], writes=[Bog])
            if debug:
                k_ = p - OWN0
                P.dma("sp", lambda e, k_=k_, hp=hp: e.dma_start(out=L["d_ohg"][k_ * 128:(k_ + 1) * 128, hp * 512:(hp + 1) * 512], in_=og[:, :]),
                      reads=[Bog])
            for h in range(4):
                pt, Bp = next_ps("a")
                P.op("pe", lambda e, pt=pt, h=h: e.transpose(out=pt[:, 0:128], in_=og[:, h * 128:(h + 1) * 128], identity=ident[:, :]),
                     reads=[Bog, B_ident], writes=[Bp])
                P.op("dve", lambda e, pt=pt, h=h, p=p, hp=hp: e.tensor_copy(
                    out=o_hgT[:, 4 * hp + h, (p - OWN0) * 128:(p - OWN0 + 1) * 128], in_=pt[:, 0:128]), reads=[Bp], writes=[B_ohgT])
        P.barrier()


def tail_moe(nc, P, sc, sbt, next_ps, L):
    xf = L["xf"]
    ident, B_ident = L["ident"], L["B_ident"]
    ones_c, B_ones = L["ones_c"], L["B_ones"]
    o_nsaT, B_onsaT, o_hgT, B_ohgT = L["o_nsaT"], L["B_onsaT"], L["o_hgT"], L["B_ohgT"]
    debug = L["debug"]
    y_out = L["y_out"]
    xf_v = xf.rearrange("(kc p) t -> p kc t", p=128)
    T0 = OWN0 * 128

    zacc, B_zacc = sbt(sc, "zacc", [128, KC, NT], F32)
    hT, B_hT = L["ohT"], L["B_oh"]
    lncol, B_lncol = sbt(sc, "lncol", [128, 4, KC], F32)
    for i, nm in enumerate(("ln1_g", "ln1_b", "ln2_g", "ln2_b")):
        P.dma("sp", lambda e, i=i, nm=nm: e.dma_start(out=lncol[:, i, :], in_=L[nm].rearrange("(kc p) -> p kc", p=128),
                                                      allow_slow_non_contiguous=True), writes=[B_lncol])
    eps_c, B_eps = sbt(sc, "eps_t", [128, 1], F32)
    P.op("dve", lambda e: e.memset(eps_c[:], LN_EPS), writes=[B_eps])
    onesr, B_onesr = sbt(sc, "onesr", [1, 128], F32)
    P.op("dve", lambda e: e.memset(onesr[:], 1.0), writes=[B_onesr])

    def layer_norm(gi, bi, emit_out):
        for (t0, tw) in TT:
            p1, Bp1 = next_ps("b")
            p2, Bp2 = next_ps("b")
            for m in range(KC):
                P.op("pe", lambda e, p1=p1, m=m, t0=t0, tw=tw: e.matmul(p1[0:1, 0:tw], lhsT=ones_c[:, 0:1], rhs=zacc[:, m, t0:t0 + tw],
                                                                        start=(m == 0), stop=(m == KC - 1)),
                     reads=[B_ones, B_zacc], writes=[Bp1])
                P.op("act", lambda e, m=m, t0=t0, tw=tw: e.activation(out=sqs[:, 0:tw], in_=zacc[:, m, t0:t0 + tw], func=AF.Square),
                     reads=[B_zacc], writes=[B_sqs])
                P.op("pe", lambda e, p2=p2, m=m, tw=tw: e.matmul(p2[0:1, 0:tw], lhsT=ones_c[:, 0:1], rhs=sqs[:, 0:tw],
                                                                 start=(m == 0), stop=(m == KC - 1)),
                     reads=[B_ones, B_sqs], writes=[Bp2])
            P.op("dve", lambda e, p1=p1, t0=t0, tw=tw: e.tensor_scalar(out=stat[:, 0, t0:t0 + tw], in0=p1[0:1, 0:tw], scalar1=1.0 / D_MODEL,
                                                                       scalar2=None, op0=ALU.mult), reads=[Bp1], writes=[B_stat])
            P.op("dve", lambda e, p2=p2, t0=t0, tw=tw: e.tensor_scalar(out=stat[:, 1, t0:t0 + tw], in0=p2[0:1, 0:tw], scalar1=1.0 / D_MODEL,
                                                                       scalar2=None, op0=ALU.mult), reads=[Bp2], writes=[B_stat])
            P.op("dve", lambda e, t0=t0, tw=tw: e.tensor_tensor(out=sqs[0:1, 0:tw], in0=stat[:, 0, t0:t0 + tw], in1=stat[:, 0, t0:t0 + tw],
                                                                op=ALU.mult), reads=[B_stat], writes=[B_sqs])
            P.op("dve", lambda e, t0=t0, tw=tw: e.tensor_tensor(out=stat[:, 1, t0:t0 + tw], in0=stat[:, 1, t0:t0 + tw], in1=sqs[0:1, 0:tw],
                                                                op=ALU.subtract), reads=[B_stat, B_sqs], writes=[B_stat])
            P.op("act", lambda e, t0=t0, tw=tw: e.activation(out=stat[:, 1, t0:t0 + tw], in_=stat[:, 1, t0:t0 + tw], func=AF.Sqrt,
                                                             bias=eps_c[0:1, :]), reads=[B_stat, B_eps], writes=[B_stat])
            P.op("dve", lambda e, t0=t0, tw=tw: e.reciprocal(out=stat[:, 1, t0:t0 + tw], in_=stat[:, 1, t0:t0 + tw]),
                 reads=[B_stat], writes=[B_stat])
            for r in range(2):
                pb, Bpb = next_ps("b")
                P.op("pe", lambda e, pb=pb, r=r, t0=t0, tw=tw: e.matmul(pb[:, 0:tw], lhsT=onesr[0:1, :], rhs=stat[:, r, t0:t0 + tw],
                                                                        start=True, stop=True), reads=[B_onesr, B_stat], writes=[Bpb])
                P.op("act", lambda e, pb=pb, r=r, t0=t0, tw=tw: e.activation(out=mbc[:, r, t0:t0 + tw], in_=pb[:, 0:tw], func=AF.Identity),
                     reads=[Bpb], writes=[B_mbc])
        for m in range(KC):
            P.op("dve", lambda e, m=m: e.tensor_tensor(out=zacc[:, m, :], in0=zacc[:, m, :], in1=mbc[:, 0, :], op=ALU.subtract),
                 reads=[B_zacc, B_mbc], writes=[B_zacc])
            P.op("dve", lambda e, m=m: e.tensor_tensor(out=zacc[:, m, :], in0=zacc[:, m, :], in1=mbc[:, 1, :], op=ALU.mult),
                 reads=[B_zacc, B_mbc], writes=[B_zacc])
            P.op("dve", lambda e, m=m: e.tensor_scalar(out=zacc[:, m, :], in0=zacc[:, m, :], scalar1=lncol[:, gi, m:m + 1],
                                                       scalar2=lncol[:, bi, m:m + 1], op0=ALU.mult, op1=ALU.add),
                 reads=[B_zacc, B_lncol], writes=[B_zacc])
            emit_out(m)

    t1 = contextlib.ExitStack()
    with t1:
        mT, B_mT = sbt(t1, "mT", [128, KC, NT], BF16)
        t1a = t1.enter_context(contextlib.ExitStack())
        xt_, B_xt = sbt(t1a, "xt_", [128, KC, 512], BF16)
        wpa = [sbt(t1a, "wpa%d" % i, [128, 8, 128], BF16) for i in range(2)]
        wpb = [sbt(t1a, "wpb%d" % i, [128, 8, 128], BF16) for i in range(2)]
        wmg = [sbt(t1a, "wmg%d" % i, [128, KC, 256], BF16) for i in range(2)]
        bmg, B_bmg = sbt(t1a, "bmg", [128, 32], F32)
        sga, B_sga = sbt(t1a, "sga", [128, 512], F32)
        sgb, B_sgb = sbt(t1a, "sgb", [128, 512], F32)
        P.dma("sp", lambda e: e.dma_start(out=bmg[:], in_=L["b_mg"].rearrange("(cb p) -> p cb", p=128), allow_slow_non_contiguous=True),
              writes=[B_bmg])
        wmg_v = L["w_mg"].rearrange("(kc p) c -> p kc c", p=128)
        wpa_v = L["w_pa"].rearrange("(kc p) c -> p kc c", p=128)
        wpb_v = L["w_pb"].rearrange("(kc p) c -> p kc c", p=128)
        it = 0
        for (t0, tw) in TT:
            for q in range(2):
                P.dma("pool", lambda e, q=q, t0=t0, tw=tw: e.dma_start(out=xt_[:, 8 * q:8 * q + 8, 0:tw],
                                                                       in_=xf_v[:, 8 * q:8 * q + 8, T0 + t0:T0 + t0 + tw]), writes=[B_xt])
            for m in range(KC):
                wm, Bwm = wmg[it % 2]
                wa, Bwa = wpa[it % 2]
                wb, Bwb = wpb[it % 2]
                it += 1
                P.dma("pool", lambda e, wm=wm, m=m: e.dma_start(out=wm[:, :, 0:128], in_=wmg_v[:, :, m * 128:(m + 1) * 128]), writes=[Bwm])
                P.dma("pool", lambda e, wm=wm, m=m: e.dma_start(out=wm[:, :, 128:256], in_=wmg_v[:, :, 2048 + m * 128:2048 + (m + 1) * 128]),
                      writes=[Bwm])
                P.dma("pool", lambda e, wa=wa, m=m: e.dma_start(out=wa[:, :, :], in_=wpa_v[:, :, m * 128:(m + 1) * 128]), writes=[Bwa])
                P.dma("pool", lambda e, wb=wb, m=m: e.dma_start(out=wb[:, :, :], in_=wpb_v[:, :, m * 128:(m + 1) * 128]), writes=[Bwb])
                pA, BpA = next_ps("a")
                pB, BpB = next_ps("a")
                pga, Bpga = next_ps("a")
                pgb, Bpgb = next_ps("a")
                for kc in range(8):
                    P.op("pe", lambda e, pA=pA, kc=kc, wa=wa, t0=t0, tw=tw: e.matmul(
                        pA[:, 0:tw], lhsT=wa[:, kc, :], rhs=o_nsaT[:, kc, t0:t0 + tw], start=(kc == 0), stop=(kc == 7)),
                        reads=[Bwa, B_onsaT], writes=[BpA])
                for kc in range(8):
                    P.op("pe", lambda e, pB=pB, kc=kc, wb=wb, t0=t0, tw=tw: e.matmul(
                        pB[:, 0:tw], lhsT=wb[:, kc, :], rhs=o_hgT[:, kc, t0:t0 + tw], start=(kc == 0), stop=(kc == 7)),
                        reads=[Bwb, B_ohgT], writes=[BpB])
                for kc in range(KC):
                    P.op("pe", lambda e, pga=pga, kc=kc, wm=wm, tw=tw: e.matmul(
                        pga[:, 0:tw], lhsT=wm[:, kc, 0:128], rhs=xt_[:, kc, 0:tw], start=(kc == 0), stop=(kc == KC - 1)),
                        reads=[Bwm, B_xt], writes=[Bpga])
                for kc in range(KC):
                    P.op("pe", lambda e, pgb=pgb, kc=kc, wm=wm, tw=tw: e.matmul(
                        pgb[:, 0:tw], lhsT=wm[:, kc, 128:256], rhs=xt_[:, kc, 0:tw], start=(kc == 0), stop=(kc == KC - 1)),
                        reads=[Bwm, B_xt], writes=[Bpgb])
                P.op("act", lambda e, pga=pga, m=m, tw=tw: e.activation(out=sga[:, 0:tw], in_=pga[:, 0:tw], func=AF.Sigmoid,
                                                                        bias=bmg[:, m:m + 1]), reads=[Bpga, B_bmg], writes=[B_sga])
                P.op("act", lambda e, pgb=pgb, m=m, tw=tw: e.activation(out=sgb[:, 0:tw], in_=pgb[:, 0:tw], func=AF.Sigmoid,
                                                                        bias=bmg[:, 16 + m:17 + m]), reads=[Bpgb, B_bmg], writes=[B_sgb])
                P.op("dve", lambda e, pA=pA, tw=tw: e.tensor_tensor(out=sga[:, 0:tw], in0=sga[:, 0:tw], in1=pA[:, 0:tw], op=ALU.mult),
                     reads=[BpA, B_sga], writes=[B_sga])
                P.op("dve", lambda e, pB=pB, tw=tw: e.tensor_tensor(out=sgb[:, 0:tw], in0=sgb[:, 0:tw], in1=pB[:, 0:tw], op=ALU.mult),
                     reads=[BpB, B_sgb], writes=[B_sgb])
                P.op("dve", lambda e, m=m, t0=t0, tw=tw: e.tensor_tensor(out=mT[:, m, t0:t0 + tw], in0=sga[:, 0:tw], in1=sgb[:, 0:tw], op=ALU.add),
                     reads=[B_sga, B_sgb], writes=[B_mT])
        P.barrier()
        t1a.close()
        wout = [sbt(t1, "wout%d" % i, [128, KC, 128], BF16) for i in range(2)]
        xres = [sbt(t1, "xres%d" % i, [128, NT], F32) for i in range(2)]
        wout_v = L["w_out"].rearrange("(kc p) c -> p kc c", p=128)
        for m in range(KC):
            xr, Bxr = xres[m % 2]
            wo, Bwo = wout[m % 2]
            P.dma("pool", lambda e, wo=wo, m=m: e.dma_start(out=wo[:, :, :], in_=wout_v[:, :, m * 128:(m + 1) * 128]), writes=[Bwo])
            P.dma("sp", lambda e, xr=xr, m=m: e.dma_start(out=xr[:, :], in_=xf[m * 128:(m + 1) * 128, T0:T0 + NT]), writes=[Bxr])
            for (t0, tw) in TT:
                pt, Bp = next_ps("a")
                for kc in range(KC):
                    P.op("pe", lambda e, pt=pt, kc=kc, wo=wo, t0=t0, tw=tw: e.matmul(
                        pt[:, 0:tw], lhsT=wo[:, kc, :], rhs=mT[:, kc, t0:t0 + tw], start=(kc == 0), stop=(kc == KC - 1)),
                        reads=[Bwo, B_mT], writes=[Bp])
                P.op("dve", lambda e, pt=pt, xr=xr, m=m, t0=t0, tw=tw: e.scalar_tensor_tensor(
                    out=zacc[:, m, t0:t0 + tw], in0=xr[:, t0:t0 + tw], scalar=ALPHA, in1=pt[:, 0:tw], op0=ALU.mult, op1=ALU.add),
                    reads=[Bp, Bxr], writes=[B_zacc])
        P.barrier()

    stat, B_stat = sbt(sc, "stat", [1, 2, NT], F32)
    mbc, B_mbc = sbt(sc, "mbc", [128, 2, NT], F32)
    sqs, B_sqs = sbt(sc, "sqs", [128, 512], F32)

    def after_ln1(m):
        P.op("act", lambda e, m=m: e.activation(out=hT[:, m, :], in_=zacc[:, m, :], func=AF.Identity), reads=[B_zacc], writes=[B_hT])
        if debug:
            P.dma("sp", lambda e, m=m: e.dma_start(out=L["d_h"][m * 128:(m + 1) * 128, :], in_=zacc[:, m, :]), reads=[B_zacc])
        P.op("dve", lambda e, m=m: e.tensor_scalar(out=zacc[:, m, :], in0=zacc[:, m, :], scalar1=ALPHA, scalar2=None, op0=ALU.mult),
             reads=[B_zacc], writes=[B_zacc])
    layer_norm(0, 1, after_ln1)
    P.barrier()

    t3 = contextlib.ExitStack()
    with t3:
        wr, B_wr = sbt(t3, "wr", [128, KC, 36], BF16)
        brt, B_brt = sbt(t3, "brt", [128, 36], F32)
        lg, B_lg = sbt(t3, "lg", [128, 36], F32)
        lem, B_lem = sbt(t3, "lem", [128, 32], F32)
        sm, B_sm = sbt(t3, "sm", [128, 16], F32)
        m8, B_m8 = sbt(t3, "m8r", [128, 8], F32)
        gate, B_gate = sbt(t3, "gate", [128, 32], F32)
        gate2, B_gate2 = sbt(t3, "gate2", [128, 32], F32)
        gT, B_gT = sbt(t3, "gT", [32, NT], F32)
        gThi, B_gThi = sbt(t3, "gThi", [32, NT], BF16)
        gTlo, B_gTlo = sbt(t3, "gTlo", [32, NT], BF16)
        selE, B_selE = sbt(t3, "selE", [32, 32 * 128], BF16)
        gbc, B_gbc = sbt(t3, "gbc", [128, NT], F32)
        hid, B_hid = sbt(t3, "hid", [128, 4, NT], BF16)
        sil, B_sil = sbt(t3, "sil", [128, 512], F32)
        ug, B_ug = sbt(t3, "ugm", [128, 512], F32)
        wgu = [sbt(t3, "wgu%d" % i, [128, KC, 2, 128], BF16) for i in range(2)]
        wd = [sbt(t3, "wd%d" % i, [128, 4, D_MODEL], BF16) for i in range(_NWD)]
        P.dma("pool", lambda e: e.dma_start(out=wr[:], in_=L["w_r"].rearrange("(kc p) c -> p kc c", p=128)), writes=[B_wr])
        P.dma("sp", lambda e: e.dma_start(out=brt[:], in_=L["b_r"].partition_broadcast(128)), writes=[B_brt])
        P.dma("sp", lambda e: e.dma_start(out=selE[:], in_=L["t_selE"]), writes=[B_selE])
        for blk in range(NOWN + 1):
            bs_ = slice(blk * 128, (blk + 1) * 128)
            pt, Bp = next_ps("b")
            for kc in range(KC):
                P.op("pe", lambda e, pt=pt, kc=kc, bs_=bs_: e.matmul(pt[:, 0:36], lhsT=hT[:, kc, bs_], rhs=wr[:, kc, :],
                                                                     start=(kc == 0), stop=(kc == KC - 1)), reads=[B_hT, B_wr], writes=[Bp])
            P.op("dve", lambda e, pt=pt: e.tensor_tensor(out=lg[:], in0=pt[:, 0:36], in1=brt[:], op=ALU.add), reads=[Bp, B_brt], writes=[B_lg])
            P.op("dve", lambda e: e.reduce_max(out=sm[:, 0:1], in_=lg[:, 0:4], axis=mybir.AxisListType.X), reads=[B_lg], writes=[B_sm])
            P.op("dve", lambda e: e.tensor_scalar(out=sm[:, 1:2], in0=sm[:, 0:1], scalar1=-1.0, scalar2=None, op0=ALU.mult),
                 reads=[B_sm], writes=[B_sm])
            P.op("act", lambda e: e.activation(out=sm[:, 4:8], in_=lg[:, 0:4], func=AF.Exp, bias=sm[:, 1:2]), reads=[B_lg, B_sm], writes=[B_sm])
            P.op("dve", lambda e: e.reduce_sum(out=sm[:, 2:3], in_=sm[:, 4:8], axis=mybir.AxisListType.X), reads=[B_sm], writes=[B_sm])
            P.op("dve", lambda e: e.reciprocal(out=sm[:, 2:3], in_=sm[:, 2:3]), reads=[B_sm], writes=[B_sm])
            P.op("dve", lambda e: e.tensor_scalar(out=sm[:, 8:12], in0=lg[:, 0:4], scalar1=sm[:, 0:1], scalar2=None, op0=ALU.is_ge),
                 reads=[B_lg, B_sm], writes=[B_sm])
            P.op("dve", lambda e: e.tensor_scalar(out=sm[:, 8:12], in0=sm[:, 8:12], scalar1=1e30, scalar2=-1e30, op0=ALU.mult, op1=ALU.add),
                 reads=[B_sm], writes=[B_sm])
            for g in range(4):
                P.op("dve", lambda e, g=g: e.tensor_scalar(out=lem[:, g * 8:(g + 1) * 8], in0=lg[:, 4 + g * 8:4 + (g + 1) * 8],
                                                           scalar1=sm[:, 8 + g:9 + g], scalar2=None, op0=ALU.add),
                     reads=[B_lg, B_sm], writes=[B_lem])
            P.op("dve", lambda e: e.max(out=m8[:, 0:8], in_=lem[:, :]), reads=[B_lem], writes=[B_m8])
            P.op("dve", lambda e: e.tensor_tensor(out=sm[:, 12:13], in0=m8[:, 0:1], in1=m8[:, 1:2], op=ALU.subtract), reads=[B_m8], writes=[B_sm])
            P.op("act", lambda e: e.activation(out=sm[:, 13:14], in_=sm[:, 12:13], func=AF.Sigmoid), reads=[B_sm], writes=[B_sm])
            P.op("act", lambda e: e.activation(out=sm[:, 14:15], in_=sm[:, 12:13], func=AF.Sigmoid, scale=-1.0), reads=[B_sm], writes=[B_sm])
            P.op("dve", lambda e: e.tensor_scalar(out=sm[:, 13:15], in0=sm[:, 13:15], scalar1=sm[:, 2:3], scalar2=None, op0=ALU.mult),
                 reads=[B_sm], writes=[B_sm])
            P.op("dve", lambda e: e.tensor_scalar(out=gate[:], in0=lem[:], scalar1=m8[:, 0:1], scalar2=sm[:, 13:14], op0=ALU.is_equal, op1=ALU.mult),
                 reads=[B_lem, B_m8, B_sm], writes=[B_gate])
            P.op("dve", lambda e: e.tensor_scalar(out=gate2[:], in0=lem[:], scalar1=m8[:, 1:2], scalar2=sm[:, 14:15], op0=ALU.is_equal, op1=ALU.mult),
                 reads=[B_lem, B_m8, B_sm], writes=[B_gate2])
            P.op("dve", lambda e: e.tensor_tensor(out=gate[:], in0=gate[:], in1=gate2[:], op=ALU.add), reads=[B_gate, B_gate2], writes=[B_gate])
            pt, Bp = next_ps("b")
            P.op("pe", lambda e, pt=pt: e.transpose(out=pt[0:32, 0:128], in_=gate[:, :], identity=ident[:, :]), reads=[B_gate, B_ident], writes=[Bp])
            P.op("dve", lambda e, pt=pt, bs_=bs_: e.tensor_copy(out=gT[:, bs_], in_=pt[0:32, 0:128]), reads=[Bp], writes=[B_gT])
        if debug:
            P.dma("sp", lambda e: e.dma_start(out=L["d_gate"], in_=gT[:, :]), reads=[B_gT])
        P.op("dve", lambda e: e.tensor_copy(out=gThi[:], in_=gT[:]), reads=[B_gT], writes=[B_gThi])
        P.op("dve", lambda e: e.tensor_tensor(out=gT[:], in0=gT[:], in1=gThi[:], op=ALU.subtract), reads=[B_gT, B_gThi], writes=[B_gT])
        P.op("dve", lambda e: e.tensor_copy(out=gTlo[:], in_=gT[:]), reads=[B_gT], writes=[B_gTlo])

        L["pools"]["a"] = [0, 1, 2, 3, 4, 5, 6]
        L["pools"]["b"] = [7]
        NE = L["n_experts"]
        wg_v = L["w_gate"].rearrange("e (kc p) f -> e p kc f", p=128)
        wu_v = L["w_up"].rearrange("e (kc p) f -> e p kc f", p=128)
        wd_v = L["w_down"].rearrange("e (fc p) d -> e p fc d", p=128)
        def load_wgu(ex, fc):
            wt, Bwt = wgu[(ex * 4 + fc) % 2]
            P.dma("pool", lambda e, wt=wt, ex=ex, fc=fc: e.dma_start(out=wt[:, :, 0, :], in_=wg_v[ex, :, :, fc * 128:(fc + 1) * 128]), writes=[Bwt])
            P.dma("pool", lambda e, wt=wt, ex=ex, fc=fc: e.dma_start(out=wt[:, :, 1, :], in_=wu_v[ex, :, :, fc * 128:(fc + 1) * 128]), writes=[Bwt])

        def load_wd(ex):
            wdt, Bwd = wd[ex % len(wd)]
            for q in range(2):
                P.dma("pool", lambda e, wdt=wdt, ex=ex, q=q: e.dma_start(out=wdt[:, 2 * q:2 * q + 2, :], in_=wd_v[ex, :, 2 * q:2 * q + 2, :]), writes=[Bwd])

        load_wgu(0, 0)
        load_wd(0)
        for ex in range(NE):
            wdt, Bwd = wd[ex % len(wd)]
            for (t0, tw) in TT:
                pb, Bpb = next_ps("b")
                P.op("pe", lambda e, pb=pb, ex=ex, t0=t0, tw=tw: e.matmul(pb[:, 0:tw], lhsT=selE[:, ex * 128:(ex + 1) * 128], rhs=gThi[:, t0:t0 + tw],
                                                                          start=True, stop=False), reads=[B_selE, B_gThi], writes=[Bpb])
                P.op("pe", lambda e, pb=pb, ex=ex, t0=t0, tw=tw: e.matmul(pb[:, 0:tw], lhsT=selE[:, ex * 128:(ex + 1) * 128], rhs=gTlo[:, t0:t0 + tw],
                                                                          start=False, stop=True), reads=[B_selE, B_gTlo], writes=[Bpb])
                P.op("act", lambda e, pb=pb, t0=t0, tw=tw: e.activation(out=gbc[:, t0:t0 + tw], in_=pb[:, 0:tw], func=AF.Identity),
                     reads=[Bpb], writes=[B_gbc])
            for fc in range(4):
                wt, Bwt = wgu[(ex * 4 + fc) % 2]
                if fc < 3:
                    load_wgu(ex, fc + 1)
                elif ex + 1 < NE:
                    load_wgu(ex + 1, 0)
                    if len(wd) > 1:
                        load_wd(ex + 1)
                if True:
                    for (t0, tw) in TT:
                        pg, Bpg = next_ps("a")
                        pu, Bpu = next_ps("a")
                        for kc in range(KC):
                            P.op("pe", lambda e, pg=pg, kc=kc, wt=wt, t0=t0, tw=tw: e.matmul(
                                pg[:, 0:tw], lhsT=wt[:, kc, 0, :], rhs=hT[:, kc, t0:t0 + tw],
                                start=(kc == 0), stop=(kc == KC - 1)), reads=[Bwt, B_hT], writes=[Bpg])
                        for kc in range(KC):
                            P.op("pe", lambda e, pu=pu, kc=kc, wt=wt, t0=t0, tw=tw: e.matmul(
                                pu[:, 0:tw], lhsT=wt[:, kc, 1, :], rhs=hT[:, kc, t0:t0 + tw],
                                start=(kc == 0), stop=(kc == KC - 1)), reads=[Bwt, B_hT], writes=[Bpu])
                        P.op("act", lambda e, pg=pg, tw=tw: e.activation(out=sil[:, 0:tw], in_=pg[:, 0:tw], func=AF.Sigmoid),
                             reads=[Bpg], writes=[B_sil])
                        P.op("dve", lambda e, pg=pg, tw=tw: e.tensor_tensor(out=sil[:, 0:tw], in0=sil[:, 0:tw], in1=pg[:, 0:tw], op=ALU.mult),
                             reads=[Bpg, B_sil], writes=[B_sil])
                        P.op("dve", lambda e, pu=pu, t0=t0, tw=tw: e.tensor_tensor(out=ug[:, 0:tw], in0=gbc[:, t0:t0 + tw], in1=pu[:, 0:tw], op=ALU.mult),
                             reads=[Bpu, B_gbc], writes=[B_ug])
                        P.op("dve", lambda e, fc=fc, t0=t0, tw=tw: e.tensor_tensor(out=hid[:, fc, t0:t0 + tw], in0=sil[:, 0:tw], in1=ug[:, 0:tw], op=ALU.mult),
                             reads=[B_sil, B_ug], writes=[B_hid])
            for m in range(KC):
                for (t0, tw) in TT:
                    pt, Bp = next_ps("a")
                    for fc in range(4):
                        P.op("pe", lambda e, pt=pt, fc=fc, wdt=wdt, m=m, t0=t0, tw=tw: e.matmul(
                            pt[:, 0:tw], lhsT=wdt[:, fc, m * 128:(m + 1) * 128], rhs=hid[:, fc, t0:t0 + tw], start=(fc == 0), stop=(fc == 3)),
                            reads=[Bwd, B_hid], writes=[Bp])
                    P.op("dve", lambda e, pt=pt, m=m, t0=t0, tw=tw: e.tensor_tensor(out=zacc[:, m, t0:t0 + tw], in0=zacc[:, m, t0:t0 + tw],
                                                                                   in1=pt[:, 0:tw], op=ALU.add), reads=[Bp, B_zacc], writes=[B_zacc])
            if len(wd) == 1 and ex + 1 < NE:
                load_wd(ex + 1)
        P.barrier()

    L["pools"]["a"] = [0, 1, 2, 3, 4]
    L["pools"]["b"] = [5, 6, 7]
    def after_ln2(m):
        P.dma("sp", lambda e, m=m: e.dma_start(out=y_out[m * 128:(m + 1) * 128, :], in_=zacc[:, m, :]), reads=[B_zacc])
    layer_norm(2, 3, after_ln2)
    P.barrier()


def nsa_sample(nc, P, sc, sbt, next_ps, L):
    debug = L["debug"]
    xf = L["xf"]
    ident, B_ident, identb, B_identb = L["ident"], L["B_ident"], L["identb"], L["B_identb"]
    onesb, B_onesb = L["onesb"], L["B_onesb"]
    o_nsaT, B_onsaT = L["o_nsaT"], L["B_onsaT"]
    cache2d = L["cache2d"]
    SB0 = NB * 128
    SC0 = NOWN * 128

    def table(name, shape, dt, src, q="sp"):
        t, B = sbt(sc, "T" + name, shape, dt)
        P.dma(q, lambda e: e.dma_start(out=t[:], in_=src), writes=[B])
        return t, B
    cb, B_cb = table("s_cb", [128, 64], F32, L["s_cb"])
    bs, B_bs = table("s_bs", [128, 16 * 65], F32, L["s_bs"])
    sq, B_sq = table("s_sq", [1, 512], F32, L["s_sq"])
    lnt, B_lnt = table("s_lnt", [2, 128], BF16, L["s_lnt"])
    R2, B_R2 = table("s_R2", [2, 512], BF16, L["t_r2"])
    Et, B_Et = table("s_Et", [64, NB * 128], BF16, L["t_E"])
    caus, B_caus = table("s_caus", [128, 128], BF16, L["s_caus"])
    low, B_low = table("s_low", [128, 128], BF16, L["s_low"])
    keep, B_keep = table("s_keep", [128, 128], F32, L["s_keep"])
    force, B_force = table("s_force", [128, 128], F32, L["s_force"])
    iot, B_iot = table("s_iot", [128, 1], F32, L["s_iota"])
    ptb, B_ptb = sbt(sc, "s_ptb", [128, 256], mybir.dt.int32)
    P.dma("sp", lambda e: e.dma_start(out=ptb[:], in_=L["pt_core"].partition_broadcast(128)), writes=[B_ptb])
    ptf, B_ptf = sbt(sc, "s_ptf", [128, 256], F32)
    idx, B_idx = sbt(sc, "s_idx", [128, 256], mybir.dt.int32)
    P.op("dve", lambda e: e.tensor_copy(out=ptf[:], in_=ptb[:]), reads=[B_ptb], writes=[B_ptf])
    P.op("dve", lambda e: e.tensor_scalar(out=ptf[:], in0=ptf[:], scalar1=128.0, scalar2=iot[:, 0:1], op0=ALU.mult, op1=ALU.add),
         reads=[B_ptf, B_iot], writes=[B_ptf])
    P.op("dve", lambda e: e.tensor_scalar(out=ptf[:], in0=ptf[:], scalar1=2.0, scalar2=None, op0=ALU.mult), reads=[B_ptf], writes=[B_ptf])
    P.op("dve", lambda e: e.tensor_copy(out=idx[:], in_=ptf[:]), reads=[B_ptf], writes=[B_idx])
    idx1, B_idx1 = sbt(sc, "s_idx1", [128, 256], mybir.dt.int32)
    P.op("dve", lambda e: e.tensor_scalar(out=ptf[:], in0=ptf[:], scalar1=1.0, scalar2=None, op0=ALU.add), reads=[B_ptf], writes=[B_ptf])
    P.op("dve", lambda e: e.tensor_copy(out=idx1[:], in_=ptf[:]), reads=[B_ptf], writes=[B_idx1])

    qT, B_qT = sbt(sc, "s_qT", [128, 8, 128], BF16)
    gates, B_gates = sbt(sc, "s_gates", [128, 48], F32)
    knew, B_knew = sbt(sc, "s_knew", [128, 2, 2, 128], BF16)
    vnew, B_vnew = sbt(sc, "s_vnew", [128, 2, 4, 65], BF16)
    vnj, B_vnj = sbt(sc, "s_vnj", [4, 4, 2, 4, 65], BF16)
    P.op("dve", lambda e: e.memset(vnew[:, :, :, 64:65], 1.0), writes=[B_vnew])
    pq = contextlib.ExitStack()
    with pq:
        wq, B_wq = sbt(pq, "s_wq", [128, KC, 1024], BF16)
        wng, B_wng = sbt(pq, "s_wng", [128, KC, 48], BF16)
        wk, B_wk = sbt(pq, "s_wk", [128, KC, 1024], BF16)
        bq_col, B_bq = sbt(pq, "s_bq", [128, 8], F32)
        bk_col, B_bk = sbt(pq, "s_bk", [128, 12], F32)
        bk_bc, B_bkbc = sbt(pq, "s_bkbc", [128, KV_W], F32)
        bng, B_bng = sbt(pq, "s_bng", [128, 48], F32)
        xb, Bx = sbt(pq, "s_xb", [128, KC, 128], BF16)
        wq_v = L["w_q"].rearrange("(kc p) c -> p kc c", p=128)
        wkv_v = L["w_kv"].rearrange("(kc p) c -> p kc c", p=128)
        xf_v = xf.rearrange("(kc p) t -> p kc t", p=128)
        for q in range(4):
            P.dma("pool", lambda e, q=q: e.dma_start(out=wq[:, 4 * q:4 * q + 4, :], in_=wq_v[:, 4 * q:4 * q + 4, :]), writes=[B_wq])
            P.dma("pool", lambda e, q=q: e.dma_start(out=wk[:, 4 * q:4 * q + 4, :], in_=wkv_v[:, 4 * q:4 * q + 4, 512:1536]), writes=[B_wk])
        P.dma("pool", lambda e: e.dma_start(out=wng[:], in_=L["w_ng"].rearrange("(kc p) c -> p kc c", p=128)), writes=[B_wng])
        P.dma("pool", lambda e: e.dma_start(out=xb[:], in_=xf_v[:, :, SB0:SB0 + 128]), writes=[Bx])
        P.dma("sp", lambda e: e.dma_start(out=bq_col[:], in_=L["b_q"].rearrange("(cb p) -> p cb", p=128), allow_slow_non_contiguous=True),
              writes=[B_bq])
        P.dma("sp", lambda e: e.dma_start(out=bk_col[:], in_=L["b_kv"].rearrange("(cb p) -> p cb", p=128), allow_slow_non_contiguous=True),
              writes=[B_bk])
        P.dma("sp", lambda e: e.dma_start(out=bk_bc[:], in_=L["b_kv"].partition_broadcast(128)), writes=[B_bkbc])
        P.dma("sp", lambda e: e.dma_start(out=bng[:], in_=L["b_ng"].partition_broadcast(128)), writes=[B_bng])
        for cbk in range(8):
            pt, Bp = next_ps("a")
            for kc in range(KC):
                P.op("pe", lambda e, pt=pt, kc=kc, cbk=cbk: e.matmul(pt[:, 0:128], lhsT=wq[:, kc, cbk * 128:(cbk + 1) * 128], rhs=xb[:, kc, :],
                                                                     start=(kc == 0), stop=(kc == KC - 1)), reads=[B_wq, Bx], writes=[Bp])
            P.op("act", lambda e, pt=pt, cbk=cbk: e.activation(out=qT[:, cbk, :], in_=pt[:, 0:128], func=AF.Identity, bias=bq_col[:, cbk:cbk + 1]),
                 reads=[Bp, B_bq], writes=[B_qT])
        for si, (c0, cbb) in enumerate(((0, 4), (512, 8))):
            for gp in range(2):
                pt, Bp = next_ps("a")
                for kc in range(KC):
                    P.op("pe", lambda e, pt=pt, kc=kc, c0=c0, gp=gp: e.matmul(
                        pt[:, 0:128], lhsT=wk[:, kc, c0 + gp * 128:c0 + (gp + 1) * 128], rhs=xb[:, kc, :],
                        start=(kc == 0), stop=(kc == KC - 1)), reads=[B_wk, Bx], writes=[Bp])
                P.op("act", lambda e, pt=pt, si=si, gp=gp, cbb=cbb: e.activation(
                    out=knew[:, si, gp, :], in_=pt[:, 0:128], func=AF.Identity, bias=bk_col[:, cbb + gp:cbb + gp + 1]),
                    reads=[Bp, B_bk], writes=[B_knew])
        pt, Bp = next_ps("a")
        for si, c0 in enumerate((256, 768)):
            for kc in range(KC):
                P.op("pe", lambda e, pt=pt, kc=kc, si=si, c0=c0: e.matmul(pt[:, si * 256:(si + 1) * 256], lhsT=xb[:, kc, :], rhs=wk[:, kc, c0:c0 + 256],
                                                                         start=(kc == 0), stop=(kc == KC - 1)), reads=[B_wk, Bx], writes=[Bp])
        for si, c0 in enumerate((768, 1280)):
            P.op("dve", lambda e, pt=pt, si=si, c0=c0: e.tensor_tensor(
                out=vnew[:, si, :, 0:64], in0=pt[:, si * 256:(si + 1) * 256].rearrange("p (g d) -> p g d", g=4),
                in1=bk_bc[:, c0:c0 + 256].rearrange("p (g d) -> p g d", g=4), op=ALU.add), reads=[Bp, B_bkbc], writes=[B_vnew])
        for j in range(4):
            P.dma("sp", lambda e, j=j: e.dma_start(out=vnj[:, j, :, :, :], in_=vnew[32 * j:32 * j + 4, :, :, :]), reads=[B_vnew], writes=[B_vnj])
        pt, Bp = next_ps("b")
        for kc in range(KC):
            P.op("pe", lambda e, pt=pt, kc=kc: e.matmul(pt[:, 0:48], lhsT=xb[:, kc, :], rhs=wng[:, kc, :], start=(kc == 0), stop=(kc == KC - 1)),
                 reads=[B_wng, Bx], writes=[Bp])
        P.op("dve", lambda e, pt=pt: e.tensor_tensor(out=gates[:, :], in0=pt[:, 0:48], in1=bng[:, :], op=ALU.add), reads=[Bp, B_bng], writes=[B_gates])
        P.op("act", lambda e: e.activation(out=gates[:], in_=gates[:], func=AF.Sigmoid), reads=[B_gates], writes=[B_gates])
        P.barrier()

    kcs, B_kcs = sbt(sc, "s_kc", [128, 2, 512], BF16)
    vca, B_vca = sbt(sc, "s_vca", [128, 4, 4, 193], BF16)
    P.op("dve", lambda e: e.memset(vca[:, :, :, 64:65], 1.0), writes=[B_vca])
    for c in range(4):
        for g in range(4):
            P.dma("sp", lambda e, c=c, g=g: e.dma_start(out=vca[:, c, g, 65:193], in_=L["s_ovl"][:, c * 128:(c + 1) * 128]), writes=[B_vca])
    o_s, B_os = sbt(sc, "s_os", [128, 4, 256], F32)
    P.op("dve", lambda e: e.memset(o_s[:], 0.0), writes=[B_os])
    pg = [sbt(sc, "s_pg%d" % i, [128, 512], F32) for i in range(3)]
    pef, B_pef = sbt(sc, "s_pef", [128, 2, 16], F32)
    peb, B_peb = sbt(sc, "s_peb", [128, 2, 16], BF16)
    w2k, B_w2k = sbt(sc, "s_w2k", [128, 128], BF16)
    w2v, B_w2v = sbt(sc, "s_w2v", [128, 64], BF16)
    pre0, B_pre0 = sbt(sc, "s_pre0", [128, 2], F32)
    ug, B_ug = sbt(sc, "s_ug", [128, 512], F32)
    tg, B_tg = sbt(sc, "s_tg", [128, 512], F32)
    Gb, B_Gb = sbt(sc, "s_Gb", [128, 512], BF16)
    w_c1, w_c2, c_pe = L["w_c1"], L["w_c2"], L["c_pe"]
    P.dma("sp", lambda e: e.dma_start(out=pef[:], in_=c_pe.rearrange("c (jc j2) d -> (j2 d) c jc", j2=2), allow_slow_non_contiguous=True),
          writes=[B_pef])
    P.op("dve", lambda e: e.tensor_copy(out=peb[:], in_=pef[:]), reads=[B_pef], writes=[B_peb])
    P.op("dve", lambda e: e.memset(w2k[:, 0:64], 0.0), writes=[B_w2k])
    P.dma("pool", lambda e: e.dma_start(out=w2k[:, 64:128], in_=w_c2[0]), writes=[B_w2k])
    P.dma("pool", lambda e: e.dma_start(out=w2v[:], in_=w_c2[1]), writes=[B_w2v])
    pT = [sbt(sc, "s_pT%d" % i, [128, 4, 128], BF16) for i in range(4)]
    pT_rr = [0]

    def next_pT():
        k = pT_rr[0] % len(pT)
        pT_rr[0] += 1
        return pT[k]
    qsq, B_qsq = sbt(sc, "s_qsq", [128, 512], BF16)
    o_blk, B_oblk = sbt(sc, "s_oblk", [128, 256], F32)
    rs, B_rs = sbt(sc, "s_rs", [128, 4], F32)
    wgt, B_wgt = sbt(sc, "s_wgt", [128, 4], F32)
    imp, B_imp = sbt(sc, "s_imp", [128, 128], F32)
    imp3, B_imp3 = sbt(sc, "s_imp3", [128, 128], F32)
    m8, B_m8 = sbt(sc, "s_m8", [128, 16], F32)
    nsel, B_nsel = sbt(sc, "s_nsel", [128, 128], F32)
    nselT = [sbt(sc, "s_nselT%d" % i, [64, 512], BF16) for i in range(2)]
    sqt, B_sqt = sbt(sc, "s_sqt", [128, 512], BF16)
    runmax, B_runmax = sbt(sc, "s_runmax", [1, 512], F32)
    nkm, B_nkm = sbt(sc, "s_nkm", [1, 1], F32)

    def softmax_chunk(pt, Bp, w, hbase, col_fn, tab, B_tab):
        (t, Bt0) = next_pT()
        Bt = _HB.setdefault(id(Bt0), [Buf("h%d" % i) for i in range(4)])
        for hh in range(4):
            P.op("act", lambda e, pt=pt, t=t, hh=hh, w=w: e.activation(
                out=t[:w, hh, 0:32], in_=pt[:w, hh * 32:(hh + 1) * 32], func=AF.Exp, scale=SCALE,
                bias=tab[:w, col_fn(hbase + hh):col_fn(hbase + hh) + 1]), reads=[Bp, B_tab], writes=[Bt[hh]])
        return t, Bt

    def finish_branch(psO, BpO, g, br, first, gj, B_gj):
        P.op("dve", lambda e: e.tensor_scalar(out=rs[0:32, :], in0=psO[0:32, 0:260].rearrange("p (h c) -> p h c", c=65)[:, :, 64],
                                              scalar1=1e-30, scalar2=None, op0=ALU.max), reads=[BpO], writes=[B_rs])
        P.op("dve", lambda e: e.reciprocal(out=rs[0:32, :], in_=rs[0:32, :]), reads=[B_rs], writes=[B_rs])
        P.op("dve", lambda e: e.tensor_tensor(out=wgt[0:32, :], in0=rs[0:32, :], in1=gj[0:32, br * 16 + g * 4:br * 16 + g * 4 + 4], op=ALU.mult),
             reads=[B_rs, B_gj], writes=[B_wgt])
        for hh in range(4):
            oc = slice(hh * 64, hh * 64 + 64)
            if first:
                P.op("dve", lambda e, hh=hh, oc=oc: e.tensor_scalar(out=o_blk[0:32, oc], in0=psO[0:32, hh * 65:hh * 65 + 64],
                                                                    scalar1=wgt[0:32, hh:hh + 1], scalar2=None, op0=ALU.mult),
                     reads=[BpO, B_wgt], writes=[B_oblk])
            else:
                P.op("dve", lambda e, hh=hh, oc=oc: e.scalar_tensor_tensor(
                    out=o_blk[0:32, oc], in0=psO[0:32, hh * 65:hh * 65 + 64], scalar=wgt[0:32, hh:hh + 1], in1=o_blk[0:32, oc],
                    op0=ALU.mult, op1=ALU.add), reads=[BpO, B_wgt, B_oblk], writes=[B_oblk])

    def gather(j, c, half, dst, Bdst):
        col = j * 64 + c
        ix, Bix = (idx, B_idx) if half == 0 else (idx1, B_idx1)
        P.dma("pool", lambda e, col=col, ix=ix, dst=dst: e.indirect_dma_start(
            out=dst[:, :], out_offset=None, in_=cache2d[:, :],
            in_offset=bass.IndirectOffsetOnAxis(ap=ix[:, col:col + 1], axis=0)), reads=[Bix], writes=[Bdst])

    for j in range(4):
        if _LIM < 6.5 and j not in _JSEL:
            continue
        jb = contextlib.ExitStack()
        with jb:
            pa = jb.enter_context(contextlib.ExitStack())
            kcmp, B_kcmp = sbt(pa, "s_kcmp", [128, 2, NCH * 128], BF16)
            vcmp, B_vcmp = sbt(pa, "s_vcmp", [128, 2, NCH * 128], BF16)
            w1d, B_w1d = sbt(pa, "s_w1d", [128, 2, 32, 128], BF16)
            w1f, B_w1f = sbt(pa, "s_w1f", [128, 2, 16, 128], BF16)
            for half in range(2):
                P.dma("pool", lambda e, half=half, w1d=w1d: e.dma_start(out=w1d[64 * half:64 * half + 64, :, :, :], in_=w_c1.rearrange("c j d h -> d c j h")),
                      writes=[B_w1d])
            P.dma("pool", lambda e, w1f=w1f: e.dma_start(out=w1f[:], in_=w_c1.rearrange("c (jc j2) d h -> (j2 d) c jc h", j2=2)), writes=[B_w1f])
            pt, Bp = next_ps("b")
            for c in range(2):
                for jc in range(16):
                    P.op("pe", lambda e, pt=pt, c=c, jc=jc, w1f=w1f: e.matmul(pt[:, c:c + 1], lhsT=w1f[:, c, jc, :], rhs=peb[:, c, jc:jc + 1],
                                                                              start=(jc == 0), stop=(jc == 15)), reads=[B_w1f, B_peb], writes=[Bp])
            P.op("dve", lambda e, pt=pt: e.tensor_copy(out=pre0[:], in_=pt[:, 0:2]), reads=[Bp], writes=[B_pre0])
            for c in range(NCH):
                pgt, Bpg = pg[c % 3]
                gather(j, c, 0, pgt, Bpg)
                for q in range(4):
                    dst, Bd = (kcmp, B_kcmp) if q < 2 else (vcmp, B_vcmp)
                    pt, Bp = next_ps("a")
                    P.op("pe", lambda e, pt=pt, q=q, pgt=pgt: e.transpose(out=pt[:, 0:128], in_=pgt[:, q * 128:(q + 1) * 128],
                                                                          identity=ident[:, :]), reads=[Bpg, B_ident], writes=[Bp])
                    eng = "act" if q % 2 == 0 else "dve"
                    if eng == "act":
                        P.op("act", lambda e, pt=pt, c=c, q=q, dst=dst: e.activation(out=dst[:, q % 2, c * 128:(c + 1) * 128], in_=pt[:, 0:128],
                                                                                      func=AF.Identity), reads=[Bp], writes=[Bd])
                    else:
                        P.op("dve", lambda e, pt=pt, c=c, q=q, dst=dst: e.tensor_copy(out=dst[:, q % 2, c * 128:(c + 1) * 128], in_=pt[:, 0:128]),
                             reads=[Bp], writes=[Bd])
            for c in range(2):
                src, Bsrc = (kcmp, B_kcmp) if c == 0 else (vcmp, B_vcmp)
                for g in range(4):
                    gp, g2 = g // 2, g % 2
                    hs = slice(64 * g2, 64 * g2 + 64)
                    pt, Bp = next_ps("a")
                    for jj in range(32):
                        P.op("pe", lambda e, pt=pt, c=c, jj=jj, hs=hs, gp=gp, src=src, w1d=w1d: e.matmul(
                            pt[:, 0:511], lhsT=w1d[hs, c, jj, :], rhs=src[hs, gp, jj:jj + 16 * 510 + 1:16],
                            start=(jj == 0), stop=(jj == 31)), reads=[B_w1d, Bsrc], writes=[Bp])
                    P.op("act", lambda e, pt=pt, c=c: e.activation(out=ug[:, 0:511], in_=pt[:, 0:511], func=AF.Identity, bias=pre0[:, c:c + 1]),
                         reads=[Bp, B_pre0], writes=[B_ug])
                    P.op("dve", lambda e: e.tensor_tensor(out=tg[:, 0:511], in0=ug[:, 0:511], in1=ug[:, 0:511], op=ALU.mult), reads=[B_ug], writes=[B_tg])
                    P.op("dve", lambda e: e.tensor_scalar(out=tg[:, 0:511], in0=tg[:, 0:511], scalar1=0.044715, scalar2=1.0, op0=ALU.mult, op1=ALU.add),
                         reads=[B_tg], writes=[B_tg])
                    P.op("dve", lambda e: e.tensor_tensor(out=tg[:, 0:511], in0=tg[:, 0:511], in1=ug[:, 0:511], op=ALU.mult), reads=[B_tg, B_ug], writes=[B_tg])
                    P.op("act", lambda e: e.activation(out=tg[:, 0:511], in_=tg[:, 0:511], func=AF.Sigmoid, scale=1.5957691216057308),
                         reads=[B_tg], writes=[B_tg])
                    P.op("dve", lambda e: e.tensor_tensor(out=Gb[:, 0:511], in0=tg[:, 0:511], in1=ug[:, 0:511], op=ALU.mult), reads=[B_tg, B_ug], writes=[B_Gb])
                    if c == 0:
                        pt2, Bp2 = next_ps("b")
                        if g2 == 0:
                            P.op("pe", lambda e, pt2=pt2: e.matmul(pt2[0:64, 0:511], lhsT=w2k[:, 64:128], rhs=Gb[:, 0:511], start=True, stop=True),
                                 reads=[B_w2k, B_Gb], writes=[Bp2])
                        else:
                            P.op("pe", lambda e, pt2=pt2: e.matmul(pt2[:, 0:511], lhsT=w2k[:, :], rhs=Gb[:, 0:511], start=True, stop=True),
                                 reads=[B_w2k, B_Gb], writes=[Bp2])
                        P.op("dve", lambda e, pt2=pt2, hs=hs, gp=gp: e.tensor_copy(out=kcs[hs, gp, 0:511], in_=pt2[hs, 0:511]), reads=[Bp2], writes=[B_kcs])
                    else:
                        pt2, Bp2 = next_ps("b")
                        for ch in range(4):
                            w = 128 if ch < 3 else 127
                            P.op("pe", lambda e, pt2=pt2, ch=ch, w=w: e.matmul(pt2[:w, ch * 64:(ch + 1) * 64], lhsT=Gb[:, ch * 128:ch * 128 + w],
                                                                               rhs=w2v[:, :], start=True, stop=True), reads=[B_w2v, B_Gb], writes=[Bp2])
                        for ch in range(4):
                            w = 128 if ch < 3 else 127
                            P.op("dve", lambda e, pt2=pt2, ch=ch, w=w, g=g: e.tensor_copy(out=vca[:w, ch, g, 0:64], in_=pt2[:w, ch * 64:(ch + 1) * 64]),
                                 reads=[Bp2], writes=[B_vca])
            P.op("dve", lambda e: e.memset(kcs[:, :, 511:512], 0.0), writes=[B_kcs])
            P.barrier()
            pa.close()

            kslc, B_kslc = sbt(jb, "s_kslc", [128, 2, NCH * 128], BF16)
            vslc, B_vslc = sbt(jb, "s_vslc", [128, NCH, 4, 65], BF16)
            kwin, B_kwin = sbt(jb, "s_kwin", [128, 2, 512], BF16)
            vwin, B_vwin = sbt(jb, "s_vwin", [128, 4, 4, 65], BF16)
            sqj, B_sqj = sbt(jb, "s_sqj", [1, 512], F32)
            kslc_g, B_kslcg = sbt(jb, "s_kslcg", [64, (NCH + 1) * 128], BF16)
            kwin_g, B_kwing = sbt(jb, "s_kwing", [64, 5 * 128], BF16)
            kc_g, B_kcg = sbt(jb, "s_kcg", [64, 512], BF16)
            q_g, B_qg = sbt(jb, "s_qg", [64, 4, 128], BF16)
            gj, B_gj = sbt(jb, "s_gj", [32, 48], F32)
            P.dma("sp", lambda e, j=j, gj=gj: e.dma_start(out=gj[:, :], in_=gates[32 * j:32 * j + 32, :]), reads=[B_gates], writes=[B_gj])
            P.op("dve", lambda e: e.memset(vslc[:, :, :, 64:65], 1.0), writes=[B_vslc])
            P.op("dve", lambda e: e.memset(vwin[:, :, :, 64:65], 1.0), writes=[B_vwin])
            for c in range(NCH):
                pgt, Bpg = pg[c % 3]
                gather(j, c, 1, pgt, Bpg)
                for q in range(2):
                    pt, Bp = next_ps("a")
                    P.op("pe", lambda e, pt=pt, q=q, pgt=pgt: e.transpose(out=pt[:, 0:128], in_=pgt[:, q * 128:(q + 1) * 128],
                                                                          identity=ident[:, :]), reads=[Bpg, B_ident], writes=[Bp])
                    P.op("act", lambda e, pt=pt, c=c, q=q: e.activation(out=kslc[:, q, c * 128:(c + 1) * 128], in_=pt[:, 0:128], func=AF.Identity),
                         reads=[Bp], writes=[B_kslc])
                P.op("dve", lambda e, pgt=pgt, c=c: e.tensor_copy(out=vslc[:, c, :, 0:64], in_=pgt[:, 256:512].rearrange("p (g d) -> p g d", g=4)),
                     reads=[Bpg], writes=[B_vslc])
            for wch in range(4):
                pgt, Bpg = pg[wch % 3]
                P.dma("sp", lambda e, pgt=pgt, j=j, wch=wch: e.dma_start(out=pgt[:, :], in_=L["cw_in"][j, wch * 128:(wch + 1) * 128, :]), writes=[Bpg])
                for q in range(2):
                    pt, Bp = next_ps("a")
                    P.op("pe", lambda e, pt=pt, q=q, pgt=pgt: e.transpose(out=pt[:, 0:128], in_=pgt[:, q * 128:(q + 1) * 128],
                                                                          identity=ident[:, :]), reads=[Bpg, B_ident], writes=[Bp])
                    P.op("act", lambda e, pt=pt, wch=wch, q=q: e.activation(out=kwin[:, q, wch * 128:(wch + 1) * 128], in_=pt[:, 0:128], func=AF.Identity),
                         reads=[Bp], writes=[B_kwin])
                P.op("dve", lambda e, pgt=pgt, wch=wch: e.tensor_copy(out=vwin[:, wch, :, 0:64], in_=pgt[:, 256:512].rearrange("p (g d) -> p g d", g=4)),
                     reads=[Bpg], writes=[B_vwin])

            P.op("dve", lambda e: e.memset(runmax[:], 0.0), writes=[B_runmax])
            srcs = []
            for gp in range(2):
                for s_ in range(NCH * 128 // 512):
                    srcs.append((kslc, B_kslc, lambda gp=gp, s_=s_: kslc[:, gp, s_ * 512:(s_ + 1) * 512], 512))
                srcs.append((kwin, B_kwin, lambda gp=gp: kwin[:, gp, :], 512))
                srcs.append((kcs, B_kcs, lambda gp=gp: kcs[:, gp, :], 512))
                srcs.append((knew, B_knew, lambda gp=gp: knew[:, 0, gp, :], 128))
                srcs.append((knew, B_knew, lambda gp=gp: knew[:, 1, gp, :], 128))
            for (src, Bs, apf, w) in srcs:
                P.op("dve", lambda e, apf=apf, w=w: e.tensor_tensor(out=sqt[:, 0:w], in0=apf(), in1=apf(), op=ALU.mult), reads=[Bs], writes=[B_sqt])
                pt, Bp = next_ps("b")
                P.op("pe", lambda e, pt=pt, w=w: e.matmul(pt[0:1, 0:w], lhsT=onesb[:, 0:1], rhs=sqt[:, 0:w], start=True, stop=True),
                     reads=[B_onesb, B_sqt], writes=[Bp])
                P.op("dve", lambda e, pt=pt, w=w: e.tensor_tensor(out=runmax[:, 0:w], in0=runmax[:, 0:w], in1=pt[0:1, 0:w], op=ALU.max),
                     reads=[Bp, B_runmax], writes=[B_runmax])
            P.op("dve", lambda e: e.reduce_max(out=nkm[:], in_=runmax[:], axis=mybir.AxisListType.X), reads=[B_runmax], writes=[B_nkm])
            P.op("dve", lambda e: e.tensor_scalar(out=nkm[:], in0=nkm[:], scalar1=-0.5, scalar2=None, op0=ALU.mult), reads=[B_nkm], writes=[B_nkm])
            P.op("dve", lambda e: e.tensor_scalar(out=sqj[:, :], in0=sq[:, :], scalar1=nkm[0:1, 0:1], scalar2=None, op0=ALU.add),
                 reads=[B_nkm, B_sq], writes=[B_sqj])

            for g in range(4):
                gp, g2 = g // 2, g % 2
                hs0 = slice(64 * g2, 64 * g2 + 64)
                P.dma("sp", lambda e, hs0=hs0, gp=gp: e.dma_start(out=kslc_g[:, 0:NCH * 128], in_=kslc[hs0, gp, :]), reads=[B_kslc], writes=[B_kslcg])
                P.dma("sp", lambda e, hs0=hs0, gp=gp, j=j: e.dma_start(out=kslc_g[:, NCH * 128:NCH * 128 + 4], in_=knew[hs0, 0, gp, 32 * j:32 * j + 4]),
                      reads=[B_knew], writes=[B_kslcg])
                P.dma("sp", lambda e, hs0=hs0, gp=gp: e.dma_start(out=kwin_g[:, 0:512], in_=kwin[hs0, gp, :]), reads=[B_kwin], writes=[B_kwing])
                P.dma("sp", lambda e, hs0=hs0, gp=gp, j=j: e.dma_start(out=kwin_g[:, 512:516], in_=knew[hs0, 1, gp, 32 * j:32 * j + 4]),
                      reads=[B_knew], writes=[B_kwing])
                P.dma("sp", lambda e, hs0=hs0, gp=gp: e.dma_start(out=kc_g[:, :], in_=kcs[hs0, gp, :]), reads=[B_kcs], writes=[B_kcg])
                P.dma("sp", lambda e, hs0=hs0, gp=gp: e.dma_start(out=q_g[:, :, :], in_=qT[hs0, gp * 4:gp * 4 + 4, :]), reads=[B_qT], writes=[B_qg])
                hs = slice(0, 64)
                qv = q_g[:, :, 32 * j:32 * j + 32]
                P.op("dve", lambda e, qv=qv: e.tensor_tensor(out=qsq[hs, 0:128].rearrange("p (h q) -> p h q", h=4), in0=qv, in1=qv, op=ALU.mult),
                     reads=[B_qg], writes=[B_qsq])
                pt, Bp = next_ps("b")
                P.op("pe", lambda e, pt=pt: e.matmul(pt[0:1, 0:128], lhsT=onesb[hs, 0:1], rhs=qsq[hs, 0:128], start=True, stop=True),
                     reads=[B_onesb, B_qsq], writes=[Bp])
                P.op("dve", lambda e, pt=pt, g=g: e.scalar_tensor_tensor(out=R2[0:1, 0:128], in0=pt[0:1, 0:128], scalar=-0.5,
                                                                         in1=sqj[0:1, g * 128:(g + 1) * 128], op0=ALU.mult, op1=ALU.add),
                     reads=[Bp, B_sqj], writes=[B_R2])
                pts = []
                for c in range(4):
                    w = 128 if c < 3 else 127
                    pt, Bp = next_ps("a")
                    P.op("pe", lambda e, pt=pt, c=c, w=w, qv=qv: e.matmul(pt[:w, 0:128], lhsT=kc_g[hs, c * 128:c * 128 + w], rhs=qv, start=True, stop=False),
                         reads=[B_kcg, B_qg], writes=[Bp])
                    P.op("pe", lambda e, pt=pt, w=w: e.matmul(pt[:w, 0:128], lhsT=lnt[0:1, 0:w], rhs=R2[0:1, 0:128], start=False, stop=True),
                         reads=[B_lnt, B_R2], writes=[Bp])
                    t, Bt = softmax_chunk(pt, Bp, w, 4 * g, lambda h, c=c: h * 4 + c, cb, B_cb)
                    pts.append((t, Bt, w))
                psO, BpO = next_ps("b")
                psI, BpI = next_ps("b")
                for hh in range(4):
                    for c, (t, Bt, w) in enumerate(pts):
                        P.op("pe", lambda e, hh=hh, c=c, t=t, w=w, g=g, psO=psO: e.matmul(
                            psO[0:32, hh * 65:(hh + 1) * 65], lhsT=t[:w, hh, 0:32], rhs=vca[:w, c, g, 0:65], start=(c == 0), stop=(c == 3)),
                            reads=[Bt[hh], B_vca], writes=[BpO])
                for hh in range(4):
                    for c, (t, Bt, w) in enumerate(pts):
                        P.op("pe", lambda e, hh=hh, c=c, t=t, w=w, g=g, psI=psI: e.matmul(
                            psI[0:32, hh * 128:(hh + 1) * 128], lhsT=t[:w, hh, 0:32], rhs=vca[:w, c, g, 65:193], start=(c == 0), stop=(c == 3)),
                            reads=[Bt[hh], B_vca], writes=[BpI])
                finish_branch(psO, BpO, g, 0, True, gj, B_gj)
                for hh in range(4):
                    if hh == 0:
                        P.op("dve", lambda e, psI=psI: e.tensor_scalar(out=imp[0:32, :], in0=psI[0:32, 0:128], scalar1=rs[0:32, 0:1], scalar2=None, op0=ALU.mult),
                             reads=[BpI, B_rs], writes=[B_imp])
                    else:
                        P.op("dve", lambda e, hh=hh, psI=psI: e.scalar_tensor_tensor(
                            out=imp[0:32, :], in0=psI[0:32, hh * 128:(hh + 1) * 128], scalar=rs[0:32, hh:hh + 1], in1=imp[0:32, :], op0=ALU.mult, op1=ALU.add),
                            reads=[BpI, B_rs, B_imp], writes=[B_imp])
                P.op("dve", lambda e: e.tensor_scalar(out=imp[0:32, :], in0=imp[0:32, :], scalar1=1e-30, scalar2=None, op0=ALU.max), reads=[B_imp], writes=[B_imp])
                P.op("dve", lambda e: e.tensor_tensor(out=imp[0:32, :], in0=imp[0:32, :], in1=keep[0:32, :], op=ALU.mult), reads=[B_imp, B_keep], writes=[B_imp])
                P.op("dve", lambda e: e.tensor_tensor(out=imp[0:32, :], in0=imp[0:32, :], in1=force[0:32, :], op=ALU.add), reads=[B_imp, B_force], writes=[B_imp])
                P.op("dve", lambda e: e.max(out=m8[0:32, 0:8], in_=imp[0:32, :]), reads=[B_imp], writes=[B_m8])
                P.op("dve", lambda e: e.tensor_scalar(out=imp3[0:32, :], in0=imp[0:32, :], scalar1=m8[0:32, 7:8], scalar2=None, op0=ALU.is_ge),
                     reads=[B_imp, B_m8], writes=[B_imp3])
                P.op("dve", lambda e: e.scalar_tensor_tensor(out=imp3[0:32, :], in0=imp3[0:32, :], scalar=-3.0e38, in1=imp[0:32, :], op0=ALU.mult, op1=ALU.add),
                     reads=[B_imp, B_imp3], writes=[B_imp3])
                P.op("dve", lambda e: e.max(out=m8[0:32, 8:16], in_=imp3[0:32, :]), reads=[B_imp3], writes=[B_m8])
                P.op("dve", lambda e: e.tensor_scalar(out=nsel[0:32, :], in0=imp[0:32, :], scalar1=m8[0:32, 14:15], scalar2=None, op0=ALU.is_ge),
                     reads=[B_imp, B_m8], writes=[B_nsel])
                P.op("dve", lambda e: e.tensor_scalar(out=nsel[0:32, :], in0=nsel[0:32, :], scalar1=-NEGM, scalar2=NEGM, op0=ALU.mult, op1=ALU.add),
                     reads=[B_nsel], writes=[B_nsel])
                for hf in range(2):
                    nt_, Bnt = nselT[hf]
                    pt, Bp = next_ps("b")
                    P.op("pe", lambda e, pt=pt, hf=hf: e.transpose(out=pt[0:64, 0:32], in_=nsel[0:32, hf * 64:(hf + 1) * 64], identity=ident[0:32, 0:32]),
                         reads=[B_nsel, B_ident], writes=[Bp])
                    for hh in range(4):
                        P.op("act", lambda e, pt=pt, hh=hh, nt_=nt_: e.activation(out=nt_[:, hh * 32:(hh + 1) * 32], in_=pt[0:64, 0:32], func=AF.Identity),
                             reads=[Bp], writes=[Bnt])
                psO, BpO = next_ps("b")
                for c in range(NCH + 1):
                    last = c == NCH
                    w = 4 if last else 128
                    pt, Bp = next_ps("a")
                    P.op("pe", lambda e, pt=pt, c=c, w=w, qv=qv: e.matmul(pt[:w, 0:128], lhsT=kslc_g[hs, c * 128:c * 128 + w], rhs=qv, start=True, stop=False),
                         reads=[B_kslcg, B_qg], writes=[Bp])
                    if not last:
                        nt_, Bnt = nselT[c // 32]
                        cc = c % 32
                        P.op("pe", lambda e, pt=pt, cc=cc, nt_=nt_: e.matmul(pt[:, 0:128], lhsT=Et[:, cc * 128:(cc + 1) * 128], rhs=nt_[:, 0:128], start=False, stop=False),
                             reads=[B_Et, Bnt], writes=[Bp])
                    P.op("pe", lambda e, pt=pt, w=w, last=last: e.matmul(pt[:w, 0:128], lhsT=lnt[0:2, 0:w], rhs=R2[0:2, 0:128], start=False, stop=(not last)),
                         reads=[B_lnt, B_R2], writes=[Bp])
                    if last:
                        P.op("pe", lambda e, pt=pt: e.matmul(pt[:4, 0:128], lhsT=identb[:4, :4], rhs=caus[:4, 0:128], start=False, stop=True),
                             reads=[B_identb, B_caus], writes=[Bp])
                    t, Bt = softmax_chunk(pt, Bp, w, 4 * g, lambda h, d=NCH - c: h * 65 + d, bs, B_bs)
                    for hh in range(4):
                        if last:
                            P.op("pe", lambda e, hh=hh, t=t, g=g, j=j, psO=psO: e.matmul(
                                psO[0:32, hh * 65:(hh + 1) * 65], lhsT=t[:4, hh, 0:32], rhs=vnj[0:4, j, 0, g, :], start=False, stop=True),
                                reads=[Bt[hh], B_vnj], writes=[BpO])
                        else:
                            P.op("pe", lambda e, hh=hh, t=t, c=c, g=g, psO=psO: e.matmul(
                                psO[0:32, hh * 65:(hh + 1) * 65], lhsT=t[:, hh, 0:32], rhs=vslc[:, c, g, :], start=(c == 0), stop=False),
                                reads=[Bt[hh], B_vslc], writes=[BpO])
                finish_branch(psO, BpO, g, 1, False, gj, B_gj)
                psO, BpO = next_ps("b")
                for c in range(5):
                    last = c == 4
                    w = 4 if last else 128
                    pt, Bp = next_ps("a")
                    P.op("pe", lambda e, pt=pt, c=c, w=w, qv=qv: e.matmul(pt[:w, 0:128], lhsT=kwin_g[hs, c * 128:c * 128 + w], rhs=qv, start=True, stop=False),
                         reads=[B_kwing, B_qg], writes=[Bp])
                    edge = last or c == 0
                    P.op("pe", lambda e, pt=pt, w=w, edge=edge: e.matmul(pt[:w, 0:128], lhsT=lnt[0:2, 0:w], rhs=R2[0:2, 0:128], start=False, stop=(not edge)),
                         reads=[B_lnt, B_R2], writes=[Bp])
                    if edge:
                        mk, Bmk = (caus, B_caus) if last else (low, B_low)
                        P.op("pe", lambda e, pt=pt, mk=mk, w=w: e.matmul(pt[:w, 0:128], lhsT=identb[:w, :w], rhs=mk[:w, 0:128], start=False, stop=True),
                             reads=[B_identb, Bmk], writes=[Bp])
                    t, Bt = softmax_chunk(pt, Bp, w, 4 * g, lambda h, d=4 - c: h * 65 + d, bs, B_bs)
                    for hh in range(4):
                        if last:
                            P.op("pe", lambda e, hh=hh, t=t, g=g, j=j, psO=psO: e.matmul(
                                psO[0:32, hh * 65:(hh + 1) * 65], lhsT=t[:4, hh, 0:32], rhs=vnj[0:4, j, 1, g, :], start=False, stop=True),
                                reads=[Bt[hh], B_vnj], writes=[BpO])
                        else:
                            P.op("pe", lambda e, hh=hh, t=t, c=c, g=g, psO=psO: e.matmul(
                                psO[0:32, hh * 65:(hh + 1) * 65], lhsT=t[:, hh, 0:32], rhs=vwin[:, c, g, :], start=(c == 0), stop=False),
                                reads=[Bt[hh], B_vwin], writes=[BpO])
                finish_branch(psO, BpO, g, 2, False, gj, B_gj)
                P.dma("sp", lambda e, j=j, g=g: e.dma_start(out=o_s[32 * j:32 * j + 4, g, :], in_=o_blk[0:4, :]),
                      reads=[B_oblk], writes=[B_os])
            P.barrier()
    for g in range(4):
        if debug:
            P.dma("sp", lambda e, g=g: e.dma_start(out=L["d_onsa_s"][:, g * 256:(g + 1) * 256], in_=o_s[:, g, :]), reads=[B_os])
        for jj in range(2):
            pt, Bp = next_ps("a")
            P.op("pe", lambda e, pt=pt, g=g, jj=jj: e.transpose(out=pt[:, 0:128], in_=o_s[:, g, jj * 128:(jj + 1) * 128], identity=ident[:, :]),
                 reads=[B_os, B_ident], writes=[Bp])
            P.op("dve", lambda e, pt=pt, g=g, jj=jj: e.tensor_copy(out=o_nsaT[:, 2 * g + jj, SC0:SC0 + 128], in_=pt[:, 0:128]),
                 reads=[Bp], writes=[B_onsaT])
    P.barrier()


NEGM = -30000.0
_LIM = 99
_NEXP = 32
_KSEL = [(0, 0)]
_JSEL = [0]
_HB = {}
_NWD = 1
SCALE = 0.125
NB = 32
OWN0 = 24
NOWN = 8
XF_T = (NB + 1) * 128
QOFF, NGOFF, HQOFF, HGOFF, MGOFF = 0, 2560, 2608, 5680, 6704
SLOPES = [2.0 ** (-8.0 * (h + 1) / 16) for h in range(16)]
NT = (NOWN + 1) * 128
TT = [(0, 512), (512, 512), (1024, 128)]
ALPHA = 2.0 ** 0.25
LN_EPS = 1e-5
N_EXPERTS = 32
NCH = 64
N_PHYS = 2560


def build_nc(debug=False):
    nc = bass.Bass("TRN2", target_bir_lowering=False)
    P = Prog()

    def din(name, shape, dt=F32):
        return nc.dram_tensor(name, list(shape), dt, kind="ExternalInput").ap()

    def dout(name, shape, dt=F32):
        return nc.dram_tensor(name, list(shape), dt, kind="ExternalOutput").ap()

    xf = din("xf", [D_MODEL, XF_T])
    xTb = din("xTb", [D_MODEL, SEQ])
    w_kv = din("w_kv", [D_MODEL, KV_W])
    b_kv = din("b_kv", [KV_W])
    w_q = din("w_q", [D_MODEL, 1024])
    b_q = din("b_q", [1024])
    w_ng = din("w_ng", [D_MODEL, 48])
    b_ng = din("b_ng", [48])
    w_st = din("w_st", [D_MODEL, 512])
    b_st = din("b_st", [512])
    g_st = din("g_st", [2, 256])
    w_ss = din("w_ss", [D_MODEL, 2048])
    b_ss = din("b_ss", [2048])
    g_ss = din("g_ss", [2, 1024])
    st_in = din("st_in", [4, 8, 128, 128])
    cw_in = din("cw_in", [4, 512, 512])
    c_lm = din("c_lm", [128, 128])
    c_lm4 = din("c_lm4", [4, 4])
    w_c1 = din("w_c1", [2, 32, 64, 128])
    w_c2 = din("w_c2", [2, 128, 64])
    c_pe = din("c_pe", [2, 32, 64])
    t_cb = din("t_cb", [128, 256])
    t_cmask = din("t_cmask", [128, NOWN * 512], BF16)
    t_bs = din("t_bs", [128, 512])
    t_sq = din("t_sq", [1, 2048])
    t_ln = din("t_ln", [2, NB * 128], BF16)
    t_r2 = din("t_r2", [2, 512], BF16)
    t_E = din("t_E", [64, NB * 128], BF16)
    t_caus = din("t_caus", [128, 512], BF16)
    t_low = din("t_low", [128, 512], BF16)
    t_keep = din("t_keep", [128, NOWN * 64])
    t_force = din("t_force", [128, NOWN * 64])
    t_ovl = din("t_ovl", [128, 2 * 64], BF16)
    w_h3 = din("w_h3", [2, D_MODEL, 2048])
    b_h3 = din("b_h3", [2, 2048])
    n_h3 = din("n_h3", [2, 512])
    g_h3 = din("g_h3", [2, 2, 512])
    t_u32 = din("t_u32", [128, 128])
    t_l32 = din("t_l32", [128, 128])
    t_ind4 = din("t_ind4", [128, 4])
    t_vmask = din("t_vmask", [128, NB + 1])
    w_pa = din("w_pa", [1024, D_MODEL])
    w_pb = din("w_pb", [1024, D_MODEL])
    w_mg = din("w_mg", [D_MODEL, 4096])
    b_mg = din("b_mg", [4096])
    w_out = din("w_out", [D_MODEL, D_MODEL])
    ln1_g = din("ln1_g", [D_MODEL])
    ln1_b = din("ln1_b", [D_MODEL])
    ln2_g = din("ln2_g", [D_MODEL])
    ln2_b = din("ln2_b", [D_MODEL])
    w_r = din("w_r", [D_MODEL, 36])
    b_r = din("b_r", [36])
    t_selE = din("t_selE", [32, 32 * 128], BF16)
    n_experts = _NEXP
    w_gate = din("w_gate", [n_experts, D_MODEL, 512])
    w_up = din("w_up", [n_experts, D_MODEL, 512])
    w_down = din("w_down", [n_experts, 512, D_MODEL])
    cache2d = din("cache2d", [N_PHYS * 128 * 2, 512])
    pt_core = din("pt_core", [256], mybir.dt.int32)
    s_cb = din("s_cb", [128, 64])
    s_bs = din("s_bs", [128, 16 * 65])
    s_sq = din("s_sq", [1, 512])
    s_lnt = din("s_lnt", [2, 128], BF16)
    s_caus = din("s_caus", [128, 128], BF16)
    s_low = din("s_low", [128, 128], BF16)
    s_keep = din("s_keep", [128, 128])
    s_force = din("s_force", [128, 128])
    s_ovl = din("s_ovl", [128, 4 * 128], BF16)
    s_iota = din("s_iota", [128, 1])
    t_id = din("t_id", [128, 128])
    t_idb = din("t_idb", [128, 128], BF16)

    kv_out = dout("kv_out", [(NOWN + 1) * 128, KV_W])
    win_s = dout("win_s", [4, 512, 512])
    st_p = dout("st_p", [2, 128, 128])
    st_s = dout("st_s", [4, 8, 128, 128])
    y_out = dout("y_out", [D_MODEL, NT])
    if debug:
        d_onsa = dout("d_onsa", [NOWN * 128, 1024])
        d_kc = dout("d_kc", [128, 2 * 256])
        d_vc = dout("d_vc", [128, 2 * 4 * 129])
        d_imp = dout("d_imp", [NOWN * 4 * 128, 64])
        d_ohg = dout("d_ohg", [NT, 1024])
        d_onsa_s = dout("d_onsa_s", [128, 1024])
        d_h = dout("d_h", [D_MODEL, NT])
        d_gate = dout("d_gate", [32, NT])

    top = contextlib.ExitStack()
    stopped = False
    if True:
      try:
          _names = {}

          def sbt(stack, name, shape, dt=F32):
              n = _names.get(name, 0)
              _names[name] = n + 1
              if n:
                  name = "%s_r%d" % (name, n)
              return stack.enter_context(nc.sbuf_tensor(name, list(shape), dt)), Buf(name)

          ps = [top.enter_context(nc.psum_tensor("ps%d" % i, [128, 512], F32)) for i in range(8)]
          B_ps = [Buf("ps%d" % i) for i in range(8)]
          pools = {"a": [0, 1, 2, 3, 4], "b": [5, 6, 7]}
          rr = {"a": 0, "b": 0}

          def next_ps(pool="a"):
              lst = pools[pool]
              k = lst[rr[pool] % len(lst)]
              rr[pool] += 1
              return ps[k], B_ps[k]

          ones_c, B_ones = sbt(top, "ones_c", [128, 1], F32)
          onesb, B_onesb = sbt(top, "onesb", [128, 128], BF16)
          ident, B_ident = sbt(top, "ident", [128, 128], F32)
          identb, B_identb = sbt(top, "identb", [128, 128], BF16)
          P.op("dve", lambda e: e.memset(ones_c[:], 1.0), writes=[B_ones])
          P.op("dve", lambda e: e.memset(onesb[:], 1.0), writes=[B_onesb])
          P.dma("sp", lambda e: e.dma_start(out=ident[:], in_=t_id), writes=[B_ident])
          P.dma("sp", lambda e: e.dma_start(out=identb[:], in_=t_idb), writes=[B_identb])
          ohT, B_oh = sbt(top, "ohT", [128, 16, (NOWN + 1) * 128], BF16)
          P.op("pool", lambda e: e.memset(ohT[:], 0.0), writes=[B_oh])
          o_nsaT, B_onsaT = ohT[:, 0:8, :], B_oh
          o_hgT, B_ohgT = ohT[:, 8:16, :], B_oh

          st = contextlib.ExitStack()
          with st:
              stage_states(nc, P, st, sbt, next_ps, ones_c, B_ones,
                           dict(xTb=xTb, xf=xf, w_st=w_st, b_st=b_st, g_st=g_st, w_ss=w_ss, b_ss=b_ss, g_ss=g_ss,
                                st_in=st_in, c_lm=c_lm, c_lm4=c_lm4, st_p=st_p, st_s=st_s))
              P.barrier()

          ns = contextlib.ExitStack()
          with ns:
              kslcT, B_kslcT = sbt(ns, "kslcT", [128, 2, NB * 128], BF16)
              kwinT, B_kwinT = sbt(ns, "kwinT", [128, 2, 12 * 128], BF16)
              vslc, B_vslc = sbt(ns, "vslc", [128, NB, 4, 65], BF16)
              vwin, B_vwin = sbt(ns, "vwin", [128, 12, 4, 65], BF16)
              kcT, B_kcT = sbt(ns, "kcT", [128, 2, 256], BF16)
              vca, B_vca = sbt(ns, "vca", [128, 2, 4, 129], BF16)
              P.op("dve", lambda e: e.memset(vslc[:, :, :, 64:65], 1.0), writes=[B_vslc])
              P.op("dve", lambda e: e.memset(vwin[:, :, :, 64:65], 1.0), writes=[B_vwin])
              P.op("dve", lambda e: e.memset(vca[:, :, :, 64:65], 1.0), writes=[B_vca])
              for c in range(2):
                  for g in range(4):
                      P.dma("sp", lambda e, c=c, g=g: e.dma_start(out=vca[:, c, g, 65:129], in_=t_ovl[:, c * 64:(c + 1) * 64]),
                            writes=[B_vca])

              p1 = contextlib.ExitStack()
              with p1:
                  p1a = p1.enter_context(contextlib.ExitStack())
                  kcmpT, B_kcmpT = sbt(p1, "kcmpT", [128, 2, NB * 128], BF16)
                  vcmpT, B_vcmpT = sbt(p1, "vcmpT", [128, 2, NB * 128], BF16)
                  wkv, B_wkv = sbt(p1a, "wkv", [128, KC, KV_W], BF16)
                  bkv_bc, B_bkvbc = sbt(p1a, "bkv_bc", [128, KV_W], F32)
                  bkv_col, B_bkvcol = sbt(p1a, "bkv_col", [128, 12], F32)
                  xs = [sbt(p1a, "xs%d" % i, [128, KC, 256], BF16) for i in range(2)]
                  osb = [sbt(p1a, "osb%d" % i, [128, KV_W], F32) for i in range(2)]
                  wkv_v = w_kv.rearrange("(kc p) c -> p kc c", p=128)
                  for q in range(4):
                      P.dma("pool", lambda e, q=q: e.dma_start(out=wkv[:, 4 * q:4 * q + 4, :], in_=wkv_v[:, 4 * q:4 * q + 4, :]),
                            writes=[B_wkv])
                  P.dma("sp", lambda e: e.dma_start(out=bkv_bc[:], in_=b_kv.partition_broadcast(128)), writes=[B_bkvbc])
                  with nc.allow_non_contiguous_dma(reason="tiny bias column layout"):
                      P.dma("sp", lambda e: e.dma_start(out=bkv_col[:], in_=b_kv.rearrange("(cb p) -> p cb", p=128), allow_slow_non_contiguous=True),
                            writes=[B_bkvcol])
                  xf_v = xf.rearrange("(kc p) t -> p kc t", p=128)
                  nti = 0
                  for tt in range((NB + 1) * 128 // 256 + 1):
                      t0 = tt * 256
                      if t0 >= XF_T:
                          break
                      tw = min(256, XF_T - t0)
                      (xb, Bx) = xs[tt % 2]
                      for q in range(2):
                          P.dma("pool", lambda e, xb=xb, t0=t0, tw=tw, q=q: e.dma_start(
                              out=xb[:, 8 * q:8 * q + 8, 0:tw], in_=xf_v[:, 8 * q:8 * q + 8, t0:t0 + tw]), writes=[Bx])
                      is_prompt = t0 < NB * 128
                      if is_prompt:
                          for slot, dst, Bd, lo in ((0, kcmpT, B_kcmpT, 0), (1, vcmpT, B_vcmpT, 0), (2, kslcT, B_kslcT, 0),
                                                    (4, kwinT, B_kwinT, 20 * 128)):
                              if t0 < lo:
                                  continue
                              for gp in range(2):
                                  cb = slot * 2 + gp
                                  pt, Bp = next_ps("a")
                                  for kc in range(KC):
                                      P.op("pe", lambda e, pt=pt, kc=kc, cb=cb, xb=xb, tw=tw: e.matmul(
                                          pt[:, 0:tw], lhsT=wkv[:, kc, cb * 128:(cb + 1) * 128], rhs=xb[:, kc, 0:tw],
                                          start=(kc == 0), stop=(kc == KC - 1)), reads=[B_wkv, Bx], writes=[Bp])
                                  P.op("act", lambda e, pt=pt, dst=dst, gp=gp, cb=cb, t0=t0, tw=tw, lo=lo: e.activation(
                                      out=dst[:, gp, t0 - lo:t0 - lo + tw], in_=pt[:, 0:tw], func=AF.Identity,
                                      bias=bkv_col[:, cb:cb + 1]), reads=[Bp, B_bkvcol], writes=[Bd])
                      for bi in range(tw // 128):
                          p = (t0 + bi * 128) // 128
                          tl = slice(bi * 128, (bi + 1) * 128)
                          if p < NB:
                              pt, Bp = next_ps("a")
                              for half, c0 in ((0, 768), (1, 1280)):
                                  for kc in range(KC):
                                      P.op("pe", lambda e, pt=pt, kc=kc, xb=xb, tl=tl, half=half, c0=c0: e.matmul(
                                          pt[:, half * 256:(half + 1) * 256], lhsT=xb[:, kc, tl], rhs=wkv[:, kc, c0:c0 + 256],
                                          start=(kc == 0), stop=(kc == KC - 1)), reads=[B_wkv, Bx], writes=[Bp])
                              P.op("dve", lambda e, pt=pt, p=p: e.tensor_tensor(
                                  out=vslc[:, p, :, 0:64], in0=pt[:, 0:256].rearrange("p (g d) -> p g d", g=4),
                                  in1=bkv_bc[:, 768:1024].rearrange("p (g d) -> p g d", g=4), op=ALU.add),
                                  reads=[Bp, B_bkvbc], writes=[B_vslc])
                              if p >= 20:
                                  P.op("dve", lambda e, pt=pt, p=p: e.tensor_tensor(
                                      out=vwin[:, p - 20, :, 0:64], in0=pt[:, 256:512].rearrange("p (g d) -> p g d", g=4),
                                      in1=bkv_bc[:, 1280:1536].rearrange("p (g d) -> p g d", g=4), op=ALU.add),
                                      reads=[Bp, B_bkvbc], writes=[B_vwin])
                          if p >= OWN0:
                              (o, Bo) = osb[nti % 2]
                              nti += 1
                              for cg in range(3):
                                  pt, Bp = next_ps("a")
                                  for kc in range(KC):
                                      P.op("pe", lambda e, pt=pt, kc=kc, xb=xb, tl=tl, cg=cg: e.matmul(
                                          pt[:, :], lhsT=xb[:, kc, tl], rhs=wkv[:, kc, cg * 512:(cg + 1) * 512],
                                          start=(kc == 0), stop=(kc == KC - 1)), reads=[B_wkv, Bx], writes=[Bp])
                                  P.op("dve", lambda e, pt=pt, o=o, cg=cg: e.tensor_tensor(
                                      out=o[:, cg * 512:(cg + 1) * 512], in0=pt[:, :], in1=bkv_bc[:, cg * 512:(cg + 1) * 512],
                                      op=ALU.add), reads=[Bp, B_bkvbc], writes=[Bo])
                              r0 = (p - OWN0) * 128
                              P.dma("sp", lambda e, o=o, r0=r0: e.dma_start(out=kv_out[r0:r0 + 128, :], in_=o[:, :]), reads=[Bo])
                              if p == NB:
                                  for j in range(4):
                                      P.dma("sp", lambda e, o=o, j=j: e.dma_start(
                                          out=win_s[j, 508:512, :], in_=o[32 * j:32 * j + 4, 1024:1536]), reads=[Bo])
                  for j in range(4):
                      P.dma("sp", lambda e, j=j: e.dma_start(out=win_s[j, 0:508, :], in_=cw_in[j, 4:512, :]))

                  P.barrier()
                  p1a.close()
                  if _LIM < 3:
                      raise _Stop()
                  w1d, B_w1d = sbt(p1, "w1d", [128, 2, 32, 128], BF16)
                  w1f, B_w1f = sbt(p1, "w1f", [128, 2, 16, 128], BF16)
                  pef, B_pef = sbt(p1, "pef", [128, 2, 16], F32)
                  peb, B_peb = sbt(p1, "peb", [128, 2, 16], BF16)
                  w2k, B_w2k = sbt(p1, "w2k", [128, 128], BF16)
                  w2v, B_w2v = sbt(p1, "w2v", [128, 64], BF16)
                  pre0, B_pre0 = sbt(p1, "pre0", [128, 2], F32)
                  ug, B_ug = sbt(p1, "ug", [128, 256], F32)
                  tg, B_tg = sbt(p1, "tg", [128, 256], F32)
                  Gb, B_Gb = sbt(p1, "Gb", [128, 256], BF16)
                  for half in range(2):
                      P.dma("pool", lambda e, half=half: e.dma_start(
                          out=w1d[64 * half:64 * half + 64, :, :, :], in_=w_c1.rearrange("c j d h -> d c j h")), writes=[B_w1d])
                  P.dma("pool", lambda e: e.dma_start(out=w1f[:], in_=w_c1.rearrange("c (jc j2) d h -> (j2 d) c jc h", j2=2)),
                        writes=[B_w1f])
                  with nc.allow_non_contiguous_dma(reason="tiny pe layout"):
                      P.dma("sp", lambda e: e.dma_start(out=pef[:], in_=c_pe.rearrange("c (jc j2) d -> (j2 d) c jc", j2=2), allow_slow_non_contiguous=True),
                            writes=[B_pef])
                  P.op("dve", lambda e: e.tensor_copy(out=peb[:], in_=pef[:]), reads=[B_pef], writes=[B_peb])
                  P.op("dve", lambda e: e.memset(w2k[:, 0:64], 0.0), writes=[B_w2k])
                  P.dma("pool", lambda e: e.dma_start(out=w2k[:, 64:128], in_=w_c2[0]), writes=[B_w2k])
                  P.dma("pool", lambda e: e.dma_start(out=w2v[:], in_=w_c2[1]), writes=[B_w2v])
                  pt, Bp = next_ps("b")
                  for c in range(2):
                      for jc in range(16):
                          P.op("pe", lambda e, pt=pt, c=c, jc=jc: e.matmul(
                              pt[:, c:c + 1], lhsT=w1f[:, c, jc, :], rhs=peb[:, c, jc:jc + 1], start=(jc == 0), stop=(jc == 15)),
                              reads=[B_w1f, B_peb], writes=[Bp])
                  P.op("dve", lambda e, pt=pt: e.tensor_copy(out=pre0[:], in_=pt[:, 0:2]), reads=[Bp], writes=[B_pre0])
                  for c in range(2):
                      src, Bsrc = (kcmpT, B_kcmpT) if c == 0 else (vcmpT, B_vcmpT)
                      for g in range(4):
                          gp, g2 = g // 2, g % 2
                          hs = slice(64 * g2, 64 * g2 + 64)
                          pt, Bp = next_ps("a")
                          for j in range(32):
                              P.op("pe", lambda e, pt=pt, c=c, j=j, hs=hs, gp=gp, src=src: e.matmul(
                                  pt[:, 0:255], lhsT=w1d[hs, c, j, :], rhs=src[hs, gp, j:j + 16 * 254 + 1:16],
                                  start=(j == 0), stop=(j == 31)), reads=[B_w1d, Bsrc], writes=[Bp])
                          P.op("act", lambda e, pt=pt, c=c: e.activation(out=ug[:, 0:255], in_=pt[:, 0:255], func=AF.Identity,
                                                                         bias=pre0[:, c:c + 1]), reads=[Bp, B_pre0], writes=[B_ug])
                          P.op("dve", lambda e: e.tensor_tensor(out=tg[:, 0:255], in0=ug[:, 0:255], in1=ug[:, 0:255], op=ALU.mult),
                               reads=[B_ug], writes=[B_tg])
                          P.op("dve", lambda e: e.tensor_scalar(out=tg[:, 0:255], in0=tg[:, 0:255], scalar1=0.044715, scalar2=1.0,
                                                                op0=ALU.mult, op1=ALU.add), reads=[B_tg], writes=[B_tg])
                          P.op("dve", lambda e: e.tensor_tensor(out=tg[:, 0:255], in0=tg[:, 0:255], in1=ug[:, 0:255], op=ALU.mult),
                               reads=[B_tg, B_ug], writes=[B_tg])
                          P.op("act", lambda e: e.activation(out=tg[:, 0:255], in_=tg[:, 0:255], func=AF.Sigmoid,
                                                             scale=1.5957691216057308), reads=[B_tg], writes=[B_tg])
                          P.op("dve", lambda e: e.tensor_tensor(out=Gb[:, 0:255], in0=tg[:, 0:255], in1=ug[:, 0:255], op=ALU.mult),
                               reads=[B_tg, B_ug], writes=[B_Gb])
                          if c == 0:
                              pt2, Bp2 = next_ps("b")
                              if g2 == 0:
                                  P.op("pe", lambda e, pt2=pt2: e.matmul(pt2[0:64, 0:255], lhsT=w2k[:, 64:128], rhs=Gb[:, 0:255],
                                                                         start=True, stop=True), reads=[B_w2k, B_Gb], writes=[Bp2])
                              else:
                                  P.op("pe", lambda e, pt2=pt2: e.matmul(pt2[:, 0:255], lhsT=w2k[:, :], rhs=Gb[:, 0:255],
                                                                         start=True, stop=True), reads=[B_w2k, B_Gb], writes=[Bp2])
                              P.op("dve", lambda e, pt2=pt2, hs=hs, gp=gp: e.tensor_copy(out=kcT[hs, gp, 0:255], in_=pt2[hs, 0:255]),
                                   reads=[Bp2], writes=[B_kcT])
                          else:
                              pt2, Bp2 = next_ps("b")
                              for ch in range(2):
                                  w = 128 if ch == 0 else 127
                                  P.op("pe", lambda e, pt2=pt2, ch=ch, w=w: e.matmul(
                                      pt2[:w, ch * 64:(ch + 1) * 64], lhsT=Gb[:, ch * 128:ch * 128 + w], rhs=w2v[:, :],
                                      start=True, stop=True), reads=[B_w2v, B_Gb], writes=[Bp2])
                              for ch in range(2):
                                  w = 128 if ch == 0 else 127
                                  P.op("dve", lambda e, pt2=pt2, ch=ch, w=w, g=g: e.tensor_copy(
                                      out=vca[:w, ch, g, 0:64], in_=pt2[:w, ch * 64:(ch + 1) * 64]), reads=[Bp2], writes=[B_vca])
                  P.op("dve", lambda e: e.memset(kcT[:, :, 255:256], 0.0), writes=[B_kcT])
                  if debug:
                      dk, B_dk = sbt(p1, "dk", [128, 512], F32)
                      P.op("dve", lambda e: e.tensor_copy(out=dk[:], in_=kcT[:].rearrange("p a b -> p (a b)")), reads=[B_kcT], writes=[B_dk])
                      P.dma("sp", lambda e: e.dma_start(out=d_kc, in_=dk[:]), reads=[B_dk])
                      dv, B_dv = sbt(p1, "dv", [128, 2 * 4 * 129], F32)
                      P.op("dve", lambda e: e.tensor_copy(out=dv[:], in_=vca[:].rearrange("p a b c -> p (a b c)")), reads=[B_vca], writes=[B_dv])
                      P.dma("sp", lambda e: e.dma_start(out=d_vc, in_=dv[:]), reads=[B_dv])
                  P.barrier()

              p4 = contextlib.ExitStack()
              with p4:
                  if _LIM < 4:
                      raise _Stop()
                  nsa_prompt(nc, P, p4, sbt, next_ps, locals())
                  P.barrier()

          if _LIM < 6.2:
              raise _Stop()
          sm = contextlib.ExitStack()
          with sm:
              nsa_sample(nc, P, sm, sbt, next_ps, locals())
          if _LIM < 7:
              raise _Stop()
          hg = contextlib.ExitStack()
          with hg:
              hgrn_outputs(nc, P, hg, sbt, next_ps, locals())
          if _LIM < 8:
              raise _Stop()
          tl = contextlib.ExitStack()
          with tl:
              tail_moe(nc, P, tl, sbt, next_ps, locals())

      except _Stop:
        stopped = True
      if True:
          P.finish("sp")
          P.emit(nc)
      if not stopped:
          top.close()
    return nc


def _bf16(a):
    import ml_dtypes
    return np.ascontiguousarray(np.asarray(a, np.float32).astype(ml_dtypes.bfloat16))


def sample_tables():
    T = {}
    nl = np.arange(128)
    cb = np.zeros((128, 64), np.float32)
    for h in range(16):
        for c in range(4):
            n = 128 * c + nl
            v = SLOPES[h] * (16.0 * n + 31 - 8192)
            cb[:, h * 4 + c] = np.where(n >= 511, NEGM, v)
    T["s_cb"] = cb
    bsx = np.zeros((128, 16 * 65), np.float32)
    for h in range(16):
        for d in range(65):
            bsx[:, h * 65 + d] = SLOPES[h] * (nl - 128.0 * d)
    T["s_bs"] = bsx
    sv = np.arange(32)
    sq = np.zeros((16, 32), np.float32)
    for h in range(16):
        sq[h] = -SLOPES[h] * sv / SCALE
    T["s_sq"] = sq.reshape(1, 512)
    ln = np.zeros((2, 128), np.float32)
    ln[0] = 1.0
    T["s_lnt"] = _bf16(ln)
    T["s_caus"] = _bf16(np.tile(np.where(nl[:, None] > sv[None, :], NEGM, 0.0), (1, 4)))
    T["s_low"] = _bf16(np.tile(np.where(nl[:, None] <= sv[None, :], NEGM, 0.0), (1, 4)))
    keep = np.ones((128, 128), np.float32)
    force = np.zeros((128, 128), np.float32)
    keep[:, 0] = 0.0
    force[:, 0] = 1e30
    T["s_keep"], T["s_force"] = keep, force
    j = np.arange(128)[None, :]
    ov = np.zeros((128, 4, 128), np.float32)
    for c in range(4):
        n = (128 * c + nl)[:, None]
        ov[:, c, :] = ((n >= 4 * j - 1) & (n <= 4 * j + 3)).astype(np.float32)
    T["s_ovl"] = _bf16(ov.reshape(128, 512))
    T["s_iota"] = nl.astype(np.float32).reshape(128, 1)
    return T


def host_tables(nnull):
    T = {}
    nl = np.arange(128)
    t_cb = np.zeros((128, 256), np.float32)
    for h in range(16):
        for k in range(NOWN):
            B = OWN0 + k
            for c in range(2):
                n = 128 * c + nl
                v = SLOPES[h] * (16.0 * n + 31 - 128 * B)
                v = np.where((n < 8 * nnull) | (n >= 255), NEGM, v)
                t_cb[:, (h * NOWN + k) * 2 + c] = v
    T["t_cb"] = t_cb
    cm = np.zeros((128, NOWN, 4, 128), np.float32)
    ql = np.arange(128)
    for k in range(NOWN):
        B = OWN0 + k
        inval = (16 * (128 + nl)[:, None] + 31) > (128 * B + ql[None, :])
        cm[:, k, :, :] = np.where(inval, NEGM, 0.0)[:, None, :]
    T["t_cmask"] = _bf16(cm.reshape(128, NOWN * 512))
    t_bs = np.zeros((128, 512), np.float32)
    for h in range(16):
        for d in range(32):
            t_bs[:, h * 32 + d] = SLOPES[h] * (nl - 128.0 * d)
    T["t_bs"] = t_bs
    sq = np.zeros((16, 128), np.float32)
    for h in range(16):
        sq[h] = -SLOPES[h] * ql / SCALE
    T["t_sq"] = sq.reshape(1, 2048)
    ln = np.zeros((2, NB, 128), np.float32)
    ln[0] = 1.0
    ln[1, :nnull] = 1.0
    T["t_ln"] = _bf16(ln.reshape(2, NB * 128))
    r2 = np.zeros((2, 512), np.float32)
    r2[1] = NEGM
    T["t_r2"] = _bf16(r2)
    E = np.zeros((64, NB, 128), np.float32)
    for c in range(NB):
        E[2 * c, c, :64] = 1.0
        E[2 * c + 1, c, 64:] = 1.0
    T["t_E"] = _bf16(E.reshape(64, NB * 128))
    tq = nl[:, None] > ql[None, :]
    T["t_caus"] = _bf16(np.tile(np.where(tq, NEGM, 0.0), (1, 4)))
    T["t_low"] = _bf16(np.tile(np.where(~tq, NEGM, 0.0), (1, 4)))
    keep = np.zeros((128, NOWN, 64), np.float32)
    force = np.zeros((128, NOWN, 64), np.float32)
    j = np.arange(64)[None, :]
    j0 = 2 * nnull
    for k in range(NOWN):
        B = OWN0 + k
        cur = (2 * B + (ql >= 64))[:, None]
        neg = (j > cur) | (j < j0)
        big = ((j == cur) | (j == j0)) & ~neg
        keep[:, k, :] = np.where(neg | big, 0.0, 1.0)
        force[:, k, :] = np.where(neg, -1e30, np.where(big, 1e30, 0.0))
    T["t_keep"] = keep.reshape(128, NOWN * 64)
    T["t_force"] = force.reshape(128, NOWN * 64)
    ov = np.zeros((128, 2, 64), np.float32)
    for c in range(2):
        n = (128 * c + nl)[:, None]
        ov[:, c, :] = ((n >= 4 * j - 1) & (n <= 4 * j + 3)).astype(np.float32)
    T["t_ovl"] = _bf16(ov.reshape(128, 128))
    a = np.arange(128)
    same = (a[:, None] // 32) == (a[None, :] // 32)
    T["t_u32"] = (same & (a[:, None] <= a[None, :])).astype(np.float32)
    T["t_l32"] = (same & (a[:, None] > a[None, :])).astype(np.float32)
    T["t_ind4"] = ((a[:, None] // 32) == np.arange(4)[None, :]).astype(np.float32)
    vm = np.ones((128, NB + 1), np.float32)
    vm[:, :nnull] = 0.0
    T["t_vmask"] = vm
    se = np.zeros((32, 32, 128), np.float32)
    for e_ in range(32):
        se[e_, e_, :] = 1.0
    T["t_selE"] = _bf16(se.reshape(32, 32 * 128))
    T["t_id"] = np.eye(128, dtype=np.float32)
    T["t_idb"] = _bf16(np.eye(128))
    return T


_NC_CACHE = {}
_TAB_CACHE = {}


def _run(inputs, debug=False):
    f = lambda a: np.ascontiguousarray(np.asarray(a, dtype=np.float32))
    x_prompt, x_sample = f(inputs["x_prompt"]), f(inputs["x_sample"])
    w_in, b_in = f(inputs["w_in"])[0], f(inputs["b_in"])[0]
    hgrn_gamma, state_hgrn, cache_win = f(inputs["hgrn_gamma"]), f(inputs["state_hgrn"])[0], f(inputs["cache_win"])[0]
    key = "nc_dbg" if debug else "nc"
    if key not in _NC_CACHE:
        _NC_CACHE[key] = build_nc(debug)
    nc = _NC_CACHE[key]

    lmask = np.tril(np.ones((128, 128), np.float32), -1)
    w_kv = np.ascontiguousarray(w_in[:, KV_OFF:KV_OFF + KV_W])
    b_kv = np.ascontiguousarray(b_in[KV_OFF:KV_OFF + KV_W])
    perm = np.zeros(1024, np.int64)
    for gp in range(2):
        for hh in range(4):
            for g2 in range(2):
                dst = ((gp * 4 + hh) * 2 + g2) * 64
                src = (4 * (2 * gp + g2) + hh) * 64
                perm[dst:dst + 64] = np.arange(src, src + 64)
    w_q = np.ascontiguousarray(w_in[:, QOFF:QOFF + 1024][:, perm])
    b_q = np.ascontiguousarray(b_in[QOFF:QOFF + 1024][perm])
    w_ng = np.ascontiguousarray(w_in[:, NGOFF:NGOFF + 48])
    b_ng = np.ascontiguousarray(b_in[NGOFF:NGOFF + 48])
    w_ss = np.ascontiguousarray(np.concatenate([w_in[:, HF_OFF:HF_OFF + 1024], w_in[:, HI_OFF:HI_OFF + 1024]], axis=1))
    b_ss = np.ascontiguousarray(np.concatenate([b_in[HF_OFF:HF_OFF + 1024], b_in[HI_OFF:HI_OFF + 1024]]))
    xTb_all = [np.ascontiguousarray(x_prompt[b].T) for b in range(2)]
    hn = f(inputs["hgrn_norm"])[0].reshape(1024)
    w_h3, b_h3, n_h3, g_h3 = [], [], [], []
    for hp in range(2):
        cs = slice(512 * hp, 512 * (hp + 1))
        w_h3.append(np.concatenate([w_in[:, o:o + 1024][:, cs] for o in (HQOFF, HF_OFF, HI_OFF, HGOFF)], axis=1))
        b_h3.append(np.concatenate([b_in[o:o + 1024][cs] for o in (HQOFF, HF_OFF, HI_OFF, HGOFF)]))
        n_h3.append(hn[cs])
        g_h3.append(hgrn_gamma[:, cs])
    shared = {
        "w_h3": np.ascontiguousarray(np.stack(w_h3)), "b_h3": np.ascontiguousarray(np.stack(b_h3)),
        "n_h3": np.ascontiguousarray(np.stack(n_h3)), "g_h3": np.ascontiguousarray(np.stack(g_h3)),
        "w_pa": f(inputs["w_pa"])[0], "w_pb": f(inputs["w_pb"])[0],
        "w_mg": np.ascontiguousarray(w_in[:, MGOFF:MGOFF + 4096]), "b_mg": np.ascontiguousarray(b_in[MGOFF:MGOFF + 4096]),
        "w_out": f(inputs["w_out"])[0],
        "ln1_g": f(inputs["ln1_g"])[0], "ln1_b": f(inputs["ln1_b"])[0], "ln2_g": f(inputs["ln2_g"])[0], "ln2_b": f(inputs["ln2_b"])[0],
        "w_r": np.ascontiguousarray(np.concatenate([f(inputs["w_rg"])[0], f(inputs["w_re"])[0]], axis=1)),
        "b_r": np.ascontiguousarray(np.concatenate([f(inputs["b_rg"])[0], f(inputs["b_re"])[0]])),
        "w_gate": np.ascontiguousarray(f(inputs["w_gate"])[0][:_NEXP]), "w_up": np.ascontiguousarray(f(inputs["w_up"])[0][:_NEXP]),
        "w_down": np.ascontiguousarray(f(inputs["w_down"])[0][:_NEXP]),
    }
    shared.update(sample_tables())
    shared["cache2d"] = np.ascontiguousarray(f(inputs["cache_kv"])[0].reshape(N_PHYS * 128 * 2, 512))
    page_table = np.ascontiguousarray(np.asarray(inputs["page_table"], dtype=np.int32))
    in_maps = []
    for c in range(NCORES):
        b, i = c // 4, c % 4
        nnull = OWN0 - 8 * i
        if nnull not in _TAB_CACHE:
            _TAB_CACHE[nnull] = host_tables(nnull)
        xfr = np.zeros((XF_T, D_MODEL), np.float32)
        nreal = (NB - nnull) * 128
        xfr[nnull * 128:NB * 128] = x_prompt[b, 0:nreal]
        for j in range(4):
            xfr[NB * 128 + 32 * j:NB * 128 + 32 * j + 4] = x_sample[4 * c + j]
        hs = slice(256 * i, 256 * (i + 1))
        w_st = np.concatenate([w_in[:, HF_OFF:HF_OFF + 1024][:, hs], w_in[:, HI_OFF:HI_OFF + 1024][:, hs]], axis=1)
        b_st = np.concatenate([b_in[HF_OFF:HF_OFF + 1024][hs], b_in[HI_OFF:HI_OFF + 1024][hs]])
        m = {
            "xf": np.ascontiguousarray(xfr.T), "xTb": xTb_all[b],
            "w_kv": w_kv, "b_kv": b_kv, "w_q": w_q, "b_q": b_q, "w_ng": w_ng, "b_ng": b_ng,
            "w_st": np.ascontiguousarray(w_st), "b_st": np.ascontiguousarray(b_st),
            "g_st": np.ascontiguousarray(hgrn_gamma[:, hs]),
            "w_ss": w_ss, "b_ss": b_ss, "g_ss": hgrn_gamma,
            "st_in": np.ascontiguousarray(state_hgrn[4 * c:4 * c + 4]),
            "cw_in": np.ascontiguousarray(cache_win[4 * c:4 * c + 4].reshape(4, 512, 512)),
            "c_lm": lmask, "c_lm4": np.ascontiguousarray(lmask[:4, :4]),
            "w_c1": f(inputs["w_cmp1"])[0], "w_c2": f(inputs["w_cmp2"])[0], "c_pe": f(inputs["cmp_pe"])[0],
        }
        m.update(_TAB_CACHE[nnull])
        m.update(shared)
        m["pt_core"] = np.ascontiguousarray(page_table[4 * c:4 * c + 4].reshape(256))
        in_maps.append(m)
    res = run_bass_kernel_spmd(nc, in_maps, core_ids=list(range(NCORES)))
    return res.results


def _assemble(R):
    y_prompt = np.zeros((2, SEQ, D_MODEL), np.float32)
    y_sample = np.zeros((32, 4, D_MODEL), np.float32)
    new_kv_prompt = np.zeros((1, 2, SEQ, 4, 4, 64), np.float32)
    new_kv_sample = np.zeros((1, 32, 4, 4, 4, 64), np.float32)
    new_win_prompt = np.zeros((1, 2, 512, 2, 4, 64), np.float32)
    new_win_sample = np.zeros((1, 32, 512, 2, 4, 64), np.float32)
    new_state_prompt = np.zeros((1, 2, 8, 128, 128), np.float32)
    new_state_sample = np.zeros((1, 32, 8, 128, 128), np.float32)
    for c in range(NCORES):
        b, i = c // 4, c % 4
        kvo = np.asarray(R[c]["kv_out"])
        new_kv_prompt[0, b, 1024 * i:1024 * (i + 1)] = kvo[:1024, :1024].reshape(1024, 4, 4, 64)
        smp = kvo[1024:].reshape(4, 32, KV_W)[:, :4]
        new_kv_sample[0, 4 * c:4 * c + 4] = smp[:, :, :1024].reshape(4, 4, 4, 4, 64)
        if i == 3:
            new_win_prompt[0, b] = kvo[512:1024, 1024:1536].reshape(512, 2, 4, 64)
        new_win_sample[0, 4 * c:4 * c + 4] = np.asarray(R[c]["win_s"]).reshape(4, 512, 2, 4, 64)
        new_state_prompt[0, b, 2 * i:2 * i + 2] = np.asarray(R[c]["st_p"])
        new_state_sample[0, 4 * c:4 * c + 4] = np.asarray(R[c]["st_s"])
        if "y_out" in R[c]:
            yo = np.asarray(R[c]["y_out"])
            y_prompt[b, 1024 * i:1024 * (i + 1)] = yo[:, :1024].T
            y_sample[4 * c:4 * c + 4] = yo[:, 1024:].T.reshape(4, 32, D_MODEL)[:, :4]
    return (y_prompt, y_sample, new_kv_prompt, new_kv_sample, new_win_prompt, new_win_sample,
            new_state_prompt, new_state_sample)


def kernel(**inputs):
    return _assemble(_run(inputs, debug=False))
```
